# Optimizing a Trainium2 kernel written in Bass

```python
import jax, jax.numpy as jnp
from jax import lax
import numpy as np

D_MODEL = 1024
BATCH = 4
SEQ = 4096
DEPTH = 2

EPS = 1e-6
GRID_W = 64
NEG = -1e30

POOL_WINDOWS = (2, 4, 8, 16)
POOL_GROUP = D_MODEL // 16
POOL_WIDTH = POOL_GROUP * len(POOL_WINDOWS)

RET_HEADS = 4
RET_DK = D_MODEL // 16
RET_DV = 2 * RET_DK
RET_QK = RET_HEADS * RET_DK
RET_V = RET_HEADS * RET_DV
RET_CHUNK = 128
ROPE_BASE = 10000.0

NA_HEADS = 4
NA_DH = D_MODEL // 16
NA_WIDTH = NA_HEADS * NA_DH
NA_ROWS_MAX = 8
NA_COLS = 16

N_BRANCH = 3
IN_SPLITS = (POOL_WIDTH, RET_QK, RET_QK, RET_V, RET_V, NA_WIDTH, NA_WIDTH, NA_WIDTH, N_BRANCH * D_MODEL)
IN_WIDTH = sum(IN_SPLITS)

PEER_KEYS = 128
PEER_EXPERTS = PEER_KEYS * PEER_KEYS
PEER_HEADS = 8
PEER_DK = 128
PEER_TOPK = 16
PEER_BLOCK = 128

kernel_name = "hybrid_pool_retention_natten_peer_encoder"


def rmsnorm(x, g):
    xf = x.astype(jnp.float32)
    y = xf * lax.rsqrt(jnp.mean(xf * xf, axis=-1, keepdims=True) + EPS)
    return (y * g.astype(jnp.float32)).astype(x.dtype)


def rms_heads(y):
    yf = y.astype(jnp.float32)
    return yf * lax.rsqrt(jnp.mean(yf * yf, axis=-1, keepdims=True) + EPS)


def rotary(x):
    S, dk = x.shape[2], x.shape[3]
    half = dk // 2
    inv = 1.0 / (ROPE_BASE ** jnp.linspace(0.0, 1.0, half, dtype=jnp.float32))
    ang = jnp.arange(S, dtype=jnp.float32)[:, None] * inv[None, :]
    cos, sin = jnp.cos(ang), jnp.sin(ang)
    x1, x2 = x[..., :half], x[..., half:]
    return jnp.concatenate([x1 * cos - x2 * sin, x1 * sin + x2 * cos], axis=-1)


def pool_mixer(xp, w_group, scale):
    B, S, _ = xp.shape
    xf = xp.astype(jnp.float32)
    cs = jnp.concatenate([jnp.zeros((B, 1, POOL_WIDTH), jnp.float32), jnp.cumsum(xf, axis=1)], axis=1)
    t = jnp.arange(S)
    outs = []
    for gi, w in enumerate(POOL_WINDOWS):
        lo, hi = w // 2, w - w // 2
        a = jnp.clip(t - lo, 0, S)
        b = jnp.clip(t + hi, 0, S)
        sl = slice(gi * POOL_GROUP, (gi + 1) * POOL_GROUP)
        csg = cs[..., sl]
        cnt = (b - a).astype(jnp.float32)[None, :, None]
        mean = (jnp.take(csg, b, axis=1) - jnp.take(csg, a, axis=1)) / cnt
        outs.append(jnp.einsum('bsc,ce->bse', mean - xf[..., sl], w_group[gi].astype(jnp.float32)))
    return (jnp.concatenate(outs, axis=-1) * scale).astype(xp.dtype)


def retention_direction(q, k, v, log_gamma, strict):
    B, H, S, dk = q.shape
    dv = v.shape[-1]
    C = RET_CHUNK
    n = S // C
    qc = q.reshape(B, H, n, C, dk)
    kc = k.reshape(B, H, n, C, dk)
    vc = v.reshape(B, H, n, C, dv)
    pos = jnp.arange(C, dtype=jnp.float32)
    lg = log_gamma.astype(jnp.float32)
    diff = pos[:, None] - pos[None, :]
    mask = (diff > 0) if strict else (diff >= 0)
    d_intra = jnp.where(mask, jnp.exp(lg[:, None, None] * jnp.where(mask, diff, 0.0)), 0.0)
    scores = jnp.einsum('bhncd,bhnmd->bhncm', qc, kc) * d_intra[None, :, None]
    intra = jnp.einsum('bhncm,bhnme->bhnce', scores, vc)
    lgv = lg[None, :, None, None, None]
    k_dec = kc * jnp.exp(lgv * (C - 1 - pos)[:, None])
    chunk_kv = jnp.einsum('bhncd,bhnce->nbhde', k_dec, vc)
    chunk_decay = jnp.exp(lg * C)[None, :, None, None]

    def step(state, kv):
        return state * chunk_decay + kv, state

    _, prev = lax.scan(step, jnp.zeros((B, H, dk, dv), jnp.float32), chunk_kv.astype(jnp.float32))
    q_dec = qc * jnp.exp(lgv * (pos + 1.0)[:, None])
    cross = jnp.einsum('bhncd,nbhde->bhnce', q_dec, prev)
    return (intra + cross).reshape(B, H, S, dv)


def retention_mixer(q, k, v, g, decay_logit):
    B, S, _ = q.shape
    heads = lambda t, d: t.reshape(B, S, RET_HEADS, d).transpose(0, 2, 1, 3)
    qh = rotary(heads(q, RET_DK))
    kh = rotary(heads(k, RET_DK)) * (RET_DK ** -0.5)
    vh = heads(v, RET_DV)
    lg = jax.nn.log_sigmoid(decay_logit.astype(jnp.float32))
    fwd = retention_direction(qh, kh, vh, lg[0], strict=False)
    bwd = retention_direction(jnp.flip(qh, 2), jnp.flip(kh, 2), jnp.flip(vh, 2), lg[1], strict=True)
    y = rms_heads(fwd + jnp.flip(bwd, 2))
    y = y.transpose(0, 2, 1, 3).reshape(B, S, RET_V)
    return (jax.nn.silu(g.astype(jnp.float32)) * y).astype(q.dtype)


def neighbourhood_attention(q, k, v, rpb):
    B, S, _ = q.shape
    rows = S // GRID_W
    kr = min(NA_ROWS_MAX, rows)
    grid = lambda t: t.reshape(B, rows, GRID_W, NA_HEADS, NA_DH).transpose(0, 3, 1, 2, 4)
    r = jnp.arange(rows)
    row_idx = jnp.clip(r - kr // 2, 0, rows - kr)[:, None] + jnp.arange(kr)[None, :]
    c = jnp.arange(GRID_W)
    col_start = jnp.clip(c - NA_COLS // 2, 0, GRID_W - NA_COLS)
    col_in = (c[None, :] >= col_start[:, None]) & (c[None, :] < col_start[:, None] + NA_COLS)
    di = row_idx - r[:, None] + (NA_ROWS_MAX - 1)
    dj = jnp.clip(c[None, :] - c[:, None], -(NA_COLS - 1), NA_COLS - 1) + (NA_COLS - 1)
    bias = rpb[:, di[:, None, :, None], dj[None, :, None, :]].astype(jnp.float32)
    bias = jnp.where(col_in[None, None, :, None, :], bias, NEG)
    qg = grid(q) * (NA_DH ** -0.5)
    kg = grid(k)[:, :, row_idx]
    vg = grid(v)[:, :, row_idx]
    s = jnp.einsum('bhrcd,bhrivd->bhrciv', qg, kg).astype(jnp.float32) + bias[None]
    p = jax.nn.softmax(s.reshape(B, NA_HEADS, rows, GRID_W, kr * GRID_W), axis=-1)
    p = p.reshape(s.shape).astype(v.dtype)
    o = jnp.einsum('bhrciv,bhrivd->bhrcd', p, vg)
    return o.transpose(0, 2, 3, 1, 4).reshape(B, S, NA_WIDTH)


def mixer_block(xn, w_in, pool_w, pool_scale, ret_decay, na_rpb, w_br_pool, w_br_ret, w_br_na, w_out):
    B, S, D = xn.shape
    proj = jnp.einsum('bsd,de->bse', xn, w_in)
    splits = [int(i) for i in np.cumsum(IN_SPLITS)[:-1]]
    p_pool, r_q, r_k, r_v, r_g, n_q, n_k, n_v, gate_logits = jnp.split(proj, splits, axis=-1)
    y_pool = pool_mixer(p_pool, pool_w, pool_scale)
    y_ret = retention_mixer(r_q, r_k, r_v, r_g, ret_decay)
    y_na = neighbourhood_attention(n_q, n_k, n_v, na_rpb)
    gates = jax.nn.sigmoid(gate_logits.astype(jnp.float32)).reshape(B, S, N_BRANCH, D).astype(xn.dtype)
    merged = (gates[:, :, 0] * jnp.einsum('bsc,cd->bsd', y_pool, w_br_pool)
              + gates[:, :, 1] * jnp.einsum('bsc,cd->bsd', y_ret, w_br_ret)
              + gates[:, :, 2] * jnp.einsum('bsc,cd->bsd', y_na, w_br_na))
    return jnp.einsum('bsd,de->bse', merged, w_out).astype(xn.dtype)


def peer_ffn(xn, w_query, sub_keys, w_u, w_v):
    B, S, D = xn.shape
    T = B * S
    xt = xn.reshape(T, D)
    q = jnp.einsum('td,de->te', xt, w_query).reshape(T, PEER_HEADS, 2, PEER_DK)
    s = jnp.einsum('thpd,pnd->thpn', q, sub_keys).astype(jnp.float32)
    s1, i1 = lax.top_k(s[:, :, 0], PEER_TOPK)
    s2, i2 = lax.top_k(s[:, :, 1], PEER_TOPK)
    cand_s = (s1[..., :, None] + s2[..., None, :]).reshape(T, PEER_HEADS, PEER_TOPK * PEER_TOPK)
    cand_i = (i1[..., :, None] * PEER_KEYS + i2[..., None, :]).reshape(T, PEER_HEADS, PEER_TOPK * PEER_TOPK)
    top_s, top_pos = lax.top_k(cand_s, PEER_TOPK)
    idx = jnp.take_along_axis(cand_i, top_pos, axis=-1).reshape(T, PEER_HEADS * PEER_TOPK)
    gate = jax.nn.softmax(top_s, axis=-1).reshape(T, PEER_HEADS * PEER_TOPK)
    nb = T // PEER_BLOCK

    def expert_block(args):
        xb, ib, gb = args
        a = jnp.einsum('td,tkd->tk', xb, w_u[ib]).astype(jnp.float32)
        h = (jax.nn.gelu(a, approximate=False) * gb).astype(xb.dtype)
        return jnp.einsum('tk,tkd->td', h, w_v[ib]).astype(xb.dtype)

    out = lax.map(expert_block, (xt.reshape(nb, PEER_BLOCK, D),
                                 idx.reshape(nb, PEER_BLOCK, -1),
                                 gate.reshape(nb, PEER_BLOCK, -1)))
    return out.reshape(B, S, D)


def setup_inputs(seed: int = 0) -> dict:
    key = jax.random.key(seed)
    ks = jax.random.split(key, 20)
    f32 = jnp.float32
    nrm = lambda k, shape, sc: jax.random.normal(k, shape, f32) * sc
    gam = 1.0 - 2.0 ** (-5.0 - np.arange(RET_HEADS, dtype=np.float64))
    logit0 = jnp.asarray(np.log(gam / (1.0 - gam)), dtype=f32)
    return {
        "x": nrm(ks[0], (BATCH, SEQ, D_MODEL), 1.0),
        "norm_mix": 1.0 + nrm(ks[1], (DEPTH, D_MODEL), 0.02),
        "w_in": nrm(ks[2], (DEPTH, D_MODEL, IN_WIDTH), D_MODEL ** -0.5),
        "pool_w": nrm(ks[3], (DEPTH, len(POOL_WINDOWS), POOL_GROUP, POOL_GROUP), POOL_GROUP ** -0.5),
        "pool_scale": 1.0 + nrm(ks[4], (DEPTH, POOL_WIDTH), 0.02),
        "ret_decay": logit0[None, None, :] + nrm(ks[5], (DEPTH, 2, RET_HEADS), 0.1),
        "na_rpb": nrm(ks[6], (DEPTH, NA_HEADS, 2 * NA_ROWS_MAX - 1, 2 * NA_COLS - 1), 0.1),
        "w_br_pool": nrm(ks[7], (DEPTH, POOL_WIDTH, D_MODEL), POOL_WIDTH ** -0.5),
        "w_br_ret": nrm(ks[8], (DEPTH, RET_V, D_MODEL), RET_V ** -0.5),
        "w_br_na": nrm(ks[9], (DEPTH, NA_WIDTH, D_MODEL), NA_WIDTH ** -0.5),
        "w_out": nrm(ks[10], (DEPTH, D_MODEL, D_MODEL), D_MODEL ** -0.5),
        "norm_ffn": 1.0 + nrm(ks[11], (DEPTH, D_MODEL), 0.02),
        "peer_w_query": nrm(ks[12], (DEPTH, D_MODEL, PEER_HEADS * 2 * PEER_DK), D_MODEL ** -0.5),
        "peer_sub_keys": nrm(ks[13], (DEPTH, 2, PEER_KEYS, PEER_DK), PEER_DK ** -0.5),
        "peer_u": nrm(ks[14], (DEPTH, PEER_EXPERTS, D_MODEL), D_MODEL ** -0.5),
        "peer_v": nrm(ks[15], (DEPTH, PEER_EXPERTS, D_MODEL), 0.25),
        "norm_final": 1.0 + nrm(ks[16], (D_MODEL,), 0.02),
    }


def reference(x, norm_mix, w_in, pool_w, pool_scale, ret_decay, na_rpb, w_br_pool, w_br_ret, w_br_na,
              w_out, norm_ffn, peer_w_query, peer_sub_keys, peer_u, peer_v, norm_final):
    for l in range(DEPTH):
        xn = rmsnorm(x, norm_mix[l])
        x = x + mixer_block(xn, w_in[l], pool_w[l], pool_scale[l], ret_decay[l], na_rpb[l],
                            w_br_pool[l], w_br_ret[l], w_br_na[l], w_out[l])
        hn = rmsnorm(x, norm_ffn[l])
        x = x + peer_ffn(hn, peer_w_query[l], peer_sub_keys[l], peer_u[l], peer_v[l])
    return rmsnorm(x, norm_final)
```

```python
import numpy as np
from contextlib import ExitStack
import concourse.bass as bass
import concourse.mybir as mybir
from concourse.bass_utils import run_bass_kernel_spmd

F32 = mybir.dt.float32
BF16 = mybir.dt.bfloat16
U32 = mybir.dt.uint32
AF = mybir.ActivationFunctionType
ALU = mybir.AluOpType
AX = mybir.AxisListType

import re as _re
_PSUM_KEY = _re.compile(r"^(bk|pp|gps|aps|ops|tps)\d*$")
EPOCH = 20000
NDMA = 12
EPS = 1e-6


class _Eng:
    def __init__(self, name, strict):
        self.name = name
        self.strict = strict
        self.count = 0
        self.sems = []
        self.known = {}
        self.dsems = []
        self.dcount = []
        self.dnext = 0


class Sched:
    def __init__(self, nc, es):
        self.nc = nc
        self.es = es
        self.E = {
            'pe': _Eng('pe', False),
            'act': _Eng('act', True),
            'dve': _Eng('dve', True),
            'pool': _Eng('pool', True),
            'sp': _Eng('sp', False),
        }
        self.lastw = {}
        self.readers = {}
        self.nsem = 0
        self.prog = {k: [] for k in self.E}

    def _newsem(self, nm):
        self.nsem += 1
        return self.es.enter_context(self.nc.semaphore(f"{nm}_{self.nsem}"))

    def _cur_sem(self, e):
        ep = e.count // EPOCH
        while len(e.sems) <= ep:
            e.sems.append(self._newsem(e.name))
        return e.sems[ep]

    def _wait(self, e, dep):
        sem, val, src = dep
        if src is e and not e.strict:
            return
        k = id(sem)
        if e.known.get(k, 0) >= val:
            return
        self.prog[e.name].append(('w', sem, val))
        e.known[k] = val

    def _deps(self, reads, writes):
        deps = []
        for b in reads:
            if b in self.lastw:
                deps.append(self.lastw[b])
        for b in writes:
            if b in self.lastw:
                deps.append(self.lastw[b])
            deps.extend(self.readers.get(b, []))
        return deps

    def _commit(self, dep, reads, writes):
        for b in reads:
            self.readers.setdefault(b, []).append(dep)
        for b in writes:
            self.lastw[b] = dep
            self.readers[b] = []

    def op(self, eng, meth, reads, writes, *a, **kw):
        fn = lambda h: getattr(h, meth)(*a, **kw)
        e = self.E[eng]
        px = [b for b in reads if _PSUM_KEY.match(b)]
        if px:
            reads = [b for b in reads if b not in px]
            writes = list(writes) + px
        for d in self._deps(reads, writes):
            self._wait(e, d)
        sem = self._cur_sem(e)
        val = e.count % EPOCH + 1
        self.prog[e.name].append(('i', fn, sem, 1))
        e.count += 1
        self._commit((sem, val, e), reads, writes)

    def dma(self, eng, reads, writes, meth='dma_start', **kw):
        fn = lambda h: getattr(h, meth)(**kw)
        e = self.E[eng]
        if not e.dsems:
            e.dsems = [self._newsem(e.name + "d") for _ in range(NDMA)]
            e.dcount = [0] * NDMA
        s = e.dnext
        e.dnext = (e.dnext + 1) % NDMA
        sem = e.dsems[s]
        if e.dcount[s] > 0:
            self._wait(e, (sem, 16 * e.dcount[s], None))
        for d in self._deps(reads, writes):
            self._wait(e, d)
        e.dcount[s] += 1
        self.prog[e.name].append(('i', fn, sem, 16))
        self._commit((sem, 16 * e.dcount[s], None), reads, writes)

    def end_phase(self):
        e = self.E['sp']
        for o in self.E.values():
            if o.count > 0:
                sem = o.sems[(o.count - 1) // EPOCH]
                self._wait(e, (sem, (o.count - 1) % EPOCH + 1, None))
            for s, c in enumerate(o.dcount):
                if c > 0:
                    self._wait(e, (o.dsems[s], 16 * c, None))
        with self.nc.Block() as blk:
            def replay(name):
                def f(h):
                    for it in self.prog[name]:
                        if it[0] == 'w':
                            h.wait_ge(it[1], it[2])
                        else:
                            it[1](h).then_inc(it[2], it[3])
                return f
            blk.sync(replay('sp'))
            blk.scalar(replay('act'))
            blk.vector(replay('dve'))
            blk.gpsimd(replay('pool'))
            blk.tensor(replay('pe'))
        self.prog = {k: [] for k in self.E}
        self.lastw = {}
        self.readers = {}


class Ctx:
    n = 0

    def __init__(self, nc, es):
        self.nc = nc
        self.es = es

    def sb(self, nm, shp, dt):
        Ctx.n += 1
        return self.es.enter_context(self.nc.sbuf_tensor(f"{nm}_{Ctx.n}", list(shp), dt))

    def ps(self, nm, shp, dt):
        Ctx.n += 1
        return self.es.enter_context(self.nc.psum_tensor(f"{nm}_{Ctx.n}", list(shp), dt))


def rmsnorm_tile(S, x_ap, x_key, g_tile, g_key, out_ap, out_key, tmp, ssq, rstd, pfx):
    S.op('act', 'activation', [x_key], [pfx + 'tmp', pfx + 'ssq'], out=tmp[:], in_=x_ap, func=AF.Square, accum_out=ssq[:])
    S.op('act', 'activation', [pfx + 'ssq'], [pfx + 'rstd'], out=rstd[:], in_=ssq[:], func=AF.Sqrt, scale=1.0 / 1024, bias=EPS)
    S.op('dve', 'reciprocal', [pfx + 'rstd'], [pfx + 'rstd'], out=rstd[:], in_=rstd[:])
    S.op('dve', 'scalar_tensor_tensor', [x_key, pfx + 'rstd', g_key], [out_key], out=out_ap, in0=x_ap, scalar=rstd[:, 0:1],
         in1=g_tile[:], op0=ALU.mult, op1=ALU.mult)


def peer_phase(nc, S, D, NT, n_iblk=128, final=False):
    with ExitStack() as es:
        C = Ctx(nc, es)
        sb, ps = C.sb, C.ps
        identf = sb("identf", [128, 128], F32)
        identb = sb("identb", [128, 128], BF16)
        iotaf = sb("iotaf", [128, 128], F32)
        iotab = sb("iotab", [128, 128], BF16)
        c16 = sb("c16", [128, 16], F32)
        skT = sb("skT", [128, 2, 128], F32)
        gB = sb("gB", [128, 1024], F32)
        gFin = sb("gFin", [128, 1024], F32) if final else None
        Gbuf = sb("Gbuf", [128, 256, 128], BF16)
        xt = [sb(f"xt{r}", [128, 2, 1024], F32) for r in range(2)]
        hnT = [sb(f"hnT{r}", [128, 8, 256], BF16) for r in range(2)]
        Wqc = [sb(f"Wqc{r}", [128, 8, 128], BF16) for r in range(2)]
        tmp = sb("tmp", [128, 1024], BF16)
        ssq = sb("ssq", [128, 1], F32)
        rstd = sb("rstd", [128, 1], F32)
        hn = sb("hn", [128, 1024], BF16)
        qc = [sb(f"qc{r}", [128, 256], F32) for r in range(2)]
        sall = sb("sall", [128, 2, 16, 128], F32)
        s2 = sb("s2", [128, 256], F32)
        Vt = sb("Vt", [128, 8, 2, 16], F32)
        It = sb("It", [128, 8, 2, 16], U32)
        cand = sb("cand", [128, 8, 256], F32)
        TV = sb("TV", [128, 8, 16], F32)
        TP = sb("TP", [128, 8, 16], U32)
        TPf = sb("TPf", [128, 8, 16], F32)
        TVs = sb("TVs", [128, 8, 16], F32)
        Ex = sb("Ex", [128, 8, 16], F32)
        Z = sb("Z", [128, 8], F32)
        rZ = sb("rZ", [128, 8], F32)
        I1f = sb("I1f", [128, 8, 16], F32)
        I2f = sb("I2f", [128, 8, 16], F32)
        OH = sb("OH", [128, 8, 16, 16], F32)
        OH2 = sb("OH2", [128, 8, 16, 16], F32)
        af = sb("af", [128, 8, 16], F32)
        bf = sb("bf", [128, 8, 16], F32)
        iF = sb("iF", [128, 128], F32)
        jF = sb("jF", [128, 128], F32)
        gFt = sb("gFt", [128, 128], F32)
        iT = sb("iT", [128, 256], F32)
        jT = sb("jT", [128, 256], F32)
        gT = sb("gT", [128, 256], F32)
        NR = 8
        OI = [sb(f"OI{r}", [128, 128], BF16) for r in range(NR)]
        OJ = [sb(f"OJ{r}", [128, 128], BF16) for r in range(NR)]
        NSLOT = 5
        UTb = [sb(f"UTb{r}", [128, 8, 128], BF16) for r in range(NSLOT)]
        Vb = [sb(f"Vb{r}", [128, 1024], BF16) for r in range(NSLOT)]
        hf = [sb(f"hf{r}", [128, 256], F32) for r in range(2)]
        hG = [sb(f"hG{r}", [128, 256], BF16) for r in range(2)]
        yo = sb("yo", [128, 2, 1024], F32)
        ops = [ps(f"ops{r}", [128, 512], F32) for r in range(4)]
        aps = [ps(f"aps{r}", [128, 512], F32) for r in range(2)]
        gps = [ps(f"gps{r}", [128, 4, 128], F32) for r in range(2)]
        tps = gps[1][:].rearrange("p a b -> p (a b)").bitcast(BF16).rearrange("p (c t) -> p c t", c=8)

        S.dma('sp', [], ['identf'], out=identf[:], in_=D['ident'])
        S.dma('sp', [], ['iotaf'], out=iotaf[:], in_=D['iota'])
        S.dma('sp', [], ['c16'], out=c16[:], in_=D['cst16'])
        S.dma('sp', [], ['skT'], out=skT[:], in_=D['skT'])
        S.dma('sp', [], ['gB'], out=gB[:], in_=D['g'].partition_broadcast(128))
        if final:
            S.dma('sp', [], ['gFin'], out=gFin[:], in_=D['gfin'].partition_broadcast(128))
        S.op('dve', 'tensor_copy', ['identf'], ['identb'], out=identb[:], in_=identf[:])
        S.op('dve', 'tensor_copy', ['iotaf'], ['iotab'], out=iotab[:], in_=iotaf[:])

        ntile = NT // 256
        x_in = D['x_in'].rearrange("(t g p) d -> t p g d", g=2, p=128)
        x_out = D['x_out'].rearrange("(t g p) d -> t p g d", g=2, p=128)
        iota16_b = iotaf[:, 0:16].unsqueeze(1).unsqueeze(1).to_broadcast([128, 8, 16, 16])
        c16_b = c16[:].unsqueeze(1).unsqueeze(1).to_broadcast([128, 8, 16, 16])
        B4 = [128, 8, 16, 16]

        uT2 = D['uT'].rearrange("i p c e -> i p (c e)")
        for i0 in range(128):
            S.dma('pool', [], ['cvU%d' % i0], out=D['uTb'][i0], in_=uT2[i0])
            S.dma('pool', [], ['cvV%d' % i0], out=D['vb'][i0], in_=D['v'][i0 * 128:(i0 + 1) * 128, :])

        def wdma(i):
            sl = i % NSLOT
            S.dma('sp', ['cvU%d' % i], ['UTb%d' % sl], out=UTb[sl][:].rearrange("p c e -> p (c e)"), in_=D['uTb'][i])
            S.dma('sp', ['cvV%d' % i], ['Vb%d' % sl], out=Vb[sl][:], in_=D['vb'][i])

        def stage1(i, par):
            sl = i % NSLOT
            hv = i % 2
            for dc in range(8):
                S.op('pe', 'matmul', ['UTb%d' % sl, 'hnT%d' % par], ['aps%d' % hv], aps[hv][:, 0:256], lhsT=UTb[sl][:, dc, :],
                     rhs=hnT[par][:, dc, :], start=(dc == 0), stop=(dc == 7))

        def prep(ti, par):
            xk, hk = 'xt%d' % par, 'hnT%d' % par
            S.dma('sp', [], [xk], out=xt[par][:], in_=x_in[ti])
            yield
            for g in range(2):
                rmsnorm_tile(S, xt[par][:, g, :], xk, gB, 'gB', hn[:], 'hn', tmp, ssq, rstd, 'p')
                yield
                for c in range(8):
                    S.op('pe', 'transpose', ['hn', 'identb'], ['gps1'], out=tps[:, c, :], in_=hn[:, c * 128:(c + 1) * 128], identity=identb[:])
                S.op('act', 'copy', ['gps1'], [hk], out=hnT[par][:, :, g * 128:(g + 1) * 128], in_=tps)
                yield
            S.dma('pool', [], ['Wqc0'], out=Wqc[0][:], in_=D['wq'][0])
            for c in range(16):
                gp = gps[c % 2]
                gk = 'gps%d' % (c % 2)
                qk = 'qc%d' % (c % 2)
                wk = 'Wqc%d' % (c % 2)
                if c + 1 < 16:
                    S.dma('pool', [], ['Wqc%d' % ((c + 1) % 2)], out=Wqc[(c + 1) % 2][:], in_=D['wq'][c + 1])
                qv = gp[:, 0:2, :].rearrange("p a b -> p (a b)")
                for dc in range(8):
                    S.op('pe', 'matmul', [hk, wk], [gk], qv, lhsT=Wqc[c % 2][:, dc, :], rhs=hnT[par][:, dc, :],
                         start=(dc == 0), stop=(dc == 7))
                S.op('act', 'copy', [gk], [qk], out=qc[c % 2][:], in_=qv)
                yield
                for g in range(2):
                    S.op('pe', 'matmul', [qk, 'skT'], [gk], gp[:, 2 + g, :], lhsT=qc[c % 2][:, g * 128:(g + 1) * 128], rhs=skT[:, c % 2, :],
                         start=True, stop=True)
                S.op('act', 'copy', [gk], ['sall'], out=sall[:, :, c, :], in_=gp[:, 2:4, :])
                yield
            for g in range(2):
                for c in range(16):
                    h, p = divmod(c, 2)
                    src = sall[:, g, c, :]
                    S.op('dve', 'max', ['sall'], ['Vt'], out=Vt[:, h, p, 0:8], in_=src)
                    yield
                    S.op('dve', 'max_index', ['sall', 'Vt'], ['It'], out=It[:, h, p, 0:8], in_max=Vt[:, h, p, 0:8], in_values=src)
                    yield
                    S.op('dve', 'match_replace', ['sall', 'Vt'], ['s2'], out=s2[:, 0:128], in_to_replace=Vt[:, h, p, 0:8], in_values=src, imm_value=-1e30)
                    yield
                    S.op('dve', 'max', ['s2'], ['Vt'], out=Vt[:, h, p, 8:16], in_=s2[:, 0:128])
                    yield
                    S.op('dve', 'max_index', ['s2', 'Vt'], ['It'], out=It[:, h, p, 8:16], in_max=Vt[:, h, p, 8:16], in_values=s2[:, 0:128])
                    yield
                S.op('dve', 'tensor_tensor', ['Vt'], ['cand'], out=cand[:].rearrange("p h (a b) -> p h a b", a=16),
                     in0=Vt[:, :, 0, :].unsqueeze(3).to_broadcast(B4), in1=Vt[:, :, 1, :].unsqueeze(2).to_broadcast(B4), op=ALU.add)
                yield
                for h in range(8):
                    src = cand[:, h, :]
                    S.op('dve', 'max', ['cand'], ['TV'], out=TV[:, h, 0:8], in_=src)
                    yield
                    S.op('dve', 'max_index', ['cand', 'TV'], ['TP'], out=TP[:, h, 0:8], in_max=TV[:, h, 0:8], in_values=src)
                    yield
                    S.op('dve', 'match_replace', ['cand', 'TV'], ['s2'], out=s2[:], in_to_replace=TV[:, h, 0:8], in_values=src, imm_value=-1e30)
                    yield
                    S.op('dve', 'max', ['s2'], ['TV'], out=TV[:, h, 8:16], in_=s2[:])
                    yield
                    S.op('dve', 'max_index', ['s2', 'TV'], ['TP'], out=TP[:, h, 8:16], in_max=TV[:, h, 8:16], in_values=s2[:])
                    yield
                S.op('dve', 'tensor_tensor', ['TV'], ['TVs'], out=TVs[:], in0=TV[:], in1=TV[:, :, 0:1].to_broadcast([128, 8, 16]), op=ALU.subtract)
                yield
                S.op('act', 'activation', ['TVs'], ['Ex'], out=Ex[:], in_=TVs[:], func=AF.Exp)
                S.op('dve', 'tensor_copy', ['TP'], ['TPf'], out=TPf[:], in_=TP[:])
                yield
                S.op('dve', 'tensor_copy', ['It'], ['I1f'], out=I1f[:], in_=It[:, :, 0, :])
                yield
                S.op('dve', 'tensor_copy', ['It'], ['I2f'], out=I2f[:], in_=It[:, :, 1, :])
                yield
                S.op('dve', 'tensor_reduce', ['Ex'], ['Z'], out=Z[:], in_=Ex[:], axis=AX.X, op=ALU.add)
                yield
                S.op('dve', 'reciprocal', ['Z'], ['rZ'], out=rZ[:], in_=Z[:])
                yield
                S.op('dve', 'tensor_tensor', ['Ex', 'rZ'], ['gFt'], out=gFt[:].rearrange("p (h r) -> p h r", h=8), in0=Ex[:],
                     in1=rZ[:].unsqueeze(2).to_broadcast([128, 8, 16]), op=ALU.mult)
                yield
                S.op('dve', 'tensor_tensor', ['TPf', 'c16'], ['OH'], out=OH[:], in0=TPf[:].unsqueeze(3).to_broadcast(B4), in1=c16_b, op=ALU.subtract)
                yield
                OHf = OH[:].rearrange("p a b c -> p (a b c)")
                OH2f = OH2[:].rearrange("p a b c -> p (a b c)")
                S.op('dve', 'tensor_scalar', ['OH'], ['OH2'], out=OH2f, in0=OHf, scalar1=-8.0, scalar2=None, op0=ALU.is_gt)
                yield
                S.op('dve', 'scalar_tensor_tensor', ['OH', 'OH2'], ['OH'], out=OHf, in0=OHf, scalar=8.0, in1=OH2f, op0=ALU.is_lt, op1=ALU.mult)
                yield
                S.op('dve', 'tensor_tensor', ['OH', 'I1f'], ['OH2'], out=OH2[:], in0=OH[:], in1=I1f[:].unsqueeze(2).to_broadcast(B4), op=ALU.mult)
                yield
                S.op('dve', 'tensor_reduce', ['OH2'], ['iF'], out=iF[:].rearrange("p (h r) -> p h r", h=8), in_=OH2[:], axis=AX.X, op=ALU.add)
                yield
                S.op('dve', 'tensor_tensor', ['OH', 'iotaf'], ['OH2'], out=OH2[:], in0=OH[:], in1=iota16_b, op=ALU.mult)
                yield
                S.op('dve', 'tensor_reduce', ['OH2'], ['af'], out=af[:], in_=OH2[:], axis=AX.X, op=ALU.add)
                yield
                S.op('dve', 'scalar_tensor_tensor', ['af', 'TPf'], ['bf'], out=bf[:].rearrange("p a b -> p (a b)"),
                     in0=af[:].rearrange("p a b -> p (a b)"), scalar=-16.0, in1=TPf[:].rearrange("p a b -> p (a b)"), op0=ALU.mult, op1=ALU.add)
                yield
                S.op('dve', 'tensor_tensor', ['bf', 'iotaf'], ['OH'], out=OH[:], in0=iota16_b, in1=bf[:].unsqueeze(3).to_broadcast(B4), op=ALU.is_equal)
                yield
                S.op('dve', 'tensor_tensor', ['OH', 'I2f'], ['OH2'], out=OH2[:], in0=OH[:], in1=I2f[:].unsqueeze(2).to_broadcast(B4), op=ALU.mult)
                yield
                S.op('dve', 'tensor_reduce', ['OH2'], ['jF'], out=jF[:].rearrange("p (h r) -> p h r", h=8), in_=OH2[:], axis=AX.X, op=ALU.add)
                yield
                for k3, (src, sk, dst, dk) in enumerate(((iF, 'iF', iT, 'iT'), (jF, 'jF', jT, 'jT'), (gFt, 'gFt', gT, 'gT'))):
                    S.op('pe', 'transpose', [sk, 'identf'], ['gps0'], out=gps[0][:, k3, :], in_=src[:], identity=identf[:])
                for k3, (src, sk, dst, dk) in enumerate(((iF, 'iF', iT, 'iT'), (jF, 'jF', jT, 'jT'), (gFt, 'gFt', gT, 'gT'))):
                    S.op('act', 'copy', ['gps0'], [dk], out=dst[:, g * 128:(g + 1) * 128], in_=gps[0][:, k3, :])
                yield

        def drain(gen, n=None):
            k = 0
            for _ in gen:
                k += 1
                if n is not None and k >= n:
                    return False
            return True

        cur = prep(0, 0)
        n_yield = sum(1 for _ in cur)
        per_it = max(1, -(-n_yield // max(1, n_iblk - 6)))
        for ti in range(ntile):
            par = ti % 2
            xk = 'xt%d' % par
            for i0 in range(min(NSLOT - 1, n_iblk)):
                wdma(i0)
            for t in range(256):
                r = t % NR
                bk = (t // 4) % 2
                S.op('dve', 'tensor_scalar', ['iotab', 'iT'], ['OI%d' % r], out=OI[r][:], in0=iotab[:], scalar1=iT[:, t:t + 1], scalar2=None,
                     op0=ALU.is_equal)
                S.op('dve', 'tensor_scalar', ['iotab', 'jT', 'gT'], ['OJ%d' % r], out=OJ[r][:], in0=iotab[:], scalar1=jT[:, t:t + 1],
                     scalar2=gT[:, t:t + 1], op0=ALU.is_equal, op1=ALU.mult)
                S.op('pe', 'matmul', ['OI%d' % r, 'OJ%d' % r], ['gps%d' % bk], gps[bk][:, t % 4, :], lhsT=OJ[r][:], rhs=OI[r][:], start=True, stop=True)
                if t % 4 == 3:
                    S.op('act', 'copy', ['gps%d' % bk], ['Gbuf'], out=Gbuf[:, t - 3:t + 1, :], in_=gps[bk][:])
            nxt = prep(ti + 1, 1 - par) if ti + 1 < ntile else None
            stage1(0, par)
            for i in range(n_iblk):
                if i + NSLOT - 1 < n_iblk:
                    wdma(i + NSLOT - 1)
                if i + 1 < n_iblk:
                    stage1(i + 1, par)
                sl = i % NSLOT
                hv = i % 2
                S.op('act', 'activation', ['aps%d' % hv], ['hf%d' % hv], out=hf[hv][:], in_=aps[hv][:, 0:256], func=AF.Gelu)
                S.op('dve', 'tensor_tensor', ['hf%d' % hv, 'Gbuf'], ['hG%d' % hv], out=hG[hv][:], in0=hf[hv][:], in1=Gbuf[:, :, i], op=ALU.mult)
                for tg in range(2):
                    for dh in range(2):
                        S.op('pe', 'matmul', ['hG%d' % hv, 'Vb%d' % sl], ['ops%d' % (tg * 2 + dh)], ops[tg * 2 + dh][:],
                             lhsT=hG[hv][:, tg * 128:(tg + 1) * 128], rhs=Vb[sl][:, dh * 512:(dh + 1) * 512], start=(i == 0), stop=(i == n_iblk - 1))
                if nxt is not None and i >= 2:
                    if drain(nxt, per_it):
                        nxt = None
            if nxt is not None:
                drain(nxt)
            for tg in range(2):
                for dh in range(2):
                    S.op('dve', 'tensor_tensor', ['ops%d' % (tg * 2 + dh), xk], ['yo'], out=yo[:, tg, dh * 512:(dh + 1) * 512], in0=ops[tg * 2 + dh][:],
                         in1=xt[par][:, tg, dh * 512:(dh + 1) * 512], op=ALU.add)
            if final:
                for tg in range(2):
                    rmsnorm_tile(S, yo[:, tg, :], 'yo', gFin, 'gFin', xt[par][:, tg, :], xk, tmp, ssq, rstd, 'f')
                S.dma('sp', [xk], [], out=x_out[ti], in_=xt[par][:])
            else:
                S.dma('sp', ['yo'], [], out=x_out[ti], in_=yo[:])
        S.end_phase()


def mixer_a(nc, S, D, own):
    t0, nt = own
    with ExitStack() as es:
        C = Ctx(nc, es)
        sb, ps = C.sb, C.ps
        identf = sb("identf", [128, 128], F32)
        identb = sb("identb", [128, 128], BF16)
        Win = sb("Win", [128, 8, 5632], BF16)
        gB = sb("gB", [128, 1024], F32)
        tmp = sb("tmp", [128, 1024], BF16)
        ssq = sb("ssq", [128, 1], F32)
        rstd = sb("rstd", [128, 1], F32)
        xn = sb("xn", [128, 1024], BF16)
        xnT = sb("xnT", [128, 8, 128], BF16)
        t1 = sb("t1", [128, 8, 32], F32)
        t2 = sb("t2", [128, 8, 32], F32)
        NB = 2
        xt = [sb(f"xt{r}", [128, 1024], F32) for r in range(NB)]
        cs = [sb(f"cs{r}", [128, 2, 32], F32) for r in range(NB)]
        Pp = [sb(f"Pp{r}", [128, 256], F32) for r in range(NB)]
        rin = [sb(f"rin{r}", [128, 8, 64], F32) for r in range(NB)]
        rot = [sb(f"rot{r}", [128, 8, 64], F32) for r in range(NB)]
        Rq = [sb(f"Rq{r}", [128, 256], BF16) for r in range(NB)]
        Rk = [sb(f"Rk{r}", [128, 256], BF16) for r in range(NB)]
        Rv = [sb(f"Rv{r}", [128, 512], BF16) for r in range(NB)]
        Rg = [sb(f"Rg{r}", [128, 512], F32) for r in range(NB)]
        Nq = [sb(f"Nq{r}", [128, 256], BF16) for r in range(NB)]
        Nk = [sb(f"Nk{r}", [128, 256], BF16) for r in range(NB)]
        Nv = [sb(f"Nv{r}", [128, 256], BF16) for r in range(NB)]
        Gt = [sb(f"Gt{r}", [128, 3072], F32) for r in range(NB)]
        pp = [ps(f"pp{r}", [128, 512], F32) for r in range(4)]
        tps = ps("tps", [128, 8, 128], BF16)

        S.dma('sp', [], ['identf'], out=identf[:], in_=D['ident'])
        S.dma('sp', [], ['gB'], out=gB[:], in_=D['g'].partition_broadcast(128))
        w_v = D['w_in'].rearrange("(c p) n -> p c n", p=128)
        for k in range(11):
            S.dma('pool', [], ['Win%d' % k], out=Win[:, :, k * 512:(k + 1) * 512], in_=w_v[:, :, k * 512:(k + 1) * 512])
        S.op('dve', 'tensor_copy', ['identf'], ['identb'], out=identb[:], in_=identf[:])
        B3 = [128, 8, 32]
        kc = 0
        import os as _os
        for i in range(int(_os.environ.get('MA_NT', '32'))):
            b = i % NB
            sfx = str(b)
            is_own = t0 <= i < t0 + nt
            rows = slice(i * 128, (i + 1) * 128)
            S.dma('sp', [], ['xt' + sfx], out=xt[b][:], in_=D['x_full'][rows, :])
            S.dma('sp', [], ['cs' + sfx], out=cs[b][:, 0, :], in_=D['cos'][rows, :])
            S.dma('sp', [], ['cs' + sfx], out=cs[b][:, 1, :], in_=D['sin'][rows, :])
            _sub = int(_os.environ.get('MA_SUB', '9'))
            if _sub <= 2:
                S.end_phase()
                return
            rmsnorm_tile(S, xt[b][:], 'xt' + sfx, gB, 'gB', xn[:], 'xn', tmp, ssq, rstd, 'a')
            if _sub <= 3:
                S.end_phase()
                return
            for c in range(8):
                S.op('pe', 'transpose', ['xn', 'identb'], ['tps'], out=tps[:, c, :], in_=xn[:, c * 128:(c + 1) * 128], identity=identb[:])
            S.op('act', 'copy', ['tps'], ['xnT'], out=xnT[:], in_=tps[:])
            if _sub <= 4:
                S.end_phase()
                return
            chunks = list(range(11)) if is_own else [0, 1, 2, 4]
            _lvl = int(_os.environ.get('MA_LVL', '9'))
            if _lvl == 0:
                chunks = [0]
            for ci in chunks:
                bank = pp[kc % 4]
                bk = 'pp%d' % (kc % 4)
                kc += 1
                for dc in range(8):
                    S.op('pe', 'matmul', ['xnT', 'Win%d' % ci], [bk], bank[:], lhsT=xnT[:, dc, :], rhs=Win[:, dc, ci * 512:(ci + 1) * 512],
                         start=(dc == 0), stop=(dc == 7))
                lo, hi = bank[:, 0:256], bank[:, 256:512]
                rq = rin[b][:, 0:4, :].rearrange("p a b -> p (a b)")
                rk = rin[b][:, 4:8, :].rearrange("p a b -> p (a b)")
                if ci == 0:
                    S.op('act', 'copy', [bk], ['Pp' + sfx], out=Pp[b][:], in_=lo)
                    S.op('dve', 'tensor_copy', [bk], ['rin' + sfx], out=rq, in_=hi)
                elif ci == 1:
                    S.op('dve', 'tensor_copy', [bk], ['rin' + sfx], out=rk, in_=lo)
                    S.op('act', 'copy', [bk], ['Rv' + sfx], out=Rv[b][:, 0:256], in_=hi)
                elif ci == 2:
                    S.op('act', 'copy', [bk], ['Rv' + sfx], out=Rv[b][:, 256:512], in_=lo)
                    if is_own:
                        S.op('act', 'activation', [bk], ['Rg' + sfx], out=Rg[b][:, 0:256], in_=hi, func=AF.Silu)
                elif ci == 3:
                    S.op('act', 'activation', [bk], ['Rg' + sfx], out=Rg[b][:, 256:512], in_=lo, func=AF.Silu)
                    S.op('dve', 'tensor_copy', [bk], ['Nq' + sfx], out=Nq[b][:], in_=hi)
                elif ci == 4:
                    S.op('dve', 'tensor_copy', [bk], ['Nk' + sfx], out=Nk[b][:], in_=lo)
                    S.op('act', 'copy', [bk], ['Nv' + sfx], out=Nv[b][:], in_=hi)
                else:
                    S.op('act', 'activation', [bk], ['Gt' + sfx], out=Gt[b][:, (ci - 5) * 512:(ci - 4) * 512], in_=bank[:], func=AF.Sigmoid)
            if _lvl == 0:
                S.dma('sp', ['Pp' + sfx], [], out=D['Pp'][rows, :], in_=Pp[b][:])
                continue
            x1, x2 = rin[b][:, :, 0:32], rin[b][:, :, 32:64]
            cosb = cs[b][:, 0, :].unsqueeze(1).to_broadcast(B3)
            sinb = cs[b][:, 1, :].unsqueeze(1).to_broadcast(B3)
            rk_ = ['rin' + sfx, 'cs' + sfx]
            S.op('dve', 'tensor_tensor', rk_, ['t1'], out=t1[:], in0=x1, in1=cosb, op=ALU.mult)
            S.op('dve', 'tensor_tensor', rk_, ['t2'], out=t2[:], in0=x2, in1=sinb, op=ALU.mult)
            S.op('dve', 'tensor_tensor', ['t1', 't2'], ['rot' + sfx], out=rot[b][:, :, 0:32], in0=t1[:], in1=t2[:], op=ALU.subtract)
            S.op('dve', 'tensor_tensor', rk_, ['t1'], out=t1[:], in0=x1, in1=sinb, op=ALU.mult)
            S.op('dve', 'tensor_tensor', rk_, ['t2'], out=t2[:], in0=x2, in1=cosb, op=ALU.mult)
            S.op('dve', 'tensor_tensor', ['t1', 't2'], ['rot' + sfx], out=rot[b][:, :, 32:64], in0=t1[:], in1=t2[:], op=ALU.add)
            S.op('act', 'copy', ['rot' + sfx], ['Rq' + sfx], out=Rq[b][:], in_=rot[b][:, 0:4, :].rearrange("p a b -> p (a b)"))
            S.op('act', 'mul', ['rot' + sfx], ['Rk' + sfx], Rk[b][:], rot[b][:, 4:8, :].rearrange("p a b -> p (a b)"), 0.125)
            S.dma('sp', ['Pp' + sfx], [], out=D['Pp'][rows, :], in_=Pp[b][:])
            S.dma('sp', ['Rk' + sfx], [], out=D['Rk'][rows, :], in_=Rk[b][:])
            S.dma('sp', ['Rv' + sfx], [], out=D['Rv'][rows, :], in_=Rv[b][:])
            S.dma('sp', ['Nk' + sfx], [], out=D['Nk'][rows, :], in_=Nk[b][:])
            S.dma('sp', ['Nv' + sfx], [], out=D['Nv'][rows, :], in_=Nv[b][:])
            if is_own:
                S.dma('sp', ['Rq' + sfx], [], out=D['Rq'][rows, :], in_=Rq[b][:])
                S.dma('sp', ['Rg' + sfx], [], out=D['Rg'][rows, :], in_=Rg[b][:])
                S.dma('sp', ['Nq' + sfx], [], out=D['Nq'][rows, :], in_=Nq[b][:])
                S.dma('sp', ['Gt' + sfx], [], out=D['Gt'][rows, :], in_=Gt[b][:])
        S.end_phase()


def mixer_b(nc, S, D, own):
    t0, nt = own
    with ExitStack() as es:
        C = Ctx(nc, es)
        sb, ps = C.sb, C.ps
        identf = sb("identf", [128, 128], F32)
        identb = sb("identb", [128, 128], BF16)
        Wbp = sb("Wbp", [64, 4, 1024], BF16)
        Wbr = sb("Wbr", [128, 4, 1024], BF16)
        Wbn = sb("Wbn", [128, 2, 1024], BF16)
        Wout = sb("Wout", [128, 8, 1024], BF16)
        poolw = sb("poolw", [64, 4, 64], F32)
        pscale = sb("pscale", [64, 4], F32)
        amat = sb("amat", [128, 4, 5, 128], F32)
        Et = [sb(f"Et{r}", [128, 5, 4, 128], BF16) for r in range(5)]
        rstage = sb("rstage", [128, 5, 4, 128], F32)
        nmask = sb("nmask", [128, 5, 128], F32)
        dl = sb("dl", [128, 8], F32)
        lg = sb("lg", [128, 8], F32)
        pos = sb("pos", [128, 2], F32)
        zf = sb("zf", [128, 4], F32)
        zb = sb("zb", [128, 4], F32)
        dC = sb("dC", [128, 8], F32)
        dmat = sb("dmat", [128, 4, 128], F32)
        xrow = sb("xrow", [128, 2, 128], F32)
        Dt = sb("Dt", [128, 4, 128], F32)
        E1 = sb("E1", [128, 128], F32)
        E2 = sb("E2", [128, 128], F32)
        XF = sb("XF", [64, 4, 128], F32)
        XB = sb("XB", [64, 4, 128], F32)
        Sst = sb("Sst", [64, 4, 128], F32)
        Sob = [sb(f"Sob{r}", [64, 4, 128], BF16) for r in range(2)]
        kd = sb("kd", [128, 4, 64], BF16)
        NB = 2
        xt = [sb(f"xt{r}", [128, 1024], F32) for r in range(NB)]
        Gt = [sb(f"Gt{r}", [128, 3072], F32) for r in range(NB)]
        Rq = [sb(f"Rq{r}", [128, 256], BF16) for r in range(NB)]
        Rk = [sb(f"Rk{r}", [128, 256], BF16) for r in range(NB)]
        Rv = [sb(f"Rv{r}", [128, 512], BF16) for r in range(NB)]
        Rg = [sb(f"Rg{r}", [128, 512], F32) for r in range(NB)]
        SFt = [sb(f"SFt{r}", [64, 4, 128], BF16) for r in range(NB)]
        SBt = [sb(f"SBt{r}", [64, 4, 128], BF16) for r in range(NB)]
        Nq = [sb(f"Nq{r}", [128, 256], BF16) for r in range(NB)]
        Nk = [[sb(f"Nk{r}_{o}", [128, 256], BF16) for o in range(5)] for r in range(NB)]
        Nv1 = [[sb(f"Nv{r}_{o}", [128, 4, 65], BF16) for o in range(5)] for r in range(NB)]
        Pp = [[sb(f"Pp{r}_{o}", [128, 256], F32) for o in range(3)] for r in range(NB)]
        qkT = sb("qkT", [64, 8, 128], BF16)
        qfT = sb("qfT", [64, 4, 128], BF16)
        qbT = sb("qbT", [64, 4, 128], BF16)
        PD = sb("PD", [128, 4, 128], BF16)
        ysq = sb("ysq", [128, 4, 128], F32)
        ssq4 = sb("ssq4", [128, 4], F32)
        rs4 = sb("rs4", [128, 4], F32)
        yr = sb("yr", [128, 4, 128], F32)
        yrb = sb("yrb", [128, 512], BF16)
        yrT = sb("yrT", [128, 4, 128], BF16)
        NTa = sb("NTa", [64, 24, 128], BF16)
        Pm = sb("Pm", [128, 4, 128], BF16)
        PEm = sb("PEm", [128, 4, 128], BF16)
        rden = sb("rden", [128, 4], F32)
        ynb = sb("ynb", [128, 4, 64], BF16)
        ynT = sb("ynT", [128, 2, 128], BF16)
        dT = sb("dT", [64, 4, 128], F32)
        ypT = sb("ypT", [64, 4, 128], BF16)
        mg = sb("mg", [128, 1024], F32)
        tg_ = sb("tg", [128, 1024], F32)
        mgb = sb("mgb", [128, 1024], BF16)
        mT = sb("mT", [128, 8, 128], BF16)
        xo = [sb(f"xo{r}", [128, 1024], F32) for r in range(NB)]
        bank = [ps(f"bk{r}", [128, 512], F32) for r in range(8)]
        sc_ps = bank[0][:].rearrange("p (h c) -> p h c", h=4)
        y_ps = bank[1][:].rearrange("p (h c) -> p h c", h=4)
        st_ps = [bank[2][:].rearrange("p (h c) -> p h c", h=4), bank[6][:].rearrange("p (h c) -> p h c", h=4)]
        st_k = ['bk2', 'bk6']
        o_ps = bank[3][:, 0:260].rearrange("p (h c) -> p h c", h=4)
        pl_ps = bank[4][0:64, :].rearrange("p (h c) -> p h c", h=4)
        tr_ps = bank[5][:].bitcast(BF16).rearrange("p (c t) -> p c t", c=8)

        S.dma('sp', [], ['identf'], out=identf[:], in_=D['ident'])
        S.op('dve', 'tensor_copy', ['identf'], ['identb'], out=identb[:], in_=identf[:])
        for g in range(4):
            S.dma('pool', [], ['Wbp'], out=Wbp[:, g, :], in_=D['wbp'][:, g, :])
        S.dma('pool', [], ['Wbr'], out=Wbr[:], in_=D['wbr'].rearrange("(h e) n -> e h n", e=128))
        S.dma('pool', [], ['Wbn'], out=Wbn[:], in_=D['wbn'].rearrange("(h e) n -> e h n", e=128))
        S.dma('pool', [], ['Wout'], out=Wout[:], in_=D['wout'].rearrange("(h e) n -> e h n", e=128))
        S.dma('sp', [], ['poolw'], out=poolw[:], in_=D['poolw'])
        S.dma('sp', [], ['pscale'], out=pscale[:], in_=D['pscale'])
        S.dma('sp', [], ['amat'], out=amat[:], in_=D['amat'])
        S.dma('sp', [], ['dl'], out=dl[:], in_=D['decay'].partition_broadcast(128))
        S.dma('sp', [], ['pos'], out=pos[:], in_=D['pos'])
        S.dma('sp', [], ['dmat'], out=dmat[:], in_=D['dmat'].rearrange("k p c -> p k c"))
        S.dma('sp', [], ['xrow'], out=xrow[:], in_=D['xrow'])
        for r in range(NB):
            for o in range(5):
                S.op('dve', 'memset', [], ['Nv%d_%d' % (r, o)], Nv1[r][o][:], 1.0)
        for ty in range(5):
            S.dma('sp', [], ['rstage'], out=rstage[:], in_=D['rpbT'][ty])
            S.dma('sp', [], ['nmask'], out=nmask[:], in_=D['nmask'][ty])
            S.op('act', 'activation', ['rstage'], ['rstage'], out=rstage[:], in_=rstage[:], func=AF.Exp)
            S.op('dve', 'tensor_tensor', ['rstage', 'nmask'], ['Et%d' % ty], out=Et[ty][:], in0=rstage[:],
                 in1=nmask[:].unsqueeze(2).to_broadcast([128, 5, 4, 128]), op=ALU.mult)
        S.op('act', 'activation', ['dl'], ['lg'], out=lg[:], in_=dl[:], func=AF.Exp, scale=-1.0)
        S.op('act', 'activation', ['lg'], ['lg'], out=lg[:], in_=lg[:], func=AF.Ln, bias=1.0)
        S.op('act', 'mul', ['lg'], ['lg'], lg[:], lg[:], -1.0)
        S.op('act', 'activation', ['lg', 'pos'], ['zf'], out=zf[:], in_=lg[:, 0:4], func=AF.Exp, scale=pos[:, 0:1])
        S.op('act', 'activation', ['lg', 'pos'], ['zb'], out=zb[:], in_=lg[:, 4:8], func=AF.Exp, scale=pos[:, 1:2])
        S.op('act', 'activation', ['lg'], ['dC'], out=dC[:], in_=lg[:], func=AF.Exp, scale=128.0)
        for h in range(4):
            S.op('act', 'activation', ['lg', 'dmat'], ['E1'], out=E1[:], in_=dmat[:, 0, :], func=AF.Exp, scale=lg[:, h:h + 1])
            S.op('act', 'activation', ['lg', 'dmat'], ['E2'], out=E2[:], in_=dmat[:, 2, :], func=AF.Exp, scale=lg[:, 4 + h:5 + h])
            S.op('dve', 'tensor_tensor', ['E1', 'dmat'], ['E1'], out=E1[:], in0=E1[:], in1=dmat[:, 1, :], op=ALU.mult)
            S.op('dve', 'tensor_tensor', ['E2', 'dmat'], ['E2'], out=E2[:], in0=E2[:], in1=dmat[:, 3, :], op=ALU.mult)
            S.op('dve', 'tensor_tensor', ['E1', 'E2'], ['Dt'], out=Dt[:, h, :], in0=E1[:], in1=E2[:], op=ALU.add)
            S.op('act', 'activation', ['lg', 'xrow'], ['XF'], out=XF[:, h, :], in_=xrow[0:64, 0, :], func=AF.Exp, scale=lg[0:64, h:h + 1])
            S.op('act', 'activation', ['lg', 'xrow'], ['XB'], out=XB[:, h, :], in_=xrow[0:64, 1, :], func=AF.Exp, scale=lg[0:64, 4 + h:5 + h])

        def sweep(order, zt, zk, dcol, dst, dkey, store_pred, upd_pred):
            S.op('dve', 'memset', [], ['Sst'], Sst[:], 0.0)
            for cnt, n in enumerate(order):
                b = cnt % NB
                sfx = str(b)
                rows = slice(n * 128, (n + 1) * 128)
                if store_pred(n):
                    S.op('act', 'copy', ['Sst'], ['Sob' + sfx], out=Sob[b][:], in_=Sst[:])
                    S.dma('sp', ['Sob' + sfx], [dkey + str(n)], out=dst[n], in_=Sob[b][:])
                if not upd_pred(n):
                    continue
                S.dma('sp', [], ['Rk' + sfx], out=Rk[b][:], in_=D['Rk'][rows, :])
                S.dma('sp', [], ['Rv' + sfx], out=Rv[b][:], in_=D['Rv'][rows, :])
                S.op('dve', 'tensor_tensor', ['Rk' + sfx, zk], ['kd'], out=kd[:], in0=Rk[b][:].rearrange("p (h d) -> p h d", h=4),
                     in1=zt[:].unsqueeze(2).to_broadcast([128, 4, 64]), op=ALU.mult)
                for h in range(4):
                    S.op('pe', 'matmul', ['kd', 'Rv' + sfx], ['bk0'], bank[0][0:64, h * 128:(h + 1) * 128], lhsT=kd[:, h, :],
                         rhs=Rv[b][:, h * 128:(h + 1) * 128], start=True, stop=True)
                S.op('dve', 'tensor_tensor', ['Sst', 'dC'], ['Sst'], out=Sst[:], in0=Sst[:],
                     in1=dC[0:64, dcol:dcol + 4].unsqueeze(2).to_broadcast([64, 4, 128]), op=ALU.mult)
                S.op('dve', 'tensor_tensor', ['Sst', 'bk0'], ['Sst'], out=Sst[:], in0=Sst[:],
                     in1=bank[0][0:64, :].rearrange("p (h c) -> p h c", h=4), op=ALU.add)

        last = t0 + nt - 1
        sweep(list(range(0, last + 1)), zf, 'zf', 0, D['SF'], 'dSF', lambda n: n >= t0, lambda n: n < last)
        sweep(list(range(31, t0 - 1, -1)), zb, 'zb', 4, D['SB'], 'dSB', lambda n: n <= last, lambda n: n > t0)

        B4 = [128, 4, 128]
        for it in range(nt):
            i = t0 + it
            b = it % NB
            sfx = str(b)
            rows = slice(i * 128, (i + 1) * 128)
            ty = {0: 1, 1: 2, 30: 3, 31: 4}.get(i, 0)
            offs = [o for o in range(NA_OMIN[ty], NA_OMIN[ty] + 5) if 0 <= i + o <= 31]
            poffs = [o for o in range(-1, 2) if 0 <= i + o <= 31]
            S.dma('sp', [], ['xt' + sfx], out=xt[b][:], in_=D['x_full'][rows, :])
            S.dma('sp', [], ['Gt' + sfx], out=Gt[b][:], in_=D['Gt'][rows, :])
            S.dma('sp', [], ['Rq' + sfx], out=Rq[b][:], in_=D['Rq'][rows, :])
            S.dma('sp', [], ['Rk' + sfx], out=Rk[b][:], in_=D['Rk'][rows, :])
            S.dma('sp', [], ['Rv' + sfx], out=Rv[b][:], in_=D['Rv'][rows, :])
            S.dma('sp', [], ['Rg' + sfx], out=Rg[b][:], in_=D['Rg'][rows, :])
            S.dma('sp', ['dSF%d' % i], ['SFt' + sfx], out=SFt[b][:], in_=D['SF'][i])
            S.dma('sp', ['dSB%d' % i], ['SBt' + sfx], out=SBt[b][:], in_=D['SB'][i])
            S.dma('sp', [], ['Nq' + sfx], out=Nq[b][:], in_=D['Nq'][rows, :])
            for oi, o in enumerate(offs):
                r2 = slice((i + o) * 128, (i + o + 1) * 128)
                S.dma('sp', [], ['Nk%s_%d' % (sfx, oi)], out=Nk[b][oi][:], in_=D['Nk'][r2, :])
                S.dma('sp', [], ['Nv%s_%d' % (sfx, oi)], out=Nv1[b][oi][:, :, 0:64], in_=D['Nv'][r2, :].rearrange("p (h d) -> p h d", h=4))
            for oi, o in enumerate(poffs):
                r2 = slice((i + o) * 128, (i + o + 1) * 128)
                S.dma('sp', [], ['Pp%s_%d' % (sfx, oi)], out=Pp[b][oi][:], in_=D['Pp'][r2, :])

            for h in range(4):
                S.op('pe', 'transpose', ['Rq' + sfx, 'identb'], ['bk5'], out=tr_ps[0:64, h, :], in_=Rq[b][:, h * 64:(h + 1) * 64], identity=identb[:])
                S.op('pe', 'transpose', ['Rk' + sfx, 'identb'], ['bk5'], out=tr_ps[0:64, 4 + h, :], in_=Rk[b][:, h * 64:(h + 1) * 64], identity=identb[:])
            S.op('act', 'copy', ['bk5'], ['qkT'], out=qkT[:], in_=tr_ps[0:64, :, :])
            S.op('dve', 'tensor_tensor', ['qkT', 'XF'], ['qfT'], out=qfT[:], in0=qkT[:, 0:4, :], in1=XF[:], op=ALU.mult)
            S.op('dve', 'tensor_tensor', ['qkT', 'XB'], ['qbT'], out=qbT[:], in0=qkT[:, 0:4, :], in1=XB[:], op=ALU.mult)
            for h in range(4):
                S.op('pe', 'matmul', ['qkT'], ['bk0'], sc_ps[:, h, :], lhsT=qkT[:, 4 + h, :], rhs=qkT[:, h, :], start=True, stop=True)
            S.op('dve', 'tensor_tensor', ['bk0', 'Dt'], ['PD'], out=PD[:], in0=sc_ps, in1=Dt[:], op=ALU.mult)
            for h in range(4):
                S.op('pe', 'matmul', ['PD', 'Rv' + sfx], ['bk1'], y_ps[:, h, :], lhsT=PD[:, h, :], rhs=Rv[b][:, h * 128:(h + 1) * 128],
                     start=(h == 0), stop=False, skip_group_check=True)
                S.op('pe', 'matmul', ['qfT', 'SFt' + sfx], ['bk1'], y_ps[:, h, :], lhsT=qfT[:, h, :], rhs=SFt[b][:, h, :],
                     start=False, stop=False, skip_group_check=True)
                S.op('pe', 'matmul', ['qbT', 'SBt' + sfx], ['bk1'], y_ps[:, h, :], lhsT=qbT[:, h, :], rhs=SBt[b][:, h, :],
                     start=False, stop=(h == 3), skip_group_check=True)
            S.op('act', 'activation', ['bk1'], ['ysq'], out=ysq[:], in_=y_ps, func=AF.Square)
            S.op('dve', 'tensor_reduce', ['ysq'], ['ssq4'], out=ssq4[:], in_=ysq[:], axis=AX.X, op=ALU.add)
            S.op('act', 'activation', ['ssq4'], ['rs4'], out=rs4[:], in_=ssq4[:], func=AF.Sqrt, scale=1.0 / 128, bias=EPS)
            S.op('dve', 'reciprocal', ['rs4'], ['rs4'], out=rs4[:], in_=rs4[:])
            S.op('dve', 'tensor_tensor', ['bk1', 'rs4'], ['yr'], out=yr[:], in0=y_ps, in1=rs4[:].unsqueeze(2).to_broadcast(B4), op=ALU.mult)
            S.op('dve', 'tensor_tensor', ['yr', 'Rg' + sfx], ['yrb'], out=yrb[:], in0=yr[:].rearrange("p h c -> p (h c)"), in1=Rg[b][:], op=ALU.mult)
            for h in range(4):
                S.op('pe', 'transpose', ['yrb', 'identb'], ['bk5'], out=tr_ps[:, h, :], in_=yrb[:, h * 128:(h + 1) * 128], identity=identb[:])
            S.op('act', 'copy', ['bk5'], ['yrT'], out=yrT[:], in_=tr_ps[:, 0:4, :])

            srcs = [(Nq[b], 'Nq' + sfx)] + [(Nk[b][oi], 'Nk%s_%d' % (sfx, oi)) for oi in range(len(offs))]
            for r0 in range(0, len(srcs), 2):
                grp = srcs[r0:r0 + 2]
                for gi, (src, sk) in enumerate(grp):
                    for h in range(4):
                        S.op('pe', 'transpose', [sk, 'identb'], ['bk5'], out=tr_ps[0:64, gi * 4 + h, :], in_=src[:, h * 64:(h + 1) * 64], identity=identb[:])
                n_ = 4 * len(grp)
                S.op('act', 'copy', ['bk5'], ['NTa'], out=NTa[:, r0 * 4:r0 * 4 + n_, :], in_=tr_ps[0:64, 0:n_, :])
            for oi, o in enumerate(offs):
                sp_, sk_ = st_ps[oi % 2], st_k[oi % 2]
                for h in range(4):
                    S.op('pe', 'matmul', ['NTa'], [sk_], sp_[:, h, :], lhsT=NTa[:, 4 + 4 * oi + h, :], rhs=NTa[:, h, :], start=True, stop=True)
                S.op('act', 'activation', [sk_], ['Pm'], out=Pm[:], in_=sp_, func=AF.Exp, scale=0.125)
                S.op('dve', 'tensor_tensor', ['Pm', 'Et%d' % ty], ['PEm'], out=PEm[:], in0=Pm[:], in1=Et[ty][:, o - NA_OMIN[ty], :, :], op=ALU.mult)
                for h in range(4):
                    S.op('pe', 'matmul', ['PEm', 'Nv%s_%d' % (sfx, oi)], ['bk3'], o_ps[:, h, :], lhsT=PEm[:, h, :], rhs=Nv1[b][oi][:, h, :],
                         start=(oi == 0 and h == 0), stop=(oi == len(offs) - 1 and h == 3), skip_group_check=True)
            S.op('dve', 'reciprocal', ['bk3'], ['rden'], out=rden[:], in_=o_ps[:, :, 64:65].rearrange("p h c -> p (h c)"))
            S.op('dve', 'tensor_tensor', ['bk3', 'rden'], ['ynb'], out=ynb[:], in0=o_ps[:, :, 0:64], in1=rden[:].unsqueeze(2).to_broadcast([128, 4, 64]), op=ALU.mult)
            ynf = ynb[:].rearrange("p h d -> p (h d)")
            for c in range(2):
                S.op('pe', 'transpose', ['ynb', 'identb'], ['bk5'], out=tr_ps[:, c, :], in_=ynf[:, c * 128:(c + 1) * 128], identity=identb[:])
            S.op('act', 'copy', ['bk5'], ['ynT'], out=ynT[:], in_=tr_ps[:, 0:2, :])

            for g in range(4):
                for oi, o in enumerate(poffs):
                    kind = {-1: 0, 1: 2}.get(o, 3 if i == 0 else (4 if i == 31 else 1))
                    S.op('pe', 'matmul', ['Pp%s_%d' % (sfx, oi), 'amat'], ['bk4'], pl_ps[:, g, :], lhsT=Pp[b][oi][:, g * 64:(g + 1) * 64],
                         rhs=amat[:, g, kind, :], start=(oi == 0), stop=(oi == len(poffs) - 1))
            S.op('act', 'copy', ['bk4'], ['dT'], out=dT[:], in_=pl_ps)
            for g in range(4):
                S.op('pe', 'matmul', ['dT', 'poolw'], ['bk4'], pl_ps[:, g, :], lhsT=poolw[:, g, :], rhs=dT[:, g, :], start=True, stop=True)
            S.op('dve', 'tensor_tensor', ['bk4', 'pscale'], ['ypT'], out=ypT[:], in0=pl_ps, in1=pscale[:].unsqueeze(2).to_broadcast([64, 4, 128]), op=ALU.mult)

            def branch(nk, lhs_list, w_tile, goff, first, lastb):
                for nh in range(2):
                    bkk = bank[6 + nh]
                    for kk in range(nk):
                        S.op('pe', 'matmul', lhs_list[1] + [w_tile[1]], ['bk%d' % (6 + nh)], bkk[:], lhsT=lhs_list[0][:, kk, :],
                             rhs=w_tile[0][:, kk, nh * 512:(nh + 1) * 512], start=(kk == 0), stop=(kk == nk - 1))
                    cols = slice(nh * 512, (nh + 1) * 512)
                    gsl = Gt[b][:, goff + nh * 512: goff + (nh + 1) * 512]
                    if first:
                        S.op('dve', 'tensor_tensor', ['bk%d' % (6 + nh), 'Gt' + sfx], ['mg'], out=mg[:, cols], in0=bkk[:], in1=gsl, op=ALU.mult)
                    else:
                        S.op('dve', 'tensor_tensor', ['bk%d' % (6 + nh), 'Gt' + sfx], ['tg'], out=tg_[:, cols], in0=bkk[:], in1=gsl, op=ALU.mult)
                        if lastb:
                            S.op('dve', 'tensor_tensor', ['mg', 'tg'], ['mgb'], out=mgb[:, cols], in0=mg[:, cols], in1=tg_[:, cols], op=ALU.add)
                        else:
                            S.op('dve', 'tensor_tensor', ['mg', 'tg'], ['mg'], out=mg[:, cols], in0=mg[:, cols], in1=tg_[:, cols], op=ALU.add)

            branch(4, (ypT, ['ypT']), (Wbp, 'Wbp'), 0, True, False)
            branch(4, (yrT, ['yrT']), (Wbr, 'Wbr'), 1024, False, False)
            branch(2, (ynT, ['ynT']), (Wbn, 'Wbn'), 2048, False, True)
            for c in range(8):
                S.op('pe', 'transpose', ['mgb', 'identb'], ['bk5'], out=tr_ps[:, c, :], in_=mgb[:, c * 128:(c + 1) * 128], identity=identb[:])
            S.op('act', 'copy', ['bk5'], ['mT'], out=mT[:], in_=tr_ps)
            for nh in range(2):
                bkk = bank[6 + nh]
                for dc in range(8):
                    S.op('pe', 'matmul', ['mT', 'Wout'], ['bk%d' % (6 + nh)], bkk[:], lhsT=mT[:, dc, :], rhs=Wout[:, dc, nh * 512:(nh + 1) * 512],
                         start=(dc == 0), stop=(dc == 7))
                cols = slice(nh * 512, (nh + 1) * 512)
                S.op('dve', 'tensor_tensor', ['bk%d' % (6 + nh), 'xt' + sfx], ['xo' + sfx], out=xo[b][:, cols], in0=bkk[:], in1=xt[b][:, cols], op=ALU.add)
            S.dma('sp', ['xo' + sfx], [], out=D['x_out'][it * 128:(it + 1) * 128, :], in_=xo[b][:])
        S.end_phase()


POOL_WINDOWS = (2, 4, 8, 16)
SEQ = 4096
NA_OMIN = (-2, 0, -2, -2, -3)
NA_NOFF = (5, 4, 4, 4, 4)


def _const_tables():
    T = {}
    T['ident'] = np.eye(128, dtype=np.float32)
    T['iota'] = np.tile(np.arange(128, dtype=np.float32)[None, :], (128, 1))
    T['cst16'] = np.tile((16 * np.arange(16, dtype=np.float32) + 7.5)[None, :], (128, 1))
    half = 32
    inv = (1.0 / (np.float32(10000.0) ** np.linspace(0.0, 1.0, half, dtype=np.float32))).astype(np.float32)
    ang = (np.arange(SEQ, dtype=np.float32)[:, None] * inv[None, :]).astype(np.float32)
    T['cos'] = np.cos(ang).astype(np.float32)
    T['sin'] = np.sin(ang).astype(np.float32)
    p = np.arange(128, dtype=np.float32)
    T['pos'] = np.stack([127.0 - p, p], axis=1).astype(np.float32)
    m = p[:, None]
    c = p[None, :]
    T['dmat'] = np.stack([np.maximum(c - m, 0), (c >= m).astype(np.float32), np.maximum(m - c, 0), (m > c).astype(np.float32)]).astype(np.float32)
    xr = np.stack([p + 1.0, 128.0 - p], axis=0)
    T['xrow'] = np.tile(xr[None], (128, 1, 1)).astype(np.float32)
    A = np.zeros((128, 4, 5, 128), np.float32)
    for g, w in enumerate(POOL_WINDOWS):
        lo, hi = w // 2, w - w // 2
        for kind, base in ((1, 1024), (3, 0), (4, SEQ - 128)):
            for t in range(128):
                ta = base + t
                a_, b_ = max(ta - lo, 0), min(ta + hi, SEQ)
                cnt = float(b_ - a_)
                for s in range(a_, b_):
                    rel = s - base
                    if 0 <= rel < 128:
                        A[rel, g, kind, t] += 1.0 / cnt
                    elif rel < 0 and kind == 1:
                        A[rel + 128, g, 0, t] += 1.0 / cnt
                    elif rel >= 128 and kind == 1:
                        A[rel - 128, g, 2, t] += 1.0 / cnt
                A[t, g, kind, t] -= 1.0
    T['amat'] = A
    di = np.zeros((5, 128, 5, 128), np.int64)
    dj = np.zeros((5, 128, 5, 128), np.int64)
    mk = np.zeros((5, 128, 5, 128), np.float32)
    q = np.arange(128)
    k = np.arange(128)
    for ty, i in enumerate((5, 0, 1, 30, 31)):
        for oi in range(5):
            o = NA_OMIN[ty] + oi
            j = i + o
            if j < 0 or j > 31:
                continue
            qr = 2 * i + q // 64
            qc = q % 64
            kr = (2 * j + k // 64)[:, None]
            kcol = (k % 64)[:, None]
            rs = np.clip(qr - 4, 0, 56)[None, :]
            vr = (kr >= rs) & (kr < rs + 8)
            cst = np.clip(qc - 8, 0, 48)[None, :]
            vc = (kcol >= cst) & (kcol < cst + 16)
            di[ty, :, oi, :] = np.clip(kr - qr[None, :] + 7, 0, 14)
            dj[ty, :, oi, :] = np.clip(kcol - qc[None, :], -15, 15) + 15
            mk[ty, :, oi, :] = (vr & vc).astype(np.float32)
    T['na_di'], T['na_dj'], T['nmask'] = di, dj, mk
    return T


_TABLES = None


def tables():
    global _TABLES
    if _TABLES is None:
        _TABLES = _const_tables()
    return _TABLES


def layer_layout(l, w_in, pool_w, pool_scale, ret_decay, na_rpb, w_br_pool, w_br_ret, w_br_na, w_out,
                 peer_w_query, peer_sub_keys, peer_u, peer_v):
    T = tables()
    L = {}
    L['w_in'] = np.ascontiguousarray(w_in[l])
    L['wbp'] = np.ascontiguousarray(w_br_pool[l].reshape(4, 64, 1024).transpose(1, 0, 2))
    L['wbr'] = np.ascontiguousarray(w_br_ret[l])
    L['wbn'] = np.ascontiguousarray(w_br_na[l])
    L['wout'] = np.ascontiguousarray(w_out[l])
    L['poolw'] = np.ascontiguousarray(pool_w[l].transpose(1, 0, 2))
    L['pscale'] = np.ascontiguousarray(pool_scale[l].reshape(4, 64).T)
    L['decay'] = np.ascontiguousarray(ret_decay[l].reshape(1, 8))
    rp = na_rpb[l]
    g = rp[:, T['na_di'], T['na_dj']]
    L['rpbT'] = np.ascontiguousarray(g.transpose(1, 2, 3, 0, 4)).astype(np.float32)
    L['wq'] = np.ascontiguousarray(peer_w_query[l].reshape(8, 128, 16, 128).transpose(2, 1, 0, 3))
    L['skT'] = np.ascontiguousarray(peer_sub_keys[l].transpose(2, 0, 1))
    L['uT'] = np.ascontiguousarray(peer_u[l].reshape(128, 128, 8, 128).transpose(0, 3, 2, 1))
    L['v'] = np.ascontiguousarray(peer_v[l])
    return L


N_CORES = 4
NTOK = 4096
DEPTH = 2
_PER_LAYER = ('g_mix', 'w_in', 'wbp', 'wbr', 'wbn', 'wout', 'poolw', 'pscale', 'decay', 'rpbT', 'g_ffn', 'wq', 'skT', 'uT', 'v')
_SHAPES = dict(g_mix=[1, 1024], w_in=[1024, 5632], wbp=[64, 4, 1024], wbr=[512, 1024], wbn=[256, 1024], wout=[1024, 1024],
               poolw=[64, 4, 64], pscale=[64, 4], decay=[1, 8], rpbT=[5, 128, 5, 4, 128], g_ffn=[1, 1024], wq=[16, 128, 8, 128],
               skT=[128, 2, 128], uT=[128, 128, 8, 128], v=[16384, 1024])
_CONST = dict(ident=[128, 128], iota=[128, 128], cst16=[128, 16], cos=[4096, 32], sin=[4096, 32], pos=[128, 2], dmat=[4, 128, 128],
              xrow=[128, 2, 128], amat=[128, 4, 5, 128], nmask=[5, 128, 5, 128])


def build_program(n_iblk=128):
    nc = bass.Bass("TRN2", target_bir_lowering=False)
    dt = lambda nm, shp, ty=F32, kind="ExternalInput": nc.dram_tensor(nm, list(shp), ty, kind=kind).ap()
    x = dt("x", [NTOK, 1024])
    y = dt("y", [NTOK, 1024], F32, "ExternalOutput")
    gfin = dt("gfin", [1, 1024])
    Cn = {k: dt(k, s) for k, s in _CONST.items()}
    W = [{k: dt(f"{k}_{l}", _SHAPES[k]) for k in _PER_LAYER} for l in range(DEPTH)]
    I = "Internal"
    Sc = dict(Pp=dt("s_Pp", [4096, 256], F32, I), Rq=dt("s_Rq", [4096, 256], BF16, I), Rk=dt("s_Rk", [4096, 256], BF16, I),
              Rv=dt("s_Rv", [4096, 512], BF16, I), Rg=dt("s_Rg", [4096, 512], F32, I), Nq=dt("s_Nq", [4096, 256], BF16, I),
              Nk=dt("s_Nk", [4096, 256], BF16, I), Nv=dt("s_Nv", [4096, 256], BF16, I), Gt=dt("s_Gt", [4096, 3072], F32, I),
              SF=dt("s_SF", [32, 64, 4, 128], BF16, I), SB=dt("s_SB", [32, 64, 4, 128], BF16, I))
    x1 = dt("s_x1", [NTOK, 1024], F32, I)
    uTb = dt("s_uTb", [128, 128, 1024], BF16, I)
    vb = dt("s_vb", [128, 128, 1024], BF16, I)
    x2 = dt("s_x2", [NTOK, 1024], F32, I)
    own = (0, NTOK // 128)
    with ExitStack() as es:
        S = Sched(nc, es)
        cur = x
        for l in range(DEPTH):
            D = dict(Cn)
            D.update(Sc)
            D.update(W[l])
            D.update(x_full=cur, g=W[l]['g_mix'], x_out=x1)
            mixer_a(nc, S, D, own)
            mixer_b(nc, S, D, own)
            last = (l == DEPTH - 1)
            P = dict(Cn)
            P.update(W[l])
            P.update(x_in=x1, x_out=(y if last else x2), g=W[l]['g_ffn'], gfin=gfin, uTb=uTb, vb=vb)
            peer_phase(nc, S, P, NTOK, n_iblk=n_iblk, final=last)
            cur = x2
    return nc


_NC_CACHE = {}


def kernel(x, norm_mix, w_in, pool_w, pool_scale, ret_decay, na_rpb, w_br_pool, w_br_ret, w_br_na, w_out, norm_ffn,
           peer_w_query, peer_sub_keys, peer_u, peer_v, norm_final):
    f = lambda a: np.ascontiguousarray(np.asarray(a), dtype=np.float32)
    x = f(x)
    T = tables()
    shared = {k: np.ascontiguousarray(T[k]) for k in _CONST}
    shared['gfin'] = f(norm_final).reshape(1, 1024)
    args = [f(a) for a in (w_in, pool_w, pool_scale, ret_decay, na_rpb, w_br_pool, w_br_ret, w_br_na, w_out,
                           peer_w_query, peer_sub_keys, peer_u, peer_v)]
    nm, nf = f(norm_mix), f(norm_ffn)
    for l in range(DEPTH):
        L = layer_layout(l, *args)
        L['g_mix'] = nm[l].reshape(1, 1024)
        L['g_ffn'] = nf[l].reshape(1, 1024)
        for k in _PER_LAYER:
            shared[f"{k}_{l}"] = L[k]
    if 'nc' not in _NC_CACHE:
        _NC_CACHE['nc'] = build_program()
    nc = _NC_CACHE['nc']
    in_maps = []
    for c in range(N_CORES):
        m = dict(shared)
        m['x'] = np.ascontiguousarray(x[c])
        in_maps.append(m)
    res = run_bass_kernel_spmd(nc, in_maps, core_ids=list(range(N_CORES)))
    out = np.stack([np.asarray(res.results[c]['y'], dtype=np.float32) for c in range(N_CORES)], axis=0)
    return out
```

```python
import numpy as np
from contextlib import ExitStack
import concourse.bass as bass
import concourse.mybir as mybir
from concourse.bass_utils import run_bass_kernel_spmd

F32 = mybir.dt.float32
BF16 = mybir.dt.bfloat16
U32 = mybir.dt.uint32
AF = mybir.ActivationFunctionType
ALU = mybir.AluOpType
AX = mybir.AxisListType

import re as _re
_PSUM_KEY = _re.compile(r"^(bk|pp|gps|aps|ops|tps)\d*$")
EPOCH = 20000
NDMA = 16
EPS = 1e-6


class _Eng:
    def __init__(self, name, strict):
        self.name = name
        self.strict = strict
        self.count = 0
        self.sems = []
        self.known = {}
        self.dsems = []
        self.dcount = []
        self.dnext = 0


class Sched:
    def __init__(self, nc, es):
        self.nc = nc
        self.es = es
        self.E = {
            'pe': _Eng('pe', False),
            'act': _Eng('act', True),
            'dve': _Eng('dve', True),
            'pool': _Eng('pool', True),
            'sp': _Eng('sp', False),
        }
        self.lastw = {}
        self.readers = {}
        self.nsem = 0
        self.prog = {k: [] for k in self.E}

    def _newsem(self, nm):
        self.nsem += 1
        return self.es.enter_context(self.nc.semaphore(f"{nm}_{self.nsem}"))

    def _cur_sem(self, e):
        ep = e.count // EPOCH
        while len(e.sems) <= ep:
            e.sems.append(self._newsem(e.name))
        return e.sems[ep]

    def _wait(self, e, dep):
        sem, val, src = dep
        if src is e and not e.strict:
            return
        k = id(sem)
        if e.known.get(k, 0) >= val:
            return
        self.prog[e.name].append(('w', sem, val))
        e.known[k] = val

    def _deps(self, reads, writes):
        deps = []
        for b in reads:
            if b in self.lastw:
                deps.append(self.lastw[b])
        for b in writes:
            if b in self.lastw:
                deps.append(self.lastw[b])
            deps.extend(self.readers.get(b, []))
        return deps

    def _commit(self, dep, reads, writes):
        for b in reads:
            self.readers.setdefault(b, []).append(dep)
        for b in writes:
            self.lastw[b] = dep
            self.readers[b] = []

    def op(self, eng, meth, reads, writes, *a, **kw):
        fn = lambda h: getattr(h, meth)(*a, **kw)
        e = self.E[eng]
        px = [b for b in reads if _PSUM_KEY.match(b)]
        if px:
            reads = [b for b in reads if b not in px]
            writes = list(writes) + px
        for d in self._deps(reads, writes):
            self._wait(e, d)
        sem = self._cur_sem(e)
        val = e.count % EPOCH + 1
        self.prog[e.name].append(('i', fn, sem, 1))
        e.count += 1
        self._commit((sem, val, e), reads, writes)

    def dma(self, eng, reads, writes, meth='dma_start', **kw):
        fn = lambda h: getattr(h, meth)(**kw)
        e = self.E[eng]
        if not e.dsems:
            e.dsems = [self._newsem(e.name + "d") for _ in range(NDMA)]
            e.dcount = [0] * NDMA
        s = e.dnext
        e.dnext = (e.dnext + 1) % NDMA
        sem = e.dsems[s]
        if e.dcount[s] > 0:
            self._wait(e, (sem, 16 * e.dcount[s], None))
        for d in self._deps(reads, writes):
            self._wait(e, d)
        e.dcount[s] += 1
        self.prog[e.name].append(('i', fn, sem, 16))
        self._commit((sem, 16 * e.dcount[s], None), reads, writes)

    def end_phase(self):
        e = self.E['sp']
        for o in self.E.values():
            if o.count > 0:
                sem = o.sems[(o.count - 1) // EPOCH]
                self._wait(e, (sem, (o.count - 1) % EPOCH + 1, None))
            for s, c in enumerate(o.dcount):
                if c > 0:
                    self._wait(e, (o.dsems[s], 16 * c, None))
        with self.nc.Block() as blk:
            def replay(name):
                def f(h):
                    for it in self.prog[name]:
                        if it[0] == 'w':
                            h.wait_ge(it[1], it[2])
                        else:
                            it[1](h).then_inc(it[2], it[3])
                return f
            blk.sync(replay('sp'))
            blk.scalar(replay('act'))
            blk.vector(replay('dve'))
            blk.gpsimd(replay('pool'))
            blk.tensor(replay('pe'))
        self.prog = {k: [] for k in self.E}
        self.lastw = {}
        self.readers = {}


class Ctx:
    n = 0

    def __init__(self, nc, es):
        self.nc = nc
        self.es = es

    def sb(self, nm, shp, dt):
        Ctx.n += 1
        return self.es.enter_context(self.nc.sbuf_tensor(f"{nm}_{Ctx.n}", list(shp), dt))

    def ps(self, nm, shp, dt):
        Ctx.n += 1
        return self.es.enter_context(self.nc.psum_tensor(f"{nm}_{Ctx.n}", list(shp), dt))


def rmsnorm_tile(S, x_ap, x_key, g_tile, g_key, out_ap, out_key, tmp, ssq, rstd, pfx):
    S.op('act', 'activation', [x_key], [pfx + 'tmp', pfx + 'ssq'], out=tmp[:], in_=x_ap, func=AF.Square, accum_out=ssq[:])
    S.op('act', 'activation', [pfx + 'ssq'], [pfx + 'rstd'], out=rstd[:], in_=ssq[:], func=AF.Sqrt, scale=1.0 / 1024, bias=EPS)
    S.op('dve', 'reciprocal', [pfx + 'rstd'], [pfx + 'rstd'], out=rstd[:], in_=rstd[:])
    S.op('dve', 'scalar_tensor_tensor', [x_key, pfx + 'rstd', g_key], [out_key], out=out_ap, in0=x_ap, scalar=rstd[:, 0:1],
         in1=g_tile[:], op0=ALU.mult, op1=ALU.mult)


def peer_phase(nc, S, D, NT, n_iblk=128, final=False):
    with ExitStack() as es:
        C = Ctx(nc, es)
        sb, ps = C.sb, C.ps
        identf = sb("identf", [128, 128], F32)
        identb = sb("identb", [128, 128], BF16)
        iotaf = sb("iotaf", [128, 128], F32)
        iotab = sb("iotab", [128, 128], BF16)
        c16 = sb("c16", [128, 16], F32)
        skT = sb("skT", [128, 2, 128], F32)
        gB = sb("gB", [128, 1024], F32)
        gFin = sb("gFin", [128, 1024], F32) if final else None
        Gbuf = sb("Gbuf", [128, 256, 128], BF16)
        xt = [sb(f"xt{r}", [128, 2, 1024], F32) for r in range(2)]
        hnT = [sb(f"hnT{r}", [128, 8, 256], BF16) for r in range(2)]
        Wqc = [sb(f"Wqc{r}", [128, 8, 128], BF16) for r in range(2)]
        tmp = sb("tmp", [128, 1024], BF16)
        ssq = sb("ssq", [128, 1], F32)
        rstd = sb("rstd", [128, 1], F32)
        hn = sb("hn", [128, 1024], BF16)
        qc = [sb(f"qc{r}", [128, 256], F32) for r in range(2)]
        sall = sb("sall", [128, 2, 16, 128], F32)
        s2 = sb("s2", [128, 256], F32)
        Vt = sb("Vt", [128, 8, 2, 16], F32)
        It = sb("It", [128, 8, 2, 16], U32)
        cand = sb("cand", [128, 8, 256], F32)
        TV = sb("TV", [128, 8, 16], F32)
        TP = sb("TP", [128, 8, 16], U32)
        TPf = sb("TPf", [128, 8, 16], F32)
        TVs = sb("TVs", [128, 8, 16], F32)
        Ex = sb("Ex", [128, 8, 16], F32)
        Z = sb("Z", [128, 8], F32)
        rZ = sb("rZ", [128, 8], F32)
        I1f = sb("I1f", [128, 8, 16], F32)
        I2f = sb("I2f", [128, 8, 16], F32)
        OH = sb("OH", [128, 8, 16, 16], F32)
        OH2 = sb("OH2", [128, 8, 16, 16], F32)
        af = sb("af", [128, 8, 16], F32)
        bf = sb("bf", [128, 8, 16], F32)
        iF = sb("iF", [128, 128], F32)
        jF = sb("jF", [128, 128], F32)
        gFt = sb("gFt", [128, 128], F32)
        iT = sb("iT", [128, 256], F32)
        jT = sb("jT", [128, 256], F32)
        gT = sb("gT", [128, 256], F32)
        NR = 8
        OI = [sb(f"OI{r}", [128, 128], BF16) for r in range(NR)]
        OJ = [sb(f"OJ{r}", [128, 128], BF16) for r in range(NR)]
        NSLOT = 5
        UTb = [sb(f"UTb{r}", [128, 8, 128], BF16) for r in range(NSLOT)]
        Vb = [sb(f"Vb{r}", [128, 1024], BF16) for r in range(NSLOT)]
        hf = [sb(f"hf{r}", [128, 256], F32) for r in range(2)]
        hG = [sb(f"hG{r}", [128, 256], BF16) for r in range(2)]
        yo = sb("yo", [128, 2, 1024], F32)
        ops = [ps(f"ops{r}", [128, 512], F32) for r in range(4)]
        aps = [ps(f"aps{r}", [128, 512], F32) for r in range(2)]
        gps = [ps(f"gps{r}", [128, 4, 128], F32) for r in range(2)]
        tps = gps[1][:].rearrange("p a b -> p (a b)").bitcast(BF16).rearrange("p (c t) -> p c t", c=8)

        S.dma('sp', [], ['identf'], out=identf[:], in_=D['ident'])
        S.dma('sp', [], ['iotaf'], out=iotaf[:], in_=D['iota'])
        S.dma('sp', [], ['c16'], out=c16[:], in_=D['cst16'])
        S.dma('sp', [], ['skT'], out=skT[:], in_=D['skT'])
        S.dma('sp', [], ['gB'], out=gB[:], in_=D['g'].partition_broadcast(128))
        if final:
            S.dma('sp', [], ['gFin'], out=gFin[:], in_=D['gfin'].partition_broadcast(128))
        S.op('dve', 'tensor_copy', ['identf'], ['identb'], out=identb[:], in_=identf[:])
        S.op('dve', 'tensor_copy', ['iotaf'], ['iotab'], out=iotab[:], in_=iotaf[:])

        ntile = NT // 256
        x_in = D['x_in'].rearrange("(t g p) d -> t p g d", g=2, p=128)
        x_out = D['x_out'].rearrange("(t g p) d -> t p g d", g=2, p=128)
        iota16_b = iotaf[:, 0:16].unsqueeze(1).unsqueeze(1).to_broadcast([128, 8, 16, 16])
        c16_b = c16[:].unsqueeze(1).unsqueeze(1).to_broadcast([128, 8, 16, 16])
        B4 = [128, 8, 16, 16]

        if not D.get('preconverted', False):
            uT2 = D['uT'].rearrange("i p c e -> i p (c e)")
            for i0 in range(128):
                S.dma('pool', [], ['cvU%d' % i0], out=D['uTb'][i0], in_=uT2[i0])
                S.dma('pool', [], ['cvV%d' % i0], out=D['vb'][i0], in_=D['v'][i0 * 128:(i0 + 1) * 128, :])

        def wdma(i):
            sl = i % NSLOT
            S.dma('sp', ['cvU%d' % i], ['UTb%d' % sl], out=UTb[sl][:].rearrange("p c e -> p (c e)"), in_=D['uTb'][i])
            S.dma('sp', ['cvV%d' % i], ['Vb%d' % sl], out=Vb[sl][:], in_=D['vb'][i])

        def stage1(i, par):
            sl = i % NSLOT
            hv = i % 2
            for dc in range(8):
                S.op('pe', 'matmul', ['UTb%d' % sl, 'hnT%d' % par], ['aps%d' % hv], aps[hv][:, 0:256], lhsT=UTb[sl][:, dc, :],
                     rhs=hnT[par][:, dc, :], start=(dc == 0), stop=(dc == 7))

        def prep(ti, par):
            xk, hk = 'xt%d' % par, 'hnT%d' % par
            S.dma('sp', [], [xk], out=xt[par][:], in_=x_in[ti])
            yield
            for g in range(2):
                rmsnorm_tile(S, xt[par][:, g, :], xk, gB, 'gB', hn[:], 'hn', tmp, ssq, rstd, 'p')
                yield
                for c in range(8):
                    S.op('pe', 'transpose', ['hn', 'identb'], ['gps1'], out=tps[:, c, :], in_=hn[:, c * 128:(c + 1) * 128], identity=identb[:])
                S.op('act', 'copy', ['gps1'], [hk], out=hnT[par][:, :, g * 128:(g + 1) * 128], in_=tps)
                yield
            S.dma('pool', [], ['Wqc0'], out=Wqc[0][:], in_=D['wq'][0])
            for c in range(16):
                gp = gps[c % 2]
                gk = 'gps%d' % (c % 2)
                qk = 'qc%d' % (c % 2)
                wk = 'Wqc%d' % (c % 2)
                if c + 1 < 16:
                    S.dma('pool', [], ['Wqc%d' % ((c + 1) % 2)], out=Wqc[(c + 1) % 2][:], in_=D['wq'][c + 1])
                qv = gp[:, 0:2, :].rearrange("p a b -> p (a b)")
                for dc in range(8):
                    S.op('pe', 'matmul', [hk, wk], [gk], qv, lhsT=Wqc[c % 2][:, dc, :], rhs=hnT[par][:, dc, :],
                         start=(dc == 0), stop=(dc == 7))
                S.op('act', 'copy', [gk], [qk], out=qc[c % 2][:], in_=qv)
                yield
                for g in range(2):
                    S.op('pe', 'matmul', [qk, 'skT'], [gk], gp[:, 2 + g, :], lhsT=qc[c % 2][:, g * 128:(g + 1) * 128], rhs=skT[:, c % 2, :],
                         start=True, stop=True)
                S.op('act', 'copy', [gk], ['sall'], out=sall[:, :, c, :], in_=gp[:, 2:4, :])
                yield
            for g in range(2):
                for c in range(16):
                    h, p = divmod(c, 2)
                    src = sall[:, g, c, :]
                    S.op('dve', 'max', ['sall'], ['Vt'], out=Vt[:, h, p, 0:8], in_=src)
                    yield
                    S.op('dve', 'max_index', ['sall', 'Vt'], ['It'], out=It[:, h, p, 0:8], in_max=Vt[:, h, p, 0:8], in_values=src)
                    yield
                    S.op('dve', 'match_replace', ['sall', 'Vt'], ['s2'], out=s2[:, 0:128], in_to_replace=Vt[:, h, p, 0:8], in_values=src, imm_value=-1e30)
                    yield
                    S.op('dve', 'max', ['s2'], ['Vt'], out=Vt[:, h, p, 8:16], in_=s2[:, 0:128])
                    yield
                    S.op('dve', 'max_index', ['s2', 'Vt'], ['It'], out=It[:, h, p, 8:16], in_max=Vt[:, h, p, 8:16], in_values=s2[:, 0:128])
                    yield
                S.op('dve', 'tensor_tensor', ['Vt'], ['cand'], out=cand[:].rearrange("p h (a b) -> p h a b", a=16),
                     in0=Vt[:, :, 0, :].unsqueeze(3).to_broadcast(B4), in1=Vt[:, :, 1, :].unsqueeze(2).to_broadcast(B4), op=ALU.add)
                yield
                for h in range(8):
                    src = cand[:, h, :]
                    S.op('dve', 'max', ['cand'], ['TV'], out=TV[:, h, 0:8], in_=src)
                    yield
                    S.op('dve', 'max_index', ['cand', 'TV'], ['TP'], out=TP[:, h, 0:8], in_max=TV[:, h, 0:8], in_values=src)
                    yield
                    S.op('dve', 'match_replace', ['cand', 'TV'], ['s2'], out=s2[:], in_to_replace=TV[:, h, 0:8], in_values=src, imm_value=-1e30)
                    yield
                    S.op('dve', 'max', ['s2'], ['TV'], out=TV[:, h, 8:16], in_=s2[:])
                    yield
                    S.op('dve', 'max_index', ['s2', 'TV'], ['TP'], out=TP[:, h, 8:16], in_max=TV[:, h, 8:16], in_values=s2[:])
                    yield
                S.op('dve', 'tensor_tensor', ['TV'], ['TVs'], out=TVs[:], in0=TV[:], in1=TV[:, :, 0:1].to_broadcast([128, 8, 16]), op=ALU.subtract)
                yield
                S.op('act', 'activation', ['TVs'], ['Ex'], out=Ex[:], in_=TVs[:], func=AF.Exp)
                S.op('dve', 'tensor_copy', ['TP'], ['TPf'], out=TPf[:], in_=TP[:])
                yield
                S.op('dve', 'tensor_copy', ['It'], ['I1f'], out=I1f[:], in_=It[:, :, 0, :])
                yield
                S.op('dve', 'tensor_copy', ['It'], ['I2f'], out=I2f[:], in_=It[:, :, 1, :])
                yield
                S.op('dve', 'tensor_reduce', ['Ex'], ['Z'], out=Z[:], in_=Ex[:], axis=AX.X, op=ALU.add)
                yield
                S.op('dve', 'reciprocal', ['Z'], ['rZ'], out=rZ[:], in_=Z[:])
                yield
                S.op('dve', 'tensor_tensor', ['Ex', 'rZ'], ['gFt'], out=gFt[:].rearrange("p (h r) -> p h r", h=8), in0=Ex[:],
                     in1=rZ[:].unsqueeze(2).to_broadcast([128, 8, 16]), op=ALU.mult)
                yield
                S.op('dve', 'tensor_tensor', ['TPf', 'c16'], ['OH'], out=OH[:], in0=TPf[:].unsqueeze(3).to_broadcast(B4), in1=c16_b, op=ALU.subtract)
                yield
                OHf = OH[:].rearrange("p a b c -> p (a b c)")
                OH2f = OH2[:].rearrange("p a b c -> p (a b c)")
                S.op('dve', 'tensor_scalar', ['OH'], ['OH2'], out=OH2f, in0=OHf, scalar1=-8.0, scalar2=None, op0=ALU.is_gt)
                yield
                S.op('dve', 'scalar_tensor_tensor', ['OH', 'OH2'], ['OH'], out=OHf, in0=OHf, scalar=8.0, in1=OH2f, op0=ALU.is_lt, op1=ALU.mult)
                yield
                S.op('dve', 'tensor_tensor', ['OH', 'I1f'], ['OH2'], out=OH2[:], in0=OH[:], in1=I1f[:].unsqueeze(2).to_broadcast(B4), op=ALU.mult)
                yield
                S.op('dve', 'tensor_reduce', ['OH2'], ['iF'], out=iF[:].rearrange("p (h r) -> p h r", h=8), in_=OH2[:], axis=AX.X, op=ALU.add)
                yield
                S.op('dve', 'tensor_tensor', ['OH', 'iotaf'], ['OH2'], out=OH2[:], in0=OH[:], in1=iota16_b, op=ALU.mult)
                yield
                S.op('dve', 'tensor_reduce', ['OH2'], ['af'], out=af[:], in_=OH2[:], axis=AX.X, op=ALU.add)
                yield
                S.op('dve', 'scalar_tensor_tensor', ['af', 'TPf'], ['bf'], out=bf[:].rearrange("p a b -> p (a b)"),
                     in0=af[:].rearrange("p a b -> p (a b)"), scalar=-16.0, in1=TPf[:].rearrange("p a b -> p (a b)"), op0=ALU.mult, op1=ALU.add)
                yield
                S.op('dve', 'tensor_tensor', ['bf', 'iotaf'], ['OH'], out=OH[:], in0=iota16_b, in1=bf[:].unsqueeze(3).to_broadcast(B4), op=ALU.is_equal)
                yield
                S.op('dve', 'tensor_tensor', ['OH', 'I2f'], ['OH2'], out=OH2[:], in0=OH[:], in1=I2f[:].unsqueeze(2).to_broadcast(B4), op=ALU.mult)
                yield
                S.op('dve', 'tensor_reduce', ['OH2'], ['jF'], out=jF[:].rearrange("p (h r) -> p h r", h=8), in_=OH2[:], axis=AX.X, op=ALU.add)
                yield
                for k3, (src, sk, dst, dk) in enumerate(((iF, 'iF', iT, 'iT'), (jF, 'jF', jT, 'jT'), (gFt, 'gFt', gT, 'gT'))):
                    S.op('pe', 'transpose', [sk, 'identf'], ['gps0'], out=gps[0][:, k3, :], in_=src[:], identity=identf[:])
                for k3, (src, sk, dst, dk) in enumerate(((iF, 'iF', iT, 'iT'), (jF, 'jF', jT, 'jT'), (gFt, 'gFt', gT, 'gT'))):
                    S.op('act', 'copy', ['gps0'], [dk], out=dst[:, g * 128:(g + 1) * 128], in_=gps[0][:, k3, :])
                yield

        def drain(gen, n=None):
            k = 0
            for _ in gen:
                k += 1
                if n is not None and k >= n:
                    return False
            return True

        cur = prep(0, 0)
        n_yield = sum(1 for _ in cur)
        per_it = max(1, -(-n_yield // max(1, n_iblk - 6)))
        for ti in range(ntile):
            par = ti % 2
            xk = 'xt%d' % par
            for i0 in range(min(NSLOT - 1, n_iblk)):
                wdma(i0)
            for t in range(256):
                r = t % NR
                bk = (t // 4) % 2
                S.op('dve', 'tensor_scalar', ['iotab', 'iT'], ['OI%d' % r], out=OI[r][:], in0=iotab[:], scalar1=iT[:, t:t + 1], scalar2=None,
                     op0=ALU.is_equal)
                S.op('dve', 'tensor_scalar', ['iotab', 'jT', 'gT'], ['OJ%d' % r], out=OJ[r][:], in0=iotab[:], scalar1=jT[:, t:t + 1],
                     scalar2=gT[:, t:t + 1], op0=ALU.is_equal, op1=ALU.mult)
                S.op('pe', 'matmul', ['OI%d' % r, 'OJ%d' % r], ['gps%d' % bk], gps[bk][:, t % 4, :], lhsT=OJ[r][:], rhs=OI[r][:], start=True, stop=True)
                if t % 4 == 3:
                    S.op('act', 'copy', ['gps%d' % bk], ['Gbuf'], out=Gbuf[:, t - 3:t + 1, :], in_=gps[bk][:])
            nxt = prep(ti + 1, 1 - par) if ti + 1 < ntile else None
            stage1(0, par)
            for i in range(n_iblk):
                if i + NSLOT - 1 < n_iblk:
                    wdma(i + NSLOT - 1)
                if i + 1 < n_iblk:
                    stage1(i + 1, par)
                sl = i % NSLOT
                hv = i % 2
                S.op('act', 'activation', ['aps%d' % hv], ['hf%d' % hv], out=hf[hv][:], in_=aps[hv][:, 0:256], func=AF.Gelu)
                S.op('dve', 'tensor_tensor', ['hf%d' % hv, 'Gbuf'], ['hG%d' % hv], out=hG[hv][:], in0=hf[hv][:], in1=Gbuf[:, :, i], op=ALU.mult)
                for tg in range(2):
                    for dh in range(2):
                        S.op('pe', 'matmul', ['hG%d' % hv, 'Vb%d' % sl], ['ops%d' % (tg * 2 + dh)], ops[tg * 2 + dh][:],
                             lhsT=hG[hv][:, tg * 128:(tg + 1) * 128], rhs=Vb[sl][:, dh * 512:(dh + 1) * 512], start=(i == 0), stop=(i == n_iblk - 1))
                if nxt is not None and i >= 2:
                    if drain(nxt, per_it):
                        nxt = None
            if nxt is not None:
                drain(nxt)
            for tg in range(2):
                for dh in range(2):
                    S.op('dve', 'tensor_tensor', ['ops%d' % (tg * 2 + dh), xk], ['yo'], out=yo[:, tg, dh * 512:(dh + 1) * 512], in0=ops[tg * 2 + dh][:],
                         in1=xt[par][:, tg, dh * 512:(dh + 1) * 512], op=ALU.add)
            if final:
                for tg in range(2):
                    rmsnorm_tile(S, yo[:, tg, :], 'yo', gFin, 'gFin', xt[par][:, tg, :], xk, tmp, ssq, rstd, 'f')
                S.dma('pool', [xk], [], out=x_out[ti], in_=xt[par][:])
            else:
                S.dma('pool', ['yo'], [], out=x_out[ti], in_=yo[:])
        S.end_phase()


def mixer_a(nc, S, D, own):
    t0, nt = own
    with ExitStack() as es:
        C = Ctx(nc, es)
        sb, ps = C.sb, C.ps
        identf = sb("identf", [128, 128], F32)
        identb = sb("identb", [128, 128], BF16)
        Win = sb("Win", [128, 8, 5632], BF16)
        gB = sb("gB", [128, 1024], F32)
        tmp = sb("tmp", [128, 1024], BF16)
        ssq = sb("ssq", [128, 1], F32)
        rstd = sb("rstd", [128, 1], F32)
        xn = sb("xn", [128, 1024], BF16)
        xnT = sb("xnT", [128, 8, 128], BF16)
        t1 = sb("t1", [128, 8, 32], F32)
        t2 = sb("t2", [128, 8, 32], F32)
        NB = 2
        xt = [sb(f"xt{r}", [128, 1024], F32) for r in range(NB)]
        cs = [sb(f"cs{r}", [128, 2, 32], F32) for r in range(NB)]
        Pp = [sb(f"Pp{r}", [128, 256], F32) for r in range(NB)]
        rin = [sb(f"rin{r}", [128, 8, 64], F32) for r in range(NB)]
        rot = [sb(f"rot{r}", [128, 8, 64], F32) for r in range(NB)]
        Rq = [sb(f"Rq{r}", [128, 256], BF16) for r in range(NB)]
        Rk = [sb(f"Rk{r}", [128, 256], BF16) for r in range(NB)]
        Rv = [sb(f"Rv{r}", [128, 512], BF16) for r in range(NB)]
        Rg = [sb(f"Rg{r}", [128, 512], F32) for r in range(NB)]
        Nq = [sb(f"Nq{r}", [128, 256], BF16) for r in range(NB)]
        Nk = [sb(f"Nk{r}", [128, 256], BF16) for r in range(NB)]
        Nv = [sb(f"Nv{r}", [128, 256], BF16) for r in range(NB)]
        Gt = [sb(f"Gt{r}", [128, 3072], F32) for r in range(NB)]
        pp = [ps(f"pp{r}", [128, 512], F32) for r in range(4)]
        tps = ps("tps", [128, 8, 128], BF16)

        S.dma('sp', [], ['identf'], out=identf[:], in_=D['ident'])
        S.dma('sp', [], ['gB'], out=gB[:], in_=D['g'].partition_broadcast(128))
        w_v = D['w_in'].rearrange("(c p) n -> p c n", p=128)
        for k in range(11):
            S.dma('pool', [], ['Win%d' % k], out=Win[:, :, k * 512:(k + 1) * 512], in_=w_v[:, :, k * 512:(k + 1) * 512])
        S.op('dve', 'tensor_copy', ['identf'], ['identb'], out=identb[:], in_=identf[:])
        B3 = [128, 8, 32]
        kc = 0
        import os as _os
        for i in range(int(_os.environ.get('MA_NT', '32'))):
            b = i % NB
            sfx = str(b)
            is_own = t0 <= i < t0 + nt
            rows = slice(i * 128, (i + 1) * 128)
            S.dma('sp', [], ['xt' + sfx], out=xt[b][:], in_=D['x_full'][rows, :])
            S.dma('sp', [], ['cs' + sfx], out=cs[b][:, 0, :], in_=D['cos'][rows, :])
            S.dma('sp', [], ['cs' + sfx], out=cs[b][:, 1, :], in_=D['sin'][rows, :])
            _sub = int(_os.environ.get('MA_SUB', '9'))
            if _sub <= 2:
                S.end_phase()
                return
            rmsnorm_tile(S, xt[b][:], 'xt' + sfx, gB, 'gB', xn[:], 'xn', tmp, ssq, rstd, 'a')
            if _sub <= 3:
                S.end_phase()
                return
            for c in range(8):
                S.op('pe', 'transpose', ['xn', 'identb'], ['tps'], out=tps[:, c, :], in_=xn[:, c * 128:(c + 1) * 128], identity=identb[:])
            S.op('act', 'copy', ['tps'], ['xnT'], out=xnT[:], in_=tps[:])
            if _sub <= 4:
                S.end_phase()
                return
            chunks = list(range(11)) if is_own else [0, 1, 2, 4]
            _lvl = int(_os.environ.get('MA_LVL', '9'))
            if _lvl == 0:
                chunks = [0]
            for ci in chunks:
                bank = pp[kc % 4]
                bk = 'pp%d' % (kc % 4)
                kc += 1
                for dc in range(8):
                    S.op('pe', 'matmul', ['xnT', 'Win%d' % ci], [bk], bank[:], lhsT=xnT[:, dc, :], rhs=Win[:, dc, ci * 512:(ci + 1) * 512],
                         start=(dc == 0), stop=(dc == 7))
                lo, hi = bank[:, 0:256], bank[:, 256:512]
                rq = rin[b][:, 0:4, :].rearrange("p a b -> p (a b)")
                rk = rin[b][:, 4:8, :].rearrange("p a b -> p (a b)")
                if ci == 0:
                    S.op('act', 'copy', [bk], ['Pp' + sfx], out=Pp[b][:], in_=lo)
                    S.op('dve', 'tensor_copy', [bk], ['rin' + sfx], out=rq, in_=hi)
                elif ci == 1:
                    S.op('dve', 'tensor_copy', [bk], ['rin' + sfx], out=rk, in_=lo)
                    S.op('act', 'copy', [bk], ['Rv' + sfx], out=Rv[b][:, 0:256], in_=hi)
                elif ci == 2:
                    S.op('act', 'copy', [bk], ['Rv' + sfx], out=Rv[b][:, 256:512], in_=lo)
                    if is_own:
                        S.op('act', 'activation', [bk], ['Rg' + sfx], out=Rg[b][:, 0:256], in_=hi, func=AF.Silu)
                elif ci == 3:
                    S.op('act', 'activation', [bk], ['Rg' + sfx], out=Rg[b][:, 256:512], in_=lo, func=AF.Silu)
                    S.op('dve', 'tensor_copy', [bk], ['Nq' + sfx], out=Nq[b][:], in_=hi)
                elif ci == 4:
                    S.op('dve', 'tensor_copy', [bk], ['Nk' + sfx], out=Nk[b][:], in_=lo)
                    S.op('act', 'copy', [bk], ['Nv' + sfx], out=Nv[b][:], in_=hi)
                else:
                    S.op('act', 'activation', [bk], ['Gt' + sfx], out=Gt[b][:, (ci - 5) * 512:(ci - 4) * 512], in_=bank[:], func=AF.Sigmoid)
            if _lvl == 0:
                S.dma('pool', ['Pp' + sfx], [], out=D['Pp'][rows, :], in_=Pp[b][:])
                continue
            x1, x2 = rin[b][:, :, 0:32], rin[b][:, :, 32:64]
            cosb = cs[b][:, 0, :].unsqueeze(1).to_broadcast(B3)
            sinb = cs[b][:, 1, :].unsqueeze(1).to_broadcast(B3)
            rk_ = ['rin' + sfx, 'cs' + sfx]
            S.op('dve', 'tensor_tensor', rk_, ['t1'], out=t1[:], in0=x1, in1=cosb, op=ALU.mult)
            S.op('dve', 'tensor_tensor', rk_, ['t2'], out=t2[:], in0=x2, in1=sinb, op=ALU.mult)
            S.op('dve', 'tensor_tensor', ['t1', 't2'], ['rot' + sfx], out=rot[b][:, :, 0:32], in0=t1[:], in1=t2[:], op=ALU.subtract)
            S.op('dve', 'tensor_tensor', rk_, ['t1'], out=t1[:], in0=x1, in1=sinb, op=ALU.mult)
            S.op('dve', 'tensor_tensor', rk_, ['t2'], out=t2[:], in0=x2, in1=cosb, op=ALU.mult)
            S.op('dve', 'tensor_tensor', ['t1', 't2'], ['rot' + sfx], out=rot[b][:, :, 32:64], in0=t1[:], in1=t2[:], op=ALU.add)
            S.op('act', 'copy', ['rot' + sfx], ['Rq' + sfx], out=Rq[b][:], in_=rot[b][:, 0:4, :].rearrange("p a b -> p (a b)"))
            S.op('act', 'mul', ['rot' + sfx], ['Rk' + sfx], Rk[b][:], rot[b][:, 4:8, :].rearrange("p a b -> p (a b)"), 0.125)
            S.dma('pool', ['Pp' + sfx], [], out=D['Pp'][rows, :], in_=Pp[b][:])
            S.dma('pool', ['Rk' + sfx], [], out=D['Rk'][rows, :], in_=Rk[b][:])
            S.dma('pool', ['Rv' + sfx], [], out=D['Rv'][rows, :], in_=Rv[b][:])
            S.dma('pool', ['Nk' + sfx], [], out=D['Nk'][rows, :], in_=Nk[b][:])
            S.dma('pool', ['Nv' + sfx], [], out=D['Nv'][rows, :], in_=Nv[b][:])
            if is_own:
                S.dma('pool', ['Rq' + sfx], [], out=D['Rq'][rows, :], in_=Rq[b][:])
                S.dma('pool', ['Rg' + sfx], [], out=D['Rg'][rows, :], in_=Rg[b][:])
                S.dma('pool', ['Nq' + sfx], [], out=D['Nq'][rows, :], in_=Nq[b][:])
                S.dma('pool', ['Gt' + sfx], [], out=D['Gt'][rows, :], in_=Gt[b][:])
        S.end_phase()


def mixer_b(nc, S, D, own):
    t0, nt = own
    with ExitStack() as es:
        C = Ctx(nc, es)
        sb, ps = C.sb, C.ps
        identf = sb("identf", [128, 128], F32)
        identb = sb("identb", [128, 128], BF16)
        Wbp = sb("Wbp", [64, 4, 1024], BF16)
        Wbr = sb("Wbr", [128, 4, 1024], BF16)
        Wbn = sb("Wbn", [128, 2, 1024], BF16)
        Wout = sb("Wout", [128, 8, 1024], BF16)
        poolw = sb("poolw", [64, 4, 64], F32)
        pscale = sb("pscale", [64, 4], F32)
        amat = sb("amat", [128, 4, 5, 128], F32)
        Et = [sb(f"Et{r}", [128, 5, 4, 128], BF16) for r in range(5)]
        rstage = sb("rstage", [128, 5, 4, 128], F32)
        nmask = sb("nmask", [128, 5, 128], F32)
        dl = sb("dl", [128, 8], F32)
        lg = sb("lg", [128, 8], F32)
        pos = sb("pos", [128, 2], F32)
        zf = sb("zf", [128, 4], F32)
        zb = sb("zb", [128, 4], F32)
        dC = sb("dC", [128, 8], F32)
        dmat = sb("dmat", [128, 4, 128], F32)
        xrow = sb("xrow", [128, 2, 128], F32)
        Dt = sb("Dt", [128, 4, 128], F32)
        E1 = sb("E1", [128, 128], F32)
        E2 = sb("E2", [128, 128], F32)
        XF = sb("XF", [64, 4, 128], F32)
        XB = sb("XB", [64, 4, 128], F32)
        Sst = sb("Sst", [64, 4, 128], F32)
        Sob = [sb(f"Sob{r}", [64, 4, 128], BF16) for r in range(2)]
        kd = sb("kd", [128, 4, 64], BF16)
        NB = 2
        xt = [sb(f"xt{r}", [128, 1024], F32) for r in range(NB)]
        Gt = [sb(f"Gt{r}", [128, 3072], F32) for r in range(NB)]
        Rq = [sb(f"Rq{r}", [128, 256], BF16) for r in range(NB)]
        Rk = [sb(f"Rk{r}", [128, 256], BF16) for r in range(NB)]
        Rv = [sb(f"Rv{r}", [128, 512], BF16) for r in range(NB)]
        Rg = [sb(f"Rg{r}", [128, 512], F32) for r in range(NB)]
        SFt = [sb(f"SFt{r}", [64, 4, 128], BF16) for r in range(NB)]
        SBt = [sb(f"SBt{r}", [64, 4, 128], BF16) for r in range(NB)]
        Nq = [sb(f"Nq{r}", [128, 256], BF16) for r in range(NB)]
        Nk = [[sb(f"Nk{r}_{o}", [128, 256], BF16) for o in range(5)] for r in range(NB)]
        Nv1 = [[sb(f"Nv{r}_{o}", [128, 4, 65], BF16) for o in range(5)] for r in range(NB)]
        Pp = [[sb(f"Pp{r}_{o}", [128, 256], F32) for o in range(3)] for r in range(NB)]
        qkT = sb("qkT", [64, 8, 128], BF16)
        qfT = sb("qfT", [64, 4, 128], BF16)
        qbT = sb("qbT", [64, 4, 128], BF16)
        PD = sb("PD", [128, 4, 128], BF16)
        ysq = sb("ysq", [128, 4, 128], F32)
        ssq4 = sb("ssq4", [128, 4], F32)
        rs4 = sb("rs4", [128, 4], F32)
        yr = sb("yr", [128, 4, 128], F32)
        yrb = sb("yrb", [128, 512], BF16)
        yrT = sb("yrT", [128, 4, 128], BF16)
        NTa = sb("NTa", [64, 24, 128], BF16)
        Pm = sb("Pm", [128, 4, 128], BF16)
        PEm = sb("PEm", [128, 4, 128], BF16)
        rden = sb("rden", [128, 4], F32)
        ynb = sb("ynb", [128, 4, 64], BF16)
        ynT = sb("ynT", [128, 2, 128], BF16)
        dT = sb("dT", [64, 4, 128], F32)
        ypT = sb("ypT", [64, 4, 128], BF16)
        mg = sb("mg", [128, 1024], F32)
        tg_ = sb("tg", [128, 1024], F32)
        mgb = sb("mgb", [128, 1024], BF16)
        mT = sb("mT", [128, 8, 128], BF16)
        xo = [sb(f"xo{r}", [128, 1024], F32) for r in range(NB)]
        bank = [ps(f"bk{r}", [128, 512], F32) for r in range(8)]
        sc_ps = bank[0][:].rearrange("p (h c) -> p h c", h=4)
        y_ps = bank[1][:].rearrange("p (h c) -> p h c", h=4)
        st_ps = [bank[2][:].rearrange("p (h c) -> p h c", h=4), bank[6][:].rearrange("p (h c) -> p h c", h=4)]
        st_k = ['bk2', 'bk6']
        o_ps = bank[3][:, 0:260].rearrange("p (h c) -> p h c", h=4)
        pl_ps = bank[4][0:64, :].rearrange("p (h c) -> p h c", h=4)
        tr_ps = bank[5][:].bitcast(BF16).rearrange("p (c t) -> p c t", c=8)

        S.dma('sp', [], ['identf'], out=identf[:], in_=D['ident'])
        S.op('dve', 'tensor_copy', ['identf'], ['identb'], out=identb[:], in_=identf[:])
        for g in range(4):
            S.dma('pool', [], ['Wbp'], out=Wbp[:, g, :], in_=D['wbp'][:, g, :])
        S.dma('pool', [], ['Wbr'], out=Wbr[:], in_=D['wbr'].rearrange("(h e) n -> e h n", e=128))
        S.dma('pool', [], ['Wbn'], out=Wbn[:], in_=D['wbn'].rearrange("(h e) n -> e h n", e=128))
        S.dma('pool', [], ['Wout'], out=Wout[:], in_=D['wout'].rearrange("(h e) n -> e h n", e=128))
        S.dma('sp', [], ['poolw'], out=poolw[:], in_=D['poolw'])
        S.dma('sp', [], ['pscale'], out=pscale[:], in_=D['pscale'])
        S.dma('sp', [], ['amat'], out=amat[:], in_=D['amat'])
        S.dma('sp', [], ['dl'], out=dl[:], in_=D['decay'].partition_broadcast(128))
        S.dma('sp', [], ['pos'], out=pos[:], in_=D['pos'])
        S.dma('sp', [], ['dmat'], out=dmat[:], in_=D['dmat'].rearrange("k p c -> p k c"))
        S.dma('sp', [], ['xrow'], out=xrow[:], in_=D['xrow'])
        for r in range(NB):
            for o in range(5):
                S.op('dve', 'memset', [], ['Nv%d_%d' % (r, o)], Nv1[r][o][:], 1.0)
        for ty in range(5):
            S.dma('sp', [], ['rstage'], out=rstage[:], in_=D['rpbT'][ty])
            S.dma('sp', [], ['nmask'], out=nmask[:], in_=D['nmask'][ty])
            S.op('act', 'activation', ['rstage'], ['rstage'], out=rstage[:], in_=rstage[:], func=AF.Exp)
            S.op('dve', 'tensor_tensor', ['rstage', 'nmask'], ['Et%d' % ty], out=Et[ty][:], in0=rstage[:],
                 in1=nmask[:].unsqueeze(2).to_broadcast([128, 5, 4, 128]), op=ALU.mult)
        S.op('act', 'activation', ['dl'], ['lg'], out=lg[:], in_=dl[:], func=AF.Exp, scale=-1.0)
        S.op('act', 'activation', ['lg'], ['lg'], out=lg[:], in_=lg[:], func=AF.Ln, bias=1.0)
        S.op('act', 'mul', ['lg'], ['lg'], lg[:], lg[:], -1.0)
        S.op('act', 'activation', ['lg', 'pos'], ['zf'], out=zf[:], in_=lg[:, 0:4], func=AF.Exp, scale=pos[:, 0:1])
        S.op('act', 'activation', ['lg', 'pos'], ['zb'], out=zb[:], in_=lg[:, 4:8], func=AF.Exp, scale=pos[:, 1:2])
        S.op('act', 'activation', ['lg'], ['dC'], out=dC[:], in_=lg[:], func=AF.Exp, scale=128.0)
        for h in range(4):
            S.op('act', 'activation', ['lg', 'dmat'], ['E1'], out=E1[:], in_=dmat[:, 0, :], func=AF.Exp, scale=lg[:, h:h + 1])
            S.op('act', 'activation', ['lg', 'dmat'], ['E2'], out=E2[:], in_=dmat[:, 2, :], func=AF.Exp, scale=lg[:, 4 + h:5 + h])
            S.op('dve', 'tensor_tensor', ['E1', 'dmat'], ['E1'], out=E1[:], in0=E1[:], in1=dmat[:, 1, :], op=ALU.mult)
            S.op('dve', 'tensor_tensor', ['E2', 'dmat'], ['E2'], out=E2[:], in0=E2[:], in1=dmat[:, 3, :], op=ALU.mult)
            S.op('dve', 'tensor_tensor', ['E1', 'E2'], ['Dt'], out=Dt[:, h, :], in0=E1[:], in1=E2[:], op=ALU.add)
            S.op('act', 'activation', ['lg', 'xrow'], ['XF'], out=XF[:, h, :], in_=xrow[0:64, 0, :], func=AF.Exp, scale=lg[0:64, h:h + 1])
            S.op('act', 'activation', ['lg', 'xrow'], ['XB'], out=XB[:, h, :], in_=xrow[0:64, 1, :], func=AF.Exp, scale=lg[0:64, 4 + h:5 + h])

        def sweep(order, zt, zk, dcol, dst, dkey, store_pred, upd_pred):
            S.op('dve', 'memset', [], ['Sst'], Sst[:], 0.0)
            for cnt, n in enumerate(order):
                b = cnt % NB
                sfx = str(b)
                rows = slice(n * 128, (n + 1) * 128)
                if store_pred(n):
                    S.op('act', 'copy', ['Sst'], ['Sob' + sfx], out=Sob[b][:], in_=Sst[:])
                    S.dma('pool', ['Sob' + sfx], [dkey + str(n)], out=dst[n], in_=Sob[b][:])
                if not upd_pred(n):
                    continue
                S.dma('sp', [], ['Rk' + sfx], out=Rk[b][:], in_=D['Rk'][rows, :])
                S.dma('sp', [], ['Rv' + sfx], out=Rv[b][:], in_=D['Rv'][rows, :])
                S.op('dve', 'tensor_tensor', ['Rk' + sfx, zk], ['kd'], out=kd[:], in0=Rk[b][:].rearrange("p (h d) -> p h d", h=4),
                     in1=zt[:].unsqueeze(2).to_broadcast([128, 4, 64]), op=ALU.mult)
                for h in range(4):
                    S.op('pe', 'matmul', ['kd', 'Rv' + sfx], ['bk0'], bank[0][0:64, h * 128:(h + 1) * 128], lhsT=kd[:, h, :],
                         rhs=Rv[b][:, h * 128:(h + 1) * 128], start=True, stop=True)
                S.op('dve', 'tensor_tensor', ['Sst', 'dC'], ['Sst'], out=Sst[:], in0=Sst[:],
                     in1=dC[0:64, dcol:dcol + 4].unsqueeze(2).to_broadcast([64, 4, 128]), op=ALU.mult)
                S.op('dve', 'tensor_tensor', ['Sst', 'bk0'], ['Sst'], out=Sst[:], in0=Sst[:],
                     in1=bank[0][0:64, :].rearrange("p (h c) -> p h c", h=4), op=ALU.add)

        last = t0 + nt - 1
        sweep(list(range(0, last + 1)), zf, 'zf', 0, D['SF'], 'dSF', lambda n: n >= t0, lambda n: n < last)
        sweep(list(range(31, t0 - 1, -1)), zb, 'zb', 4, D['SB'], 'dSB', lambda n: n <= last, lambda n: n > t0)

        B4 = [128, 4, 128]
        for it in range(nt):
            i = t0 + it
            b = it % NB
            sfx = str(b)
            rows = slice(i * 128, (i + 1) * 128)
            ty = {0: 1, 1: 2, 30: 3, 31: 4}.get(i, 0)
            offs = [o for o in range(NA_OMIN[ty], NA_OMIN[ty] + 5) if 0 <= i + o <= 31]
            poffs = [o for o in range(-1, 2) if 0 <= i + o <= 31]
            S.dma('sp', [], ['xt' + sfx], out=xt[b][:], in_=D['x_full'][rows, :])
            S.dma('sp', [], ['Gt' + sfx], out=Gt[b][:], in_=D['Gt'][rows, :])
            S.dma('sp', [], ['Rq' + sfx], out=Rq[b][:], in_=D['Rq'][rows, :])
            S.dma('sp', [], ['Rk' + sfx], out=Rk[b][:], in_=D['Rk'][rows, :])
            S.dma('sp', [], ['Rv' + sfx], out=Rv[b][:], in_=D['Rv'][rows, :])
            S.dma('sp', [], ['Rg' + sfx], out=Rg[b][:], in_=D['Rg'][rows, :])
            S.dma('sp', ['dSF%d' % i], ['SFt' + sfx], out=SFt[b][:], in_=D['SF'][i])
            S.dma('sp', ['dSB%d' % i], ['SBt' + sfx], out=SBt[b][:], in_=D['SB'][i])
            S.dma('sp', [], ['Nq' + sfx], out=Nq[b][:], in_=D['Nq'][rows, :])
            for oi, o in enumerate(offs):
                r2 = slice((i + o) * 128, (i + o + 1) * 128)
                S.dma('sp', [], ['Nk%s_%d' % (sfx, oi)], out=Nk[b][oi][:], in_=D['Nk'][r2, :])
                S.dma('sp', [], ['Nv%s_%d' % (sfx, oi)], out=Nv1[b][oi][:, :, 0:64], in_=D['Nv'][r2, :].rearrange("p (h d) -> p h d", h=4))
            for oi, o in enumerate(poffs):
                r2 = slice((i + o) * 128, (i + o + 1) * 128)
                S.dma('sp', [], ['Pp%s_%d' % (sfx, oi)], out=Pp[b][oi][:], in_=D['Pp'][r2, :])

            if 'uTb' in D:
                cpt = -(-128 // nt)
                uT2 = D['uT'].rearrange("i p c e -> i p (c e)")
                for i0 in range(it * cpt, min(128, (it + 1) * cpt)):
                    S.dma('pool', [], [], out=D['uTb'][i0], in_=uT2[i0])
                    S.dma('pool', [], [], out=D['vb'][i0], in_=D['v'][i0 * 128:(i0 + 1) * 128, :])
            for h in range(4):
                S.op('pe', 'transpose', ['Rq' + sfx, 'identb'], ['bk5'], out=tr_ps[0:64, h, :], in_=Rq[b][:, h * 64:(h + 1) * 64], identity=identb[:])
                S.op('pe', 'transpose', ['Rk' + sfx, 'identb'], ['bk5'], out=tr_ps[0:64, 4 + h, :], in_=Rk[b][:, h * 64:(h + 1) * 64], identity=identb[:])
            S.op('act', 'copy', ['bk5'], ['qkT'], out=qkT[:], in_=tr_ps[0:64, :, :])
            S.op('dve', 'tensor_tensor', ['qkT', 'XF'], ['qfT'], out=qfT[:], in0=qkT[:, 0:4, :], in1=XF[:], op=ALU.mult)
            S.op('dve', 'tensor_tensor', ['qkT', 'XB'], ['qbT'], out=qbT[:], in0=qkT[:, 0:4, :], in1=XB[:], op=ALU.mult)
            for h in range(4):
                S.op('pe', 'matmul', ['qkT'], ['bk0'], sc_ps[:, h, :], lhsT=qkT[:, 4 + h, :], rhs=qkT[:, h, :], start=True, stop=True)
            S.op('dve', 'tensor_tensor', ['bk0', 'Dt'], ['PD'], out=PD[:], in0=sc_ps, in1=Dt[:], op=ALU.mult)
            for h in range(4):
                S.op('pe', 'matmul', ['PD', 'Rv' + sfx], ['bk1'], y_ps[:, h, :], lhsT=PD[:, h, :], rhs=Rv[b][:, h * 128:(h + 1) * 128],
                     start=(h == 0), stop=False, skip_group_check=True)
                S.op('pe', 'matmul', ['qfT', 'SFt' + sfx], ['bk1'], y_ps[:, h, :], lhsT=qfT[:, h, :], rhs=SFt[b][:, h, :],
                     start=False, stop=False, skip_group_check=True)
                S.op('pe', 'matmul', ['qbT', 'SBt' + sfx], ['bk1'], y_ps[:, h, :], lhsT=qbT[:, h, :], rhs=SBt[b][:, h, :],
                     start=False, stop=(h == 3), skip_group_check=True)
            S.op('act', 'activation', ['bk1'], ['ysq'], out=ysq[:], in_=y_ps, func=AF.Square)
            S.op('dve', 'tensor_reduce', ['ysq'], ['ssq4'], out=ssq4[:], in_=ysq[:], axis=AX.X, op=ALU.add)
            S.op('act', 'activation', ['ssq4'], ['rs4'], out=rs4[:], in_=ssq4[:], func=AF.Sqrt, scale=1.0 / 128, bias=EPS)
            S.op('dve', 'reciprocal', ['rs4'], ['rs4'], out=rs4[:], in_=rs4[:])
            S.op('dve', 'tensor_tensor', ['bk1', 'rs4'], ['yr'], out=yr[:], in0=y_ps, in1=rs4[:].unsqueeze(2).to_broadcast(B4), op=ALU.mult)
            S.op('dve', 'tensor_tensor', ['yr', 'Rg' + sfx], ['yrb'], out=yrb[:], in0=yr[:].rearrange("p h c -> p (h c)"), in1=Rg[b][:], op=ALU.mult)
            for h in range(4):
                S.op('pe', 'transpose', ['yrb', 'identb'], ['bk5'], out=tr_ps[:, h, :], in_=yrb[:, h * 128:(h + 1) * 128], identity=identb[:])
            S.op('act', 'copy', ['bk5'], ['yrT'], out=yrT[:], in_=tr_ps[:, 0:4, :])

            srcs = [(Nq[b], 'Nq' + sfx)] + [(Nk[b][oi], 'Nk%s_%d' % (sfx, oi)) for oi in range(len(offs))]
            for r0 in range(0, len(srcs), 2):
                grp = srcs[r0:r0 + 2]
                for gi, (src, sk) in enumerate(grp):
                    for h in range(4):
                        S.op('pe', 'transpose', [sk, 'identb'], ['bk5'], out=tr_ps[0:64, gi * 4 + h, :], in_=src[:, h * 64:(h + 1) * 64], identity=identb[:])
                n_ = 4 * len(grp)
                S.op('act', 'copy', ['bk5'], ['NTa'], out=NTa[:, r0 * 4:r0 * 4 + n_, :], in_=tr_ps[0:64, 0:n_, :])
            for oi, o in enumerate(offs):
                sp_, sk_ = st_ps[oi % 2], st_k[oi % 2]
                for h in range(4):
                    S.op('pe', 'matmul', ['NTa'], [sk_], sp_[:, h, :], lhsT=NTa[:, 4 + 4 * oi + h, :], rhs=NTa[:, h, :], start=True, stop=True)
                S.op('act', 'activation', [sk_], ['Pm'], out=Pm[:], in_=sp_, func=AF.Exp, scale=0.125)
                S.op('dve', 'tensor_tensor', ['Pm', 'Et%d' % ty], ['PEm'], out=PEm[:], in0=Pm[:], in1=Et[ty][:, o - NA_OMIN[ty], :, :], op=ALU.mult)
                for h in range(4):
                    S.op('pe', 'matmul', ['PEm', 'Nv%s_%d' % (sfx, oi)], ['bk3'], o_ps[:, h, :], lhsT=PEm[:, h, :], rhs=Nv1[b][oi][:, h, :],
                         start=(oi == 0 and h == 0), stop=(oi == len(offs) - 1 and h == 3), skip_group_check=True)
            S.op('dve', 'reciprocal', ['bk3'], ['rden'], out=rden[:], in_=o_ps[:, :, 64:65].rearrange("p h c -> p (h c)"))
            S.op('dve', 'tensor_tensor', ['bk3', 'rden'], ['ynb'], out=ynb[:], in0=o_ps[:, :, 0:64], in1=rden[:].unsqueeze(2).to_broadcast([128, 4, 64]), op=ALU.mult)
            ynf = ynb[:].rearrange("p h d -> p (h d)")
            for c in range(2):
                S.op('pe', 'transpose', ['ynb', 'identb'], ['bk5'], out=tr_ps[:, c, :], in_=ynf[:, c * 128:(c + 1) * 128], identity=identb[:])
            S.op('act', 'copy', ['bk5'], ['ynT'], out=ynT[:], in_=tr_ps[:, 0:2, :])

            for g in range(4):
                for oi, o in enumerate(poffs):
                    kind = {-1: 0, 1: 2}.get(o, 3 if i == 0 else (4 if i == 31 else 1))
                    S.op('pe', 'matmul', ['Pp%s_%d' % (sfx, oi), 'amat'], ['bk4'], pl_ps[:, g, :], lhsT=Pp[b][oi][:, g * 64:(g + 1) * 64],
                         rhs=amat[:, g, kind, :], start=(oi == 0), stop=(oi == len(poffs) - 1))
            S.op('act', 'copy', ['bk4'], ['dT'], out=dT[:], in_=pl_ps)
            for g in range(4):
                S.op('pe', 'matmul', ['dT', 'poolw'], ['bk4'], pl_ps[:, g, :], lhsT=poolw[:, g, :], rhs=dT[:, g, :], start=True, stop=True)
            S.op('dve', 'tensor_tensor', ['bk4', 'pscale'], ['ypT'], out=ypT[:], in0=pl_ps, in1=pscale[:].unsqueeze(2).to_broadcast([64, 4, 128]), op=ALU.mult)

            def branch(nk, lhs_list, w_tile, goff, first, lastb):
                for nh in range(2):
                    bkk = bank[6 + nh]
                    for kk in range(nk):
                        S.op('pe', 'matmul', lhs_list[1] + [w_tile[1]], ['bk%d' % (6 + nh)], bkk[:], lhsT=lhs_list[0][:, kk, :],
                             rhs=w_tile[0][:, kk, nh * 512:(nh + 1) * 512], start=(kk == 0), stop=(kk == nk - 1))
                    cols = slice(nh * 512, (nh + 1) * 512)
                    gsl = Gt[b][:, goff + nh * 512: goff + (nh + 1) * 512]
                    if first:
                        S.op('dve', 'tensor_tensor', ['bk%d' % (6 + nh), 'Gt' + sfx], ['mg'], out=mg[:, cols], in0=bkk[:], in1=gsl, op=ALU.mult)
                    else:
                        S.op('dve', 'tensor_tensor', ['bk%d' % (6 + nh), 'Gt' + sfx], ['tg'], out=tg_[:, cols], in0=bkk[:], in1=gsl, op=ALU.mult)
                        if lastb:
                            S.op('dve', 'tensor_tensor', ['mg', 'tg'], ['mgb'], out=mgb[:, cols], in0=mg[:, cols], in1=tg_[:, cols], op=ALU.add)
                        else:
                            S.op('dve', 'tensor_tensor', ['mg', 'tg'], ['mg'], out=mg[:, cols], in0=mg[:, cols], in1=tg_[:, cols], op=ALU.add)

            branch(4, (ypT, ['ypT']), (Wbp, 'Wbp'), 0, True, False)
            branch(4, (yrT, ['yrT']), (Wbr, 'Wbr'), 1024, False, False)
            branch(2, (ynT, ['ynT']), (Wbn, 'Wbn'), 2048, False, True)
            for c in range(8):
                S.op('pe', 'transpose', ['mgb', 'identb'], ['bk5'], out=tr_ps[:, c, :], in_=mgb[:, c * 128:(c + 1) * 128], identity=identb[:])
            S.op('act', 'copy', ['bk5'], ['mT'], out=mT[:], in_=tr_ps)
            for nh in range(2):
                bkk = bank[6 + nh]
                for dc in range(8):
                    S.op('pe', 'matmul', ['mT', 'Wout'], ['bk%d' % (6 + nh)], bkk[:], lhsT=mT[:, dc, :], rhs=Wout[:, dc, nh * 512:(nh + 1) * 512],
                         start=(dc == 0), stop=(dc == 7))
                cols = slice(nh * 512, (nh + 1) * 512)
                S.op('dve', 'tensor_tensor', ['bk%d' % (6 + nh), 'xt' + sfx], ['xo' + sfx], out=xo[b][:, cols], in0=bkk[:], in1=xt[b][:, cols], op=ALU.add)
            S.dma('pool', ['xo' + sfx], [], out=D['x_out'][it * 128:(it + 1) * 128, :], in_=xo[b][:])
        S.end_phase()


POOL_WINDOWS = (2, 4, 8, 16)
SEQ = 4096
NA_OMIN = (-2, 0, -2, -2, -3)
NA_NOFF = (5, 4, 4, 4, 4)


def _const_tables():
    T = {}
    T['ident'] = np.eye(128, dtype=np.float32)
    T['iota'] = np.tile(np.arange(128, dtype=np.float32)[None, :], (128, 1))
    T['cst16'] = np.tile((16 * np.arange(16, dtype=np.float32) + 7.5)[None, :], (128, 1))
    half = 32
    inv = (1.0 / (np.float32(10000.0) ** np.linspace(0.0, 1.0, half, dtype=np.float32))).astype(np.float32)
    ang = (np.arange(SEQ, dtype=np.float32)[:, None] * inv[None, :]).astype(np.float32)
    T['cos'] = np.cos(ang).astype(np.float32)
    T['sin'] = np.sin(ang).astype(np.float32)
    p = np.arange(128, dtype=np.float32)
    T['pos'] = np.stack([127.0 - p, p], axis=1).astype(np.float32)
    m = p[:, None]
    c = p[None, :]
    T['dmat'] = np.stack([np.maximum(c - m, 0), (c >= m).astype(np.float32), np.maximum(m - c, 0), (m > c).astype(np.float32)]).astype(np.float32)
    xr = np.stack([p + 1.0, 128.0 - p], axis=0)
    T['xrow'] = np.tile(xr[None], (128, 1, 1)).astype(np.float32)
    A = np.zeros((128, 4, 5, 128), np.float32)
    for g, w in enumerate(POOL_WINDOWS):
        lo, hi = w // 2, w - w // 2
        for kind, base in ((1, 1024), (3, 0), (4, SEQ - 128)):
            for t in range(128):
                ta = base + t
                a_, b_ = max(ta - lo, 0), min(ta + hi, SEQ)
                cnt = float(b_ - a_)
                for s in range(a_, b_):
                    rel = s - base
                    if 0 <= rel < 128:
                        A[rel, g, kind, t] += 1.0 / cnt
                    elif rel < 0 and kind == 1:
                        A[rel + 128, g, 0, t] += 1.0 / cnt
                    elif rel >= 128 and kind == 1:
                        A[rel - 128, g, 2, t] += 1.0 / cnt
                A[t, g, kind, t] -= 1.0
    T['amat'] = A
    di = np.zeros((5, 128, 5, 128), np.int64)
    dj = np.zeros((5, 128, 5, 128), np.int64)
    mk = np.zeros((5, 128, 5, 128), np.float32)
    q = np.arange(128)
    k = np.arange(128)
    for ty, i in enumerate((5, 0, 1, 30, 31)):
        for oi in range(5):
            o = NA_OMIN[ty] + oi
            j = i + o
            if j < 0 or j > 31:
                continue
            qr = 2 * i + q // 64
            qc = q % 64
            kr = (2 * j + k // 64)[:, None]
            kcol = (k % 64)[:, None]
            rs = np.clip(qr - 4, 0, 56)[None, :]
            vr = (kr >= rs) & (kr < rs + 8)
            cst = np.clip(qc - 8, 0, 48)[None, :]
            vc = (kcol >= cst) & (kcol < cst + 16)
            di[ty, :, oi, :] = np.clip(kr - qr[None, :] + 7, 0, 14)
            dj[ty, :, oi, :] = np.clip(kcol - qc[None, :], -15, 15) + 15
            mk[ty, :, oi, :] = (vr & vc).astype(np.float32)
    T['na_di'], T['na_dj'], T['nmask'] = di, dj, mk
    return T


_TABLES = None


def tables():
    global _TABLES
    if _TABLES is None:
        _TABLES = _const_tables()
    return _TABLES


def layer_layout(l, w_in, pool_w, pool_scale, ret_decay, na_rpb, w_br_pool, w_br_ret, w_br_na, w_out,
                 peer_w_query, peer_sub_keys, peer_u, peer_v):
    T = tables()
    L = {}
    L['w_in'] = np.ascontiguousarray(w_in[l])
    L['wbp'] = np.ascontiguousarray(w_br_pool[l].reshape(4, 64, 1024).transpose(1, 0, 2))
    L['wbr'] = np.ascontiguousarray(w_br_ret[l])
    L['wbn'] = np.ascontiguousarray(w_br_na[l])
    L['wout'] = np.ascontiguousarray(w_out[l])
    L['poolw'] = np.ascontiguousarray(pool_w[l].transpose(1, 0, 2))
    L['pscale'] = np.ascontiguousarray(pool_scale[l].reshape(4, 64).T)
    L['decay'] = np.ascontiguousarray(ret_decay[l].reshape(1, 8))
    rp = na_rpb[l]
    g = rp[:, T['na_di'], T['na_dj']]
    L['rpbT'] = np.ascontiguousarray(g.transpose(1, 2, 3, 0, 4)).astype(np.float32)
    L['wq'] = np.ascontiguousarray(peer_w_query[l].reshape(8, 128, 16, 128).transpose(2, 1, 0, 3))
    L['skT'] = np.ascontiguousarray(peer_sub_keys[l].transpose(2, 0, 1))
    L['uT'] = np.ascontiguousarray(peer_u[l].reshape(128, 128, 8, 128).transpose(0, 3, 2, 1))
    L['v'] = np.ascontiguousarray(peer_v[l])
    return L


N_CORES = 4
NTOK = 4096
DEPTH = 2
_PER_LAYER = ('g_mix', 'w_in', 'wbp', 'wbr', 'wbn', 'wout', 'poolw', 'pscale', 'decay', 'rpbT', 'g_ffn', 'wq', 'skT', 'uT', 'v')
_SHAPES = dict(g_mix=[1, 1024], w_in=[1024, 5632], wbp=[64, 4, 1024], wbr=[512, 1024], wbn=[256, 1024], wout=[1024, 1024],
               poolw=[64, 4, 64], pscale=[64, 4], decay=[1, 8], rpbT=[5, 128, 5, 4, 128], g_ffn=[1, 1024], wq=[16, 128, 8, 128],
               skT=[128, 2, 128], uT=[128, 128, 8, 128], v=[16384, 1024])
_CONST = dict(ident=[128, 128], iota=[128, 128], cst16=[128, 16], cos=[4096, 32], sin=[4096, 32], pos=[128, 2], dmat=[4, 128, 128],
              xrow=[128, 2, 128], amat=[128, 4, 5, 128], nmask=[5, 128, 5, 128])


def build_program(n_iblk=128):
    nc = bass.Bass("TRN2", target_bir_lowering=False)
    dt = lambda nm, shp, ty=F32, kind="ExternalInput": nc.dram_tensor(nm, list(shp), ty, kind=kind).ap()
    x = dt("x", [NTOK, 1024])
    y = dt("y", [NTOK, 1024], F32, "ExternalOutput")
    gfin = dt("gfin", [1, 1024])
    Cn = {k: dt(k, s) for k, s in _CONST.items()}
    W = [{k: dt(f"{k}_{l}", _SHAPES[k]) for k in _PER_LAYER} for l in range(DEPTH)]
    I = "Internal"
    Sc = dict(Pp=dt("s_Pp", [4096, 256], F32, I), Rq=dt("s_Rq", [4096, 256], BF16, I), Rk=dt("s_Rk", [4096, 256], BF16, I),
              Rv=dt("s_Rv", [4096, 512], BF16, I), Rg=dt("s_Rg", [4096, 512], F32, I), Nq=dt("s_Nq", [4096, 256], BF16, I),
              Nk=dt("s_Nk", [4096, 256], BF16, I), Nv=dt("s_Nv", [4096, 256], BF16, I), Gt=dt("s_Gt", [4096, 3072], F32, I),
              SF=dt("s_SF", [32, 64, 4, 128], BF16, I), SB=dt("s_SB", [32, 64, 4, 128], BF16, I))
    x1 = dt("s_x1", [NTOK, 1024], F32, I)
    uTb = dt("s_uTb", [128, 128, 1024], BF16, I)
    vb = dt("s_vb", [128, 128, 1024], BF16, I)
    x2 = dt("s_x2", [NTOK, 1024], F32, I)
    own = (0, NTOK // 128)
    with ExitStack() as es:
        S = Sched(nc, es)
        cur = x
        for l in range(DEPTH):
            D = dict(Cn)
            D.update(Sc)
            D.update(W[l])
            D.update(x_full=cur, g=W[l]['g_mix'], x_out=x1, uTb=uTb, vb=vb)
            mixer_a(nc, S, D, own)
            mixer_b(nc, S, D, own)
            last = (l == DEPTH - 1)
            P = dict(Cn)
            P.update(W[l])
            P.update(x_in=x1, x_out=(y if last else x2), g=W[l]['g_ffn'], gfin=gfin, uTb=uTb, vb=vb, preconverted=True)
            peer_phase(nc, S, P, NTOK, n_iblk=n_iblk, final=last)
            cur = x2
    return nc


_NC_CACHE = {}


def kernel(x, norm_mix, w_in, pool_w, pool_scale, ret_decay, na_rpb, w_br_pool, w_br_ret, w_br_na, w_out, norm_ffn,
           peer_w_query, peer_sub_keys, peer_u, peer_v, norm_final):
    f = lambda a: np.ascontiguousarray(np.asarray(a), dtype=np.float32)
    x = f(x)
    T = tables()
    shared = {k: np.ascontiguousarray(T[k]) for k in _CONST}
    shared['gfin'] = f(norm_final).reshape(1, 1024)
    args = [f(a) for a in (w_in, pool_w, pool_scale, ret_decay, na_rpb, w_br_pool, w_br_ret, w_br_na, w_out,
                           peer_w_query, peer_sub_keys, peer_u, peer_v)]
    nm, nf = f(norm_mix), f(norm_ffn)
    for l in range(DEPTH):
        L = layer_layout(l, *args)
        L['g_mix'] = nm[l].reshape(1, 1024)
        L['g_ffn'] = nf[l].reshape(1, 1024)
        for k in _PER_LAYER:
            shared[f"{k}_{l}"] = L[k]
    if 'nc' not in _NC_CACHE:
        _NC_CACHE['nc'] = build_program()
    nc = _NC_CACHE['nc']
    in_maps = []
    for c in range(N_CORES):
        m = dict(shared)
        m['x'] = np.ascontiguousarray(x[c])
        in_maps.append(m)
    res = run_bass_kernel_spmd(nc, in_maps, core_ids=list(range(N_CORES)))
    out = np.stack([np.asarray(res.results[c]['y'], dtype=np.float32) for c in range(N_CORES)], axis=0)
    return out
```

```python
import numpy as np
from contextlib import ExitStack
import concourse.bass as bass
import concourse.mybir as mybir
from concourse.bass_utils import run_bass_kernel_spmd

F32 = mybir.dt.float32
BF16 = mybir.dt.bfloat16
U32 = mybir.dt.uint32
AF = mybir.ActivationFunctionType
ALU = mybir.AluOpType
AX = mybir.AxisListType

import re as _re
_PSUM_KEY = _re.compile(r"^(bk|pp|gps|aps|ops|tps)\d*$")
EPOCH = 20000
NDMA = 16
EPS = 1e-6


class _Eng:
    def __init__(self, name, strict):
        self.name = name
        self.strict = strict
        self.count = 0
        self.sems = []
        self.known = {}
        self.dsems = []
        self.dcount = []
        self.dnext = 0


class Sched:
    def __init__(self, nc, es):
        self.nc = nc
        self.es = es
        self.E = {
            'pe': _Eng('pe', False),
            'act': _Eng('act', True),
            'dve': _Eng('dve', True),
            'pool': _Eng('pool', True),
            'sp': _Eng('sp', False),
        }
        self.lastw = {}
        self.readers = {}
        self.nsem = 0
        self.prog = {k: [] for k in self.E}

    def _newsem(self, nm):
        self.nsem += 1
        return self.es.enter_context(self.nc.semaphore(f"{nm}_{self.nsem}"))

    def _cur_sem(self, e):
        ep = e.count // EPOCH
        while len(e.sems) <= ep:
            e.sems.append(self._newsem(e.name))
        return e.sems[ep]

    def _wait(self, e, dep):
        sem, val, src = dep
        if src is e and not e.strict:
            return
        k = id(sem)
        if e.known.get(k, 0) >= val:
            return
        self.prog[e.name].append(('w', sem, val))
        e.known[k] = val

    def _deps(self, reads, writes):
        deps = []
        for b in reads:
            if b in self.lastw:
                deps.append(self.lastw[b])
        for b in writes:
            if b in self.lastw:
                deps.append(self.lastw[b])
            deps.extend(self.readers.get(b, []))
        return deps

    def _commit(self, dep, reads, writes):
        for b in reads:
            self.readers.setdefault(b, []).append(dep)
        for b in writes:
            self.lastw[b] = dep
            self.readers[b] = []

    def op(self, eng, meth, reads, writes, *a, **kw):
        fn = lambda h: getattr(h, meth)(*a, **kw)
        e = self.E[eng]
        px = [b for b in reads if _PSUM_KEY.match(b)]
        if px:
            reads = [b for b in reads if b not in px]
            writes = list(writes) + px
        for d in self._deps(reads, writes):
            self._wait(e, d)
        sem = self._cur_sem(e)
        val = e.count % EPOCH + 1
        self.prog[e.name].append(('i', fn, sem, 1))
        e.count += 1
        self._commit((sem, val, e), reads, writes)

    def dma(self, eng, reads, writes, meth='dma_start', **kw):
        fn = lambda h: getattr(h, meth)(**kw)
        e = self.E[eng]
        if not e.dsems:
            e.dsems = [self._newsem(e.name + "d") for _ in range(NDMA)]
            e.dcount = [0] * NDMA
        s = e.dnext
        e.dnext = (e.dnext + 1) % NDMA
        sem = e.dsems[s]
        if e.dcount[s] > 0:
            self._wait(e, (sem, 16 * e.dcount[s], None))
        for d in self._deps(reads, writes):
            self._wait(e, d)
        e.dcount[s] += 1
        self.prog[e.name].append(('i', fn, sem, 16))
        self._commit((sem, 16 * e.dcount[s], None), reads, writes)

    def end_phase(self):
        e = self.E['sp']
        for o in self.E.values():
            if o.count > 0:
                sem = o.sems[(o.count - 1) // EPOCH]
                self._wait(e, (sem, (o.count - 1) % EPOCH + 1, None))
            for s, c in enumerate(o.dcount):
                if c > 0:
                    self._wait(e, (o.dsems[s], 16 * c, None))
        with self.nc.Block() as blk:
            def replay(name):
                def f(h):
                    for it in self.prog[name]:
                        if it[0] == 'w':
                            h.wait_ge(it[1], it[2])
                        else:
                            it[1](h).then_inc(it[2], it[3])
                return f
            blk.sync(replay('sp'))
            blk.scalar(replay('act'))
            blk.vector(replay('dve'))
            blk.gpsimd(replay('pool'))
            blk.tensor(replay('pe'))
        self.prog = {k: [] for k in self.E}
        self.lastw = {}
        self.readers = {}


class Ctx:
    n = 0

    def __init__(self, nc, es):
        self.nc = nc
        self.es = es

    def sb(self, nm, shp, dt):
        Ctx.n += 1
        return self.es.enter_context(self.nc.sbuf_tensor(f"{nm}_{Ctx.n}", list(shp), dt))

    def ps(self, nm, shp, dt):
        Ctx.n += 1
        return self.es.enter_context(self.nc.psum_tensor(f"{nm}_{Ctx.n}", list(shp), dt))


def rmsnorm_tile(S, x_ap, x_key, g_tile, g_key, out_ap, out_key, tmp, ssq, rstd, pfx):
    S.op('act', 'activation', [x_key], [pfx + 'tmp', pfx + 'ssq'], out=tmp[:], in_=x_ap, func=AF.Square, accum_out=ssq[:])
    S.op('act', 'activation', [pfx + 'ssq'], [pfx + 'rstd'], out=rstd[:], in_=ssq[:], func=AF.Sqrt, scale=1.0 / 1024, bias=EPS)
    S.op('dve', 'reciprocal', [pfx + 'rstd'], [pfx + 'rstd'], out=rstd[:], in_=rstd[:])
    S.op('dve', 'scalar_tensor_tensor', [x_key, pfx + 'rstd', g_key], [out_key], out=out_ap, in0=x_ap, scalar=rstd[:, 0:1],
         in1=g_tile[:], op0=ALU.mult, op1=ALU.mult)


def peer_phase(nc, S, D, NT, n_iblk=128, final=False):
    with ExitStack() as es:
        C = Ctx(nc, es)
        sb, ps = C.sb, C.ps
        identf = sb("identf", [128, 128], F32)
        identb = sb("identb", [128, 128], BF16)
        iotaf = sb("iotaf", [128, 128], F32)
        iotab = sb("iotab", [128, 128], BF16)
        c16 = sb("c16", [128, 16], F32)
        skT = sb("skT", [128, 2, 128], F32)
        gB = sb("gB", [128, 1024], F32)
        gFin = sb("gFin", [128, 1024], F32) if final else None
        Gbuf = sb("Gbuf", [128, 256, 128], BF16)
        xt = [sb(f"xt{r}", [128, 2, 1024], F32) for r in range(2)]
        hnT = [sb(f"hnT{r}", [128, 8, 256], BF16) for r in range(2)]
        Wqc = [sb(f"Wqc{r}", [128, 8, 128], BF16) for r in range(2)]
        tmp = sb("tmp", [128, 1024], BF16)
        ssq = sb("ssq", [128, 1], F32)
        rstd = sb("rstd", [128, 1], F32)
        hn = sb("hn", [128, 1024], BF16)
        qc = [sb(f"qc{r}", [128, 256], F32) for r in range(2)]
        sall = sb("sall", [128, 2, 16, 128], F32)
        s2 = sb("s2", [128, 256], F32)
        Vt = sb("Vt", [128, 8, 2, 16], F32)
        It = sb("It", [128, 8, 2, 16], U32)
        cand = sb("cand", [128, 8, 256], F32)
        TV = sb("TV", [128, 8, 16], F32)
        TP = sb("TP", [128, 8, 16], U32)
        TPf = sb("TPf", [128, 8, 16], F32)
        TVs = sb("TVs", [128, 8, 16], F32)
        Ex = sb("Ex", [128, 8, 16], F32)
        Z = sb("Z", [128, 8], F32)
        rZ = sb("rZ", [128, 8], F32)
        I1f = sb("I1f", [128, 8, 16], F32)
        I2f = sb("I2f", [128, 8, 16], F32)
        OH = sb("OH", [128, 8, 16, 16], F32)
        OH2 = sb("OH2", [128, 8, 16, 16], F32)
        af = sb("af", [128, 8, 16], F32)
        bf = sb("bf", [128, 8, 16], F32)
        iF = sb("iF", [128, 128], F32)
        jF = sb("jF", [128, 128], F32)
        gFt = sb("gFt", [128, 128], F32)
        iT = sb("iT", [128, 256], F32)
        jT = sb("jT", [128, 256], F32)
        gT = sb("gT", [128, 256], F32)
        NR = 8
        OI = [sb(f"OI{r}", [128, 128], BF16) for r in range(NR)]
        OJ = [sb(f"OJ{r}", [128, 128], BF16) for r in range(NR)]
        NSLOT = 5
        UTb = [sb(f"UTb{r}", [128, 8, 128], BF16) for r in range(NSLOT)]
        Vb = [sb(f"Vb{r}", [128, 1024], BF16) for r in range(NSLOT)]
        hf = [sb(f"hf{r}", [128, 256], F32) for r in range(2)]
        hG = [sb(f"hG{r}", [128, 256], BF16) for r in range(2)]
        yo = sb("yo", [128, 2, 1024], F32)
        ops = [ps(f"ops{r}", [128, 512], F32) for r in range(4)]
        aps = [ps(f"aps{r}", [128, 512], F32) for r in range(2)]
        gps = [ps(f"gps{r}", [128, 4, 128], F32) for r in range(2)]
        tps = gps[1][:].rearrange("p a b -> p (a b)").bitcast(BF16).rearrange("p (c t) -> p c t", c=8)

        S.dma('sp', [], ['identf'], out=identf[:], in_=D['ident'])
        S.dma('sp', [], ['iotaf'], out=iotaf[:], in_=D['iota'])
        S.dma('sp', [], ['c16'], out=c16[:], in_=D['cst16'])
        S.dma('sp', [], ['skT'], out=skT[:], in_=D['skT'])
        S.dma('sp', [], ['gB'], out=gB[:], in_=D['g'].partition_broadcast(128))
        if final:
            S.dma('sp', [], ['gFin'], out=gFin[:], in_=D['gfin'].partition_broadcast(128))
        S.op('dve', 'tensor_copy', ['identf'], ['identb'], out=identb[:], in_=identf[:])
        S.op('dve', 'tensor_copy', ['iotaf'], ['iotab'], out=iotab[:], in_=iotaf[:])

        ntile = NT // 256
        x_in = D['x_in'].rearrange("(t g p) d -> t p g d", g=2, p=128)
        x_out = D['x_out'].rearrange("(t g p) d -> t p g d", g=2, p=128)
        iota16_b = iotaf[:, 0:16].unsqueeze(1).unsqueeze(1).to_broadcast([128, 8, 16, 16])
        c16_b = c16[:].unsqueeze(1).unsqueeze(1).to_broadcast([128, 8, 16, 16])
        B4 = [128, 8, 16, 16]

        if not D.get('preconverted', False):
            uT2 = D['uT'].rearrange("i p c e -> i p (c e)")
            for i0 in range(128):
                S.dma('pool', [], ['cvU%d' % i0], out=D['uTb'][i0], in_=uT2[i0])
                S.dma('pool', [], ['cvV%d' % i0], out=D['vb'][i0], in_=D['v'][i0 * 128:(i0 + 1) * 128, :])

        def wdma(i):
            sl = i % NSLOT
            S.dma('sp', ['cvU%d' % i], ['UTb%d' % sl], out=UTb[sl][:].rearrange("p c e -> p (c e)"), in_=D['uTb'][i])
            S.dma('sp', ['cvV%d' % i], ['Vb%d' % sl], out=Vb[sl][:], in_=D['vb'][i])

        def stage1(i, par):
            sl = i % NSLOT
            hv = i % 2
            for dc in range(8):
                S.op('pe', 'matmul', ['UTb%d' % sl, 'hnT%d' % par], ['aps%d' % hv], aps[hv][:, 0:256], lhsT=UTb[sl][:, dc, :],
                     rhs=hnT[par][:, dc, :], start=(dc == 0), stop=(dc == 7))

        def prep(ti, par):
            xk, hk = 'xt%d' % par, 'hnT%d' % par
            S.dma('sp', [], [xk], out=xt[par][:], in_=x_in[ti])
            yield
            for g in range(2):
                rmsnorm_tile(S, xt[par][:, g, :], xk, gB, 'gB', hn[:], 'hn', tmp, ssq, rstd, 'p')
                yield
                for c in range(8):
                    S.op('pe', 'transpose', ['hn', 'identb'], ['gps1'], out=tps[:, c, :], in_=hn[:, c * 128:(c + 1) * 128], identity=identb[:])
                S.op('act', 'copy', ['gps1'], [hk], out=hnT[par][:, :, g * 128:(g + 1) * 128], in_=tps)
                yield
            S.dma('pool', [], ['Wqc0'], out=Wqc[0][:], in_=D['wq'][0])
            for c in range(16):
                gp = gps[c % 2]
                gk = 'gps%d' % (c % 2)
                qk = 'qc%d' % (c % 2)
                wk = 'Wqc%d' % (c % 2)
                if c + 1 < 16:
                    S.dma('pool', [], ['Wqc%d' % ((c + 1) % 2)], out=Wqc[(c + 1) % 2][:], in_=D['wq'][c + 1])
                qv = gp[:, 0:2, :].rearrange("p a b -> p (a b)")
                for dc in range(8):
                    S.op('pe', 'matmul', [hk, wk], [gk], qv, lhsT=Wqc[c % 2][:, dc, :], rhs=hnT[par][:, dc, :],
                         start=(dc == 0), stop=(dc == 7))
                S.op('act', 'copy', [gk], [qk], out=qc[c % 2][:], in_=qv)
                yield
                for g in range(2):
                    S.op('pe', 'matmul', [qk, 'skT'], [gk], gp[:, 2 + g, :], lhsT=qc[c % 2][:, g * 128:(g + 1) * 128], rhs=skT[:, c % 2, :],
                         start=True, stop=True)
                S.op('act', 'copy', [gk], ['sall'], out=sall[:, :, c, :], in_=gp[:, 2:4, :])
                yield
            for g in range(2):
                for c in range(16):
                    h, p = divmod(c, 2)
                    src = sall[:, g, c, :]
                    S.op('dve', 'max', ['sall'], ['Vt'], out=Vt[:, h, p, 0:8], in_=src)
                    yield
                    S.op('dve', 'max_index', ['sall', 'Vt'], ['It'], out=It[:, h, p, 0:8], in_max=Vt[:, h, p, 0:8], in_values=src)
                    yield
                    S.op('dve', 'match_replace', ['sall', 'Vt'], ['s2'], out=s2[:, 0:128], in_to_replace=Vt[:, h, p, 0:8], in_values=src, imm_value=-1e30)
                    yield
                    S.op('dve', 'max', ['s2'], ['Vt'], out=Vt[:, h, p, 8:16], in_=s2[:, 0:128])
                    yield
                    S.op('dve', 'max_index', ['s2', 'Vt'], ['It'], out=It[:, h, p, 8:16], in_max=Vt[:, h, p, 8:16], in_values=s2[:, 0:128])
                    yield
                S.op('dve', 'tensor_tensor', ['Vt'], ['cand'], out=cand[:].rearrange("p h (a b) -> p h a b", a=16),
                     in0=Vt[:, :, 0, :].unsqueeze(3).to_broadcast(B4), in1=Vt[:, :, 1, :].unsqueeze(2).to_broadcast(B4), op=ALU.add)
                yield
                for h in range(8):
                    src = cand[:, h, :]
                    S.op('dve', 'max', ['cand'], ['TV'], out=TV[:, h, 0:8], in_=src)
                    yield
                    S.op('dve', 'max_index', ['cand', 'TV'], ['TP'], out=TP[:, h, 0:8], in_max=TV[:, h, 0:8], in_values=src)
                    yield
                    S.op('dve', 'match_replace', ['cand', 'TV'], ['s2'], out=s2[:], in_to_replace=TV[:, h, 0:8], in_values=src, imm_value=-1e30)
                    yield
                    S.op('dve', 'max', ['s2'], ['TV'], out=TV[:, h, 8:16], in_=s2[:])
                    yield
                    S.op('dve', 'max_index', ['s2', 'TV'], ['TP'], out=TP[:, h, 8:16], in_max=TV[:, h, 8:16], in_values=s2[:])
                    yield
                S.op('dve', 'tensor_tensor', ['TV'], ['TVs'], out=TVs[:], in0=TV[:], in1=TV[:, :, 0:1].to_broadcast([128, 8, 16]), op=ALU.subtract)
                yield
                S.op('act', 'activation', ['TVs'], ['Ex'], out=Ex[:], in_=TVs[:], func=AF.Exp)
                S.op('dve', 'tensor_copy', ['TP'], ['TPf'], out=TPf[:], in_=TP[:])
                yield
                S.op('dve', 'tensor_copy', ['It'], ['I1f'], out=I1f[:], in_=It[:, :, 0, :])
                yield
                S.op('dve', 'tensor_copy', ['It'], ['I2f'], out=I2f[:], in_=It[:, :, 1, :])
                yield
                S.op('dve', 'tensor_reduce', ['Ex'], ['Z'], out=Z[:], in_=Ex[:], axis=AX.X, op=ALU.add)
                yield
                S.op('dve', 'reciprocal', ['Z'], ['rZ'], out=rZ[:], in_=Z[:])
                yield
                S.op('dve', 'tensor_tensor', ['Ex', 'rZ'], ['gFt'], out=gFt[:].rearrange("p (h r) -> p h r", h=8), in0=Ex[:],
                     in1=rZ[:].unsqueeze(2).to_broadcast([128, 8, 16]), op=ALU.mult)
                yield
                S.op('dve', 'tensor_tensor', ['TPf', 'c16'], ['OH'], out=OH[:], in0=TPf[:].unsqueeze(3).to_broadcast(B4), in1=c16_b, op=ALU.subtract)
                yield
                OHf = OH[:].rearrange("p a b c -> p (a b c)")
                OH2f = OH2[:].rearrange("p a b c -> p (a b c)")
                S.op('dve', 'tensor_scalar', ['OH'], ['OH2'], out=OH2f, in0=OHf, scalar1=-8.0, scalar2=None, op0=ALU.is_gt)
                yield
                S.op('dve', 'scalar_tensor_tensor', ['OH', 'OH2'], ['OH'], out=OHf, in0=OHf, scalar=8.0, in1=OH2f, op0=ALU.is_lt, op1=ALU.mult)
                yield
                S.op('dve', 'tensor_tensor', ['OH', 'I1f'], ['OH2'], out=OH2[:], in0=OH[:], in1=I1f[:].unsqueeze(2).to_broadcast(B4), op=ALU.mult)
                yield
                S.op('dve', 'tensor_reduce', ['OH2'], ['iF'], out=iF[:].rearrange("p (h r) -> p h r", h=8), in_=OH2[:], axis=AX.X, op=ALU.add)
                yield
                S.op('dve', 'tensor_tensor', ['OH', 'iotaf'], ['OH2'], out=OH2[:], in0=OH[:], in1=iota16_b, op=ALU.mult)
                yield
                S.op('dve', 'tensor_reduce', ['OH2'], ['af'], out=af[:], in_=OH2[:], axis=AX.X, op=ALU.add)
                yield
                S.op('dve', 'scalar_tensor_tensor', ['af', 'TPf'], ['bf'], out=bf[:].rearrange("p a b -> p (a b)"),
                     in0=af[:].rearrange("p a b -> p (a b)"), scalar=-16.0, in1=TPf[:].rearrange("p a b -> p (a b)"), op0=ALU.mult, op1=ALU.add)
                yield
                S.op('dve', 'tensor_tensor', ['bf', 'iotaf'], ['OH'], out=OH[:], in0=iota16_b, in1=bf[:].unsqueeze(3).to_broadcast(B4), op=ALU.is_equal)
                yield
                S.op('dve', 'tensor_tensor', ['OH', 'I2f'], ['OH2'], out=OH2[:], in0=OH[:], in1=I2f[:].unsqueeze(2).to_broadcast(B4), op=ALU.mult)
                yield
                S.op('dve', 'tensor_reduce', ['OH2'], ['jF'], out=jF[:].rearrange("p (h r) -> p h r", h=8), in_=OH2[:], axis=AX.X, op=ALU.add)
                yield
                for k3, (src, sk, dst, dk) in enumerate(((iF, 'iF', iT, 'iT'), (jF, 'jF', jT, 'jT'), (gFt, 'gFt', gT, 'gT'))):
                    S.op('pe', 'transpose', [sk, 'identf'], ['gps0'], out=gps[0][:, k3, :], in_=src[:], identity=identf[:])
                for k3, (src, sk, dst, dk) in enumerate(((iF, 'iF', iT, 'iT'), (jF, 'jF', jT, 'jT'), (gFt, 'gFt', gT, 'gT'))):
                    S.op('act', 'copy', ['gps0'], [dk], out=dst[:, g * 128:(g + 1) * 128], in_=gps[0][:, k3, :])
                yield

        def drain(gen, n=None):
            k = 0
            for _ in gen:
                k += 1
                if n is not None and k >= n:
                    return False
            return True

        cur = prep(0, 0)
        n_yield = sum(1 for _ in cur)
        per_it = max(1, -(-n_yield // max(1, n_iblk - 6)))
        for ti in range(ntile):
            par = ti % 2
            xk = 'xt%d' % par
            for i0 in range(min(NSLOT - 1, n_iblk)):
                wdma(i0)
            for t in range(256):
                r = t % NR
                bk = (t // 4) % 2
                S.op('dve', 'tensor_scalar', ['iotab', 'iT'], ['OI%d' % r], out=OI[r][:], in0=iotab[:], scalar1=iT[:, t:t + 1], scalar2=None,
                     op0=ALU.is_equal)
                S.op('dve', 'tensor_scalar', ['iotab', 'jT', 'gT'], ['OJ%d' % r], out=OJ[r][:], in0=iotab[:], scalar1=jT[:, t:t + 1],
                     scalar2=gT[:, t:t + 1], op0=ALU.is_equal, op1=ALU.mult)
                S.op('pe', 'matmul', ['OI%d' % r, 'OJ%d' % r], ['gps%d' % bk], gps[bk][:, t % 4, :], lhsT=OJ[r][:], rhs=OI[r][:], start=True, stop=True)
                if t % 4 == 3:
                    S.op('act', 'copy', ['gps%d' % bk], ['Gbuf'], out=Gbuf[:, t - 3:t + 1, :], in_=gps[bk][:])
            nxt = prep(ti + 1, 1 - par) if ti + 1 < ntile else None
            stage1(0, par)
            for i in range(n_iblk):
                if i + NSLOT - 1 < n_iblk:
                    wdma(i + NSLOT - 1)
                if i + 1 < n_iblk:
                    stage1(i + 1, par)
                sl = i % NSLOT
                hv = i % 2
                S.op('act', 'activation', ['aps%d' % hv], ['hf%d' % hv], out=hf[hv][:], in_=aps[hv][:, 0:256], func=AF.Gelu)
                S.op('dve', 'tensor_tensor', ['hf%d' % hv, 'Gbuf'], ['hG%d' % hv], out=hG[hv][:], in0=hf[hv][:], in1=Gbuf[:, :, i], op=ALU.mult)
                for tg in range(2):
                    for dh in range(2):
                        S.op('pe', 'matmul', ['hG%d' % hv, 'Vb%d' % sl], ['ops%d' % (tg * 2 + dh)], ops[tg * 2 + dh][:],
                             lhsT=hG[hv][:, tg * 128:(tg + 1) * 128], rhs=Vb[sl][:, dh * 512:(dh + 1) * 512], start=(i == 0), stop=(i == n_iblk - 1))
                if nxt is not None and i >= 2:
                    if drain(nxt, per_it):
                        nxt = None
            if nxt is not None:
                drain(nxt)
            for tg in range(2):
                for dh in range(2):
                    S.op('dve', 'tensor_tensor', ['ops%d' % (tg * 2 + dh), xk], ['yo'], out=yo[:, tg, dh * 512:(dh + 1) * 512], in0=ops[tg * 2 + dh][:],
                         in1=xt[par][:, tg, dh * 512:(dh + 1) * 512], op=ALU.add)
            if final:
                for tg in range(2):
                    rmsnorm_tile(S, yo[:, tg, :], 'yo', gFin, 'gFin', xt[par][:, tg, :], xk, tmp, ssq, rstd, 'f')
                S.dma('pool', [xk], [], out=x_out[ti], in_=xt[par][:])
            else:
                S.dma('pool', ['yo'], [], out=x_out[ti], in_=yo[:])
        S.end_phase()


def mixer_a(nc, S, D, own):
    t0, nt = own
    with ExitStack() as es:
        C = Ctx(nc, es)
        sb, ps = C.sb, C.ps
        identf = sb("identf", [128, 128], F32)
        identb = sb("identb", [128, 128], BF16)
        Win = sb("Win", [128, 8, 5632], BF16)
        gB = sb("gB", [128, 1024], F32)
        tmp = sb("tmp", [128, 1024], BF16)
        ssq = sb("ssq", [128, 1], F32)
        rstd = sb("rstd", [128, 1], F32)
        xn = sb("xn", [128, 1024], BF16)
        xnT = sb("xnT", [128, 8, 128], BF16)
        t1 = sb("t1", [128, 8, 32], F32)
        t2 = sb("t2", [128, 8, 32], F32)
        NB = 2
        xt = [sb(f"xt{r}", [128, 1024], F32) for r in range(NB)]
        cs = [sb(f"cs{r}", [128, 2, 32], F32) for r in range(NB)]
        Pp = [sb(f"Pp{r}", [128, 256], F32) for r in range(NB)]
        rin = [sb(f"rin{r}", [128, 8, 64], F32) for r in range(NB)]
        rot = [sb(f"rot{r}", [128, 8, 64], F32) for r in range(NB)]
        Rq = [sb(f"Rq{r}", [128, 256], BF16) for r in range(NB)]
        Rk = [sb(f"Rk{r}", [128, 256], BF16) for r in range(NB)]
        Rv = [sb(f"Rv{r}", [128, 512], BF16) for r in range(NB)]
        Rg = [sb(f"Rg{r}", [128, 512], F32) for r in range(NB)]
        Nq = [sb(f"Nq{r}", [128, 256], BF16) for r in range(NB)]
        Nk = [sb(f"Nk{r}", [128, 256], BF16) for r in range(NB)]
        Nv = [sb(f"Nv{r}", [128, 256], BF16) for r in range(NB)]
        Gt = [sb(f"Gt{r}", [128, 3072], F32) for r in range(NB)]
        pp = [ps(f"pp{r}", [128, 512], F32) for r in range(4)]
        tps = ps("tps", [128, 8, 128], BF16)

        S.dma('sp', [], ['identf'], out=identf[:], in_=D['ident'])
        S.dma('sp', [], ['gB'], out=gB[:], in_=D['g'].partition_broadcast(128))
        w_v = D['w_in'].rearrange("(c p) n -> p c n", p=128)
        for k in range(11):
            S.dma('pool', [], ['Win%d' % k], out=Win[:, :, k * 512:(k + 1) * 512], in_=w_v[:, :, k * 512:(k + 1) * 512])
        S.op('dve', 'tensor_copy', ['identf'], ['identb'], out=identb[:], in_=identf[:])
        B3 = [128, 8, 32]
        kc = 0
        import os as _os
        for i in range(int(_os.environ.get('MA_NT', '32'))):
            b = i % NB
            sfx = str(b)
            is_own = t0 <= i < t0 + nt
            rows = slice(i * 128, (i + 1) * 128)
            S.dma('sp', [], ['xt' + sfx], out=xt[b][:], in_=D['x_full'][rows, :])
            S.dma('sp', [], ['cs' + sfx], out=cs[b][:, 0, :], in_=D['cos'][rows, :])
            S.dma('sp', [], ['cs' + sfx], out=cs[b][:, 1, :], in_=D['sin'][rows, :])
            _sub = int(_os.environ.get('MA_SUB', '9'))
            if _sub <= 2:
                S.end_phase()
                return
            rmsnorm_tile(S, xt[b][:], 'xt' + sfx, gB, 'gB', xn[:], 'xn', tmp, ssq, rstd, 'a')
            if _sub <= 3:
                S.end_phase()
                return
            for c in range(8):
                S.op('pe', 'transpose', ['xn', 'identb'], ['tps'], out=tps[:, c, :], in_=xn[:, c * 128:(c + 1) * 128], identity=identb[:])
            S.op('act', 'copy', ['tps'], ['xnT'], out=xnT[:], in_=tps[:])
            if _sub <= 4:
                S.end_phase()
                return
            chunks = list(range(11)) if is_own else [0, 1, 2, 4]
            _lvl = int(_os.environ.get('MA_LVL', '9'))
            if _lvl == 0:
                chunks = [0]
            for ci in chunks:
                bank = pp[kc % 4]
                bk = 'pp%d' % (kc % 4)
                kc += 1
                for dc in range(8):
                    S.op('pe', 'matmul', ['xnT', 'Win%d' % ci], [bk], bank[:], lhsT=xnT[:, dc, :], rhs=Win[:, dc, ci * 512:(ci + 1) * 512],
                         start=(dc == 0), stop=(dc == 7))
                lo, hi = bank[:, 0:256], bank[:, 256:512]
                rq = rin[b][:, 0:4, :].rearrange("p a b -> p (a b)")
                rk = rin[b][:, 4:8, :].rearrange("p a b -> p (a b)")
                if ci == 0:
                    S.op('act', 'copy', [bk], ['Pp' + sfx], out=Pp[b][:], in_=lo)
                    S.op('dve', 'tensor_copy', [bk], ['rin' + sfx], out=rq, in_=hi)
                elif ci == 1:
                    S.op('dve', 'tensor_copy', [bk], ['rin' + sfx], out=rk, in_=lo)
                    S.op('act', 'copy', [bk], ['Rv' + sfx], out=Rv[b][:, 0:256], in_=hi)
                elif ci == 2:
                    S.op('act', 'copy', [bk], ['Rv' + sfx], out=Rv[b][:, 256:512], in_=lo)
                    if is_own:
                        S.op('act', 'activation', [bk], ['Rg' + sfx], out=Rg[b][:, 0:256], in_=hi, func=AF.Silu)
                elif ci == 3:
                    S.op('act', 'activation', [bk], ['Rg' + sfx], out=Rg[b][:, 256:512], in_=lo, func=AF.Silu)
                    S.op('dve', 'tensor_copy', [bk], ['Nq' + sfx], out=Nq[b][:], in_=hi)
                elif ci == 4:
                    S.op('dve', 'tensor_copy', [bk], ['Nk' + sfx], out=Nk[b][:], in_=lo)
                    S.op('act', 'copy', [bk], ['Nv' + sfx], out=Nv[b][:], in_=hi)
                else:
                    S.op('act', 'activation', [bk], ['Gt' + sfx], out=Gt[b][:, (ci - 5) * 512:(ci - 4) * 512], in_=bank[:], func=AF.Sigmoid)
            if _lvl == 0:
                S.dma('pool', ['Pp' + sfx], [], out=D['Pp'][rows, :], in_=Pp[b][:])
                continue
            x1, x2 = rin[b][:, :, 0:32], rin[b][:, :, 32:64]
            cosb = cs[b][:, 0, :].unsqueeze(1).to_broadcast(B3)
            sinb = cs[b][:, 1, :].unsqueeze(1).to_broadcast(B3)
            rk_ = ['rin' + sfx, 'cs' + sfx]
            S.op('dve', 'tensor_tensor', rk_, ['t1'], out=t1[:], in0=x1, in1=cosb, op=ALU.mult)
            S.op('dve', 'tensor_tensor', rk_, ['t2'], out=t2[:], in0=x2, in1=sinb, op=ALU.mult)
            S.op('dve', 'tensor_tensor', ['t1', 't2'], ['rot' + sfx], out=rot[b][:, :, 0:32], in0=t1[:], in1=t2[:], op=ALU.subtract)
            S.op('dve', 'tensor_tensor', rk_, ['t1'], out=t1[:], in0=x1, in1=sinb, op=ALU.mult)
            S.op('dve', 'tensor_tensor', rk_, ['t2'], out=t2[:], in0=x2, in1=cosb, op=ALU.mult)
            S.op('dve', 'tensor_tensor', ['t1', 't2'], ['rot' + sfx], out=rot[b][:, :, 32:64], in0=t1[:], in1=t2[:], op=ALU.add)
            S.op('act', 'copy', ['rot' + sfx], ['Rq' + sfx], out=Rq[b][:], in_=rot[b][:, 0:4, :].rearrange("p a b -> p (a b)"))
            S.op('act', 'mul', ['rot' + sfx], ['Rk' + sfx], Rk[b][:], rot[b][:, 4:8, :].rearrange("p a b -> p (a b)"), 0.125)
            S.dma('pool', ['Pp' + sfx], [], out=D['Pp'][rows, :], in_=Pp[b][:])
            S.dma('pool', ['Rk' + sfx], [], out=D['Rk'][rows, :], in_=Rk[b][:])
            S.dma('pool', ['Rv' + sfx], [], out=D['Rv'][rows, :], in_=Rv[b][:])
            S.dma('pool', ['Nk' + sfx], [], out=D['Nk'][rows, :], in_=Nk[b][:])
            S.dma('pool', ['Nv' + sfx], [], out=D['Nv'][rows, :], in_=Nv[b][:])
            if is_own:
                S.dma('pool', ['Rq' + sfx], [], out=D['Rq'][rows, :], in_=Rq[b][:])
                S.dma('pool', ['Rg' + sfx], [], out=D['Rg'][rows, :], in_=Rg[b][:])
                S.dma('pool', ['Nq' + sfx], [], out=D['Nq'][rows, :], in_=Nq[b][:])
                S.dma('pool', ['Gt' + sfx], [], out=D['Gt'][rows, :], in_=Gt[b][:])
        S.end_phase()


def mixer_b(nc, S, D, own):
    t0, nt = own
    with ExitStack() as es:
        C = Ctx(nc, es)
        sb, ps = C.sb, C.ps
        identf = sb("identf", [128, 128], F32)
        identb = sb("identb", [128, 128], BF16)
        Wbp = sb("Wbp", [64, 4, 1024], BF16)
        Wbr = sb("Wbr", [128, 4, 1024], BF16)
        Wbn = sb("Wbn", [128, 2, 1024], BF16)
        Wout = sb("Wout", [128, 8, 1024], BF16)
        poolw = sb("poolw", [64, 4, 64], F32)
        pscale = sb("pscale", [64, 4], F32)
        amat = sb("amat", [128, 4, 5, 128], F32)
        Et = [sb(f"Et{r}", [128, 5, 4, 128], BF16) for r in range(5)]
        rstage = sb("rstage", [128, 5, 4, 128], F32)
        nmask = sb("nmask", [128, 5, 128], F32)
        dl = sb("dl", [128, 8], F32)
        lg = sb("lg", [128, 8], F32)
        pos = sb("pos", [128, 2], F32)
        zf = sb("zf", [128, 4], F32)
        zb = sb("zb", [128, 4], F32)
        dC = sb("dC", [128, 8], F32)
        dmat = sb("dmat", [128, 4, 128], F32)
        xrow = sb("xrow", [128, 2, 128], F32)
        Dt = sb("Dt", [128, 4, 128], F32)
        E1 = sb("E1", [128, 128], F32)
        E2 = sb("E2", [128, 128], F32)
        XF = sb("XF", [64, 4, 128], F32)
        XB = sb("XB", [64, 4, 128], F32)
        Sst = sb("Sst", [64, 4, 128], F32)
        Sob = [sb(f"Sob{r}", [64, 4, 128], BF16) for r in range(2)]
        kd = sb("kd", [128, 4, 64], BF16)
        NB = 2
        xt = [sb(f"xt{r}", [128, 1024], F32) for r in range(NB)]
        Gt = [sb(f"Gt{r}", [128, 3072], F32) for r in range(NB)]
        Rq = [sb(f"Rq{r}", [128, 256], BF16) for r in range(NB)]
        Rk = [sb(f"Rk{r}", [128, 256], BF16) for r in range(NB)]
        Rv = [sb(f"Rv{r}", [128, 512], BF16) for r in range(NB)]
        Rg = [sb(f"Rg{r}", [128, 512], F32) for r in range(NB)]
        SFt = [sb(f"SFt{r}", [64, 4, 128], BF16) for r in range(NB)]
        SBt = [sb(f"SBt{r}", [64, 4, 128], BF16) for r in range(NB)]
        Nq = [sb(f"Nq{r}", [128, 256], BF16) for r in range(NB)]
        Nk = [[sb(f"Nk{r}_{o}", [128, 256], BF16) for o in range(5)] for r in range(NB)]
        Nv1 = [[sb(f"Nv{r}_{o}", [128, 4, 65], BF16) for o in range(5)] for r in range(NB)]
        Pp = [[sb(f"Pp{r}_{o}", [128, 256], F32) for o in range(3)] for r in range(NB)]
        qkT = sb("qkT", [64, 8, 128], BF16)
        qfT = sb("qfT", [64, 4, 128], BF16)
        qbT = sb("qbT", [64, 4, 128], BF16)
        PD = sb("PD", [128, 4, 128], BF16)
        ysq = sb("ysq", [128, 4, 128], F32)
        ssq4 = sb("ssq4", [128, 4], F32)
        rs4 = sb("rs4", [128, 4], F32)
        yr = sb("yr", [128, 4, 128], F32)
        yrb = sb("yrb", [128, 512], BF16)
        yrT = sb("yrT", [128, 4, 128], BF16)
        NTa = sb("NTa", [64, 24, 128], BF16)
        Pm = sb("Pm", [128, 4, 128], BF16)
        PEm = sb("PEm", [128, 4, 128], BF16)
        rden = sb("rden", [128, 4], F32)
        ynb = sb("ynb", [128, 4, 64], BF16)
        ynT = sb("ynT", [128, 2, 128], BF16)
        dT = sb("dT", [64, 4, 128], F32)
        ypT = sb("ypT", [64, 4, 128], BF16)
        mg = sb("mg", [128, 1024], F32)
        tg_ = sb("tg", [128, 1024], F32)
        mgb = sb("mgb", [128, 1024], BF16)
        mT = sb("mT", [128, 8, 128], BF16)
        xo = [sb(f"xo{r}", [128, 1024], F32) for r in range(NB)]
        bank = [ps(f"bk{r}", [128, 512], F32) for r in range(8)]
        sc_ps = bank[0][:].rearrange("p (h c) -> p h c", h=4)
        y_ps = bank[1][:].rearrange("p (h c) -> p h c", h=4)
        st_ps = [bank[2][:].rearrange("p (h c) -> p h c", h=4), bank[6][:].rearrange("p (h c) -> p h c", h=4)]
        st_k = ['bk2', 'bk6']
        o_ps = bank[3][:, 0:260].rearrange("p (h c) -> p h c", h=4)
        pl_ps = bank[4][0:64, :].rearrange("p (h c) -> p h c", h=4)
        tr_ps = bank[5][:].bitcast(BF16).rearrange("p (c t) -> p c t", c=8)
        tr7_ps = bank[7][:].bitcast(BF16).rearrange("p (c t) -> p c t", c=8)

        S.dma('sp', [], ['identf'], out=identf[:], in_=D['ident'])
        S.op('dve', 'tensor_copy', ['identf'], ['identb'], out=identb[:], in_=identf[:])
        for g in range(4):
            S.dma('pool', [], ['Wbp'], out=Wbp[:, g, :], in_=D['wbp'][:, g, :])
        S.dma('pool', [], ['Wbr'], out=Wbr[:], in_=D['wbr'].rearrange("(h e) n -> e h n", e=128))
        S.dma('pool', [], ['Wbn'], out=Wbn[:], in_=D['wbn'].rearrange("(h e) n -> e h n", e=128))
        S.dma('pool', [], ['Wout'], out=Wout[:], in_=D['wout'].rearrange("(h e) n -> e h n", e=128))
        S.dma('sp', [], ['poolw'], out=poolw[:], in_=D['poolw'])
        S.dma('sp', [], ['pscale'], out=pscale[:], in_=D['pscale'])
        S.dma('sp', [], ['amat'], out=amat[:], in_=D['amat'])
        S.dma('sp', [], ['dl'], out=dl[:], in_=D['decay'].partition_broadcast(128))
        S.dma('sp', [], ['pos'], out=pos[:], in_=D['pos'])
        S.dma('sp', [], ['dmat'], out=dmat[:], in_=D['dmat'].rearrange("k p c -> p k c"))
        S.dma('sp', [], ['xrow'], out=xrow[:], in_=D['xrow'])
        for r in range(NB):
            for o in range(5):
                S.op('dve', 'memset', [], ['Nv%d_%d' % (r, o)], Nv1[r][o][:], 1.0)
        for ty in range(5):
            S.dma('sp', [], ['rstage'], out=rstage[:], in_=D['rpbT'][ty])
            S.dma('sp', [], ['nmask'], out=nmask[:], in_=D['nmask'][ty])
            S.op('act', 'activation', ['rstage'], ['rstage'], out=rstage[:], in_=rstage[:], func=AF.Exp)
            S.op('dve', 'tensor_tensor', ['rstage', 'nmask'], ['Et%d' % ty], out=Et[ty][:], in0=rstage[:],
                 in1=nmask[:].unsqueeze(2).to_broadcast([128, 5, 4, 128]), op=ALU.mult)
        S.op('act', 'activation', ['dl'], ['lg'], out=lg[:], in_=dl[:], func=AF.Exp, scale=-1.0)
        S.op('act', 'activation', ['lg'], ['lg'], out=lg[:], in_=lg[:], func=AF.Ln, bias=1.0)
        S.op('act', 'mul', ['lg'], ['lg'], lg[:], lg[:], -1.0)
        S.op('act', 'activation', ['lg', 'pos'], ['zf'], out=zf[:], in_=lg[:, 0:4], func=AF.Exp, scale=pos[:, 0:1])
        S.op('act', 'activation', ['lg', 'pos'], ['zb'], out=zb[:], in_=lg[:, 4:8], func=AF.Exp, scale=pos[:, 1:2])
        S.op('act', 'activation', ['lg'], ['dC'], out=dC[:], in_=lg[:], func=AF.Exp, scale=128.0)
        for h in range(4):
            S.op('act', 'activation', ['lg', 'dmat'], ['E1'], out=E1[:], in_=dmat[:, 0, :], func=AF.Exp, scale=lg[:, h:h + 1])
            S.op('act', 'activation', ['lg', 'dmat'], ['E2'], out=E2[:], in_=dmat[:, 2, :], func=AF.Exp, scale=lg[:, 4 + h:5 + h])
            S.op('dve', 'tensor_tensor', ['E1', 'dmat'], ['E1'], out=E1[:], in0=E1[:], in1=dmat[:, 1, :], op=ALU.mult)
            S.op('dve', 'tensor_tensor', ['E2', 'dmat'], ['E2'], out=E2[:], in0=E2[:], in1=dmat[:, 3, :], op=ALU.mult)
            S.op('dve', 'tensor_tensor', ['E1', 'E2'], ['Dt'], out=Dt[:, h, :], in0=E1[:], in1=E2[:], op=ALU.add)
            S.op('act', 'activation', ['lg', 'xrow'], ['XF'], out=XF[:, h, :], in_=xrow[0:64, 0, :], func=AF.Exp, scale=lg[0:64, h:h + 1])
            S.op('act', 'activation', ['lg', 'xrow'], ['XB'], out=XB[:, h, :], in_=xrow[0:64, 1, :], func=AF.Exp, scale=lg[0:64, 4 + h:5 + h])

        def sweep(order, zt, zk, dcol, dst, dkey, store_pred, upd_pred):
            S.op('dve', 'memset', [], ['Sst'], Sst[:], 0.0)
            for cnt, n in enumerate(order):
                b = cnt % NB
                sfx = str(b)
                rows = slice(n * 128, (n + 1) * 128)
                if store_pred(n):
                    S.op('act', 'copy', ['Sst'], ['Sob' + sfx], out=Sob[b][:], in_=Sst[:])
                    S.dma('pool', ['Sob' + sfx], [dkey + str(n)], out=dst[n], in_=Sob[b][:])
                if not upd_pred(n):
                    continue
                S.dma('sp', [], ['Rk' + sfx], out=Rk[b][:], in_=D['Rk'][rows, :])
                S.dma('sp', [], ['Rv' + sfx], out=Rv[b][:], in_=D['Rv'][rows, :])
                S.op('dve', 'tensor_tensor', ['Rk' + sfx, zk], ['kd'], out=kd[:], in0=Rk[b][:].rearrange("p (h d) -> p h d", h=4),
                     in1=zt[:].unsqueeze(2).to_broadcast([128, 4, 64]), op=ALU.mult)
                for h in range(4):
                    S.op('pe', 'matmul', ['kd', 'Rv' + sfx], ['bk0'], bank[0][0:64, h * 128:(h + 1) * 128], lhsT=kd[:, h, :],
                         rhs=Rv[b][:, h * 128:(h + 1) * 128], start=True, stop=True)
                S.op('dve', 'tensor_tensor', ['Sst', 'dC'], ['Sst'], out=Sst[:], in0=Sst[:],
                     in1=dC[0:64, dcol:dcol + 4].unsqueeze(2).to_broadcast([64, 4, 128]), op=ALU.mult)
                S.op('dve', 'tensor_tensor', ['Sst', 'bk0'], ['Sst'], out=Sst[:], in0=Sst[:],
                     in1=bank[0][0:64, :].rearrange("p (h c) -> p h c", h=4), op=ALU.add)

        last = t0 + nt - 1
        sweep(list(range(0, last + 1)), zf, 'zf', 0, D['SF'], 'dSF', lambda n: n >= t0, lambda n: n < last)
        sweep(list(range(31, t0 - 1, -1)), zb, 'zb', 4, D['SB'], 'dSB', lambda n: n <= last, lambda n: n > t0)

        B4 = [128, 4, 128]
        for it in range(nt):
            i = t0 + it
            b = it % NB
            sfx = str(b)
            rows = slice(i * 128, (i + 1) * 128)
            ty = {0: 1, 1: 2, 30: 3, 31: 4}.get(i, 0)
            offs = [o for o in range(NA_OMIN[ty], NA_OMIN[ty] + 5) if 0 <= i + o <= 31]
            poffs = [o for o in range(-1, 2) if 0 <= i + o <= 31]
            S.dma('sp', [], ['xt' + sfx], out=xt[b][:], in_=D['x_full'][rows, :])
            S.dma('sp', [], ['Gt' + sfx], out=Gt[b][:], in_=D['Gt'][rows, :])
            S.dma('sp', [], ['Rq' + sfx], out=Rq[b][:], in_=D['Rq'][rows, :])
            S.dma('sp', [], ['Rk' + sfx], out=Rk[b][:], in_=D['Rk'][rows, :])
            S.dma('sp', [], ['Rv' + sfx], out=Rv[b][:], in_=D['Rv'][rows, :])
            S.dma('sp', [], ['Rg' + sfx], out=Rg[b][:], in_=D['Rg'][rows, :])
            S.dma('sp', ['dSF%d' % i], ['SFt' + sfx], out=SFt[b][:], in_=D['SF'][i])
            S.dma('sp', ['dSB%d' % i], ['SBt' + sfx], out=SBt[b][:], in_=D['SB'][i])
            S.dma('sp', [], ['Nq' + sfx], out=Nq[b][:], in_=D['Nq'][rows, :])
            for oi, o in enumerate(offs):
                r2 = slice((i + o) * 128, (i + o + 1) * 128)
                S.dma('sp', [], ['Nk%s_%d' % (sfx, oi)], out=Nk[b][oi][:], in_=D['Nk'][r2, :])
                S.dma('sp', [], ['Nv%s_%d' % (sfx, oi)], out=Nv1[b][oi][:, :, 0:64], in_=D['Nv'][r2, :].rearrange("p (h d) -> p h d", h=4))
            for oi, o in enumerate(poffs):
                r2 = slice((i + o) * 128, (i + o + 1) * 128)
                S.dma('sp', [], ['Pp%s_%d' % (sfx, oi)], out=Pp[b][oi][:], in_=D['Pp'][r2, :])

            if 'uTb' in D:
                cpt = -(-128 // nt)
                uT2 = D['uT'].rearrange("i p c e -> i p (c e)")
                for i0 in range(it * cpt, min(128, (it + 1) * cpt)):
                    S.dma('pool', [], [], out=D['uTb'][i0], in_=uT2[i0])
                    S.dma('pool', [], [], out=D['vb'][i0], in_=D['v'][i0 * 128:(i0 + 1) * 128, :])
            def gen_ret():
                for h in range(4):
                    S.op('pe', 'transpose', ['Rq' + sfx, 'identb'], ['bk5'], out=tr_ps[0:64, h, :], in_=Rq[b][:, h * 64:(h + 1) * 64], identity=identb[:])
                    S.op('pe', 'transpose', ['Rk' + sfx, 'identb'], ['bk5'], out=tr_ps[0:64, 4 + h, :], in_=Rk[b][:, h * 64:(h + 1) * 64], identity=identb[:])
                yield
                S.op('act', 'copy', ['bk5'], ['qkT'], out=qkT[:], in_=tr_ps[0:64, :, :])
                yield
                S.op('dve', 'tensor_tensor', ['qkT', 'XF'], ['qfT'], out=qfT[:], in0=qkT[:, 0:4, :], in1=XF[:], op=ALU.mult)
                S.op('dve', 'tensor_tensor', ['qkT', 'XB'], ['qbT'], out=qbT[:], in0=qkT[:, 0:4, :], in1=XB[:], op=ALU.mult)
                yield
                for h in range(4):
                    S.op('pe', 'matmul', ['qkT'], ['bk0'], sc_ps[:, h, :], lhsT=qkT[:, 4 + h, :], rhs=qkT[:, h, :], start=True, stop=True)
                yield
                S.op('dve', 'tensor_tensor', ['bk0', 'Dt'], ['PD'], out=PD[:], in0=sc_ps, in1=Dt[:], op=ALU.mult)
                yield
                for h in range(4):
                    S.op('pe', 'matmul', ['PD', 'Rv' + sfx], ['bk1'], y_ps[:, h, :], lhsT=PD[:, h, :], rhs=Rv[b][:, h * 128:(h + 1) * 128],
                         start=(h == 0), stop=False, skip_group_check=True)
                    S.op('pe', 'matmul', ['qfT', 'SFt' + sfx], ['bk1'], y_ps[:, h, :], lhsT=qfT[:, h, :], rhs=SFt[b][:, h, :],
                         start=False, stop=False, skip_group_check=True)
                    S.op('pe', 'matmul', ['qbT', 'SBt' + sfx], ['bk1'], y_ps[:, h, :], lhsT=qbT[:, h, :], rhs=SBt[b][:, h, :],
                         start=False, stop=(h == 3), skip_group_check=True)
                yield
                S.op('act', 'activation', ['bk1'], ['ysq'], out=ysq[:], in_=y_ps, func=AF.Square)
                yield
                S.op('dve', 'tensor_reduce', ['ysq'], ['ssq4'], out=ssq4[:], in_=ysq[:], axis=AX.X, op=ALU.add)
                yield
                S.op('act', 'activation', ['ssq4'], ['rs4'], out=rs4[:], in_=ssq4[:], func=AF.Sqrt, scale=1.0 / 128, bias=EPS)
                yield
                S.op('dve', 'reciprocal', ['rs4'], ['rs4'], out=rs4[:], in_=rs4[:])
                S.op('dve', 'tensor_tensor', ['bk1', 'rs4'], ['yr'], out=yr[:], in0=y_ps, in1=rs4[:].unsqueeze(2).to_broadcast(B4), op=ALU.mult)
                S.op('dve', 'tensor_tensor', ['yr', 'Rg' + sfx], ['yrb'], out=yrb[:], in0=yr[:].rearrange("p h c -> p (h c)"), in1=Rg[b][:], op=ALU.mult)
                yield
                for h in range(4):
                    S.op('pe', 'transpose', ['yrb', 'identb'], ['bk5'], out=tr_ps[:, h, :], in_=yrb[:, h * 128:(h + 1) * 128], identity=identb[:])
                yield
                S.op('act', 'copy', ['bk5'], ['yrT'], out=yrT[:], in_=tr_ps[:, 0:4, :])


                yield

            def gen_na():
                srcs = [(Nq[b], 'Nq' + sfx)] + [(Nk[b][oi], 'Nk%s_%d' % (sfx, oi)) for oi in range(len(offs))]
                for r0 in range(0, len(srcs), 2):
                    grp = srcs[r0:r0 + 2]
                    for gi, (src, sk) in enumerate(grp):
                        for h in range(4):
                            S.op('pe', 'transpose', [sk, 'identb'], ['bk7'], out=tr7_ps[0:64, gi * 4 + h, :], in_=src[:, h * 64:(h + 1) * 64], identity=identb[:])
                    n_ = 4 * len(grp)
                    yield
                    S.op('act', 'copy', ['bk7'], ['NTa'], out=NTa[:, r0 * 4:r0 * 4 + n_, :], in_=tr7_ps[0:64, 0:n_, :])
                yield
                for oi, o in enumerate(offs):
                    sp_, sk_ = st_ps[oi % 2], st_k[oi % 2]
                    for h in range(4):
                        S.op('pe', 'matmul', ['NTa'], [sk_], sp_[:, h, :], lhsT=NTa[:, 4 + 4 * oi + h, :], rhs=NTa[:, h, :], start=True, stop=True)
                    yield
                    S.op('act', 'activation', [sk_], ['Pm'], out=Pm[:], in_=sp_, func=AF.Exp, scale=0.125)
                    yield
                    S.op('dve', 'tensor_tensor', ['Pm', 'Et%d' % ty], ['PEm'], out=PEm[:], in0=Pm[:], in1=Et[ty][:, o - NA_OMIN[ty], :, :], op=ALU.mult)
                    yield
                    for h in range(4):
                        S.op('pe', 'matmul', ['PEm', 'Nv%s_%d' % (sfx, oi)], ['bk3'], o_ps[:, h, :], lhsT=PEm[:, h, :], rhs=Nv1[b][oi][:, h, :],
                             start=(oi == 0 and h == 0), stop=(oi == len(offs) - 1 and h == 3), skip_group_check=True)
                yield
                S.op('dve', 'reciprocal', ['bk3'], ['rden'], out=rden[:], in_=o_ps[:, :, 64:65].rearrange("p h c -> p (h c)"))
                S.op('dve', 'tensor_tensor', ['bk3', 'rden'], ['ynb'], out=ynb[:], in0=o_ps[:, :, 0:64], in1=rden[:].unsqueeze(2).to_broadcast([128, 4, 64]), op=ALU.mult)
                ynf = ynb[:].rearrange("p h d -> p (h d)")
                yield
                for c in range(2):
                    S.op('pe', 'transpose', ['ynb', 'identb'], ['bk7'], out=tr7_ps[:, c, :], in_=ynf[:, c * 128:(c + 1) * 128], identity=identb[:])
                yield
                S.op('act', 'copy', ['bk7'], ['ynT'], out=ynT[:], in_=tr7_ps[:, 0:2, :])


                yield

            def gen_pool():
                for g in range(4):
                    for oi, o in enumerate(poffs):
                        kind = {-1: 0, 1: 2}.get(o, 3 if i == 0 else (4 if i == 31 else 1))
                        S.op('pe', 'matmul', ['Pp%s_%d' % (sfx, oi), 'amat'], ['bk4'], pl_ps[:, g, :], lhsT=Pp[b][oi][:, g * 64:(g + 1) * 64],
                             rhs=amat[:, g, kind, :], start=(oi == 0), stop=(oi == len(poffs) - 1))
                yield
                S.op('act', 'copy', ['bk4'], ['dT'], out=dT[:], in_=pl_ps)
                yield
                for g in range(4):
                    S.op('pe', 'matmul', ['dT', 'poolw'], ['bk4'], pl_ps[:, g, :], lhsT=poolw[:, g, :], rhs=dT[:, g, :], start=True, stop=True)
                yield
                S.op('dve', 'tensor_tensor', ['bk4', 'pscale'], ['ypT'], out=ypT[:], in0=pl_ps, in1=pscale[:].unsqueeze(2).to_broadcast([64, 4, 128]), op=ALU.mult)


                yield

            gens = [gen_ret(), gen_na(), gen_pool()]
            while gens:
                for g_ in list(gens):
                    try:
                        next(g_)
                    except StopIteration:
                        gens.remove(g_)
            def branch(nk, lhs_list, w_tile, goff, first, lastb):
                for nh in range(2):
                    bkk = bank[6 + nh]
                    for kk in range(nk):
                        S.op('pe', 'matmul', lhs_list[1] + [w_tile[1]], ['bk%d' % (6 + nh)], bkk[:], lhsT=lhs_list[0][:, kk, :],
                             rhs=w_tile[0][:, kk, nh * 512:(nh + 1) * 512], start=(kk == 0), stop=(kk == nk - 1))
                    cols = slice(nh * 512, (nh + 1) * 512)
                    gsl = Gt[b][:, goff + nh * 512: goff + (nh + 1) * 512]
                    if first:
                        S.op('dve', 'tensor_tensor', ['bk%d' % (6 + nh), 'Gt' + sfx], ['mg'], out=mg[:, cols], in0=bkk[:], in1=gsl, op=ALU.mult)
                    else:
                        S.op('dve', 'tensor_tensor', ['bk%d' % (6 + nh), 'Gt' + sfx], ['tg'], out=tg_[:, cols], in0=bkk[:], in1=gsl, op=ALU.mult)
                        if lastb:
                            S.op('dve', 'tensor_tensor', ['mg', 'tg'], ['mgb'], out=mgb[:, cols], in0=mg[:, cols], in1=tg_[:, cols], op=ALU.add)
                        else:
                            S.op('dve', 'tensor_tensor', ['mg', 'tg'], ['mg'], out=mg[:, cols], in0=mg[:, cols], in1=tg_[:, cols], op=ALU.add)

            branch(4, (ypT, ['ypT']), (Wbp, 'Wbp'), 0, True, False)
            branch(4, (yrT, ['yrT']), (Wbr, 'Wbr'), 1024, False, False)
            branch(2, (ynT, ['ynT']), (Wbn, 'Wbn'), 2048, False, True)
            for c in range(8):
                S.op('pe', 'transpose', ['mgb', 'identb'], ['bk5'], out=tr_ps[:, c, :], in_=mgb[:, c * 128:(c + 1) * 128], identity=identb[:])
            S.op('act', 'copy', ['bk5'], ['mT'], out=mT[:], in_=tr_ps)
            for nh in range(2):
                bkk = bank[6 + nh]
                for dc in range(8):
                    S.op('pe', 'matmul', ['mT', 'Wout'], ['bk%d' % (6 + nh)], bkk[:], lhsT=mT[:, dc, :], rhs=Wout[:, dc, nh * 512:(nh + 1) * 512],
                         start=(dc == 0), stop=(dc == 7))
                cols = slice(nh * 512, (nh + 1) * 512)
                S.op('dve', 'tensor_tensor', ['bk%d' % (6 + nh), 'xt' + sfx], ['xo' + sfx], out=xo[b][:, cols], in0=bkk[:], in1=xt[b][:, cols], op=ALU.add)
            S.dma('pool', ['xo' + sfx], [], out=D['x_out'][it * 128:(it + 1) * 128, :], in_=xo[b][:])
        S.end_phase()


POOL_WINDOWS = (2, 4, 8, 16)
SEQ = 4096
NA_OMIN = (-2, 0, -2, -2, -3)
NA_NOFF = (5, 4, 4, 4, 4)


def _const_tables():
    T = {}
    T['ident'] = np.eye(128, dtype=np.float32)
    T['iota'] = np.tile(np.arange(128, dtype=np.float32)[None, :], (128, 1))
    T['cst16'] = np.tile((16 * np.arange(16, dtype=np.float32) + 7.5)[None, :], (128, 1))
    half = 32
    inv = (1.0 / (np.float32(10000.0) ** np.linspace(0.0, 1.0, half, dtype=np.float32))).astype(np.float32)
    ang = (np.arange(SEQ, dtype=np.float32)[:, None] * inv[None, :]).astype(np.float32)
    T['cos'] = np.cos(ang).astype(np.float32)
    T['sin'] = np.sin(ang).astype(np.float32)
    p = np.arange(128, dtype=np.float32)
    T['pos'] = np.stack([127.0 - p, p], axis=1).astype(np.float32)
    m = p[:, None]
    c = p[None, :]
    T['dmat'] = np.stack([np.maximum(c - m, 0), (c >= m).astype(np.float32), np.maximum(m - c, 0), (m > c).astype(np.float32)]).astype(np.float32)
    xr = np.stack([p + 1.0, 128.0 - p], axis=0)
    T['xrow'] = np.tile(xr[None], (128, 1, 1)).astype(np.float32)
    A = np.zeros((128, 4, 5, 128), np.float32)
    for g, w in enumerate(POOL_WINDOWS):
        lo, hi = w // 2, w - w // 2
        for kind, base in ((1, 1024), (3, 0), (4, SEQ - 128)):
            for t in range(128):
                ta = base + t
                a_, b_ = max(ta - lo, 0), min(ta + hi, SEQ)
                cnt = float(b_ - a_)
                for s in range(a_, b_):
                    rel = s - base
                    if 0 <= rel < 128:
                        A[rel, g, kind, t] += 1.0 / cnt
                    elif rel < 0 and kind == 1:
                        A[rel + 128, g, 0, t] += 1.0 / cnt
                    elif rel >= 128 and kind == 1:
                        A[rel - 128, g, 2, t] += 1.0 / cnt
                A[t, g, kind, t] -= 1.0
    T['amat'] = A
    di = np.zeros((5, 128, 5, 128), np.int64)
    dj = np.zeros((5, 128, 5, 128), np.int64)
    mk = np.zeros((5, 128, 5, 128), np.float32)
    q = np.arange(128)
    k = np.arange(128)
    for ty, i in enumerate((5, 0, 1, 30, 31)):
        for oi in range(5):
            o = NA_OMIN[ty] + oi
            j = i + o
            if j < 0 or j > 31:
                continue
            qr = 2 * i + q // 64
            qc = q % 64
            kr = (2 * j + k // 64)[:, None]
            kcol = (k % 64)[:, None]
            rs = np.clip(qr - 4, 0, 56)[None, :]
            vr = (kr >= rs) & (kr < rs + 8)
            cst = np.clip(qc - 8, 0, 48)[None, :]
            vc = (kcol >= cst) & (kcol < cst + 16)
            di[ty, :, oi, :] = np.clip(kr - qr[None, :] + 7, 0, 14)
            dj[ty, :, oi, :] = np.clip(kcol - qc[None, :], -15, 15) + 15
            mk[ty, :, oi, :] = (vr & vc).astype(np.float32)
    T['na_di'], T['na_dj'], T['nmask'] = di, dj, mk
    return T


_TABLES = None


def tables():
    global _TABLES
    if _TABLES is None:
        _TABLES = _const_tables()
    return _TABLES


def layer_layout(l, w_in, pool_w, pool_scale, ret_decay, na_rpb, w_br_pool, w_br_ret, w_br_na, w_out,
                 peer_w_query, peer_sub_keys, peer_u, peer_v):
    T = tables()
    L = {}
    L['w_in'] = np.ascontiguousarray(w_in[l])
    L['wbp'] = np.ascontiguousarray(w_br_pool[l].reshape(4, 64, 1024).transpose(1, 0, 2))
    L['wbr'] = np.ascontiguousarray(w_br_ret[l])
    L['wbn'] = np.ascontiguousarray(w_br_na[l])
    L['wout'] = np.ascontiguousarray(w_out[l])
    L['poolw'] = np.ascontiguousarray(pool_w[l].transpose(1, 0, 2))
    L['pscale'] = np.ascontiguousarray(pool_scale[l].reshape(4, 64).T)
    L['decay'] = np.ascontiguousarray(ret_decay[l].reshape(1, 8))
    rp = na_rpb[l]
    g = rp[:, T['na_di'], T['na_dj']]
    L['rpbT'] = np.ascontiguousarray(g.transpose(1, 2, 3, 0, 4)).astype(np.float32)
    L['wq'] = np.ascontiguousarray(peer_w_query[l].reshape(8, 128, 16, 128).transpose(2, 1, 0, 3))
    L['skT'] = np.ascontiguousarray(peer_sub_keys[l].transpose(2, 0, 1))
    L['uT'] = np.ascontiguousarray(peer_u[l].reshape(128, 128, 8, 128).transpose(0, 3, 2, 1))
    L['v'] = np.ascontiguousarray(peer_v[l])
    return L


N_CORES = 4
NTOK = 4096
DEPTH = 2
_PER_LAYER = ('g_mix', 'w_in', 'wbp', 'wbr', 'wbn', 'wout', 'poolw', 'pscale', 'decay', 'rpbT', 'g_ffn', 'wq', 'skT', 'uT', 'v')
_SHAPES = dict(g_mix=[1, 1024], w_in=[1024, 5632], wbp=[64, 4, 1024], wbr=[512, 1024], wbn=[256, 1024], wout=[1024, 1024],
               poolw=[64, 4, 64], pscale=[64, 4], decay=[1, 8], rpbT=[5, 128, 5, 4, 128], g_ffn=[1, 1024], wq=[16, 128, 8, 128],
               skT=[128, 2, 128], uT=[128, 128, 8, 128], v=[16384, 1024])
_CONST = dict(ident=[128, 128], iota=[128, 128], cst16=[128, 16], cos=[4096, 32], sin=[4096, 32], pos=[128, 2], dmat=[4, 128, 128],
              xrow=[128, 2, 128], amat=[128, 4, 5, 128], nmask=[5, 128, 5, 128])


def build_program(n_iblk=128):
    nc = bass.Bass("TRN2", target_bir_lowering=False)
    dt = lambda nm, shp, ty=F32, kind="ExternalInput": nc.dram_tensor(nm, list(shp), ty, kind=kind).ap()
    x = dt("x", [NTOK, 1024])
    y = dt("y", [NTOK, 1024], F32, "ExternalOutput")
    gfin = dt("gfin", [1, 1024])
    Cn = {k: dt(k, s) for k, s in _CONST.items()}
    W = [{k: dt(f"{k}_{l}", _SHAPES[k]) for k in _PER_LAYER} for l in range(DEPTH)]
    I = "Internal"
    Sc = dict(Pp=dt("s_Pp", [4096, 256], F32, I), Rq=dt("s_Rq", [4096, 256], BF16, I), Rk=dt("s_Rk", [4096, 256], BF16, I),
              Rv=dt("s_Rv", [4096, 512], BF16, I), Rg=dt("s_Rg", [4096, 512], F32, I), Nq=dt("s_Nq", [4096, 256], BF16, I),
              Nk=dt("s_Nk", [4096, 256], BF16, I), Nv=dt("s_Nv", [4096, 256], BF16, I), Gt=dt("s_Gt", [4096, 3072], F32, I),
              SF=dt("s_SF", [32, 64, 4, 128], BF16, I), SB=dt("s_SB", [32, 64, 4, 128], BF16, I))
    x1 = dt("s_x1", [NTOK, 1024], F32, I)
    uTb = dt("s_uTb", [128, 128, 1024], BF16, I)
    vb = dt("s_vb", [128, 128, 1024], BF16, I)
    x2 = dt("s_x2", [NTOK, 1024], F32, I)
    own = (0, NTOK // 128)
    with ExitStack() as es:
        S = Sched(nc, es)
        cur = x
        for l in range(DEPTH):
            D = dict(Cn)
            D.update(Sc)
            D.update(W[l])
            D.update(x_full=cur, g=W[l]['g_mix'], x_out=x1, uTb=uTb, vb=vb)
            mixer_a(nc, S, D, own)
            mixer_b(nc, S, D, own)
            last = (l == DEPTH - 1)
            P = dict(Cn)
            P.update(W[l])
            P.update(x_in=x1, x_out=(y if last else x2), g=W[l]['g_ffn'], gfin=gfin, uTb=uTb, vb=vb, preconverted=True)
            peer_phase(nc, S, P, NTOK, n_iblk=n_iblk, final=last)
            cur = x2
    return nc


_NC_CACHE = {}


def kernel(x, norm_mix, w_in, pool_w, pool_scale, ret_decay, na_rpb, w_br_pool, w_br_ret, w_br_na, w_out, norm_ffn,
           peer_w_query, peer_sub_keys, peer_u, peer_v, norm_final):
    f = lambda a: np.ascontiguousarray(np.asarray(a), dtype=np.float32)
    x = f(x)
    T = tables()
    shared = {k: np.ascontiguousarray(T[k]) for k in _CONST}
    shared['gfin'] = f(norm_final).reshape(1, 1024)
    args = [f(a) for a in (w_in, pool_w, pool_scale, ret_decay, na_rpb, w_br_pool, w_br_ret, w_br_na, w_out,
                           peer_w_query, peer_sub_keys, peer_u, peer_v)]
    nm, nf = f(norm_mix), f(norm_ffn)
    for l in range(DEPTH):
        L = layer_layout(l, *args)
        L['g_mix'] = nm[l].reshape(1, 1024)
        L['g_ffn'] = nf[l].reshape(1, 1024)
        for k in _PER_LAYER:
            shared[f"{k}_{l}"] = L[k]
    if 'nc' not in _NC_CACHE:
        _NC_CACHE['nc'] = build_program()
    nc = _NC_CACHE['nc']
    in_maps = []
    for c in range(N_CORES):
        m = dict(shared)
        m['x'] = np.ascontiguousarray(x[c])
        in_maps.append(m)
    res = run_bass_kernel_spmd(nc, in_maps, core_ids=list(range(N_CORES)))
    out = np.stack([np.asarray(res.results[c]['y'], dtype=np.float32) for c in range(N_CORES)], axis=0)
    return out
```

```python
import numpy as np
from contextlib import ExitStack
import concourse.bass as bass
import concourse.mybir as mybir
from concourse.bass_utils import run_bass_kernel_spmd

F32 = mybir.dt.float32
BF16 = mybir.dt.bfloat16
U32 = mybir.dt.uint32
AF = mybir.ActivationFunctionType
ALU = mybir.AluOpType
AX = mybir.AxisListType

import re as _re
_PSUM_KEY = _re.compile(r"^(bk|pp|gps|aps|ops|tps)\d*$")
EPOCH = 20000
NDMA = 16
EPS = 1e-6


class _Eng:
    def __init__(self, name, strict):
        self.name = name
        self.strict = strict
        self.count = 0
        self.sems = []
        self.known = {}
        self.dsems = []
        self.dcount = []
        self.dnext = 0


class Sched:
    def __init__(self, nc, es):
        self.nc = nc
        self.es = es
        self.E = {
            'pe': _Eng('pe', False),
            'act': _Eng('act', True),
            'dve': _Eng('dve', True),
            'pool': _Eng('pool', True),
            'sp': _Eng('sp', False),
        }
        self.lastw = {}
        self.readers = {}
        self.nsem = 0
        self.prog = {k: [] for k in self.E}

    def _newsem(self, nm):
        self.nsem += 1
        return self.es.enter_context(self.nc.semaphore(f"{nm}_{self.nsem}"))

    def _cur_sem(self, e):
        ep = e.count // EPOCH
        while len(e.sems) <= ep:
            e.sems.append(self._newsem(e.name))
        return e.sems[ep]

    def _wait(self, e, dep):
        sem, val, src = dep
        if src is e and not e.strict:
            return
        k = id(sem)
        if e.known.get(k, 0) >= val:
            return
        self.prog[e.name].append(('w', sem, val))
        e.known[k] = val

    def _deps(self, reads, writes):
        deps = []
        for b in reads:
            if b in self.lastw:
                deps.append(self.lastw[b])
        for b in writes:
            if b in self.lastw:
                deps.append(self.lastw[b])
            deps.extend(self.readers.get(b, []))
        return deps

    def _commit(self, dep, reads, writes):
        for b in reads:
            self.readers.setdefault(b, []).append(dep)
        for b in writes:
            self.lastw[b] = dep
            self.readers[b] = []

    def op(self, eng, meth, reads, writes, *a, **kw):
        fn = lambda h: getattr(h, meth)(*a, **kw)
        e = self.E[eng]
        px = [b for b in reads if _PSUM_KEY.match(b)]
        if px:
            reads = [b for b in reads if b not in px]
            writes = list(writes) + px
        for d in self._deps(reads, writes):
            self._wait(e, d)
        sem = self._cur_sem(e)
        val = e.count % EPOCH + 1
        self.prog[e.name].append(('i', fn, sem, 1))
        e.count += 1
        self._commit((sem, val, e), reads, writes)

    def dma(self, eng, reads, writes, meth='dma_start', **kw):
        fn = lambda h: getattr(h, meth)(**kw)
        e = self.E[eng]
        if not e.dsems:
            e.dsems = [self._newsem(e.name + "d") for _ in range(NDMA)]
            e.dcount = [0] * NDMA
        s = e.dnext
        e.dnext = (e.dnext + 1) % NDMA
        sem = e.dsems[s]
        if e.dcount[s] > 0:
            self._wait(e, (sem, 16 * e.dcount[s], None))
        for d in self._deps(reads, writes):
            self._wait(e, d)
        e.dcount[s] += 1
        self.prog[e.name].append(('i', fn, sem, 16))
        self._commit((sem, 16 * e.dcount[s], None), reads, writes)

    def end_phase(self):
        e = self.E['sp']
        for o in self.E.values():
            if o.count > 0:
                sem = o.sems[(o.count - 1) // EPOCH]
                self._wait(e, (sem, (o.count - 1) % EPOCH + 1, None))
            for s, c in enumerate(o.dcount):
                if c > 0:
                    self._wait(e, (o.dsems[s], 16 * c, None))
        with self.nc.Block() as blk:
            def replay(name):
                def f(h):
                    for it in self.prog[name]:
                        if it[0] == 'w':
                            h.wait_ge(it[1], it[2])
                        else:
                            it[1](h).then_inc(it[2], it[3])
                return f
            blk.sync(replay('sp'))
            blk.scalar(replay('act'))
            blk.vector(replay('dve'))
            blk.gpsimd(replay('pool'))
            blk.tensor(replay('pe'))
        self.prog = {k: [] for k in self.E}
        self.lastw = {}
        self.readers = {}


class Ctx:
    n = 0

    def __init__(self, nc, es):
        self.nc = nc
        self.es = es

    def sb(self, nm, shp, dt):
        Ctx.n += 1
        return self.es.enter_context(self.nc.sbuf_tensor(f"{nm}_{Ctx.n}", list(shp), dt))

    def ps(self, nm, shp, dt):
        Ctx.n += 1
        return self.es.enter_context(self.nc.psum_tensor(f"{nm}_{Ctx.n}", list(shp), dt))


def rmsnorm_tile(S, x_ap, x_key, g_tile, g_key, out_ap, out_key, tmp, ssq, rstd, pfx):
    S.op('act', 'activation', [x_key], [pfx + 'tmp', pfx + 'ssq'], out=tmp[:], in_=x_ap, func=AF.Square, accum_out=ssq[:])
    S.op('act', 'activation', [pfx + 'ssq'], [pfx + 'rstd'], out=rstd[:], in_=ssq[:], func=AF.Sqrt, scale=1.0 / 1024, bias=EPS)
    S.op('dve', 'reciprocal', [pfx + 'rstd'], [pfx + 'rstd'], out=rstd[:], in_=rstd[:])
    S.op('dve', 'scalar_tensor_tensor', [x_key, pfx + 'rstd', g_key], [out_key], out=out_ap, in0=x_ap, scalar=rstd[:, 0:1],
         in1=g_tile[:], op0=ALU.mult, op1=ALU.mult)


def peer_phase(nc, S, D, NT, n_iblk=128, final=False):
    with ExitStack() as es:
        C = Ctx(nc, es)
        sb, ps = C.sb, C.ps
        identf = sb("identf", [128, 128], F32)
        identb = sb("identb", [128, 128], BF16)
        iotaf = sb("iotaf", [128, 128], F32)
        iotab = sb("iotab", [128, 128], BF16)
        c16 = sb("c16", [128, 16], F32)
        skT = sb("skT", [128, 2, 128], F32)
        gB = sb("gB", [128, 1024], F32)
        gFin = sb("gFin", [128, 1024], F32) if final else None
        Gbuf = sb("Gbuf", [128, 256, 128], BF16)
        xt = [sb(f"xt{r}", [128, 2, 1024], F32) for r in range(2)]
        hnT = [sb(f"hnT{r}", [128, 8, 256], BF16) for r in range(2)]
        Wqc = [sb(f"Wqc{r}", [128, 8, 128], BF16) for r in range(2)]
        tmp = sb("tmp", [128, 1024], BF16)
        ssq = sb("ssq", [128, 1], F32)
        rstd = sb("rstd", [128, 1], F32)
        hn = sb("hn", [128, 1024], BF16)
        qc = [sb(f"qc{r}", [128, 256], F32) for r in range(2)]
        sall = sb("sall", [128, 2, 16, 128], F32)
        s2 = sb("s2", [128, 256], F32)
        Vt = sb("Vt", [128, 8, 2, 16], F32)
        It = sb("It", [128, 8, 2, 16], U32)
        cand = sb("cand", [128, 8, 256], F32)
        TV = sb("TV", [128, 8, 16], F32)
        TP = sb("TP", [128, 8, 16], U32)
        TPf = sb("TPf", [128, 8, 16], F32)
        TVs = sb("TVs", [128, 8, 16], F32)
        Ex = sb("Ex", [128, 8, 16], F32)
        Z = sb("Z", [128, 8], F32)
        rZ = sb("rZ", [128, 8], F32)
        I1f = sb("I1f", [128, 8, 16], F32)
        I2f = sb("I2f", [128, 8, 16], F32)
        OH = sb("OH", [128, 8, 16, 16], F32)
        OH2 = sb("OH2", [128, 8, 16, 16], BF16)
        af = sb("af", [128, 8, 16], F32)
        bf = sb("bf", [128, 8, 16], F32)
        iF = sb("iF", [128, 128], F32)
        jF = sb("jF", [128, 128], F32)
        gFt = sb("gFt", [128, 128], F32)
        iT = sb("iT", [128, 256], F32)
        jT = sb("jT", [128, 256], F32)
        gT = sb("gT", [128, 256], F32)
        NR = 8
        OI = [sb(f"OI{r}", [128, 128], BF16) for r in range(NR)]
        OJ = [sb(f"OJ{r}", [128, 128], BF16) for r in range(NR)]
        NSLOT = 5
        UTb = [sb(f"UTb{r}", [128, 8, 128], BF16) for r in range(NSLOT)]
        Vb = [sb(f"Vb{r}", [128, 1024], BF16) for r in range(NSLOT)]
        hf = [sb(f"hf{r}", [128, 256], F32) for r in range(2)]
        hG = [sb(f"hG{r}", [128, 256], BF16) for r in range(2)]
        yo = sb("yo", [128, 2, 1024], F32)
        ops = [ps(f"ops{r}", [128, 512], F32) for r in range(4)]
        aps = [ps(f"aps{r}", [128, 512], F32) for r in range(2)]
        gps = [ps(f"gps{r}", [128, 4, 128], F32) for r in range(2)]
        tps = gps[1][:].rearrange("p a b -> p (a b)").bitcast(BF16).rearrange("p (c t) -> p c t", c=8)

        S.dma('sp', [], ['identf'], out=identf[:], in_=D['ident'])
        S.dma('sp', [], ['iotaf'], out=iotaf[:], in_=D['iota'])
        S.dma('sp', [], ['c16'], out=c16[:], in_=D['cst16'])
        S.dma('sp', [], ['skT'], out=skT[:], in_=D['skT'])
        S.dma('sp', [], ['gB'], out=gB[:], in_=D['g'].partition_broadcast(128))
        if final:
            S.dma('sp', [], ['gFin'], out=gFin[:], in_=D['gfin'].partition_broadcast(128))
        S.op('dve', 'tensor_copy', ['identf'], ['identb'], out=identb[:], in_=identf[:])
        S.op('dve', 'tensor_copy', ['iotaf'], ['iotab'], out=iotab[:], in_=iotaf[:])

        ntile = NT // 256
        x_in = D['x_in'].rearrange("(t g p) d -> t p g d", g=2, p=128)
        x_out = D['x_out'].rearrange("(t g p) d -> t p g d", g=2, p=128)
        iota16_b = iotaf[:, 0:16].unsqueeze(1).unsqueeze(1).to_broadcast([128, 8, 16, 16])
        c16_b = c16[:].unsqueeze(1).unsqueeze(1).to_broadcast([128, 8, 16, 16])
        B4 = [128, 8, 16, 16]

        if not D.get('preconverted', False):
            uT2 = D['uT'].rearrange("i p c e -> i p (c e)")
            for i0 in range(128):
                S.dma('pool', [], ['cvU%d' % i0], out=D['uTb'][i0], in_=uT2[i0])
                S.dma('pool', [], ['cvV%d' % i0], out=D['vb'][i0], in_=D['v'][i0 * 128:(i0 + 1) * 128, :])

        def wdma(i):
            sl = i % NSLOT
            S.dma('sp', ['cvU%d' % i], ['UTb%d' % sl], out=UTb[sl][:].rearrange("p c e -> p (c e)"), in_=D['uTb'][i])
            S.dma('sp', ['cvV%d' % i], ['Vb%d' % sl], out=Vb[sl][:], in_=D['vb'][i])

        def stage1(i, par):
            sl = i % NSLOT
            hv = i % 2
            for dc in range(8):
                S.op('pe', 'matmul', ['UTb%d' % sl, 'hnT%d' % par], ['aps%d' % hv], aps[hv][:, 0:256], lhsT=UTb[sl][:, dc, :],
                     rhs=hnT[par][:, dc, :], start=(dc == 0), stop=(dc == 7))

        def prep(ti, par):
            xk, hk = 'xt%d' % par, 'hnT%d' % par
            S.dma('sp', [], [xk], out=xt[par][:], in_=x_in[ti])
            yield
            for g in range(2):
                rmsnorm_tile(S, xt[par][:, g, :], xk, gB, 'gB', hn[:], 'hn', tmp, ssq, rstd, 'p')
                yield
                for c in range(8):
                    S.op('pe', 'transpose', ['hn', 'identb'], ['gps1'], out=tps[:, c, :], in_=hn[:, c * 128:(c + 1) * 128], identity=identb[:])
                S.op('act', 'copy', ['gps1'], [hk], out=hnT[par][:, :, g * 128:(g + 1) * 128], in_=tps)
                yield
            S.dma('pool', [], ['Wqc0'], out=Wqc[0][:], in_=D['wq'][0])
            for c in range(16):
                gp = gps[c % 2]
                gk = 'gps%d' % (c % 2)
                qk = 'qc%d' % (c % 2)
                wk = 'Wqc%d' % (c % 2)
                if c + 1 < 16:
                    S.dma('pool', [], ['Wqc%d' % ((c + 1) % 2)], out=Wqc[(c + 1) % 2][:], in_=D['wq'][c + 1])
                qv = gp[:, 0:2, :].rearrange("p a b -> p (a b)")
                for dc in range(8):
                    S.op('pe', 'matmul', [hk, wk], [gk], qv, lhsT=Wqc[c % 2][:, dc, :], rhs=hnT[par][:, dc, :],
                         start=(dc == 0), stop=(dc == 7))
                S.op('act', 'copy', [gk], [qk], out=qc[c % 2][:], in_=qv)
                yield
                for g in range(2):
                    S.op('pe', 'matmul', [qk, 'skT'], [gk], gp[:, 2 + g, :], lhsT=qc[c % 2][:, g * 128:(g + 1) * 128], rhs=skT[:, c % 2, :],
                         start=True, stop=True)
                S.op('act', 'copy', [gk], ['sall'], out=sall[:, :, c, :], in_=gp[:, 2:4, :])
                yield
            for g in range(2):
                for c in range(16):
                    h, p = divmod(c, 2)
                    src = sall[:, g, c, :]
                    S.op('dve', 'max', ['sall'], ['Vt'], out=Vt[:, h, p, 0:8], in_=src)
                    yield
                    S.op('dve', 'max_index', ['sall', 'Vt'], ['It'], out=It[:, h, p, 0:8], in_max=Vt[:, h, p, 0:8], in_values=src)
                    yield
                    S.op('dve', 'match_replace', ['sall', 'Vt'], ['s2'], out=s2[:, 0:128], in_to_replace=Vt[:, h, p, 0:8], in_values=src, imm_value=-1e30)
                    yield
                    S.op('dve', 'max', ['s2'], ['Vt'], out=Vt[:, h, p, 8:16], in_=s2[:, 0:128])
                    yield
                    S.op('dve', 'max_index', ['s2', 'Vt'], ['It'], out=It[:, h, p, 8:16], in_max=Vt[:, h, p, 8:16], in_values=s2[:, 0:128])
                    yield
                S.op('dve', 'tensor_tensor', ['Vt'], ['cand'], out=cand[:].rearrange("p h (a b) -> p h a b", a=16),
                     in0=Vt[:, :, 0, :].unsqueeze(3).to_broadcast(B4), in1=Vt[:, :, 1, :].unsqueeze(2).to_broadcast(B4), op=ALU.add)
                yield
                for h in range(8):
                    src = cand[:, h, :]
                    S.op('dve', 'max', ['cand'], ['TV'], out=TV[:, h, 0:8], in_=src)
                    yield
                    S.op('dve', 'max_index', ['cand', 'TV'], ['TP'], out=TP[:, h, 0:8], in_max=TV[:, h, 0:8], in_values=src)
                    yield
                    S.op('dve', 'match_replace', ['cand', 'TV'], ['s2'], out=s2[:], in_to_replace=TV[:, h, 0:8], in_values=src, imm_value=-1e30)
                    yield
                    S.op('dve', 'max', ['s2'], ['TV'], out=TV[:, h, 8:16], in_=s2[:])
                    yield
                    S.op('dve', 'max_index', ['s2', 'TV'], ['TP'], out=TP[:, h, 8:16], in_max=TV[:, h, 8:16], in_values=s2[:])
                    yield
                S.op('dve', 'tensor_tensor', ['TV'], ['TVs'], out=TVs[:], in0=TV[:], in1=TV[:, :, 0:1].to_broadcast([128, 8, 16]), op=ALU.subtract)
                yield
                S.op('act', 'activation', ['TVs'], ['Ex'], out=Ex[:], in_=TVs[:], func=AF.Exp)
                S.op('dve', 'tensor_copy', ['TP'], ['TPf'], out=TPf[:], in_=TP[:])
                yield
                S.op('dve', 'tensor_copy', ['It'], ['I1f'], out=I1f[:], in_=It[:, :, 0, :])
                yield
                S.op('dve', 'tensor_copy', ['It'], ['I2f'], out=I2f[:], in_=It[:, :, 1, :])
                yield
                S.op('dve', 'tensor_reduce', ['Ex'], ['Z'], out=Z[:], in_=Ex[:], axis=AX.X, op=ALU.add)
                yield
                S.op('dve', 'reciprocal', ['Z'], ['rZ'], out=rZ[:], in_=Z[:])
                yield
                S.op('dve', 'tensor_tensor', ['Ex', 'rZ'], ['gFt'], out=gFt[:].rearrange("p (h r) -> p h r", h=8), in0=Ex[:],
                     in1=rZ[:].unsqueeze(2).to_broadcast([128, 8, 16]), op=ALU.mult)
                yield
                S.op('dve', 'tensor_tensor', ['TPf', 'c16'], ['OH'], out=OH[:], in0=TPf[:].unsqueeze(3).to_broadcast(B4), in1=c16_b, op=ALU.subtract)
                yield
                OHf = OH[:].rearrange("p a b c -> p (a b c)")
                OH2f = OH2[:].rearrange("p a b c -> p (a b c)")
                S.op('dve', 'tensor_scalar', ['OH'], ['OH2'], out=OH2f, in0=OHf, scalar1=-8.0, scalar2=None, op0=ALU.is_gt)
                yield
                S.op('dve', 'scalar_tensor_tensor', ['OH', 'OH2'], ['OH'], out=OHf, in0=OHf, scalar=8.0, in1=OH2f, op0=ALU.is_lt, op1=ALU.mult)
                yield
                S.op('dve', 'tensor_tensor', ['OH', 'I1f'], ['OH2'], out=OH2[:], in0=OH[:], in1=I1f[:].unsqueeze(2).to_broadcast(B4), op=ALU.mult)
                yield
                S.op('dve', 'tensor_reduce', ['OH2'], ['iF'], out=iF[:].rearrange("p (h r) -> p h r", h=8), in_=OH2[:], axis=AX.X, op=ALU.add)
                yield
                S.op('dve', 'tensor_tensor', ['OH', 'iotaf'], ['OH2'], out=OH2[:], in0=OH[:], in1=iota16_b, op=ALU.mult)
                yield
                S.op('dve', 'tensor_reduce', ['OH2'], ['af'], out=af[:], in_=OH2[:], axis=AX.X, op=ALU.add)
                yield
                S.op('dve', 'scalar_tensor_tensor', ['af', 'TPf'], ['bf'], out=bf[:].rearrange("p a b -> p (a b)"),
                     in0=af[:].rearrange("p a b -> p (a b)"), scalar=-16.0, in1=TPf[:].rearrange("p a b -> p (a b)"), op0=ALU.mult, op1=ALU.add)
                yield
                S.op('dve', 'tensor_tensor', ['bf', 'iotaf'], ['OH'], out=OH[:], in0=iota16_b, in1=bf[:].unsqueeze(3).to_broadcast(B4), op=ALU.is_equal)
                yield
                S.op('dve', 'tensor_tensor', ['OH', 'I2f'], ['OH2'], out=OH2[:], in0=OH[:], in1=I2f[:].unsqueeze(2).to_broadcast(B4), op=ALU.mult)
                yield
                S.op('dve', 'tensor_reduce', ['OH2'], ['jF'], out=jF[:].rearrange("p (h r) -> p h r", h=8), in_=OH2[:], axis=AX.X, op=ALU.add)
                yield
                for k3, (src, sk, dst, dk) in enumerate(((iF, 'iF', iT, 'iT'), (jF, 'jF', jT, 'jT'), (gFt, 'gFt', gT, 'gT'))):
                    S.op('pe', 'transpose', [sk, 'identf'], ['gps0'], out=gps[0][:, k3, :], in_=src[:], identity=identf[:])
                for k3, (src, sk, dst, dk) in enumerate(((iF, 'iF', iT, 'iT'), (jF, 'jF', jT, 'jT'), (gFt, 'gFt', gT, 'gT'))):
                    S.op('act', 'copy', ['gps0'], [dk], out=dst[:, g * 128:(g + 1) * 128], in_=gps[0][:, k3, :])
                yield

        def drain(gen, n=None):
            k = 0
            for _ in gen:
                k += 1
                if n is not None and k >= n:
                    return False
            return True

        cur = prep(0, 0)
        n_yield = sum(1 for _ in cur)
        per_it = max(1, -(-n_yield // max(1, n_iblk - 6)))
        for ti in range(ntile):
            par = ti % 2
            xk = 'xt%d' % par
            for i0 in range(min(NSLOT - 1, n_iblk)):
                wdma(i0)
            for t in range(256):
                r = t % NR
                bk = (t // 4) % 2
                S.op('dve', 'tensor_scalar', ['iotab', 'iT'], ['OI%d' % r], out=OI[r][:], in0=iotab[:], scalar1=iT[:, t:t + 1], scalar2=None,
                     op0=ALU.is_equal)
                S.op('dve', 'tensor_scalar', ['iotab', 'jT', 'gT'], ['OJ%d' % r], out=OJ[r][:], in0=iotab[:], scalar1=jT[:, t:t + 1],
                     scalar2=gT[:, t:t + 1], op0=ALU.is_equal, op1=ALU.mult)
                S.op('pe', 'matmul', ['OI%d' % r, 'OJ%d' % r], ['gps%d' % bk], gps[bk][:, t % 4, :], lhsT=OJ[r][:], rhs=OI[r][:], start=True, stop=True)
                if t % 4 == 3:
                    S.op('act', 'copy', ['gps%d' % bk], ['Gbuf'], out=Gbuf[:, t - 3:t + 1, :], in_=gps[bk][:])
            nxt = prep(ti + 1, 1 - par) if ti + 1 < ntile else None
            stage1(0, par)
            for i in range(n_iblk):
                if i + NSLOT - 1 < n_iblk:
                    wdma(i + NSLOT - 1)
                if i + 1 < n_iblk:
                    stage1(i + 1, par)
                sl = i % NSLOT
                hv = i % 2
                S.op('act', 'activation', ['aps%d' % hv], ['hf%d' % hv], out=hf[hv][:], in_=aps[hv][:, 0:256], func=AF.Gelu)
                S.op('dve', 'tensor_tensor', ['hf%d' % hv, 'Gbuf'], ['hG%d' % hv], out=hG[hv][:], in0=hf[hv][:], in1=Gbuf[:, :, i], op=ALU.mult)
                for tg in range(2):
                    for dh in range(2):
                        S.op('pe', 'matmul', ['hG%d' % hv, 'Vb%d' % sl], ['ops%d' % (tg * 2 + dh)], ops[tg * 2 + dh][:],
                             lhsT=hG[hv][:, tg * 128:(tg + 1) * 128], rhs=Vb[sl][:, dh * 512:(dh + 1) * 512], start=(i == 0), stop=(i == n_iblk - 1))
                if nxt is not None and i >= 2:
                    if drain(nxt, per_it):
                        nxt = None
            if nxt is not None:
                drain(nxt)
            for tg in range(2):
                for dh in range(2):
                    S.op('dve', 'tensor_tensor', ['ops%d' % (tg * 2 + dh), xk], ['yo'], out=yo[:, tg, dh * 512:(dh + 1) * 512], in0=ops[tg * 2 + dh][:],
                         in1=xt[par][:, tg, dh * 512:(dh + 1) * 512], op=ALU.add)
            if final:
                for tg in range(2):
                    rmsnorm_tile(S, yo[:, tg, :], 'yo', gFin, 'gFin', xt[par][:, tg, :], xk, tmp, ssq, rstd, 'f')
                S.dma('pool', [xk], [], out=x_out[ti], in_=xt[par][:])
            else:
                S.dma('pool', ['yo'], [], out=x_out[ti], in_=yo[:])
        S.end_phase()


def mixer_a(nc, S, D, own):
    t0, nt = own
    with ExitStack() as es:
        C = Ctx(nc, es)
        sb, ps = C.sb, C.ps
        identf = sb("identf", [128, 128], F32)
        identb = sb("identb", [128, 128], BF16)
        Win = sb("Win", [128, 8, 5632], BF16)
        gB = sb("gB", [128, 1024], F32)
        tmp = sb("tmp", [128, 1024], BF16)
        ssq = sb("ssq", [128, 1], F32)
        rstd = sb("rstd", [128, 1], F32)
        xn2 = [sb(f"xn{r}", [128, 1024], BF16) for r in range(2)]
        xnT2 = [sb(f"xnT{r}", [128, 8, 128], BF16) for r in range(2)]
        t1 = sb("t1", [128, 8, 32], F32)
        t2 = sb("t2", [128, 8, 32], F32)
        NB = 2
        xt = [sb(f"xt{r}", [128, 1024], F32) for r in range(NB)]
        cs = [sb(f"cs{r}", [128, 2, 32], F32) for r in range(NB)]
        Pp = [sb(f"Pp{r}", [128, 256], F32) for r in range(NB)]
        rin = [sb(f"rin{r}", [128, 8, 64], F32) for r in range(NB)]
        rot = [sb(f"rot{r}", [128, 8, 64], F32) for r in range(NB)]
        Rq = [sb(f"Rq{r}", [128, 256], BF16) for r in range(NB)]
        Rk = [sb(f"Rk{r}", [128, 256], BF16) for r in range(NB)]
        Rv = [sb(f"Rv{r}", [128, 512], BF16) for r in range(NB)]
        Rg = [sb(f"Rg{r}", [128, 512], F32) for r in range(NB)]
        Nq = [sb(f"Nq{r}", [128, 256], BF16) for r in range(NB)]
        Nk = [sb(f"Nk{r}", [128, 256], BF16) for r in range(NB)]
        Nv = [sb(f"Nv{r}", [128, 256], BF16) for r in range(NB)]
        Gt = [sb(f"Gt{r}", [128, 3072], F32) for r in range(NB)]
        pp = [ps(f"pp{r}", [128, 512], F32) for r in range(4)]
        tps = ps("tps", [128, 8, 128], BF16)

        S.dma('sp', [], ['identf'], out=identf[:], in_=D['ident'])
        S.dma('sp', [], ['gB'], out=gB[:], in_=D['g'].partition_broadcast(128))
        w_v = D['w_in'].rearrange("(c p) n -> p c n", p=128)
        for k in range(11):
            S.dma('pool', [], ['Win%d' % k], out=Win[:, :, k * 512:(k + 1) * 512], in_=w_v[:, :, k * 512:(k + 1) * 512])
        S.op('dve', 'tensor_copy', ['identf'], ['identb'], out=identb[:], in_=identf[:])
        B3 = [128, 8, 32]
        kc = 0
        import os as _os
        for i in range(int(_os.environ.get('MA_NT', '32'))):
            b = i % NB
            sfx = str(b)
            is_own = t0 <= i < t0 + nt
            rows = slice(i * 128, (i + 1) * 128)
            xn, xnT = xn2[b], xnT2[b]
            xnk, xtk = 'xn' + sfx, 'xnT' + sfx
            S.dma('sp', [], ['xt' + sfx], out=xt[b][:], in_=D['x_full'][rows, :])
            S.dma('sp', [], ['cs' + sfx], out=cs[b][:, 0, :], in_=D['cos'][rows, :])
            S.dma('sp', [], ['cs' + sfx], out=cs[b][:, 1, :], in_=D['sin'][rows, :])
            _sub = int(_os.environ.get('MA_SUB', '9'))
            if _sub <= 2:
                S.end_phase()
                return
            rmsnorm_tile(S, xt[b][:], 'xt' + sfx, gB, 'gB', xn[:], xnk, tmp, ssq, rstd, 'a')
            if _sub <= 3:
                S.end_phase()
                return
            for c in range(8):
                S.op('pe', 'transpose', [xnk, 'identb'], ['tps'], out=tps[:, c, :], in_=xn[:, c * 128:(c + 1) * 128], identity=identb[:])
            S.op('act', 'copy', ['tps'], [xtk], out=xnT[:], in_=tps[:])
            if _sub <= 4:
                S.end_phase()
                return
            chunks = list(range(11)) if is_own else [0, 1, 2, 4]
            _lvl = int(_os.environ.get('MA_LVL', '9'))
            if _lvl == 0:
                chunks = [0]
            for ci in chunks:
                bank = pp[kc % 4]
                bk = 'pp%d' % (kc % 4)
                kc += 1
                for dc in range(8):
                    S.op('pe', 'matmul', [xtk, 'Win%d' % ci], [bk], bank[:], lhsT=xnT[:, dc, :], rhs=Win[:, dc, ci * 512:(ci + 1) * 512],
                         start=(dc == 0), stop=(dc == 7))
                lo, hi = bank[:, 0:256], bank[:, 256:512]
                rq = rin[b][:, 0:4, :].rearrange("p a b -> p (a b)")
                rk = rin[b][:, 4:8, :].rearrange("p a b -> p (a b)")
                if ci == 0:
                    S.op('act', 'copy', [bk], ['Pp' + sfx], out=Pp[b][:], in_=lo)
                    S.op('dve', 'tensor_copy', [bk], ['rin' + sfx], out=rq, in_=hi)
                elif ci == 1:
                    S.op('dve', 'tensor_copy', [bk], ['rin' + sfx], out=rk, in_=lo)
                    S.op('act', 'copy', [bk], ['Rv' + sfx], out=Rv[b][:, 0:256], in_=hi)
                elif ci == 2:
                    S.op('act', 'copy', [bk], ['Rv' + sfx], out=Rv[b][:, 256:512], in_=lo)
                    if is_own:
                        S.op('act', 'activation', [bk], ['Rg' + sfx], out=Rg[b][:, 0:256], in_=hi, func=AF.Silu)
                elif ci == 3:
                    S.op('act', 'activation', [bk], ['Rg' + sfx], out=Rg[b][:, 256:512], in_=lo, func=AF.Silu)
                    S.op('dve', 'tensor_copy', [bk], ['Nq' + sfx], out=Nq[b][:], in_=hi)
                elif ci == 4:
                    S.op('dve', 'tensor_copy', [bk], ['Nk' + sfx], out=Nk[b][:], in_=lo)
                    S.op('act', 'copy', [bk], ['Nv' + sfx], out=Nv[b][:], in_=hi)
                else:
                    S.op('act', 'activation', [bk], ['Gt' + sfx], out=Gt[b][:, (ci - 5) * 512:(ci - 4) * 512], in_=bank[:], func=AF.Sigmoid)
            if _lvl == 0:
                S.dma('pool', ['Pp' + sfx], [], out=D['Pp'][rows, :], in_=Pp[b][:])
                continue
            x1, x2 = rin[b][:, :, 0:32], rin[b][:, :, 32:64]
            cosb = cs[b][:, 0, :].unsqueeze(1).to_broadcast(B3)
            sinb = cs[b][:, 1, :].unsqueeze(1).to_broadcast(B3)
            rk_ = ['rin' + sfx, 'cs' + sfx]
            S.op('dve', 'tensor_tensor', rk_, ['t1'], out=t1[:], in0=x1, in1=cosb, op=ALU.mult)
            S.op('dve', 'tensor_tensor', rk_, ['t2'], out=t2[:], in0=x2, in1=sinb, op=ALU.mult)
            S.op('dve', 'tensor_tensor', ['t1', 't2'], ['rot' + sfx], out=rot[b][:, :, 0:32], in0=t1[:], in1=t2[:], op=ALU.subtract)
            S.op('dve', 'tensor_tensor', rk_, ['t1'], out=t1[:], in0=x1, in1=sinb, op=ALU.mult)
            S.op('dve', 'tensor_tensor', rk_, ['t2'], out=t2[:], in0=x2, in1=cosb, op=ALU.mult)
            S.op('dve', 'tensor_tensor', ['t1', 't2'], ['rot' + sfx], out=rot[b][:, :, 32:64], in0=t1[:], in1=t2[:], op=ALU.add)
            S.op('act', 'copy', ['rot' + sfx], ['Rq' + sfx], out=Rq[b][:], in_=rot[b][:, 0:4, :].rearrange("p a b -> p (a b)"))
            S.op('act', 'mul', ['rot' + sfx], ['Rk' + sfx], Rk[b][:], rot[b][:, 4:8, :].rearrange("p a b -> p (a b)"), 0.125)
            S.dma('pool', ['Pp' + sfx], [], out=D['Pp'][rows, :], in_=Pp[b][:])
            S.dma('pool', ['Rk' + sfx], [], out=D['Rk'][rows, :], in_=Rk[b][:])
            S.dma('pool', ['Rv' + sfx], [], out=D['Rv'][rows, :], in_=Rv[b][:])
            S.dma('pool', ['Nk' + sfx], [], out=D['Nk'][rows, :], in_=Nk[b][:])
            S.dma('pool', ['Nv' + sfx], [], out=D['Nv'][rows, :], in_=Nv[b][:])
            if is_own:
                S.dma('pool', ['Rq' + sfx], [], out=D['Rq'][rows, :], in_=Rq[b][:])
                S.dma('pool', ['Rg' + sfx], [], out=D['Rg'][rows, :], in_=Rg[b][:])
                S.dma('pool', ['Nq' + sfx], [], out=D['Nq'][rows, :], in_=Nq[b][:])
                S.dma('pool', ['Gt' + sfx], [], out=D['Gt'][rows, :], in_=Gt[b][:])
        S.end_phase()


def mixer_b(nc, S, D, own):
    t0, nt = own
    with ExitStack() as es:
        C = Ctx(nc, es)
        sb, ps = C.sb, C.ps
        identf = sb("identf", [128, 128], F32)
        identb = sb("identb", [128, 128], BF16)
        Wbp = sb("Wbp", [64, 4, 1024], BF16)
        Wbr = sb("Wbr", [128, 4, 1024], BF16)
        Wbn = sb("Wbn", [128, 2, 1024], BF16)
        Wout = sb("Wout", [128, 8, 1024], BF16)
        poolw = sb("poolw", [64, 4, 64], F32)
        pscale = sb("pscale", [64, 4], F32)
        amat = sb("amat", [128, 4, 5, 128], F32)
        Et = [sb(f"Et{r}", [128, 5, 4, 128], BF16) for r in range(5)]
        rstage = sb("rstage", [128, 5, 4, 128], F32)
        nmask = sb("nmask", [128, 5, 128], F32)
        dl = sb("dl", [128, 8], F32)
        lg = sb("lg", [128, 8], F32)
        pos = sb("pos", [128, 2], F32)
        zf = sb("zf", [128, 4], F32)
        zb = sb("zb", [128, 4], F32)
        dC = sb("dC", [128, 8], F32)
        dmat = sb("dmat", [128, 4, 128], F32)
        xrow = sb("xrow", [128, 2, 128], F32)
        Dt = sb("Dt", [128, 4, 128], F32)
        E1 = sb("E1", [128, 128], F32)
        E2 = sb("E2", [128, 128], F32)
        XF = sb("XF", [64, 4, 128], F32)
        XB = sb("XB", [64, 4, 128], F32)
        Sst = sb("Sst", [64, 4, 128], F32)
        Sob = [sb(f"Sob{r}", [64, 4, 128], BF16) for r in range(2)]
        kd = sb("kd", [128, 4, 64], BF16)
        NB = 2
        xt = [sb(f"xt{r}", [128, 1024], F32) for r in range(NB)]
        Gt = [sb(f"Gt{r}", [128, 3072], F32) for r in range(NB)]
        Rq = [sb(f"Rq{r}", [128, 256], BF16) for r in range(NB)]
        Rk = [sb(f"Rk{r}", [128, 256], BF16) for r in range(NB)]
        Rv = [sb(f"Rv{r}", [128, 512], BF16) for r in range(NB)]
        Rg = [sb(f"Rg{r}", [128, 512], F32) for r in range(NB)]
        SFt = [sb(f"SFt{r}", [64, 4, 128], BF16) for r in range(NB)]
        SBt = [sb(f"SBt{r}", [64, 4, 128], BF16) for r in range(NB)]
        Nq = [sb(f"Nq{r}", [128, 256], BF16) for r in range(NB)]
        Nk = [[sb(f"Nk{r}_{o}", [128, 256], BF16) for o in range(5)] for r in range(NB)]
        Nv1 = [[sb(f"Nv{r}_{o}", [128, 4, 65], BF16) for o in range(5)] for r in range(NB)]
        Pp = [[sb(f"Pp{r}_{o}", [128, 256], F32) for o in range(3)] for r in range(NB)]
        qkT = sb("qkT", [64, 8, 128], BF16)
        qfT = sb("qfT", [64, 4, 128], BF16)
        qbT = sb("qbT", [64, 4, 128], BF16)
        PD = sb("PD", [128, 4, 128], BF16)
        ysq = sb("ysq", [128, 4, 128], F32)
        ssq4 = sb("ssq4", [128, 4], F32)
        rs4 = sb("rs4", [128, 4], F32)
        yr = sb("yr", [128, 4, 128], F32)
        yrb = sb("yrb", [128, 512], BF16)
        yrT = sb("yrT", [128, 4, 128], BF16)
        NTa = sb("NTa", [64, 24, 128], BF16)
        Pm = sb("Pm", [128, 4, 128], BF16)
        PEm = sb("PEm", [128, 4, 128], BF16)
        rden = sb("rden", [128, 4], F32)
        ynb = sb("ynb", [128, 4, 64], BF16)
        ynT = sb("ynT", [128, 2, 128], BF16)
        dT = sb("dT", [64, 4, 128], F32)
        ypT = sb("ypT", [64, 4, 128], BF16)
        mg = sb("mg", [128, 1024], F32)
        tg_ = sb("tg", [128, 1024], F32)
        mgb = sb("mgb", [128, 1024], BF16)
        mT = sb("mT", [128, 8, 128], BF16)
        xo = [sb(f"xo{r}", [128, 1024], F32) for r in range(NB)]
        bank = [ps(f"bk{r}", [128, 512], F32) for r in range(8)]
        sc_ps = bank[0][:].rearrange("p (h c) -> p h c", h=4)
        y_ps = bank[1][:].rearrange("p (h c) -> p h c", h=4)
        st_ps = [bank[2][:].rearrange("p (h c) -> p h c", h=4), bank[6][:].rearrange("p (h c) -> p h c", h=4)]
        st_k = ['bk2', 'bk6']
        o_ps = bank[3][:, 0:260].rearrange("p (h c) -> p h c", h=4)
        pl_ps = bank[4][0:64, :].rearrange("p (h c) -> p h c", h=4)
        tr_ps = bank[5][:].bitcast(BF16).rearrange("p (c t) -> p c t", c=8)
        tr7_ps = bank[7][:].bitcast(BF16).rearrange("p (c t) -> p c t", c=8)

        S.dma('sp', [], ['identf'], out=identf[:], in_=D['ident'])
        S.op('dve', 'tensor_copy', ['identf'], ['identb'], out=identb[:], in_=identf[:])
        for g in range(4):
            S.dma('pool', [], ['Wbp'], out=Wbp[:, g, :], in_=D['wbp'][:, g, :])
        S.dma('pool', [], ['Wbr'], out=Wbr[:], in_=D['wbr'].rearrange("(h e) n -> e h n", e=128))
        S.dma('pool', [], ['Wbn'], out=Wbn[:], in_=D['wbn'].rearrange("(h e) n -> e h n", e=128))
        S.dma('pool', [], ['Wout'], out=Wout[:], in_=D['wout'].rearrange("(h e) n -> e h n", e=128))
        S.dma('sp', [], ['poolw'], out=poolw[:], in_=D['poolw'])
        S.dma('sp', [], ['pscale'], out=pscale[:], in_=D['pscale'])
        S.dma('sp', [], ['amat'], out=amat[:], in_=D['amat'])
        S.dma('sp', [], ['dl'], out=dl[:], in_=D['decay'].partition_broadcast(128))
        S.dma('sp', [], ['pos'], out=pos[:], in_=D['pos'])
        S.dma('sp', [], ['dmat'], out=dmat[:], in_=D['dmat'].rearrange("k p c -> p k c"))
        S.dma('sp', [], ['xrow'], out=xrow[:], in_=D['xrow'])
        for r in range(NB):
            for o in range(5):
                S.op('dve', 'memset', [], ['Nv%d_%d' % (r, o)], Nv1[r][o][:], 1.0)
        for ty in range(5):
            S.dma('sp', [], ['rstage'], out=rstage[:], in_=D['rpbT'][ty])
            S.dma('sp', [], ['nmask'], out=nmask[:], in_=D['nmask'][ty])
            S.op('act', 'activation', ['rstage'], ['rstage'], out=rstage[:], in_=rstage[:], func=AF.Exp)
            S.op('dve', 'tensor_tensor', ['rstage', 'nmask'], ['Et%d' % ty], out=Et[ty][:], in0=rstage[:],
                 in1=nmask[:].unsqueeze(2).to_broadcast([128, 5, 4, 128]), op=ALU.mult)
        S.op('act', 'activation', ['dl'], ['lg'], out=lg[:], in_=dl[:], func=AF.Exp, scale=-1.0)
        S.op('act', 'activation', ['lg'], ['lg'], out=lg[:], in_=lg[:], func=AF.Ln, bias=1.0)
        S.op('act', 'mul', ['lg'], ['lg'], lg[:], lg[:], -1.0)
        S.op('act', 'activation', ['lg', 'pos'], ['zf'], out=zf[:], in_=lg[:, 0:4], func=AF.Exp, scale=pos[:, 0:1])
        S.op('act', 'activation', ['lg', 'pos'], ['zb'], out=zb[:], in_=lg[:, 4:8], func=AF.Exp, scale=pos[:, 1:2])
        S.op('act', 'activation', ['lg'], ['dC'], out=dC[:], in_=lg[:], func=AF.Exp, scale=128.0)
        for h in range(4):
            S.op('act', 'activation', ['lg', 'dmat'], ['E1'], out=E1[:], in_=dmat[:, 0, :], func=AF.Exp, scale=lg[:, h:h + 1])
            S.op('act', 'activation', ['lg', 'dmat'], ['E2'], out=E2[:], in_=dmat[:, 2, :], func=AF.Exp, scale=lg[:, 4 + h:5 + h])
            S.op('dve', 'tensor_tensor', ['E1', 'dmat'], ['E1'], out=E1[:], in0=E1[:], in1=dmat[:, 1, :], op=ALU.mult)
            S.op('dve', 'tensor_tensor', ['E2', 'dmat'], ['E2'], out=E2[:], in0=E2[:], in1=dmat[:, 3, :], op=ALU.mult)
            S.op('dve', 'tensor_tensor', ['E1', 'E2'], ['Dt'], out=Dt[:, h, :], in0=E1[:], in1=E2[:], op=ALU.add)
            S.op('act', 'activation', ['lg', 'xrow'], ['XF'], out=XF[:, h, :], in_=xrow[0:64, 0, :], func=AF.Exp, scale=lg[0:64, h:h + 1])
            S.op('act', 'activation', ['lg', 'xrow'], ['XB'], out=XB[:, h, :], in_=xrow[0:64, 1, :], func=AF.Exp, scale=lg[0:64, 4 + h:5 + h])

        S.end_phase()
        rsb = rstage[:].rearrange("p a b c -> p (a b c)").bitcast(BF16)
        rsf = rstage[:].rearrange("p a b c -> p (a b c)")

        class _V:
            def __init__(self, ap):
                self.ap = ap

            def __getitem__(self, k):
                return self.ap if (k == slice(None)) else self.ap[k]

        Rk2 = [_V(rsb[:, 0:256]), _V(rsb[:, 256:512])]
        Rv2 = [_V(rsb[:, 512:1024]), _V(rsb[:, 1024:1536])]
        kd2 = _V(rsb[:, 1536:1792].rearrange("p (h d) -> p h d", h=4))
        Sob2 = [_V(rsb[0:64, 1792:2304].rearrange("p (h c) -> p h c", h=4)), _V(rsb[0:64, 2304:2816].rearrange("p (h c) -> p h c", h=4))]
        Sst2 = _V(rsf[0:64, 1408:1920].rearrange("p (h c) -> p h c", h=4))

        def sweep(order, zt, zk, dcol, dst, dkey, store_pred, upd_pred, St, Sk, kdt, kdk, Sobt, Sobk, Rkt, Rkk, Rvt, Rvk, bki):
            S.op('dve', 'memset', [], [Sk], St[:], 0.0)
            yield
            for cnt, n in enumerate(order):
                b = cnt % NB
                sfx = str(b)
                rows = slice(n * 128, (n + 1) * 128)
                if store_pred(n):
                    S.op('act', 'copy', [Sk], [Sobk + sfx], out=Sobt[b][:], in_=St[:])
                    S.dma('pool', [Sobk + sfx], [dkey + str(n)], out=dst[n], in_=Sobt[b][:])
                if not upd_pred(n):
                    continue
                S.dma('sp', [], [Rkk + sfx], out=Rkt[b][:], in_=D['Rk'][rows, :])
                S.dma('sp', [], [Rvk + sfx], out=Rvt[b][:], in_=D['Rv'][rows, :])
                yield
                S.op('dve', 'tensor_tensor', [Rkk + sfx, zk], [kdk], out=kdt[:], in0=Rkt[b][:].rearrange("p (h d) -> p h d", h=4),
                     in1=zt[:].unsqueeze(2).to_broadcast([128, 4, 64]), op=ALU.mult)
                yield
                for h in range(4):
                    S.op('pe', 'matmul', [kdk, Rvk + sfx], ['bk%d' % bki], bank[bki][0:64, h * 128:(h + 1) * 128], lhsT=kdt[:, h, :],
                         rhs=Rvt[b][:, h * 128:(h + 1) * 128], start=True, stop=True)
                S.op('dve', 'tensor_tensor', [Sk, 'dC'], [Sk], out=St[:], in0=St[:],
                     in1=dC[0:64, dcol:dcol + 4].unsqueeze(2).to_broadcast([64, 4, 128]), op=ALU.mult)
                yield
                S.op('dve', 'tensor_tensor', [Sk, 'bk%d' % bki], [Sk], out=St[:], in0=St[:],
                     in1=bank[bki][0:64, :].rearrange("p (h c) -> p h c", h=4), op=ALU.add)
                yield

        last = t0 + nt - 1
        sw = [sweep(list(range(0, last + 1)), zf, 'zf', 0, D['SF'], 'dSF', lambda n: n >= t0, lambda n: n < last,
                    Sst, 'Sst', kd, 'kd', Sob, 'Sob', Rk, 'Rk', Rv, 'Rv', 0),
              sweep(list(range(31, t0 - 1, -1)), zb, 'zb', 4, D['SB'], 'dSB', lambda n: n <= last, lambda n: n > t0,
                    Sst2, 'Sst2', kd2, 'kd2', Sob2, 'Sobb', Rk2, 'Rkb', Rv2, 'Rvb', 1)]
        while sw:
            for g_ in list(sw):
                try:
                    next(g_)
                except StopIteration:
                    sw.remove(g_)

        B4 = [128, 4, 128]
        for it in range(nt):
            i = t0 + it
            b = it % NB
            sfx = str(b)
            rows = slice(i * 128, (i + 1) * 128)
            ty = {0: 1, 1: 2, 30: 3, 31: 4}.get(i, 0)
            offs = [o for o in range(NA_OMIN[ty], NA_OMIN[ty] + 5) if 0 <= i + o <= 31]
            poffs = [o for o in range(-1, 2) if 0 <= i + o <= 31]
            S.dma('sp', [], ['xt' + sfx], out=xt[b][:], in_=D['x_full'][rows, :])
            S.dma('sp', [], ['Gt' + sfx], out=Gt[b][:], in_=D['Gt'][rows, :])
            S.dma('sp', [], ['Rq' + sfx], out=Rq[b][:], in_=D['Rq'][rows, :])
            S.dma('sp', [], ['Rk' + sfx], out=Rk[b][:], in_=D['Rk'][rows, :])
            S.dma('sp', [], ['Rv' + sfx], out=Rv[b][:], in_=D['Rv'][rows, :])
            S.dma('sp', [], ['Rg' + sfx], out=Rg[b][:], in_=D['Rg'][rows, :])
            S.dma('sp', ['dSF%d' % i], ['SFt' + sfx], out=SFt[b][:], in_=D['SF'][i])
            S.dma('sp', ['dSB%d' % i], ['SBt' + sfx], out=SBt[b][:], in_=D['SB'][i])
            S.dma('sp', [], ['Nq' + sfx], out=Nq[b][:], in_=D['Nq'][rows, :])
            for oi, o in enumerate(offs):
                r2 = slice((i + o) * 128, (i + o + 1) * 128)
                S.dma('sp', [], ['Nk%s_%d' % (sfx, oi)], out=Nk[b][oi][:], in_=D['Nk'][r2, :])
                S.dma('sp', [], ['Nv%s_%d' % (sfx, oi)], out=Nv1[b][oi][:, :, 0:64], in_=D['Nv'][r2, :].rearrange("p (h d) -> p h d", h=4))
            for oi, o in enumerate(poffs):
                r2 = slice((i + o) * 128, (i + o + 1) * 128)
                S.dma('sp', [], ['Pp%s_%d' % (sfx, oi)], out=Pp[b][oi][:], in_=D['Pp'][r2, :])

            if 'uTb' in D:
                cpt = -(-128 // nt)
                uT2 = D['uT'].rearrange("i p c e -> i p (c e)")
                for i0 in range(it * cpt, min(128, (it + 1) * cpt)):
                    S.dma('pool', [], [], out=D['uTb'][i0], in_=uT2[i0])
                    S.dma('pool', [], [], out=D['vb'][i0], in_=D['v'][i0 * 128:(i0 + 1) * 128, :])
            def gen_ret():
                for h in range(4):
                    S.op('pe', 'transpose', ['Rq' + sfx, 'identb'], ['bk5'], out=tr_ps[0:64, h, :], in_=Rq[b][:, h * 64:(h + 1) * 64], identity=identb[:])
                    S.op('pe', 'transpose', ['Rk' + sfx, 'identb'], ['bk5'], out=tr_ps[0:64, 4 + h, :], in_=Rk[b][:, h * 64:(h + 1) * 64], identity=identb[:])
                yield
                S.op('act', 'copy', ['bk5'], ['qkT'], out=qkT[:], in_=tr_ps[0:64, :, :])
                yield
                S.op('dve', 'tensor_tensor', ['qkT', 'XF'], ['qfT'], out=qfT[:], in0=qkT[:, 0:4, :], in1=XF[:], op=ALU.mult)
                S.op('dve', 'tensor_tensor', ['qkT', 'XB'], ['qbT'], out=qbT[:], in0=qkT[:, 0:4, :], in1=XB[:], op=ALU.mult)
                yield
                for h in range(4):
                    S.op('pe', 'matmul', ['qkT'], ['bk0'], sc_ps[:, h, :], lhsT=qkT[:, 4 + h, :], rhs=qkT[:, h, :], start=True, stop=True)
                yield
                S.op('dve', 'tensor_tensor', ['bk0', 'Dt'], ['PD'], out=PD[:], in0=sc_ps, in1=Dt[:], op=ALU.mult)
                yield
                for h in range(4):
                    S.op('pe', 'matmul', ['PD', 'Rv' + sfx], ['bk1'], y_ps[:, h, :], lhsT=PD[:, h, :], rhs=Rv[b][:, h * 128:(h + 1) * 128],
                         start=(h == 0), stop=False, skip_group_check=True)
                    S.op('pe', 'matmul', ['qfT', 'SFt' + sfx], ['bk1'], y_ps[:, h, :], lhsT=qfT[:, h, :], rhs=SFt[b][:, h, :],
                         start=False, stop=False, skip_group_check=True)
                    S.op('pe', 'matmul', ['qbT', 'SBt' + sfx], ['bk1'], y_ps[:, h, :], lhsT=qbT[:, h, :], rhs=SBt[b][:, h, :],
                         start=False, stop=(h == 3), skip_group_check=True)
                yield
                S.op('act', 'activation', ['bk1'], ['ysq'], out=ysq[:], in_=y_ps, func=AF.Square)
                yield
                S.op('dve', 'tensor_reduce', ['ysq'], ['ssq4'], out=ssq4[:], in_=ysq[:], axis=AX.X, op=ALU.add)
                yield
                S.op('act', 'activation', ['ssq4'], ['rs4'], out=rs4[:], in_=ssq4[:], func=AF.Sqrt, scale=1.0 / 128, bias=EPS)
                yield
                S.op('dve', 'reciprocal', ['rs4'], ['rs4'], out=rs4[:], in_=rs4[:])
                S.op('dve', 'tensor_tensor', ['bk1', 'rs4'], ['yr'], out=yr[:], in0=y_ps, in1=rs4[:].unsqueeze(2).to_broadcast(B4), op=ALU.mult)
                S.op('dve', 'tensor_tensor', ['yr', 'Rg' + sfx], ['yrb'], out=yrb[:], in0=yr[:].rearrange("p h c -> p (h c)"), in1=Rg[b][:], op=ALU.mult)
                yield
                for h in range(4):
                    S.op('pe', 'transpose', ['yrb', 'identb'], ['bk5'], out=tr_ps[:, h, :], in_=yrb[:, h * 128:(h + 1) * 128], identity=identb[:])
                yield
                S.op('act', 'copy', ['bk5'], ['yrT'], out=yrT[:], in_=tr_ps[:, 0:4, :])


                yield

            def gen_na():
                srcs = [(Nq[b], 'Nq' + sfx)] + [(Nk[b][oi], 'Nk%s_%d' % (sfx, oi)) for oi in range(len(offs))]
                for r0 in range(0, len(srcs), 2):
                    grp = srcs[r0:r0 + 2]
                    for gi, (src, sk) in enumerate(grp):
                        for h in range(4):
                            S.op('pe', 'transpose', [sk, 'identb'], ['bk7'], out=tr7_ps[0:64, gi * 4 + h, :], in_=src[:, h * 64:(h + 1) * 64], identity=identb[:])
                    n_ = 4 * len(grp)
                    yield
                    S.op('act', 'copy', ['bk7'], ['NTa'], out=NTa[:, r0 * 4:r0 * 4 + n_, :], in_=tr7_ps[0:64, 0:n_, :])
                yield
                for oi, o in enumerate(offs):
                    sp_, sk_ = st_ps[oi % 2], st_k[oi % 2]
                    for h in range(4):
                        S.op('pe', 'matmul', ['NTa'], [sk_], sp_[:, h, :], lhsT=NTa[:, 4 + 4 * oi + h, :], rhs=NTa[:, h, :], start=True, stop=True)
                    yield
                    S.op('act', 'activation', [sk_], ['Pm'], out=Pm[:], in_=sp_, func=AF.Exp, scale=0.125)
                    yield
                    S.op('dve', 'tensor_tensor', ['Pm', 'Et%d' % ty], ['PEm'], out=PEm[:], in0=Pm[:], in1=Et[ty][:, o - NA_OMIN[ty], :, :], op=ALU.mult)
                    yield
                    for h in range(4):
                        S.op('pe', 'matmul', ['PEm', 'Nv%s_%d' % (sfx, oi)], ['bk3'], o_ps[:, h, :], lhsT=PEm[:, h, :], rhs=Nv1[b][oi][:, h, :],
                             start=(oi == 0 and h == 0), stop=(oi == len(offs) - 1 and h == 3), skip_group_check=True)
                yield
                S.op('dve', 'reciprocal', ['bk3'], ['rden'], out=rden[:], in_=o_ps[:, :, 64:65].rearrange("p h c -> p (h c)"))
                S.op('dve', 'tensor_tensor', ['bk3', 'rden'], ['ynb'], out=ynb[:], in0=o_ps[:, :, 0:64], in1=rden[:].unsqueeze(2).to_broadcast([128, 4, 64]), op=ALU.mult)
                ynf = ynb[:].rearrange("p h d -> p (h d)")
                yield
                for c in range(2):
                    S.op('pe', 'transpose', ['ynb', 'identb'], ['bk7'], out=tr7_ps[:, c, :], in_=ynf[:, c * 128:(c + 1) * 128], identity=identb[:])
                yield
                S.op('act', 'copy', ['bk7'], ['ynT'], out=ynT[:], in_=tr7_ps[:, 0:2, :])


                yield

            def gen_pool():
                for g in range(4):
                    for oi, o in enumerate(poffs):
                        kind = {-1: 0, 1: 2}.get(o, 3 if i == 0 else (4 if i == 31 else 1))
                        S.op('pe', 'matmul', ['Pp%s_%d' % (sfx, oi), 'amat'], ['bk4'], pl_ps[:, g, :], lhsT=Pp[b][oi][:, g * 64:(g + 1) * 64],
                             rhs=amat[:, g, kind, :], start=(oi == 0), stop=(oi == len(poffs) - 1))
                yield
                S.op('act', 'copy', ['bk4'], ['dT'], out=dT[:], in_=pl_ps)
                yield
                for g in range(4):
                    S.op('pe', 'matmul', ['dT', 'poolw'], ['bk4'], pl_ps[:, g, :], lhsT=poolw[:, g, :], rhs=dT[:, g, :], start=True, stop=True)
                yield
                S.op('dve', 'tensor_tensor', ['bk4', 'pscale'], ['ypT'], out=ypT[:], in0=pl_ps, in1=pscale[:].unsqueeze(2).to_broadcast([64, 4, 128]), op=ALU.mult)


                yield

            gens = [gen_ret(), gen_na(), gen_pool()]
            while gens:
                for g_ in list(gens):
                    try:
                        next(g_)
                    except StopIteration:
                        gens.remove(g_)
            def branch(nk, lhs_list, w_tile, goff, first, lastb):
                for nh in range(2):
                    bkk = bank[6 + nh]
                    for kk in range(nk):
                        S.op('pe', 'matmul', lhs_list[1] + [w_tile[1]], ['bk%d' % (6 + nh)], bkk[:], lhsT=lhs_list[0][:, kk, :],
                             rhs=w_tile[0][:, kk, nh * 512:(nh + 1) * 512], start=(kk == 0), stop=(kk == nk - 1))
                    cols = slice(nh * 512, (nh + 1) * 512)
                    gsl = Gt[b][:, goff + nh * 512: goff + (nh + 1) * 512]
                    if first:
                        S.op('dve', 'tensor_tensor', ['bk%d' % (6 + nh), 'Gt' + sfx], ['mg'], out=mg[:, cols], in0=bkk[:], in1=gsl, op=ALU.mult)
                    else:
                        S.op('dve', 'tensor_tensor', ['bk%d' % (6 + nh), 'Gt' + sfx], ['tg'], out=tg_[:, cols], in0=bkk[:], in1=gsl, op=ALU.mult)
                        if lastb:
                            S.op('dve', 'tensor_tensor', ['mg', 'tg'], ['mgb'], out=mgb[:, cols], in0=mg[:, cols], in1=tg_[:, cols], op=ALU.add)
                        else:
                            S.op('dve', 'tensor_tensor', ['mg', 'tg'], ['mg'], out=mg[:, cols], in0=mg[:, cols], in1=tg_[:, cols], op=ALU.add)

            branch(4, (ypT, ['ypT']), (Wbp, 'Wbp'), 0, True, False)
            branch(4, (yrT, ['yrT']), (Wbr, 'Wbr'), 1024, False, False)
            branch(2, (ynT, ['ynT']), (Wbn, 'Wbn'), 2048, False, True)
            for c in range(8):
                S.op('pe', 'transpose', ['mgb', 'identb'], ['bk5'], out=tr_ps[:, c, :], in_=mgb[:, c * 128:(c + 1) * 128], identity=identb[:])
            S.op('act', 'copy', ['bk5'], ['mT'], out=mT[:], in_=tr_ps)
            for nh in range(2):
                bkk = bank[6 + nh]
                for dc in range(8):
                    S.op('pe', 'matmul', ['mT', 'Wout'], ['bk%d' % (6 + nh)], bkk[:], lhsT=mT[:, dc, :], rhs=Wout[:, dc, nh * 512:(nh + 1) * 512],
                         start=(dc == 0), stop=(dc == 7))
                cols = slice(nh * 512, (nh + 1) * 512)
                S.op('dve', 'tensor_tensor', ['bk%d' % (6 + nh), 'xt' + sfx], ['xo' + sfx], out=xo[b][:, cols], in0=bkk[:], in1=xt[b][:, cols], op=ALU.add)
            S.dma('pool', ['xo' + sfx], [], out=D['x_out'][it * 128:(it + 1) * 128, :], in_=xo[b][:])
        S.end_phase()


POOL_WINDOWS = (2, 4, 8, 16)
SEQ = 4096
NA_OMIN = (-2, 0, -2, -2, -3)
NA_NOFF = (5, 4, 4, 4, 4)


def _const_tables():
    T = {}
    T['ident'] = np.eye(128, dtype=np.float32)
    T['iota'] = np.tile(np.arange(128, dtype=np.float32)[None, :], (128, 1))
    T['cst16'] = np.tile((16 * np.arange(16, dtype=np.float32) + 7.5)[None, :], (128, 1))
    half = 32
    inv = (1.0 / (np.float32(10000.0) ** np.linspace(0.0, 1.0, half, dtype=np.float32))).astype(np.float32)
    ang = (np.arange(SEQ, dtype=np.float32)[:, None] * inv[None, :]).astype(np.float32)
    T['cos'] = np.cos(ang).astype(np.float32)
    T['sin'] = np.sin(ang).astype(np.float32)
    p = np.arange(128, dtype=np.float32)
    T['pos'] = np.stack([127.0 - p, p], axis=1).astype(np.float32)
    m = p[:, None]
    c = p[None, :]
    T['dmat'] = np.stack([np.maximum(c - m, 0), (c >= m).astype(np.float32), np.maximum(m - c, 0), (m > c).astype(np.float32)]).astype(np.float32)
    xr = np.stack([p + 1.0, 128.0 - p], axis=0)
    T['xrow'] = np.tile(xr[None], (128, 1, 1)).astype(np.float32)
    A = np.zeros((128, 4, 5, 128), np.float32)
    for g, w in enumerate(POOL_WINDOWS):
        lo, hi = w // 2, w - w // 2
        for kind, base in ((1, 1024), (3, 0), (4, SEQ - 128)):
            for t in range(128):
                ta = base + t
                a_, b_ = max(ta - lo, 0), min(ta + hi, SEQ)
                cnt = float(b_ - a_)
                for s in range(a_, b_):
                    rel = s - base
                    if 0 <= rel < 128:
                        A[rel, g, kind, t] += 1.0 / cnt
                    elif rel < 0 and kind == 1:
                        A[rel + 128, g, 0, t] += 1.0 / cnt
                    elif rel >= 128 and kind == 1:
                        A[rel - 128, g, 2, t] += 1.0 / cnt
                A[t, g, kind, t] -= 1.0
    T['amat'] = A
    di = np.zeros((5, 128, 5, 128), np.int64)
    dj = np.zeros((5, 128, 5, 128), np.int64)
    mk = np.zeros((5, 128, 5, 128), np.float32)
    q = np.arange(128)
    k = np.arange(128)
    for ty, i in enumerate((5, 0, 1, 30, 31)):
        for oi in range(5):
            o = NA_OMIN[ty] + oi
            j = i + o
            if j < 0 or j > 31:
                continue
            qr = 2 * i + q // 64
            qc = q % 64
            kr = (2 * j + k // 64)[:, None]
            kcol = (k % 64)[:, None]
            rs = np.clip(qr - 4, 0, 56)[None, :]
            vr = (kr >= rs) & (kr < rs + 8)
            cst = np.clip(qc - 8, 0, 48)[None, :]
            vc = (kcol >= cst) & (kcol < cst + 16)
            di[ty, :, oi, :] = np.clip(kr - qr[None, :] + 7, 0, 14)
            dj[ty, :, oi, :] = np.clip(kcol - qc[None, :], -15, 15) + 15
            mk[ty, :, oi, :] = (vr & vc).astype(np.float32)
    T['na_di'], T['na_dj'], T['nmask'] = di, dj, mk
    return T


_TABLES = None


def tables():
    global _TABLES
    if _TABLES is None:
        _TABLES = _const_tables()
    return _TABLES


def layer_layout(l, w_in, pool_w, pool_scale, ret_decay, na_rpb, w_br_pool, w_br_ret, w_br_na, w_out,
                 peer_w_query, peer_sub_keys, peer_u, peer_v):
    T = tables()
    L = {}
    L['w_in'] = np.ascontiguousarray(w_in[l])
    L['wbp'] = np.ascontiguousarray(w_br_pool[l].reshape(4, 64, 1024).transpose(1, 0, 2))
    L['wbr'] = np.ascontiguousarray(w_br_ret[l])
    L['wbn'] = np.ascontiguousarray(w_br_na[l])
    L['wout'] = np.ascontiguousarray(w_out[l])
    L['poolw'] = np.ascontiguousarray(pool_w[l].transpose(1, 0, 2))
    L['pscale'] = np.ascontiguousarray(pool_scale[l].reshape(4, 64).T)
    L['decay'] = np.ascontiguousarray(ret_decay[l].reshape(1, 8))
    rp = na_rpb[l]
    g = rp[:, T['na_di'], T['na_dj']]
    L['rpbT'] = np.ascontiguousarray(g.transpose(1, 2, 3, 0, 4)).astype(np.float32)
    L['wq'] = np.ascontiguousarray(peer_w_query[l].reshape(8, 128, 16, 128).transpose(2, 1, 0, 3))
    L['skT'] = np.ascontiguousarray(peer_sub_keys[l].transpose(2, 0, 1))
    L['uT'] = np.ascontiguousarray(peer_u[l].reshape(128, 128, 8, 128).transpose(0, 3, 2, 1))
    L['v'] = np.ascontiguousarray(peer_v[l])
    return L


N_CORES = 4
NTOK = 4096
DEPTH = 2
_PER_LAYER = ('g_mix', 'w_in', 'wbp', 'wbr', 'wbn', 'wout', 'poolw', 'pscale', 'decay', 'rpbT', 'g_ffn', 'wq', 'skT', 'uT', 'v')
_SHAPES = dict(g_mix=[1, 1024], w_in=[1024, 5632], wbp=[64, 4, 1024], wbr=[512, 1024], wbn=[256, 1024], wout=[1024, 1024],
               poolw=[64, 4, 64], pscale=[64, 4], decay=[1, 8], rpbT=[5, 128, 5, 4, 128], g_ffn=[1, 1024], wq=[16, 128, 8, 128],
               skT=[128, 2, 128], uT=[128, 128, 8, 128], v=[16384, 1024])
_CONST = dict(ident=[128, 128], iota=[128, 128], cst16=[128, 16], cos=[4096, 32], sin=[4096, 32], pos=[128, 2], dmat=[4, 128, 128],
              xrow=[128, 2, 128], amat=[128, 4, 5, 128], nmask=[5, 128, 5, 128])


def build_program(n_iblk=128):
    nc = bass.Bass("TRN2", target_bir_lowering=False)
    dt = lambda nm, shp, ty=F32, kind="ExternalInput": nc.dram_tensor(nm, list(shp), ty, kind=kind).ap()
    x = dt("x", [NTOK, 1024])
    y = dt("y", [NTOK, 1024], F32, "ExternalOutput")
    gfin = dt("gfin", [1, 1024])
    Cn = {k: dt(k, s) for k, s in _CONST.items()}
    W = [{k: dt(f"{k}_{l}", _SHAPES[k]) for k in _PER_LAYER} for l in range(DEPTH)]
    I = "Internal"
    Sc = dict(Pp=dt("s_Pp", [4096, 256], F32, I), Rq=dt("s_Rq", [4096, 256], BF16, I), Rk=dt("s_Rk", [4096, 256], BF16, I),
              Rv=dt("s_Rv", [4096, 512], BF16, I), Rg=dt("s_Rg", [4096, 512], F32, I), Nq=dt("s_Nq", [4096, 256], BF16, I),
              Nk=dt("s_Nk", [4096, 256], BF16, I), Nv=dt("s_Nv", [4096, 256], BF16, I), Gt=dt("s_Gt", [4096, 3072], F32, I),
              SF=dt("s_SF", [32, 64, 4, 128], BF16, I), SB=dt("s_SB", [32, 64, 4, 128], BF16, I))
    x1 = dt("s_x1", [NTOK, 1024], F32, I)
    uTb = dt("s_uTb", [128, 128, 1024], BF16, I)
    vb = dt("s_vb", [128, 128, 1024], BF16, I)
    x2 = dt("s_x2", [NTOK, 1024], F32, I)
    own = (0, NTOK // 128)
    with ExitStack() as es:
        S = Sched(nc, es)
        cur = x
        for l in range(DEPTH):
            D = dict(Cn)
            D.update(Sc)
            D.update(W[l])
            D.update(x_full=cur, g=W[l]['g_mix'], x_out=x1, uTb=uTb, vb=vb)
            mixer_a(nc, S, D, own)
            mixer_b(nc, S, D, own)
            last = (l == DEPTH - 1)
            P = dict(Cn)
            P.update(W[l])
            P.update(x_in=x1, x_out=(y if last else x2), g=W[l]['g_ffn'], gfin=gfin, uTb=uTb, vb=vb, preconverted=True)
            peer_phase(nc, S, P, NTOK, n_iblk=n_iblk, final=last)
            cur = x2
    return nc


_NC_CACHE = {}


def kernel(x, norm_mix, w_in, pool_w, pool_scale, ret_decay, na_rpb, w_br_pool, w_br_ret, w_br_na, w_out, norm_ffn,
           peer_w_query, peer_sub_keys, peer_u, peer_v, norm_final):
    f = lambda a: np.ascontiguousarray(np.asarray(a), dtype=np.float32)
    x = f(x)
    T = tables()
    shared = {k: np.ascontiguousarray(T[k]) for k in _CONST}
    shared['gfin'] = f(norm_final).reshape(1, 1024)
    args = [f(a) for a in (w_in, pool_w, pool_scale, ret_decay, na_rpb, w_br_pool, w_br_ret, w_br_na, w_out,
                           peer_w_query, peer_sub_keys, peer_u, peer_v)]
    nm, nf = f(norm_mix), f(norm_ffn)
    for l in range(DEPTH):
        L = layer_layout(l, *args)
        L['g_mix'] = nm[l].reshape(1, 1024)
        L['g_ffn'] = nf[l].reshape(1, 1024)
        for k in _PER_LAYER:
            shared[f"{k}_{l}"] = L[k]
    if 'nc' not in _NC_CACHE:
        _NC_CACHE['nc'] = build_program()
    nc = _NC_CACHE['nc']
    in_maps = []
    for c in range(N_CORES):
        m = dict(shared)
        m['x'] = np.ascontiguousarray(x[c])
        in_maps.append(m)
    res = run_bass_kernel_spmd(nc, in_maps, core_ids=list(range(N_CORES)))
    out = np.stack([np.asarray(res.results[c]['y'], dtype=np.float32) for c in range(N_CORES)], axis=0)
    return out
```

```python
import numpy as np
from contextlib import ExitStack
import concourse.bass as bass
import concourse.mybir as mybir
from concourse.bass_utils import run_bass_kernel_spmd

F32 = mybir.dt.float32
BF16 = mybir.dt.bfloat16
U32 = mybir.dt.uint32
AF = mybir.ActivationFunctionType
ALU = mybir.AluOpType
AX = mybir.AxisListType

import re as _re
_PSUM_KEY = _re.compile(r"^(bk|pp|gps|aps|ops|tps)\d*$")
EPOCH = 20000
NDMA = 16
EPS = 1e-6


class _Eng:
    def __init__(self, name, strict):
        self.name = name
        self.strict = strict
        self.count = 0
        self.sems = []
        self.known = {}
        self.dsems = []
        self.dcount = []
        self.dnext = 0


class Sched:
    def __init__(self, nc, es):
        self.nc = nc
        self.es = es
        self.E = {
            'pe': _Eng('pe', False),
            'act': _Eng('act', True),
            'dve': _Eng('dve', True),
            'pool': _Eng('pool', True),
            'sp': _Eng('sp', False),
        }
        self.lastw = {}
        self.readers = {}
        self.nsem = 0
        self.prog = {k: [] for k in self.E}

    def _newsem(self, nm):
        self.nsem += 1
        return self.es.enter_context(self.nc.semaphore(f"{nm}_{self.nsem}"))

    def _cur_sem(self, e):
        ep = e.count // EPOCH
        while len(e.sems) <= ep:
            e.sems.append(self._newsem(e.name))
        return e.sems[ep]

    def _wait(self, e, dep):
        sem, val, src = dep
        if src is e and not e.strict:
            return
        k = id(sem)
        if e.known.get(k, 0) >= val:
            return
        self.prog[e.name].append(('w', sem, val))
        e.known[k] = val

    def _deps(self, reads, writes):
        deps = []
        for b in reads:
            if b in self.lastw:
                deps.append(self.lastw[b])
        for b in writes:
            if b in self.lastw:
                deps.append(self.lastw[b])
            deps.extend(self.readers.get(b, []))
        return deps

    def _commit(self, dep, reads, writes):
        for b in reads:
            self.readers.setdefault(b, []).append(dep)
        for b in writes:
            self.lastw[b] = dep
            self.readers[b] = []

    def op(self, eng, meth, reads, writes, *a, **kw):
        fn = lambda h: getattr(h, meth)(*a, **kw)
        e = self.E[eng]
        px = [b for b in reads if _PSUM_KEY.match(b)]
        if px:
            reads = [b for b in reads if b not in px]
            writes = list(writes) + px
        for d in self._deps(reads, writes):
            self._wait(e, d)
        sem = self._cur_sem(e)
        val = e.count % EPOCH + 1
        self.prog[e.name].append(('i', fn, sem, 1))
        e.count += 1
        self._commit((sem, val, e), reads, writes)

    def dma(self, eng, reads, writes, meth='dma_start', **kw):
        fn = lambda h: getattr(h, meth)(**kw)
        e = self.E[eng]
        if not e.dsems:
            e.dsems = [self._newsem(e.name + "d") for _ in range(NDMA)]
            e.dcount = [0] * NDMA
        s = e.dnext
        e.dnext = (e.dnext + 1) % NDMA
        sem = e.dsems[s]
        if e.dcount[s] > 0:
            self._wait(e, (sem, 16 * e.dcount[s], None))
        for d in self._deps(reads, writes):
            self._wait(e, d)
        e.dcount[s] += 1
        self.prog[e.name].append(('i', fn, sem, 16))
        self._commit((sem, 16 * e.dcount[s], None), reads, writes)

    def end_phase(self):
        e = self.E['sp']
        for o in self.E.values():
            if o.count > 0:
                sem = o.sems[(o.count - 1) // EPOCH]
                self._wait(e, (sem, (o.count - 1) % EPOCH + 1, None))
            for s, c in enumerate(o.dcount):
                if c > 0:
                    self._wait(e, (o.dsems[s], 16 * c, None))
        with self.nc.Block() as blk:
            def replay(name):
                def f(h):
                    for it in self.prog[name]:
                        if it[0] == 'w':
                            h.wait_ge(it[1], it[2])
                        else:
                            it[1](h).then_inc(it[2], it[3])
                return f
            blk.sync(replay('sp'))
            blk.scalar(replay('act'))
            blk.vector(replay('dve'))
            blk.gpsimd(replay('pool'))
            blk.tensor(replay('pe'))
        self.prog = {k: [] for k in self.E}
        self.lastw = {}
        self.readers = {}


class Ctx:
    n = 0

    def __init__(self, nc, es):
        self.nc = nc
        self.es = es

    def sb(self, nm, shp, dt):
        Ctx.n += 1
        return self.es.enter_context(self.nc.sbuf_tensor(f"{nm}_{Ctx.n}", list(shp), dt))

    def ps(self, nm, shp, dt):
        Ctx.n += 1
        return self.es.enter_context(self.nc.psum_tensor(f"{nm}_{Ctx.n}", list(shp), dt))


def rmsnorm_tile(S, x_ap, x_key, g_tile, g_key, out_ap, out_key, tmp, ssq, rstd, pfx):
    S.op('act', 'activation', [x_key], [pfx + 'tmp', pfx + 'ssq'], out=tmp[:], in_=x_ap, func=AF.Square, accum_out=ssq[:])
    S.op('act', 'activation', [pfx + 'ssq'], [pfx + 'rstd'], out=rstd[:], in_=ssq[:], func=AF.Sqrt, scale=1.0 / 1024, bias=EPS)
    S.op('dve', 'reciprocal', [pfx + 'rstd'], [pfx + 'rstd'], out=rstd[:], in_=rstd[:])
    S.op('dve', 'scalar_tensor_tensor', [x_key, pfx + 'rstd', g_key], [out_key], out=out_ap, in0=x_ap, scalar=rstd[:, 0:1],
         in1=g_tile[:], op0=ALU.mult, op1=ALU.mult)


def peer_phase(nc, S, D, NT, n_iblk=128, final=False):
    with ExitStack() as es:
        C = Ctx(nc, es)
        sb, ps = C.sb, C.ps
        identf = sb("identf", [128, 128], F32)
        identb = sb("identb", [128, 128], BF16)
        iotaf = sb("iotaf", [128, 128], F32)
        iotab = sb("iotab", [128, 128], BF16)
        c16 = sb("c16", [128, 16], F32)
        skT = sb("skT", [128, 2, 128], F32)
        gB = sb("gB", [128, 1024], F32)
        gFin = sb("gFin", [128, 1024], F32) if final else None
        Gbuf = sb("Gbuf", [128, 256, 128], BF16)
        xt = [sb(f"xt{r}", [128, 2, 1024], F32) for r in range(2)]
        hnT = [sb(f"hnT{r}", [128, 8, 256], BF16) for r in range(2)]
        Wqc = [sb(f"Wqc{r}", [128, 8, 128], BF16) for r in range(2)]
        tmp = sb("tmp", [128, 1024], BF16)
        ssq = sb("ssq", [128, 1], F32)
        rstd = sb("rstd", [128, 1], F32)
        hn = sb("hn", [128, 1024], BF16)
        qc = [sb(f"qc{r}", [128, 256], F32) for r in range(2)]
        sall = sb("sall", [128, 2, 16, 128], F32)
        s2 = sb("s2", [128, 256], F32)
        Vt = sb("Vt", [128, 8, 2, 16], F32)
        It = sb("It", [128, 8, 2, 16], U32)
        cand = sb("cand", [128, 8, 256], F32)
        TV = sb("TV", [128, 8, 16], F32)
        TP = sb("TP", [128, 8, 16], U32)
        TPf = sb("TPf", [128, 8, 16], F32)
        TVs = sb("TVs", [128, 8, 16], F32)
        Ex = sb("Ex", [128, 8, 16], F32)
        Z = sb("Z", [128, 8], F32)
        rZ = sb("rZ", [128, 8], F32)
        I1f = sb("I1f", [128, 8, 16], F32)
        I2f = sb("I2f", [128, 8, 16], F32)
        OH = sb("OH", [128, 8, 16, 16], F32)
        OH2 = sb("OH2", [128, 8, 16, 16], BF16)
        af = sb("af", [128, 8, 16], F32)
        bf = sb("bf", [128, 8, 16], F32)
        iF = sb("iF", [128, 128], F32)
        jF = sb("jF", [128, 128], F32)
        gFt = sb("gFt", [128, 128], F32)
        iT = sb("iT", [128, 256], F32)
        jT = sb("jT", [128, 256], F32)
        gT = sb("gT", [128, 256], F32)
        NR = 8
        OI = [sb(f"OI{r}", [128, 128], BF16) for r in range(NR)]
        OJ = [sb(f"OJ{r}", [128, 128], BF16) for r in range(NR)]
        NSLOT = 5
        UTb = [sb(f"UTb{r}", [128, 8, 128], BF16) for r in range(NSLOT)]
        Vb = [sb(f"Vb{r}", [128, 1024], BF16) for r in range(NSLOT)]
        hf = [sb(f"hf{r}", [128, 256], F32) for r in range(2)]
        hG = [sb(f"hG{r}", [128, 256], BF16) for r in range(2)]
        yo = sb("yo", [128, 2, 1024], F32)
        ops = [ps(f"ops{r}", [128, 512], F32) for r in range(4)]
        aps = [ps(f"aps{r}", [128, 512], F32) for r in range(2)]
        gps = [ps(f"gps{r}", [128, 4, 128], F32) for r in range(2)]
        tps = gps[1][:].rearrange("p a b -> p (a b)").bitcast(BF16).rearrange("p (c t) -> p c t", c=8)

        S.dma('sp', [], ['identf'], out=identf[:], in_=D['ident'])
        S.dma('sp', [], ['iotaf'], out=iotaf[:], in_=D['iota'])
        S.dma('sp', [], ['c16'], out=c16[:], in_=D['cst16'])
        S.dma('sp', [], ['skT'], out=skT[:], in_=D['skT'])
        S.dma('sp', [], ['gB'], out=gB[:], in_=D['g'].partition_broadcast(128))
        if final:
            S.dma('sp', [], ['gFin'], out=gFin[:], in_=D['gfin'].partition_broadcast(128))
        S.op('dve', 'tensor_copy', ['identf'], ['identb'], out=identb[:], in_=identf[:])
        S.op('dve', 'tensor_copy', ['iotaf'], ['iotab'], out=iotab[:], in_=iotaf[:])

        ntile = NT // 256
        x_in = D['x_in'].rearrange("(t g p) d -> t p g d", g=2, p=128)
        x_out = D['x_out'].rearrange("(t g p) d -> t p g d", g=2, p=128)
        iota16_b = iotaf[:, 0:16].unsqueeze(1).unsqueeze(1).to_broadcast([128, 8, 16, 16])
        c16_b = c16[:].unsqueeze(1).unsqueeze(1).to_broadcast([128, 8, 16, 16])
        B4 = [128, 8, 16, 16]

        if not D.get('preconverted', False):
            uT2 = D['uT'].rearrange("i p c e -> i p (c e)")
            for i0 in range(128):
                S.dma('pool', [], ['cvU%d' % i0], out=D['uTb'][i0], in_=uT2[i0])
                S.dma('pool', [], ['cvV%d' % i0], out=D['vb'][i0], in_=D['v'][i0 * 128:(i0 + 1) * 128, :])

        def wdma(i):
            sl = i % NSLOT
            S.dma('sp', ['cvU%d' % i], ['UTb%d' % sl], out=UTb[sl][:].rearrange("p c e -> p (c e)"), in_=D['uTb'][i])
            S.dma('sp', ['cvV%d' % i], ['Vb%d' % sl], out=Vb[sl][:], in_=D['vb'][i])

        def stage1(i, par):
            sl = i % NSLOT
            hv = i % 2
            for dc in range(8):
                S.op('pe', 'matmul', ['UTb%d' % sl, 'hnT%d' % par], ['aps%d' % hv], aps[hv][:, 0:256], lhsT=UTb[sl][:, dc, :],
                     rhs=hnT[par][:, dc, :], start=(dc == 0), stop=(dc == 7))

        def prep(ti, par):
            xk, hk = 'xt%d' % par, 'hnT%d' % par
            S.dma('sp', [], [xk], out=xt[par][:], in_=x_in[ti])
            yield
            for g in range(2):
                rmsnorm_tile(S, xt[par][:, g, :], xk, gB, 'gB', hn[:], 'hn', tmp, ssq, rstd, 'p')
                yield
                for c in range(8):
                    S.op('pe', 'transpose', ['hn', 'identb'], ['gps1'], out=tps[:, c, :], in_=hn[:, c * 128:(c + 1) * 128], identity=identb[:])
                S.op('act', 'copy', ['gps1'], [hk], out=hnT[par][:, :, g * 128:(g + 1) * 128], in_=tps)
                yield
            S.dma('pool', [], ['Wqc0'], out=Wqc[0][:], in_=D['wq'][0])
            for c in range(16):
                gp = gps[c % 2]
                gk = 'gps%d' % (c % 2)
                qk = 'qc%d' % (c % 2)
                wk = 'Wqc%d' % (c % 2)
                if c + 1 < 16:
                    S.dma('pool', [], ['Wqc%d' % ((c + 1) % 2)], out=Wqc[(c + 1) % 2][:], in_=D['wq'][c + 1])
                qv = gp[:, 0:2, :].rearrange("p a b -> p (a b)")
                for dc in range(8):
                    S.op('pe', 'matmul', [hk, wk], [gk], qv, lhsT=Wqc[c % 2][:, dc, :], rhs=hnT[par][:, dc, :],
                         start=(dc == 0), stop=(dc == 7))
                S.op('act', 'copy', [gk], [qk], out=qc[c % 2][:], in_=qv)
                yield
                for g in range(2):
                    S.op('pe', 'matmul', [qk, 'skT'], [gk], gp[:, 2 + g, :], lhsT=qc[c % 2][:, g * 128:(g + 1) * 128], rhs=skT[:, c % 2, :],
                         start=True, stop=True)
                S.op('act', 'copy', [gk], ['sall'], out=sall[:, :, c, :], in_=gp[:, 2:4, :])
                yield
            for g in range(2):
                for c in range(16):
                    h, p = divmod(c, 2)
                    src = sall[:, g, c, :]
                    S.op('dve', 'max', ['sall'], ['Vt'], out=Vt[:, h, p, 0:8], in_=src)
                    yield
                    S.op('dve', 'max_index', ['sall', 'Vt'], ['It'], out=It[:, h, p, 0:8], in_max=Vt[:, h, p, 0:8], in_values=src)
                    yield
                    S.op('dve', 'match_replace', ['sall', 'Vt'], ['s2'], out=s2[:, 0:128], in_to_replace=Vt[:, h, p, 0:8], in_values=src, imm_value=-1e30)
                    yield
                    S.op('dve', 'max', ['s2'], ['Vt'], out=Vt[:, h, p, 8:16], in_=s2[:, 0:128])
                    yield
                    S.op('dve', 'max_index', ['s2', 'Vt'], ['It'], out=It[:, h, p, 8:16], in_max=Vt[:, h, p, 8:16], in_values=s2[:, 0:128])
                    yield
                S.op('dve', 'tensor_tensor', ['Vt'], ['cand'], out=cand[:].rearrange("p h (a b) -> p h a b", a=16),
                     in0=Vt[:, :, 0, :].unsqueeze(3).to_broadcast(B4), in1=Vt[:, :, 1, :].unsqueeze(2).to_broadcast(B4), op=ALU.add)
                yield
                for h in range(8):
                    src = cand[:, h, :]
                    S.op('dve', 'max', ['cand'], ['TV'], out=TV[:, h, 0:8], in_=src)
                    yield
                    S.op('dve', 'max_index', ['cand', 'TV'], ['TP'], out=TP[:, h, 0:8], in_max=TV[:, h, 0:8], in_values=src)
                    yield
                    S.op('dve', 'match_replace', ['cand', 'TV'], ['s2'], out=s2[:], in_to_replace=TV[:, h, 0:8], in_values=src, imm_value=-1e30)
                    yield
                    S.op('dve', 'max', ['s2'], ['TV'], out=TV[:, h, 8:16], in_=s2[:])
                    yield
                    S.op('dve', 'max_index', ['s2', 'TV'], ['TP'], out=TP[:, h, 8:16], in_max=TV[:, h, 8:16], in_values=s2[:])
                    yield
                S.op('dve', 'tensor_tensor', ['TV'], ['TVs'], out=TVs[:], in0=TV[:], in1=TV[:, :, 0:1].to_broadcast([128, 8, 16]), op=ALU.subtract)
                yield
                S.op('act', 'activation', ['TVs'], ['Ex'], out=Ex[:], in_=TVs[:], func=AF.Exp)
                S.op('dve', 'tensor_copy', ['TP'], ['TPf'], out=TPf[:], in_=TP[:])
                yield
                S.op('dve', 'tensor_copy', ['It'], ['I1f'], out=I1f[:], in_=It[:, :, 0, :])
                yield
                S.op('dve', 'tensor_copy', ['It'], ['I2f'], out=I2f[:], in_=It[:, :, 1, :])
                yield
                S.op('dve', 'tensor_reduce', ['Ex'], ['Z'], out=Z[:], in_=Ex[:], axis=AX.X, op=ALU.add)
                yield
                S.op('dve', 'reciprocal', ['Z'], ['rZ'], out=rZ[:], in_=Z[:])
                yield
                S.op('dve', 'tensor_tensor', ['Ex', 'rZ'], ['gFt'], out=gFt[:].rearrange("p (h r) -> p h r", h=8), in0=Ex[:],
                     in1=rZ[:].unsqueeze(2).to_broadcast([128, 8, 16]), op=ALU.mult)
                yield
                S.op('dve', 'tensor_tensor', ['TPf', 'c16'], ['OH'], out=OH[:], in0=TPf[:].unsqueeze(3).to_broadcast(B4), in1=c16_b, op=ALU.subtract)
                yield
                OHf = OH[:].rearrange("p a b c -> p (a b c)")
                OH2f = OH2[:].rearrange("p a b c -> p (a b c)")
                S.op('dve', 'tensor_scalar', ['OH'], ['OH2'], out=OH2f, in0=OHf, scalar1=-8.0, scalar2=None, op0=ALU.is_gt)
                yield
                S.op('dve', 'scalar_tensor_tensor', ['OH', 'OH2'], ['OH'], out=OHf, in0=OHf, scalar=8.0, in1=OH2f, op0=ALU.is_lt, op1=ALU.mult)
                yield
                S.op('dve', 'tensor_tensor', ['OH', 'I1f'], ['OH2'], out=OH2[:], in0=OH[:], in1=I1f[:].unsqueeze(2).to_broadcast(B4), op=ALU.mult)
                yield
                S.op('dve', 'tensor_reduce', ['OH2'], ['iF'], out=iF[:].rearrange("p (h r) -> p h r", h=8), in_=OH2[:], axis=AX.X, op=ALU.add)
                yield
                S.op('dve', 'tensor_tensor', ['OH', 'iotaf'], ['OH2'], out=OH2[:], in0=OH[:], in1=iota16_b, op=ALU.mult)
                yield
                S.op('dve', 'tensor_reduce', ['OH2'], ['af'], out=af[:], in_=OH2[:], axis=AX.X, op=ALU.add)
                yield
                S.op('dve', 'scalar_tensor_tensor', ['af', 'TPf'], ['bf'], out=bf[:].rearrange("p a b -> p (a b)"),
                     in0=af[:].rearrange("p a b -> p (a b)"), scalar=-16.0, in1=TPf[:].rearrange("p a b -> p (a b)"), op0=ALU.mult, op1=ALU.add)
                yield
                S.op('dve', 'tensor_tensor', ['bf', 'iotaf'], ['OH'], out=OH[:], in0=iota16_b, in1=bf[:].unsqueeze(3).to_broadcast(B4), op=ALU.is_equal)
                yield
                S.op('dve', 'tensor_tensor', ['OH', 'I2f'], ['OH2'], out=OH2[:], in0=OH[:], in1=I2f[:].unsqueeze(2).to_broadcast(B4), op=ALU.mult)
                yield
                S.op('dve', 'tensor_reduce', ['OH2'], ['jF'], out=jF[:].rearrange("p (h r) -> p h r", h=8), in_=OH2[:], axis=AX.X, op=ALU.add)
                yield
                for k3, (src, sk, dst, dk) in enumerate(((iF, 'iF', iT, 'iT'), (jF, 'jF', jT, 'jT'), (gFt, 'gFt', gT, 'gT'))):
                    S.op('pe', 'transpose', [sk, 'identf'], ['gps0'], out=gps[0][:, k3, :], in_=src[:], identity=identf[:])
                for k3, (src, sk, dst, dk) in enumerate(((iF, 'iF', iT, 'iT'), (jF, 'jF', jT, 'jT'), (gFt, 'gFt', gT, 'gT'))):
                    S.op('act', 'copy', ['gps0'], [dk], out=dst[:, g * 128:(g + 1) * 128], in_=gps[0][:, k3, :])
                yield

        def drain(gen, n=None):
            k = 0
            for _ in gen:
                k += 1
                if n is not None and k >= n:
                    return False
            return True

        cur = prep(0, 0)
        n_yield = sum(1 for _ in cur)
        rate = n_yield / float(max(1, n_iblk - 10))
        for ti in range(ntile):
            par = ti % 2
            xk = 'xt%d' % par
            for i0 in range(min(NSLOT - 1, n_iblk)):
                wdma(i0)
            for t in range(256):
                r = t % NR
                bk = (t // 4) % 2
                S.op('dve', 'tensor_scalar', ['iotab', 'iT'], ['OI%d' % r], out=OI[r][:], in0=iotab[:], scalar1=iT[:, t:t + 1], scalar2=None,
                     op0=ALU.is_equal)
                S.op('dve', 'tensor_scalar', ['iotab', 'jT', 'gT'], ['OJ%d' % r], out=OJ[r][:], in0=iotab[:], scalar1=jT[:, t:t + 1],
                     scalar2=gT[:, t:t + 1], op0=ALU.is_equal, op1=ALU.mult)
                S.op('pe', 'matmul', ['OI%d' % r, 'OJ%d' % r], ['gps%d' % bk], gps[bk][:, t % 4, :], lhsT=OJ[r][:], rhs=OI[r][:], start=True, stop=True)
                if t % 4 == 3:
                    S.op('act', 'copy', ['gps%d' % bk], ['Gbuf'], out=Gbuf[:, t - 3:t + 1, :], in_=gps[bk][:])
            nxt = prep(ti + 1, 1 - par) if ti + 1 < ntile else None
            credit = 0.0
            stage1(0, par)
            for i in range(n_iblk):
                if i + NSLOT - 1 < n_iblk:
                    wdma(i + NSLOT - 1)
                if i + 1 < n_iblk:
                    stage1(i + 1, par)
                sl = i % NSLOT
                hv = i % 2
                S.op('act', 'activation', ['aps%d' % hv], ['hf%d' % hv], out=hf[hv][:], in_=aps[hv][:, 0:256], func=AF.Gelu)
                S.op('dve', 'tensor_tensor', ['hf%d' % hv, 'Gbuf'], ['hG%d' % hv], out=hG[hv][:], in0=hf[hv][:], in1=Gbuf[:, :, i], op=ALU.mult)
                for tg in range(2):
                    for dh in range(2):
                        S.op('pe', 'matmul', ['hG%d' % hv, 'Vb%d' % sl], ['ops%d' % (tg * 2 + dh)], ops[tg * 2 + dh][:],
                             lhsT=hG[hv][:, tg * 128:(tg + 1) * 128], rhs=Vb[sl][:, dh * 512:(dh + 1) * 512], start=(i == 0), stop=(i == n_iblk - 1))
                if nxt is not None and i >= 1:
                    credit += rate
                    k_ = int(credit)
                    credit -= k_
                    if k_ > 0 and drain(nxt, k_):
                        nxt = None
            if nxt is not None:
                drain(nxt)
            for tg in range(2):
                for dh in range(2):
                    S.op('dve', 'tensor_tensor', ['ops%d' % (tg * 2 + dh), xk], ['yo'], out=yo[:, tg, dh * 512:(dh + 1) * 512], in0=ops[tg * 2 + dh][:],
                         in1=xt[par][:, tg, dh * 512:(dh + 1) * 512], op=ALU.add)
            if final:
                for tg in range(2):
                    rmsnorm_tile(S, yo[:, tg, :], 'yo', gFin, 'gFin', xt[par][:, tg, :], xk, tmp, ssq, rstd, 'f')
                S.dma('pool', [xk], [], out=x_out[ti], in_=xt[par][:])
            else:
                S.dma('pool', ['yo'], [], out=x_out[ti], in_=yo[:])
        S.end_phase()


def mixer_a(nc, S, D, own):
    t0, nt = own
    with ExitStack() as es:
        C = Ctx(nc, es)
        sb, ps = C.sb, C.ps
        identf = sb("identf", [128, 128], F32)
        identb = sb("identb", [128, 128], BF16)
        Win = sb("Win", [128, 8, 5632], BF16)
        gB = sb("gB", [128, 1024], F32)
        tmp = sb("tmp", [128, 1024], BF16)
        ssq = sb("ssq", [128, 1], F32)
        rstd = sb("rstd", [128, 1], F32)
        xn2 = [sb(f"xn{r}", [128, 1024], BF16) for r in range(2)]
        xnT2 = [sb(f"xnT{r}", [128, 8, 128], BF16) for r in range(2)]
        t1 = sb("t1", [128, 8, 32], F32)
        t2 = sb("t2", [128, 8, 32], F32)
        NB = 2
        xt = [sb(f"xt{r}", [128, 1024], F32) for r in range(NB)]
        cs = [sb(f"cs{r}", [128, 2, 32], F32) for r in range(NB)]
        Pp = [sb(f"Pp{r}", [128, 256], F32) for r in range(NB)]
        rin = [sb(f"rin{r}", [128, 8, 64], F32) for r in range(NB)]
        rot = [sb(f"rot{r}", [128, 8, 64], F32) for r in range(NB)]
        Rq = [sb(f"Rq{r}", [128, 256], BF16) for r in range(NB)]
        Rk = [sb(f"Rk{r}", [128, 256], BF16) for r in range(NB)]
        Rv = [sb(f"Rv{r}", [128, 512], BF16) for r in range(NB)]
        Rg = [sb(f"Rg{r}", [128, 512], F32) for r in range(NB)]
        Nq = [sb(f"Nq{r}", [128, 256], BF16) for r in range(NB)]
        Nk = [sb(f"Nk{r}", [128, 256], BF16) for r in range(NB)]
        Nv = [sb(f"Nv{r}", [128, 256], BF16) for r in range(NB)]
        Gt = [sb(f"Gt{r}", [128, 3072], F32) for r in range(NB)]
        pp = [ps(f"pp{r}", [128, 512], F32) for r in range(4)]
        tps = ps("tps", [128, 8, 128], BF16)

        S.dma('sp', [], ['identf'], out=identf[:], in_=D['ident'])
        S.dma('sp', [], ['gB'], out=gB[:], in_=D['g'].partition_broadcast(128))
        w_v = D['w_in'].rearrange("(c p) n -> p c n", p=128)
        for k in range(11):
            S.dma('pool', [], ['Win%d' % k], out=Win[:, :, k * 512:(k + 1) * 512], in_=w_v[:, :, k * 512:(k + 1) * 512])
        S.op('dve', 'tensor_copy', ['identf'], ['identb'], out=identb[:], in_=identf[:])
        B3 = [128, 8, 32]
        kc = 0
        import os as _os
        for i in range(int(_os.environ.get('MA_NT', '32'))):
            b = i % NB
            sfx = str(b)
            is_own = t0 <= i < t0 + nt
            rows = slice(i * 128, (i + 1) * 128)
            xn, xnT = xn2[b], xnT2[b]
            xnk, xtk = 'xn' + sfx, 'xnT' + sfx
            S.dma('sp', [], ['xt' + sfx], out=xt[b][:], in_=D['x_full'][rows, :])
            S.dma('sp', [], ['cs' + sfx], out=cs[b][:, 0, :], in_=D['cos'][rows, :])
            S.dma('sp', [], ['cs' + sfx], out=cs[b][:, 1, :], in_=D['sin'][rows, :])
            _sub = int(_os.environ.get('MA_SUB', '9'))
            if _sub <= 2:
                S.end_phase()
                return
            rmsnorm_tile(S, xt[b][:], 'xt' + sfx, gB, 'gB', xn[:], xnk, tmp, ssq, rstd, 'a')
            if _sub <= 3:
                S.end_phase()
                return
            for c in range(8):
                S.op('pe', 'transpose', [xnk, 'identb'], ['tps'], out=tps[:, c, :], in_=xn[:, c * 128:(c + 1) * 128], identity=identb[:])
            S.op('act', 'copy', ['tps'], [xtk], out=xnT[:], in_=tps[:])
            if _sub <= 4:
                S.end_phase()
                return
            chunks = list(range(11)) if is_own else [0, 1, 2, 4]
            _lvl = int(_os.environ.get('MA_LVL', '9'))
            if _lvl == 0:
                chunks = [0]
            for ci in chunks:
                bank = pp[kc % 4]
                bk = 'pp%d' % (kc % 4)
                kc += 1
                for dc in range(8):
                    S.op('pe', 'matmul', [xtk, 'Win%d' % ci], [bk], bank[:], lhsT=xnT[:, dc, :], rhs=Win[:, dc, ci * 512:(ci + 1) * 512],
                         start=(dc == 0), stop=(dc == 7))
                lo, hi = bank[:, 0:256], bank[:, 256:512]
                rq = rin[b][:, 0:4, :].rearrange("p a b -> p (a b)")
                rk = rin[b][:, 4:8, :].rearrange("p a b -> p (a b)")
                if ci == 0:
                    S.op('act', 'copy', [bk], ['Pp' + sfx], out=Pp[b][:], in_=lo)
                    S.op('dve', 'tensor_copy', [bk], ['rin' + sfx], out=rq, in_=hi)
                elif ci == 1:
                    S.op('dve', 'tensor_copy', [bk], ['rin' + sfx], out=rk, in_=lo)
                    S.op('act', 'copy', [bk], ['Rv' + sfx], out=Rv[b][:, 0:256], in_=hi)
                elif ci == 2:
                    S.op('act', 'copy', [bk], ['Rv' + sfx], out=Rv[b][:, 256:512], in_=lo)
                    if is_own:
                        S.op('act', 'activation', [bk], ['Rg' + sfx], out=Rg[b][:, 0:256], in_=hi, func=AF.Silu)
                elif ci == 3:
                    S.op('act', 'activation', [bk], ['Rg' + sfx], out=Rg[b][:, 256:512], in_=lo, func=AF.Silu)
                    S.op('dve', 'tensor_copy', [bk], ['Nq' + sfx], out=Nq[b][:], in_=hi)
                elif ci == 4:
                    S.op('dve', 'tensor_copy', [bk], ['Nk' + sfx], out=Nk[b][:], in_=lo)
                    S.op('act', 'copy', [bk], ['Nv' + sfx], out=Nv[b][:], in_=hi)
                else:
                    S.op('act', 'activation', [bk], ['Gt' + sfx], out=Gt[b][:, (ci - 5) * 512:(ci - 4) * 512], in_=bank[:], func=AF.Sigmoid)
            if _lvl == 0:
                S.dma('pool', ['Pp' + sfx], [], out=D['Pp'][rows, :], in_=Pp[b][:])
                continue
            x1, x2 = rin[b][:, :, 0:32], rin[b][:, :, 32:64]
            cosb = cs[b][:, 0, :].unsqueeze(1).to_broadcast(B3)
            sinb = cs[b][:, 1, :].unsqueeze(1).to_broadcast(B3)
            rk_ = ['rin' + sfx, 'cs' + sfx]
            S.op('dve', 'tensor_tensor', rk_, ['t1'], out=t1[:], in0=x1, in1=cosb, op=ALU.mult)
            S.op('dve', 'tensor_tensor', rk_, ['t2'], out=t2[:], in0=x2, in1=sinb, op=ALU.mult)
            S.op('dve', 'tensor_tensor', ['t1', 't2'], ['rot' + sfx], out=rot[b][:, :, 0:32], in0=t1[:], in1=t2[:], op=ALU.subtract)
            S.op('dve', 'tensor_tensor', rk_, ['t1'], out=t1[:], in0=x1, in1=sinb, op=ALU.mult)
            S.op('dve', 'tensor_tensor', rk_, ['t2'], out=t2[:], in0=x2, in1=cosb, op=ALU.mult)
            S.op('dve', 'tensor_tensor', ['t1', 't2'], ['rot' + sfx], out=rot[b][:, :, 32:64], in0=t1[:], in1=t2[:], op=ALU.add)
            S.op('act', 'copy', ['rot' + sfx], ['Rq' + sfx], out=Rq[b][:], in_=rot[b][:, 0:4, :].rearrange("p a b -> p (a b)"))
            S.op('act', 'mul', ['rot' + sfx], ['Rk' + sfx], Rk[b][:], rot[b][:, 4:8, :].rearrange("p a b -> p (a b)"), 0.125)
            S.dma('pool', ['Pp' + sfx], [], out=D['Pp'][rows, :], in_=Pp[b][:])
            S.dma('pool', ['Rk' + sfx], [], out=D['Rk'][rows, :], in_=Rk[b][:])
            S.dma('pool', ['Rv' + sfx], [], out=D['Rv'][rows, :], in_=Rv[b][:])
            S.dma('pool', ['Nk' + sfx], [], out=D['Nk'][rows, :], in_=Nk[b][:])
            S.dma('pool', ['Nv' + sfx], [], out=D['Nv'][rows, :], in_=Nv[b][:])
            if is_own:
                S.dma('pool', ['Rq' + sfx], [], out=D['Rq'][rows, :], in_=Rq[b][:])
                S.dma('pool', ['Rg' + sfx], [], out=D['Rg'][rows, :], in_=Rg[b][:])
                S.dma('pool', ['Nq' + sfx], [], out=D['Nq'][rows, :], in_=Nq[b][:])
                S.dma('pool', ['Gt' + sfx], [], out=D['Gt'][rows, :], in_=Gt[b][:])
        S.end_phase()


def mixer_b(nc, S, D, own):
    t0, nt = own
    with ExitStack() as es:
        C = Ctx(nc, es)
        sb, ps = C.sb, C.ps
        identf = sb("identf", [128, 128], F32)
        identb = sb("identb", [128, 128], BF16)
        Wbp = sb("Wbp", [64, 4, 1024], BF16)
        Wbr = sb("Wbr", [128, 4, 1024], BF16)
        Wbn = sb("Wbn", [128, 2, 1024], BF16)
        Wout = sb("Wout", [128, 8, 1024], BF16)
        poolw = sb("poolw", [64, 4, 64], F32)
        pscale = sb("pscale", [64, 4], F32)
        amat = sb("amat", [128, 4, 5, 128], F32)
        Et = [sb(f"Et{r}", [128, 5, 4, 128], BF16) for r in range(5)]
        rstage = sb("rstage", [128, 5, 4, 128], F32)
        nmask = sb("nmask", [128, 5, 128], F32)
        dl = sb("dl", [128, 8], F32)
        lg = sb("lg", [128, 8], F32)
        pos = sb("pos", [128, 2], F32)
        zf = sb("zf", [128, 4], F32)
        zb = sb("zb", [128, 4], F32)
        dC = sb("dC", [128, 8], F32)
        dmat = sb("dmat", [128, 4, 128], F32)
        xrow = sb("xrow", [128, 2, 128], F32)
        Dt = sb("Dt", [128, 4, 128], F32)
        E1 = sb("E1", [128, 128], F32)
        E2 = sb("E2", [128, 128], F32)
        XF = sb("XF", [64, 4, 128], F32)
        XB = sb("XB", [64, 4, 128], F32)
        Sst = sb("Sst", [64, 4, 128], F32)
        Sob = [sb(f"Sob{r}", [64, 4, 128], BF16) for r in range(2)]
        kd = sb("kd", [128, 4, 64], BF16)
        NB = 2
        xt = [sb(f"xt{r}", [128, 1024], F32) for r in range(NB)]
        Gt = [sb(f"Gt{r}", [128, 3072], F32) for r in range(NB)]
        Rq = [sb(f"Rq{r}", [128, 256], BF16) for r in range(NB)]
        Rk = [sb(f"Rk{r}", [128, 256], BF16) for r in range(NB)]
        Rv = [sb(f"Rv{r}", [128, 512], BF16) for r in range(NB)]
        Rg = [sb(f"Rg{r}", [128, 512], F32) for r in range(NB)]
        SFt = [sb(f"SFt{r}", [64, 4, 128], BF16) for r in range(NB)]
        SBt = [sb(f"SBt{r}", [64, 4, 128], BF16) for r in range(NB)]
        Nq = [sb(f"Nq{r}", [128, 256], BF16) for r in range(NB)]
        Nk = [[sb(f"Nk{r}_{o}", [128, 256], BF16) for o in range(5)] for r in range(NB)]
        Nv1 = [[sb(f"Nv{r}_{o}", [128, 4, 65], BF16) for o in range(5)] for r in range(NB)]
        Pp = [[sb(f"Pp{r}_{o}", [128, 256], F32) for o in range(3)] for r in range(NB)]
        qkT = sb("qkT", [64, 8, 128], BF16)
        qfT = sb("qfT", [64, 4, 128], BF16)
        qbT = sb("qbT", [64, 4, 128], BF16)
        PD = sb("PD", [128, 4, 128], BF16)
        ysq = sb("ysq", [128, 4, 128], F32)
        ssq4 = sb("ssq4", [128, 4], F32)
        rs4 = sb("rs4", [128, 4], F32)
        yr = sb("yr", [128, 4, 128], F32)
        yrb = sb("yrb", [128, 512], BF16)
        yrT = sb("yrT", [128, 4, 128], BF16)
        NTa = sb("NTa", [64, 24, 128], BF16)
        Pm = sb("Pm", [128, 4, 128], BF16)
        PEm = sb("PEm", [128, 4, 128], BF16)
        rden = sb("rden", [128, 4], F32)
        ynb = sb("ynb", [128, 4, 64], BF16)
        ynT = sb("ynT", [128, 2, 128], BF16)
        dT = sb("dT", [64, 4, 128], F32)
        ypT = sb("ypT", [64, 4, 128], BF16)
        mg = sb("mg", [128, 1024], F32)
        tg_ = sb("tg", [128, 1024], F32)
        mgb = sb("mgb", [128, 1024], BF16)
        mT = sb("mT", [128, 8, 128], BF16)
        xo = [sb(f"xo{r}", [128, 1024], F32) for r in range(NB)]
        bank = [ps(f"bk{r}", [128, 512], F32) for r in range(8)]
        sc_ps = bank[0][:].rearrange("p (h c) -> p h c", h=4)
        y_ps = bank[1][:].rearrange("p (h c) -> p h c", h=4)
        st_ps = [bank[2][:].rearrange("p (h c) -> p h c", h=4), bank[6][:].rearrange("p (h c) -> p h c", h=4)]
        st_k = ['bk2', 'bk6']
        o_ps = bank[3][:, 0:260].rearrange("p (h c) -> p h c", h=4)
        pl_ps = bank[4][0:64, :].rearrange("p (h c) -> p h c", h=4)
        tr_ps = bank[5][:].bitcast(BF16).rearrange("p (c t) -> p c t", c=8)
        tr7_ps = bank[7][:].bitcast(BF16).rearrange("p (c t) -> p c t", c=8)

        S.dma('sp', [], ['identf'], out=identf[:], in_=D['ident'])
        S.op('dve', 'tensor_copy', ['identf'], ['identb'], out=identb[:], in_=identf[:])
        for g in range(4):
            S.dma('pool', [], ['Wbp'], out=Wbp[:, g, :], in_=D['wbp'][:, g, :])
        S.dma('pool', [], ['Wbr'], out=Wbr[:], in_=D['wbr'].rearrange("(h e) n -> e h n", e=128))
        S.dma('pool', [], ['Wbn'], out=Wbn[:], in_=D['wbn'].rearrange("(h e) n -> e h n", e=128))
        S.dma('pool', [], ['Wout'], out=Wout[:], in_=D['wout'].rearrange("(h e) n -> e h n", e=128))
        S.dma('sp', [], ['poolw'], out=poolw[:], in_=D['poolw'])
        S.dma('sp', [], ['pscale'], out=pscale[:], in_=D['pscale'])
        S.dma('sp', [], ['amat'], out=amat[:], in_=D['amat'])
        S.dma('sp', [], ['dl'], out=dl[:], in_=D['decay'].partition_broadcast(128))
        S.dma('sp', [], ['pos'], out=pos[:], in_=D['pos'])
        S.dma('sp', [], ['dmat'], out=dmat[:], in_=D['dmat'].rearrange("k p c -> p k c"))
        S.dma('sp', [], ['xrow'], out=xrow[:], in_=D['xrow'])
        for r in range(NB):
            for o in range(5):
                S.op('dve', 'memset', [], ['Nv%d_%d' % (r, o)], Nv1[r][o][:], 1.0)
        for ty in range(5):
            S.dma('sp', [], ['rstage'], out=rstage[:], in_=D['rpbT'][ty])
            S.dma('sp', [], ['nmask'], out=nmask[:], in_=D['nmask'][ty])
            S.op('act', 'activation', ['rstage'], ['rstage'], out=rstage[:], in_=rstage[:], func=AF.Exp)
            S.op('dve', 'tensor_tensor', ['rstage', 'nmask'], ['Et%d' % ty], out=Et[ty][:], in0=rstage[:],
                 in1=nmask[:].unsqueeze(2).to_broadcast([128, 5, 4, 128]), op=ALU.mult)
        S.op('act', 'activation', ['dl'], ['lg'], out=lg[:], in_=dl[:], func=AF.Exp, scale=-1.0)
        S.op('act', 'activation', ['lg'], ['lg'], out=lg[:], in_=lg[:], func=AF.Ln, bias=1.0)
        S.op('act', 'mul', ['lg'], ['lg'], lg[:], lg[:], -1.0)
        S.op('act', 'activation', ['lg', 'pos'], ['zf'], out=zf[:], in_=lg[:, 0:4], func=AF.Exp, scale=pos[:, 0:1])
        S.op('act', 'activation', ['lg', 'pos'], ['zb'], out=zb[:], in_=lg[:, 4:8], func=AF.Exp, scale=pos[:, 1:2])
        S.op('act', 'activation', ['lg'], ['dC'], out=dC[:], in_=lg[:], func=AF.Exp, scale=128.0)
        for h in range(4):
            S.op('act', 'activation', ['lg', 'dmat'], ['E1'], out=E1[:], in_=dmat[:, 0, :], func=AF.Exp, scale=lg[:, h:h + 1])
            S.op('act', 'activation', ['lg', 'dmat'], ['E2'], out=E2[:], in_=dmat[:, 2, :], func=AF.Exp, scale=lg[:, 4 + h:5 + h])
            S.op('dve', 'tensor_tensor', ['E1', 'dmat'], ['E1'], out=E1[:], in0=E1[:], in1=dmat[:, 1, :], op=ALU.mult)
            S.op('dve', 'tensor_tensor', ['E2', 'dmat'], ['E2'], out=E2[:], in0=E2[:], in1=dmat[:, 3, :], op=ALU.mult)
            S.op('dve', 'tensor_tensor', ['E1', 'E2'], ['Dt'], out=Dt[:, h, :], in0=E1[:], in1=E2[:], op=ALU.add)
            S.op('act', 'activation', ['lg', 'xrow'], ['XF'], out=XF[:, h, :], in_=xrow[0:64, 0, :], func=AF.Exp, scale=lg[0:64, h:h + 1])
            S.op('act', 'activation', ['lg', 'xrow'], ['XB'], out=XB[:, h, :], in_=xrow[0:64, 1, :], func=AF.Exp, scale=lg[0:64, 4 + h:5 + h])

        S.end_phase()
        rsb = rstage[:].rearrange("p a b c -> p (a b c)").bitcast(BF16)
        rsf = rstage[:].rearrange("p a b c -> p (a b c)")

        class _V:
            def __init__(self, ap):
                self.ap = ap

            def __getitem__(self, k):
                return self.ap if (k == slice(None)) else self.ap[k]

        Rk2 = [_V(rsb[:, 0:256]), _V(rsb[:, 256:512])]
        Rv2 = [_V(rsb[:, 512:1024]), _V(rsb[:, 1024:1536])]
        kd2 = _V(rsb[:, 1536:1792].rearrange("p (h d) -> p h d", h=4))
        Sob2 = [_V(rsb[0:64, 1792:2304].rearrange("p (h c) -> p h c", h=4)), _V(rsb[0:64, 2304:2816].rearrange("p (h c) -> p h c", h=4))]
        Sst2 = _V(rsf[0:64, 1408:1920].rearrange("p (h c) -> p h c", h=4))

        def sweep(order, zt, zk, dcol, dst, dkey, store_pred, upd_pred, St, Sk, kdt, kdk, Sobt, Sobk, Rkt, Rkk, Rvt, Rvk, bki):
            S.op('dve', 'memset', [], [Sk], St[:], 0.0)
            yield
            for cnt, n in enumerate(order):
                b = cnt % NB
                sfx = str(b)
                rows = slice(n * 128, (n + 1) * 128)
                if store_pred(n):
                    S.op('act', 'copy', [Sk], [Sobk + sfx], out=Sobt[b][:], in_=St[:])
                    S.dma('pool', [Sobk + sfx], [dkey + str(n)], out=dst[n], in_=Sobt[b][:])
                if not upd_pred(n):
                    continue
                S.dma('sp', [], [Rkk + sfx], out=Rkt[b][:], in_=D['Rk'][rows, :])
                S.dma('sp', [], [Rvk + sfx], out=Rvt[b][:], in_=D['Rv'][rows, :])
                yield
                S.op('dve', 'tensor_tensor', [Rkk + sfx, zk], [kdk], out=kdt[:], in0=Rkt[b][:].rearrange("p (h d) -> p h d", h=4),
                     in1=zt[:].unsqueeze(2).to_broadcast([128, 4, 64]), op=ALU.mult)
                yield
                for h in range(4):
                    S.op('pe', 'matmul', [kdk, Rvk + sfx], ['bk%d' % bki], bank[bki][0:64, h * 128:(h + 1) * 128], lhsT=kdt[:, h, :],
                         rhs=Rvt[b][:, h * 128:(h + 1) * 128], start=True, stop=True)
                S.op('dve', 'tensor_tensor', [Sk, 'dC'], [Sk], out=St[:], in0=St[:],
                     in1=dC[0:64, dcol:dcol + 4].unsqueeze(2).to_broadcast([64, 4, 128]), op=ALU.mult)
                yield
                S.op('dve', 'tensor_tensor', [Sk, 'bk%d' % bki], [Sk], out=St[:], in0=St[:],
                     in1=bank[bki][0:64, :].rearrange("p (h c) -> p h c", h=4), op=ALU.add)
                yield

        last = t0 + nt - 1
        sw = [sweep(list(range(0, last + 1)), zf, 'zf', 0, D['SF'], 'dSF', lambda n: n >= t0, lambda n: n < last,
                    Sst, 'Sst', kd, 'kd', Sob, 'Sob', Rk, 'Rk', Rv, 'Rv', 0),
              sweep(list(range(31, t0 - 1, -1)), zb, 'zb', 4, D['SB'], 'dSB', lambda n: n <= last, lambda n: n > t0,
                    Sst2, 'Sst2', kd2, 'kd2', Sob2, 'Sobb', Rk2, 'Rkb', Rv2, 'Rvb', 1)]
        while sw:
            for g_ in list(sw):
                try:
                    next(g_)
                except StopIteration:
                    sw.remove(g_)

        B4 = [128, 4, 128]
        for it in range(nt):
            i = t0 + it
            b = it % NB
            sfx = str(b)
            rows = slice(i * 128, (i + 1) * 128)
            ty = {0: 1, 1: 2, 30: 3, 31: 4}.get(i, 0)
            offs = [o for o in range(NA_OMIN[ty], NA_OMIN[ty] + 5) if 0 <= i + o <= 31]
            poffs = [o for o in range(-1, 2) if 0 <= i + o <= 31]
            S.dma('sp', [], ['xt' + sfx], out=xt[b][:], in_=D['x_full'][rows, :])
            S.dma('sp', [], ['Gt' + sfx], out=Gt[b][:], in_=D['Gt'][rows, :])
            S.dma('sp', [], ['Rq' + sfx], out=Rq[b][:], in_=D['Rq'][rows, :])
            S.dma('sp', [], ['Rk' + sfx], out=Rk[b][:], in_=D['Rk'][rows, :])
            S.dma('sp', [], ['Rv' + sfx], out=Rv[b][:], in_=D['Rv'][rows, :])
            S.dma('sp', [], ['Rg' + sfx], out=Rg[b][:], in_=D['Rg'][rows, :])
            S.dma('sp', ['dSF%d' % i], ['SFt' + sfx], out=SFt[b][:], in_=D['SF'][i])
            S.dma('sp', ['dSB%d' % i], ['SBt' + sfx], out=SBt[b][:], in_=D['SB'][i])
            S.dma('sp', [], ['Nq' + sfx], out=Nq[b][:], in_=D['Nq'][rows, :])
            for oi, o in enumerate(offs):
                r2 = slice((i + o) * 128, (i + o + 1) * 128)
                S.dma('sp', [], ['Nk%s_%d' % (sfx, oi)], out=Nk[b][oi][:], in_=D['Nk'][r2, :])
                S.dma('sp', [], ['Nv%s_%d' % (sfx, oi)], out=Nv1[b][oi][:, :, 0:64], in_=D['Nv'][r2, :].rearrange("p (h d) -> p h d", h=4))
            for oi, o in enumerate(poffs):
                r2 = slice((i + o) * 128, (i + o + 1) * 128)
                S.dma('sp', [], ['Pp%s_%d' % (sfx, oi)], out=Pp[b][oi][:], in_=D['Pp'][r2, :])

            if 'uTb' in D:
                cpt = -(-128 // nt)
                uT2 = D['uT'].rearrange("i p c e -> i p (c e)")
                for i0 in range(it * cpt, min(128, (it + 1) * cpt)):
                    S.dma('pool', [], [], out=D['uTb'][i0], in_=uT2[i0])
                    S.dma('pool', [], [], out=D['vb'][i0], in_=D['v'][i0 * 128:(i0 + 1) * 128, :])
            def gen_ret():
                for h in range(4):
                    S.op('pe', 'transpose', ['Rq' + sfx, 'identb'], ['bk5'], out=tr_ps[0:64, h, :], in_=Rq[b][:, h * 64:(h + 1) * 64], identity=identb[:])
                    S.op('pe', 'transpose', ['Rk' + sfx, 'identb'], ['bk5'], out=tr_ps[0:64, 4 + h, :], in_=Rk[b][:, h * 64:(h + 1) * 64], identity=identb[:])
                yield
                S.op('act', 'copy', ['bk5'], ['qkT'], out=qkT[:], in_=tr_ps[0:64, :, :])
                yield
                S.op('dve', 'tensor_tensor', ['qkT', 'XF'], ['qfT'], out=qfT[:], in0=qkT[:, 0:4, :], in1=XF[:], op=ALU.mult)
                S.op('dve', 'tensor_tensor', ['qkT', 'XB'], ['qbT'], out=qbT[:], in0=qkT[:, 0:4, :], in1=XB[:], op=ALU.mult)
                yield
                for h in range(4):
                    S.op('pe', 'matmul', ['qkT'], ['bk0'], sc_ps[:, h, :], lhsT=qkT[:, 4 + h, :], rhs=qkT[:, h, :], start=True, stop=True)
                yield
                S.op('dve', 'tensor_tensor', ['bk0', 'Dt'], ['PD'], out=PD[:], in0=sc_ps, in1=Dt[:], op=ALU.mult)
                yield
                for h in range(4):
                    S.op('pe', 'matmul', ['PD', 'Rv' + sfx], ['bk1'], y_ps[:, h, :], lhsT=PD[:, h, :], rhs=Rv[b][:, h * 128:(h + 1) * 128],
                         start=(h == 0), stop=False, skip_group_check=True)
                    S.op('pe', 'matmul', ['qfT', 'SFt' + sfx], ['bk1'], y_ps[:, h, :], lhsT=qfT[:, h, :], rhs=SFt[b][:, h, :],
                         start=False, stop=False, skip_group_check=True)
                    S.op('pe', 'matmul', ['qbT', 'SBt' + sfx], ['bk1'], y_ps[:, h, :], lhsT=qbT[:, h, :], rhs=SBt[b][:, h, :],
                         start=False, stop=(h == 3), skip_group_check=True)
                yield
                S.op('act', 'activation', ['bk1'], ['ysq'], out=ysq[:], in_=y_ps, func=AF.Square)
                yield
                S.op('dve', 'tensor_reduce', ['ysq'], ['ssq4'], out=ssq4[:], in_=ysq[:], axis=AX.X, op=ALU.add)
                yield
                S.op('act', 'activation', ['ssq4'], ['rs4'], out=rs4[:], in_=ssq4[:], func=AF.Sqrt, scale=1.0 / 128, bias=EPS)
                yield
                S.op('dve', 'reciprocal', ['rs4'], ['rs4'], out=rs4[:], in_=rs4[:])
                S.op('dve', 'tensor_tensor', ['bk1', 'rs4'], ['yr'], out=yr[:], in0=y_ps, in1=rs4[:].unsqueeze(2).to_broadcast(B4), op=ALU.mult)
                S.op('dve', 'tensor_tensor', ['yr', 'Rg' + sfx], ['yrb'], out=yrb[:], in0=yr[:].rearrange("p h c -> p (h c)"), in1=Rg[b][:], op=ALU.mult)
                yield
                for h in range(4):
                    S.op('pe', 'transpose', ['yrb', 'identb'], ['bk5'], out=tr_ps[:, h, :], in_=yrb[:, h * 128:(h + 1) * 128], identity=identb[:])
                yield
                S.op('act', 'copy', ['bk5'], ['yrT'], out=yrT[:], in_=tr_ps[:, 0:4, :])


                yield

            def gen_na():
                srcs = [(Nq[b], 'Nq' + sfx)] + [(Nk[b][oi], 'Nk%s_%d' % (sfx, oi)) for oi in range(len(offs))]
                for r0 in range(0, len(srcs), 2):
                    grp = srcs[r0:r0 + 2]
                    for gi, (src, sk) in enumerate(grp):
                        for h in range(4):
                            S.op('pe', 'transpose', [sk, 'identb'], ['bk7'], out=tr7_ps[0:64, gi * 4 + h, :], in_=src[:, h * 64:(h + 1) * 64], identity=identb[:])
                    n_ = 4 * len(grp)
                    yield
                    S.op('act', 'copy', ['bk7'], ['NTa'], out=NTa[:, r0 * 4:r0 * 4 + n_, :], in_=tr7_ps[0:64, 0:n_, :])
                yield
                for oi, o in enumerate(offs):
                    sp_, sk_ = st_ps[oi % 2], st_k[oi % 2]
                    for h in range(4):
                        S.op('pe', 'matmul', ['NTa'], [sk_], sp_[:, h, :], lhsT=NTa[:, 4 + 4 * oi + h, :], rhs=NTa[:, h, :], start=True, stop=True)
                    yield
                    S.op('act', 'activation', [sk_], ['Pm'], out=Pm[:], in_=sp_, func=AF.Exp, scale=0.125)
                    yield
                    S.op('dve', 'tensor_tensor', ['Pm', 'Et%d' % ty], ['PEm'], out=PEm[:], in0=Pm[:], in1=Et[ty][:, o - NA_OMIN[ty], :, :], op=ALU.mult)
                    yield
                    for h in range(4):
                        S.op('pe', 'matmul', ['PEm', 'Nv%s_%d' % (sfx, oi)], ['bk3'], o_ps[:, h, :], lhsT=PEm[:, h, :], rhs=Nv1[b][oi][:, h, :],
                             start=(oi == 0 and h == 0), stop=(oi == len(offs) - 1 and h == 3), skip_group_check=True)
                yield
                S.op('dve', 'reciprocal', ['bk3'], ['rden'], out=rden[:], in_=o_ps[:, :, 64:65].rearrange("p h c -> p (h c)"))
                S.op('dve', 'tensor_tensor', ['bk3', 'rden'], ['ynb'], out=ynb[:], in0=o_ps[:, :, 0:64], in1=rden[:].unsqueeze(2).to_broadcast([128, 4, 64]), op=ALU.mult)
                ynf = ynb[:].rearrange("p h d -> p (h d)")
                yield
                for c in range(2):
                    S.op('pe', 'transpose', ['ynb', 'identb'], ['bk7'], out=tr7_ps[:, c, :], in_=ynf[:, c * 128:(c + 1) * 128], identity=identb[:])
                yield
                S.op('act', 'copy', ['bk7'], ['ynT'], out=ynT[:], in_=tr7_ps[:, 0:2, :])


                yield

            def gen_pool():
                for g in range(4):
                    for oi, o in enumerate(poffs):
                        kind = {-1: 0, 1: 2}.get(o, 3 if i == 0 else (4 if i == 31 else 1))
                        S.op('pe', 'matmul', ['Pp%s_%d' % (sfx, oi), 'amat'], ['bk4'], pl_ps[:, g, :], lhsT=Pp[b][oi][:, g * 64:(g + 1) * 64],
                             rhs=amat[:, g, kind, :], start=(oi == 0), stop=(oi == len(poffs) - 1))
                yield
                S.op('act', 'copy', ['bk4'], ['dT'], out=dT[:], in_=pl_ps)
                yield
                for g in range(4):
                    S.op('pe', 'matmul', ['dT', 'poolw'], ['bk4'], pl_ps[:, g, :], lhsT=poolw[:, g, :], rhs=dT[:, g, :], start=True, stop=True)
                yield
                S.op('dve', 'tensor_tensor', ['bk4', 'pscale'], ['ypT'], out=ypT[:], in0=pl_ps, in1=pscale[:].unsqueeze(2).to_broadcast([64, 4, 128]), op=ALU.mult)


                yield

            gens = [gen_ret(), gen_na(), gen_pool()]
            while gens:
                for g_ in list(gens):
                    try:
                        next(g_)
                    except StopIteration:
                        gens.remove(g_)
            def branch(nk, lhs_list, w_tile, goff, first, lastb):
                for nh in range(2):
                    bkk = bank[6 + nh]
                    for kk in range(nk):
                        S.op('pe', 'matmul', lhs_list[1] + [w_tile[1]], ['bk%d' % (6 + nh)], bkk[:], lhsT=lhs_list[0][:, kk, :],
                             rhs=w_tile[0][:, kk, nh * 512:(nh + 1) * 512], start=(kk == 0), stop=(kk == nk - 1))
                    cols = slice(nh * 512, (nh + 1) * 512)
                    gsl = Gt[b][:, goff + nh * 512: goff + (nh + 1) * 512]
                    if first:
                        S.op('dve', 'tensor_tensor', ['bk%d' % (6 + nh), 'Gt' + sfx], ['mg'], out=mg[:, cols], in0=bkk[:], in1=gsl, op=ALU.mult)
                    else:
                        S.op('dve', 'tensor_tensor', ['bk%d' % (6 + nh), 'Gt' + sfx], ['tg'], out=tg_[:, cols], in0=bkk[:], in1=gsl, op=ALU.mult)
                        if lastb:
                            S.op('dve', 'tensor_tensor', ['mg', 'tg'], ['mgb'], out=mgb[:, cols], in0=mg[:, cols], in1=tg_[:, cols], op=ALU.add)
                        else:
                            S.op('dve', 'tensor_tensor', ['mg', 'tg'], ['mg'], out=mg[:, cols], in0=mg[:, cols], in1=tg_[:, cols], op=ALU.add)

            branch(4, (ypT, ['ypT']), (Wbp, 'Wbp'), 0, True, False)
            branch(4, (yrT, ['yrT']), (Wbr, 'Wbr'), 1024, False, False)
            branch(2, (ynT, ['ynT']), (Wbn, 'Wbn'), 2048, False, True)
            for c in range(8):
                S.op('pe', 'transpose', ['mgb', 'identb'], ['bk5'], out=tr_ps[:, c, :], in_=mgb[:, c * 128:(c + 1) * 128], identity=identb[:])
            S.op('act', 'copy', ['bk5'], ['mT'], out=mT[:], in_=tr_ps)
            for nh in range(2):
                bkk = bank[6 + nh]
                for dc in range(8):
                    S.op('pe', 'matmul', ['mT', 'Wout'], ['bk%d' % (6 + nh)], bkk[:], lhsT=mT[:, dc, :], rhs=Wout[:, dc, nh * 512:(nh + 1) * 512],
                         start=(dc == 0), stop=(dc == 7))
                cols = slice(nh * 512, (nh + 1) * 512)
                S.op('dve', 'tensor_tensor', ['bk%d' % (6 + nh), 'xt' + sfx], ['xo' + sfx], out=xo[b][:, cols], in0=bkk[:], in1=xt[b][:, cols], op=ALU.add)
            S.dma('pool', ['xo' + sfx], [], out=D['x_out'][it * 128:(it + 1) * 128, :], in_=xo[b][:])
        S.end_phase()


POOL_WINDOWS = (2, 4, 8, 16)
SEQ = 4096
NA_OMIN = (-2, 0, -2, -2, -3)
NA_NOFF = (5, 4, 4, 4, 4)


def _const_tables():
    T = {}
    T['ident'] = np.eye(128, dtype=np.float32)
    T['iota'] = np.tile(np.arange(128, dtype=np.float32)[None, :], (128, 1))
    T['cst16'] = np.tile((16 * np.arange(16, dtype=np.float32) + 7.5)[None, :], (128, 1))
    half = 32
    inv = (1.0 / (np.float32(10000.0) ** np.linspace(0.0, 1.0, half, dtype=np.float32))).astype(np.float32)
    ang = (np.arange(SEQ, dtype=np.float32)[:, None] * inv[None, :]).astype(np.float32)
    T['cos'] = np.cos(ang).astype(np.float32)
    T['sin'] = np.sin(ang).astype(np.float32)
    p = np.arange(128, dtype=np.float32)
    T['pos'] = np.stack([127.0 - p, p], axis=1).astype(np.float32)
    m = p[:, None]
    c = p[None, :]
    T['dmat'] = np.stack([np.maximum(c - m, 0), (c >= m).astype(np.float32), np.maximum(m - c, 0), (m > c).astype(np.float32)]).astype(np.float32)
    xr = np.stack([p + 1.0, 128.0 - p], axis=0)
    T['xrow'] = np.tile(xr[None], (128, 1, 1)).astype(np.float32)
    A = np.zeros((128, 4, 5, 128), np.float32)
    for g, w in enumerate(POOL_WINDOWS):
        lo, hi = w // 2, w - w // 2
        for kind, base in ((1, 1024), (3, 0), (4, SEQ - 128)):
            for t in range(128):
                ta = base + t
                a_, b_ = max(ta - lo, 0), min(ta + hi, SEQ)
                cnt = float(b_ - a_)
                for s in range(a_, b_):
                    rel = s - base
                    if 0 <= rel < 128:
                        A[rel, g, kind, t] += 1.0 / cnt
                    elif rel < 0 and kind == 1:
                        A[rel + 128, g, 0, t] += 1.0 / cnt
                    elif rel >= 128 and kind == 1:
                        A[rel - 128, g, 2, t] += 1.0 / cnt
                A[t, g, kind, t] -= 1.0
    T['amat'] = A
    di = np.zeros((5, 128, 5, 128), np.int64)
    dj = np.zeros((5, 128, 5, 128), np.int64)
    mk = np.zeros((5, 128, 5, 128), np.float32)
    q = np.arange(128)
    k = np.arange(128)
    for ty, i in enumerate((5, 0, 1, 30, 31)):
        for oi in range(5):
            o = NA_OMIN[ty] + oi
            j = i + o
            if j < 0 or j > 31:
                continue
            qr = 2 * i + q // 64
            qc = q % 64
            kr = (2 * j + k // 64)[:, None]
            kcol = (k % 64)[:, None]
            rs = np.clip(qr - 4, 0, 56)[None, :]
            vr = (kr >= rs) & (kr < rs + 8)
            cst = np.clip(qc - 8, 0, 48)[None, :]
            vc = (kcol >= cst) & (kcol < cst + 16)
            di[ty, :, oi, :] = np.clip(kr - qr[None, :] + 7, 0, 14)
            dj[ty, :, oi, :] = np.clip(kcol - qc[None, :], -15, 15) + 15
            mk[ty, :, oi, :] = (vr & vc).astype(np.float32)
    T['na_di'], T['na_dj'], T['nmask'] = di, dj, mk
    return T


_TABLES = None


def tables():
    global _TABLES
    if _TABLES is None:
        _TABLES = _const_tables()
    return _TABLES


def layer_layout(l, w_in, pool_w, pool_scale, ret_decay, na_rpb, w_br_pool, w_br_ret, w_br_na, w_out,
                 peer_w_query, peer_sub_keys, peer_u, peer_v):
    T = tables()
    L = {}
    L['w_in'] = np.ascontiguousarray(w_in[l])
    L['wbp'] = np.ascontiguousarray(w_br_pool[l].reshape(4, 64, 1024).transpose(1, 0, 2))
    L['wbr'] = np.ascontiguousarray(w_br_ret[l])
    L['wbn'] = np.ascontiguousarray(w_br_na[l])
    L['wout'] = np.ascontiguousarray(w_out[l])
    L['poolw'] = np.ascontiguousarray(pool_w[l].transpose(1, 0, 2))
    L['pscale'] = np.ascontiguousarray(pool_scale[l].reshape(4, 64).T)
    L['decay'] = np.ascontiguousarray(ret_decay[l].reshape(1, 8))
    rp = na_rpb[l]
    g = rp[:, T['na_di'], T['na_dj']]
    L['rpbT'] = np.ascontiguousarray(g.transpose(1, 2, 3, 0, 4)).astype(np.float32)
    L['wq'] = np.ascontiguousarray(peer_w_query[l].reshape(8, 128, 16, 128).transpose(2, 1, 0, 3))
    L['skT'] = np.ascontiguousarray(peer_sub_keys[l].transpose(2, 0, 1))
    L['uT'] = np.ascontiguousarray(peer_u[l].reshape(128, 128, 8, 128).transpose(0, 3, 2, 1))
    L['v'] = np.ascontiguousarray(peer_v[l])
    return L


N_CORES = 4
NTOK = 4096
DEPTH = 2
_PER_LAYER = ('g_mix', 'w_in', 'wbp', 'wbr', 'wbn', 'wout', 'poolw', 'pscale', 'decay', 'rpbT', 'g_ffn', 'wq', 'skT', 'uT', 'v')
_SHAPES = dict(g_mix=[1, 1024], w_in=[1024, 5632], wbp=[64, 4, 1024], wbr=[512, 1024], wbn=[256, 1024], wout=[1024, 1024],
               poolw=[64, 4, 64], pscale=[64, 4], decay=[1, 8], rpbT=[5, 128, 5, 4, 128], g_ffn=[1, 1024], wq=[16, 128, 8, 128],
               skT=[128, 2, 128], uT=[128, 128, 8, 128], v=[16384, 1024])
_CONST = dict(ident=[128, 128], iota=[128, 128], cst16=[128, 16], cos=[4096, 32], sin=[4096, 32], pos=[128, 2], dmat=[4, 128, 128],
              xrow=[128, 2, 128], amat=[128, 4, 5, 128], nmask=[5, 128, 5, 128])


def build_program(n_iblk=128):
    nc = bass.Bass("TRN2", target_bir_lowering=False)
    dt = lambda nm, shp, ty=F32, kind="ExternalInput": nc.dram_tensor(nm, list(shp), ty, kind=kind).ap()
    x = dt("x", [NTOK, 1024])
    y = dt("y", [NTOK, 1024], F32, "ExternalOutput")
    gfin = dt("gfin", [1, 1024])
    Cn = {k: dt(k, s) for k, s in _CONST.items()}
    W = [{k: dt(f"{k}_{l}", _SHAPES[k]) for k in _PER_LAYER} for l in range(DEPTH)]
    I = "Internal"
    Sc = dict(Pp=dt("s_Pp", [4096, 256], F32, I), Rq=dt("s_Rq", [4096, 256], BF16, I), Rk=dt("s_Rk", [4096, 256], BF16, I),
              Rv=dt("s_Rv", [4096, 512], BF16, I), Rg=dt("s_Rg", [4096, 512], F32, I), Nq=dt("s_Nq", [4096, 256], BF16, I),
              Nk=dt("s_Nk", [4096, 256], BF16, I), Nv=dt("s_Nv", [4096, 256], BF16, I), Gt=dt("s_Gt", [4096, 3072], F32, I),
              SF=dt("s_SF", [32, 64, 4, 128], BF16, I), SB=dt("s_SB", [32, 64, 4, 128], BF16, I))
    x1 = dt("s_x1", [NTOK, 1024], F32, I)
    uTb = dt("s_uTb", [128, 128, 1024], BF16, I)
    vb = dt("s_vb", [128, 128, 1024], BF16, I)
    x2 = dt("s_x2", [NTOK, 1024], F32, I)
    own = (0, NTOK // 128)
    with ExitStack() as es:
        S = Sched(nc, es)
        cur = x
        for l in range(DEPTH):
            D = dict(Cn)
            D.update(Sc)
            D.update(W[l])
            D.update(x_full=cur, g=W[l]['g_mix'], x_out=x1, uTb=uTb, vb=vb)
            mixer_a(nc, S, D, own)
            mixer_b(nc, S, D, own)
            last = (l == DEPTH - 1)
            P = dict(Cn)
            P.update(W[l])
            P.update(x_in=x1, x_out=(y if last else x2), g=W[l]['g_ffn'], gfin=gfin, uTb=uTb, vb=vb, preconverted=True)
            peer_phase(nc, S, P, NTOK, n_iblk=n_iblk, final=last)
            cur = x2
    return nc


_NC_CACHE = {}


def kernel(x, norm_mix, w_in, pool_w, pool_scale, ret_decay, na_rpb, w_br_pool, w_br_ret, w_br_na, w_out, norm_ffn,
           peer_w_query, peer_sub_keys, peer_u, peer_v, norm_final):
    f = lambda a: np.ascontiguousarray(np.asarray(a), dtype=np.float32)
    x = f(x)
    T = tables()
    shared = {k: np.ascontiguousarray(T[k]) for k in _CONST}
    shared['gfin'] = f(norm_final).reshape(1, 1024)
    args = [f(a) for a in (w_in, pool_w, pool_scale, ret_decay, na_rpb, w_br_pool, w_br_ret, w_br_na, w_out,
                           peer_w_query, peer_sub_keys, peer_u, peer_v)]
    nm, nf = f(norm_mix), f(norm_ffn)
    for l in range(DEPTH):
        L = layer_layout(l, *args)
        L['g_mix'] = nm[l].reshape(1, 1024)
        L['g_ffn'] = nf[l].reshape(1, 1024)
        for k in _PER_LAYER:
            shared[f"{k}_{l}"] = L[k]
    if 'nc' not in _NC_CACHE:
        _NC_CACHE['nc'] = build_program()
    nc = _NC_CACHE['nc']
    in_maps = []
    for c in range(N_CORES):
        m = dict(shared)
        m['x'] = np.ascontiguousarray(x[c])
        in_maps.append(m)
    res = run_bass_kernel_spmd(nc, in_maps, core_ids=list(range(N_CORES)))
    out = np.stack([np.asarray(res.results[c]['y'], dtype=np.float32) for c in range(N_CORES)], axis=0)
    return out
```

```python
import numpy as np
from contextlib import ExitStack
import concourse.bass as bass
import concourse.mybir as mybir
from concourse.bass_utils import run_bass_kernel_spmd

F32 = mybir.dt.float32
BF16 = mybir.dt.bfloat16
U32 = mybir.dt.uint32
AF = mybir.ActivationFunctionType
ALU = mybir.AluOpType
AX = mybir.AxisListType

import re as _re
_PSUM_KEY = _re.compile(r"^(bk|pp|gps|aps|ops|tps)\d*$")
EPOCH = 20000
NDMA = 16
EPS = 1e-6


class _Eng:
    def __init__(self, name, strict):
        self.name = name
        self.strict = strict
        self.count = 0
        self.sems = []
        self.known = {}
        self.dsems = []
        self.dcount = []
        self.dnext = 0


class Sched:
    def __init__(self, nc, es):
        self.nc = nc
        self.es = es
        self.E = {
            'pe': _Eng('pe', False),
            'act': _Eng('act', True),
            'dve': _Eng('dve', True),
            'pool': _Eng('pool', True),
            'sp': _Eng('sp', False),
        }
        self.lastw = {}
        self.readers = {}
        self.nsem = 0
        self.prog = {k: [] for k in self.E}

    def _newsem(self, nm):
        self.nsem += 1
        return self.es.enter_context(self.nc.semaphore(f"{nm}_{self.nsem}"))

    def _cur_sem(self, e):
        ep = e.count // EPOCH
        while len(e.sems) <= ep:
            e.sems.append(self._newsem(e.name))
        return e.sems[ep]

    def _wait(self, e, dep):
        sem, val, src = dep
        if src is e and not e.strict:
            return
        k = id(sem)
        if e.known.get(k, 0) >= val:
            return
        self.prog[e.name].append(('w', sem, val))
        e.known[k] = val

    def _deps(self, reads, writes):
        deps = []
        for b in reads:
            if b in self.lastw:
                deps.append(self.lastw[b])
        for b in writes:
            if b in self.lastw:
                deps.append(self.lastw[b])
            deps.extend(self.readers.get(b, []))
        return deps

    def _commit(self, dep, reads, writes):
        for b in reads:
            self.readers.setdefault(b, []).append(dep)
        for b in writes:
            self.lastw[b] = dep
            self.readers[b] = []

    def op(self, eng, meth, reads, writes, *a, **kw):
        fn = lambda h: getattr(h, meth)(*a, **kw)
        e = self.E[eng]
        px = [b for b in reads if _PSUM_KEY.match(b)]
        if px:
            reads = [b for b in reads if b not in px]
            writes = list(writes) + px
        for d in self._deps(reads, writes):
            self._wait(e, d)
        sem = self._cur_sem(e)
        val = e.count % EPOCH + 1
        self.prog[e.name].append(('i', fn, sem, 1))
        e.count += 1
        self._commit((sem, val, e), reads, writes)

    def dma(self, eng, reads, writes, meth='dma_start', **kw):
        fn = lambda h: getattr(h, meth)(**kw)
        e = self.E[eng]
        if not e.dsems:
            e.dsems = [self._newsem(e.name + "d") for _ in range(NDMA)]
            e.dcount = [0] * NDMA
        s = e.dnext
        e.dnext = (e.dnext + 1) % NDMA
        sem = e.dsems[s]
        if e.dcount[s] > 0:
            self._wait(e, (sem, 16 * e.dcount[s], None))
        for d in self._deps(reads, writes):
            self._wait(e, d)
        e.dcount[s] += 1
        self.prog[e.name].append(('i', fn, sem, 16))
        self._commit((sem, 16 * e.dcount[s], None), reads, writes)

    def end_phase(self):
        e = self.E['sp']
        for o in self.E.values():
            if o.count > 0:
                sem = o.sems[(o.count - 1) // EPOCH]
                self._wait(e, (sem, (o.count - 1) % EPOCH + 1, None))
            for s, c in enumerate(o.dcount):
                if c > 0:
                    self._wait(e, (o.dsems[s], 16 * c, None))
        with self.nc.Block() as blk:
            def replay(name):
                def f(h):
                    for it in self.prog[name]:
                        if it[0] == 'w':
                            h.wait_ge(it[1], it[2])
                        else:
                            it[1](h).then_inc(it[2], it[3])
                return f
            blk.sync(replay('sp'))
            blk.scalar(replay('act'))
            blk.vector(replay('dve'))
            blk.gpsimd(replay('pool'))
            blk.tensor(replay('pe'))
        self.prog = {k: [] for k in self.E}
        self.lastw = {}
        self.readers = {}


class Ctx:
    n = 0

    def __init__(self, nc, es):
        self.nc = nc
        self.es = es

    def sb(self, nm, shp, dt):
        Ctx.n += 1
        return self.es.enter_context(self.nc.sbuf_tensor(f"{nm}_{Ctx.n}", list(shp), dt))

    def ps(self, nm, shp, dt):
        Ctx.n += 1
        return self.es.enter_context(self.nc.psum_tensor(f"{nm}_{Ctx.n}", list(shp), dt))


def rmsnorm_tile(S, x_ap, x_key, g_tile, g_key, out_ap, out_key, tmp, ssq, rstd, pfx):
    S.op('act', 'activation', [x_key], [pfx + 'tmp', pfx + 'ssq'], out=tmp[:], in_=x_ap, func=AF.Square, accum_out=ssq[:])
    S.op('act', 'activation', [pfx + 'ssq'], [pfx + 'rstd'], out=rstd[:], in_=ssq[:], func=AF.Sqrt, scale=1.0 / 1024, bias=EPS)
    S.op('dve', 'reciprocal', [pfx + 'rstd'], [pfx + 'rstd'], out=rstd[:], in_=rstd[:])
    S.op('dve', 'scalar_tensor_tensor', [x_key, pfx + 'rstd', g_key], [out_key], out=out_ap, in0=x_ap, scalar=rstd[:, 0:1],
         in1=g_tile[:], op0=ALU.mult, op1=ALU.mult)


def peer_phase(nc, S, D, NT, n_iblk=128, final=False):
    with ExitStack() as es:
        C = Ctx(nc, es)
        sb, ps = C.sb, C.ps
        identf = sb("identf", [128, 128], F32)
        identb = sb("identb", [128, 128], BF16)
        iotaf = sb("iotaf", [128, 128], F32)
        iotab = sb("iotab", [128, 128], BF16)
        c16 = sb("c16", [128, 16], F32)
        skT = sb("skT", [128, 2, 128], F32)
        gB = sb("gB", [128, 1024], F32)
        gFin = sb("gFin", [128, 1024], F32) if final else None
        Gbuf = sb("Gbuf", [128, 256, 128], BF16)
        xt = [sb(f"xt{r}", [128, 2, 1024], F32) for r in range(2)]
        hnT = [sb(f"hnT{r}", [128, 8, 256], BF16) for r in range(2)]
        Wqc = [sb(f"Wqc{r}", [128, 8, 128], BF16) for r in range(2)]
        tmp = sb("tmp", [128, 1024], BF16)
        ssq = sb("ssq", [128, 1], F32)
        rstd = sb("rstd", [128, 1], F32)
        hn = sb("hn", [128, 1024], BF16)
        qc = [sb(f"qc{r}", [128, 256], F32) for r in range(2)]
        sall = sb("sall", [128, 2, 16, 128], F32)
        s2 = sb("s2", [128, 256], F32)
        Vt = sb("Vt", [128, 8, 2, 16], F32)
        It = sb("It", [128, 8, 2, 16], U32)
        cand = sb("cand", [128, 8, 256], F32)
        TV = sb("TV", [128, 8, 16], F32)
        TP = sb("TP", [128, 8, 16], U32)
        TPf = sb("TPf", [128, 8, 16], F32)
        TVs = sb("TVs", [128, 8, 16], F32)
        Ex = sb("Ex", [128, 8, 16], F32)
        Z = sb("Z", [128, 8], F32)
        rZ = sb("rZ", [128, 8], F32)
        I1f = sb("I1f", [128, 8, 16], F32)
        I2f = sb("I2f", [128, 8, 16], F32)
        OH = sb("OH", [128, 8, 16, 16], F32)
        OH2 = sb("OH2", [128, 8, 16, 16], BF16)
        af = sb("af", [128, 8, 16], F32)
        bf = sb("bf", [128, 8, 16], F32)
        iF = sb("iF", [128, 128], F32)
        jF = sb("jF", [128, 128], F32)
        gFt = sb("gFt", [128, 128], F32)
        iT = sb("iT", [128, 256], F32)
        jT = sb("jT", [128, 256], F32)
        gT = sb("gT", [128, 256], F32)
        NR = 8
        OI = [sb(f"OI{r}", [128, 128], BF16) for r in range(NR)]
        OJ = [sb(f"OJ{r}", [128, 128], BF16) for r in range(NR)]
        NSLOT = 5
        UTb = [sb(f"UTb{r}", [128, 8, 128], BF16) for r in range(NSLOT)]
        Vb = [sb(f"Vb{r}", [128, 1024], BF16) for r in range(NSLOT)]
        hf = [sb(f"hf{r}", [128, 256], F32) for r in range(2)]
        hG = [sb(f"hG{r}", [128, 256], BF16) for r in range(2)]
        yo = sb("yo", [128, 2, 1024], F32)
        ops = [ps(f"ops{r}", [128, 512], F32) for r in range(4)]
        aps = [ps(f"aps{r}", [128, 512], F32) for r in range(2)]
        gps = [ps(f"gps{r}", [128, 4, 128], F32) for r in range(2)]
        tps = gps[1][:].rearrange("p a b -> p (a b)").bitcast(BF16).rearrange("p (c t) -> p c t", c=8)

        S.dma('sp', [], ['identf'], out=identf[:], in_=D['ident'])
        S.dma('sp', [], ['iotaf'], out=iotaf[:], in_=D['iota'])
        S.dma('sp', [], ['c16'], out=c16[:], in_=D['cst16'])
        S.dma('sp', [], ['skT'], out=skT[:], in_=D['skT'])
        S.dma('sp', [], ['gB'], out=gB[:], in_=D['g'].partition_broadcast(128))
        if final:
            S.dma('sp', [], ['gFin'], out=gFin[:], in_=D['gfin'].partition_broadcast(128))
        S.op('dve', 'tensor_copy', ['identf'], ['identb'], out=identb[:], in_=identf[:])
        S.op('dve', 'tensor_copy', ['iotaf'], ['iotab'], out=iotab[:], in_=iotaf[:])

        ntile = NT // 256
        x_in = D['x_in'].rearrange("(t g p) d -> t p g d", g=2, p=128)
        x_out = D['x_out'].rearrange("(t g p) d -> t p g d", g=2, p=128)
        iota16_b = iotaf[:, 0:16].unsqueeze(1).unsqueeze(1).to_broadcast([128, 8, 16, 16])
        c16_b = c16[:].unsqueeze(1).unsqueeze(1).to_broadcast([128, 8, 16, 16])
        B4 = [128, 8, 16, 16]

        if not D.get('preconverted', False):
            uT2 = D['uT'].rearrange("i p c e -> i p (c e)")
            for i0 in range(128):
                S.dma('pool', [], ['cvU%d' % i0], out=D['uTb'][i0], in_=uT2[i0])
                S.dma('pool', [], ['cvV%d' % i0], out=D['vb'][i0], in_=D['v'][i0 * 128:(i0 + 1) * 128, :])

        def wdma(i):
            sl = i % NSLOT
            S.dma('sp', ['cvU%d' % i], ['UTb%d' % sl], out=UTb[sl][:].rearrange("p c e -> p (c e)"), in_=D['uTb'][i])
            S.dma('sp', ['cvV%d' % i], ['Vb%d' % sl], out=Vb[sl][:], in_=D['vb'][i])

        def stage1(i, par):
            sl = i % NSLOT
            hv = i % 2
            for dc in range(8):
                S.op('pe', 'matmul', ['UTb%d' % sl, 'hnT%d' % par], ['aps%d' % hv], aps[hv][:, 0:256], lhsT=UTb[sl][:, dc, :],
                     rhs=hnT[par][:, dc, :], start=(dc == 0), stop=(dc == 7))

        def prep(ti, par):
            xk, hk = 'xt%d' % par, 'hnT%d' % par
            S.dma('sp', [], [xk], out=xt[par][:], in_=x_in[ti])
            yield
            for g in range(2):
                rmsnorm_tile(S, xt[par][:, g, :], xk, gB, 'gB', hn[:], 'hn', tmp, ssq, rstd, 'p')
                yield
                for c in range(8):
                    S.op('pe', 'transpose', ['hn', 'identb'], ['gps1'], out=tps[:, c, :], in_=hn[:, c * 128:(c + 1) * 128], identity=identb[:])
                S.op('act', 'copy', ['gps1'], [hk], out=hnT[par][:, :, g * 128:(g + 1) * 128], in_=tps)
                yield
            S.dma('pool', [], ['Wqc0'], out=Wqc[0][:], in_=D['wq'][0])
            for c in range(16):
                gp = gps[c % 2]
                gk = 'gps%d' % (c % 2)
                qk = 'qc%d' % (c % 2)
                wk = 'Wqc%d' % (c % 2)
                if c + 1 < 16:
                    S.dma('pool', [], ['Wqc%d' % ((c + 1) % 2)], out=Wqc[(c + 1) % 2][:], in_=D['wq'][c + 1])
                qv = gp[:, 0:2, :].rearrange("p a b -> p (a b)")
                for dc in range(8):
                    S.op('pe', 'matmul', [hk, wk], [gk], qv, lhsT=Wqc[c % 2][:, dc, :], rhs=hnT[par][:, dc, :],
                         start=(dc == 0), stop=(dc == 7))
                S.op('act', 'copy', [gk], [qk], out=qc[c % 2][:], in_=qv)
                yield
                for g in range(2):
                    S.op('pe', 'matmul', [qk, 'skT'], [gk], gp[:, 2 + g, :], lhsT=qc[c % 2][:, g * 128:(g + 1) * 128], rhs=skT[:, c % 2, :],
                         start=True, stop=True)
                S.op('act', 'copy', [gk], ['sall'], out=sall[:, :, c, :], in_=gp[:, 2:4, :])
                yield
            for g in range(2):
                for c in range(16):
                    h, p = divmod(c, 2)
                    src = sall[:, g, c, :]
                    S.op('dve', 'max', ['sall'], ['Vt'], out=Vt[:, h, p, 0:8], in_=src)
                    yield
                    S.op('dve', 'max_index', ['sall', 'Vt'], ['It'], out=It[:, h, p, 0:8], in_max=Vt[:, h, p, 0:8], in_values=src)
                    yield
                    S.op('dve', 'match_replace', ['sall', 'Vt'], ['s2'], out=s2[:, 0:128], in_to_replace=Vt[:, h, p, 0:8], in_values=src, imm_value=-1e30)
                    yield
                    S.op('dve', 'max', ['s2'], ['Vt'], out=Vt[:, h, p, 8:16], in_=s2[:, 0:128])
                    yield
                    S.op('dve', 'max_index', ['s2', 'Vt'], ['It'], out=It[:, h, p, 8:16], in_max=Vt[:, h, p, 8:16], in_values=s2[:, 0:128])
                    yield
                S.op('dve', 'tensor_tensor', ['Vt'], ['cand'], out=cand[:].rearrange("p h (a b) -> p h a b", a=16),
                     in0=Vt[:, :, 0, :].unsqueeze(3).to_broadcast(B4), in1=Vt[:, :, 1, :].unsqueeze(2).to_broadcast(B4), op=ALU.add)
                yield
                for h in range(8):
                    src = cand[:, h, :]
                    S.op('dve', 'max', ['cand'], ['TV'], out=TV[:, h, 0:8], in_=src)
                    yield
                    S.op('dve', 'max_index', ['cand', 'TV'], ['TP'], out=TP[:, h, 0:8], in_max=TV[:, h, 0:8], in_values=src)
                    yield
                    S.op('dve', 'match_replace', ['cand', 'TV'], ['s2'], out=s2[:], in_to_replace=TV[:, h, 0:8], in_values=src, imm_value=-1e30)
                    yield
                    S.op('dve', 'max', ['s2'], ['TV'], out=TV[:, h, 8:16], in_=s2[:])
                    yield
                    S.op('dve', 'max_index', ['s2', 'TV'], ['TP'], out=TP[:, h, 8:16], in_max=TV[:, h, 8:16], in_values=s2[:])
                    yield
                S.op('dve', 'tensor_tensor', ['TV'], ['TVs'], out=TVs[:], in0=TV[:], in1=TV[:, :, 0:1].to_broadcast([128, 8, 16]), op=ALU.subtract)
                yield
                S.op('act', 'activation', ['TVs'], ['Ex'], out=Ex[:], in_=TVs[:], func=AF.Exp)
                S.op('dve', 'tensor_copy', ['TP'], ['TPf'], out=TPf[:], in_=TP[:])
                yield
                S.op('dve', 'tensor_copy', ['It'], ['I1f'], out=I1f[:], in_=It[:, :, 0, :])
                yield
                S.op('dve', 'tensor_copy', ['It'], ['I2f'], out=I2f[:], in_=It[:, :, 1, :])
                yield
                S.op('dve', 'tensor_reduce', ['Ex'], ['Z'], out=Z[:], in_=Ex[:], axis=AX.X, op=ALU.add)
                yield
                S.op('dve', 'reciprocal', ['Z'], ['rZ'], out=rZ[:], in_=Z[:])
                yield
                S.op('dve', 'tensor_tensor', ['Ex', 'rZ'], ['gFt'], out=gFt[:].rearrange("p (h r) -> p h r", h=8), in0=Ex[:],
                     in1=rZ[:].unsqueeze(2).to_broadcast([128, 8, 16]), op=ALU.mult)
                yield
                S.op('dve', 'tensor_tensor', ['TPf', 'c16'], ['OH'], out=OH[:], in0=TPf[:].unsqueeze(3).to_broadcast(B4), in1=c16_b, op=ALU.subtract)
                yield
                OHf = OH[:].rearrange("p a b c -> p (a b c)")
                OH2f = OH2[:].rearrange("p a b c -> p (a b c)")
                S.op('dve', 'tensor_scalar', ['OH'], ['OH2'], out=OH2f, in0=OHf, scalar1=-8.0, scalar2=None, op0=ALU.is_gt)
                yield
                S.op('dve', 'scalar_tensor_tensor', ['OH', 'OH2'], ['OH'], out=OHf, in0=OHf, scalar=8.0, in1=OH2f, op0=ALU.is_lt, op1=ALU.mult)
                yield
                S.op('dve', 'tensor_tensor', ['OH', 'I1f'], ['OH2'], out=OH2[:], in0=OH[:], in1=I1f[:].unsqueeze(2).to_broadcast(B4), op=ALU.mult)
                yield
                S.op('dve', 'tensor_reduce', ['OH2'], ['iF'], out=iF[:].rearrange("p (h r) -> p h r", h=8), in_=OH2[:], axis=AX.X, op=ALU.add)
                yield
                S.op('dve', 'tensor_tensor', ['OH', 'iotaf'], ['OH2'], out=OH2[:], in0=OH[:], in1=iota16_b, op=ALU.mult)
                yield
                S.op('dve', 'tensor_reduce', ['OH2'], ['af'], out=af[:], in_=OH2[:], axis=AX.X, op=ALU.add)
                yield
                S.op('dve', 'scalar_tensor_tensor', ['af', 'TPf'], ['bf'], out=bf[:].rearrange("p a b -> p (a b)"),
                     in0=af[:].rearrange("p a b -> p (a b)"), scalar=-16.0, in1=TPf[:].rearrange("p a b -> p (a b)"), op0=ALU.mult, op1=ALU.add)
                yield
                S.op('dve', 'tensor_tensor', ['bf', 'iotaf'], ['OH'], out=OH[:], in0=iota16_b, in1=bf[:].unsqueeze(3).to_broadcast(B4), op=ALU.is_equal)
                yield
                S.op('dve', 'tensor_tensor', ['OH', 'I2f'], ['OH2'], out=OH2[:], in0=OH[:], in1=I2f[:].unsqueeze(2).to_broadcast(B4), op=ALU.mult)
                yield
                S.op('dve', 'tensor_reduce', ['OH2'], ['jF'], out=jF[:].rearrange("p (h r) -> p h r", h=8), in_=OH2[:], axis=AX.X, op=ALU.add)
                yield
                for k3, (src, sk, dst, dk) in enumerate(((iF, 'iF', iT, 'iT'), (jF, 'jF', jT, 'jT'), (gFt, 'gFt', gT, 'gT'))):
                    S.op('pe', 'transpose', [sk, 'identf'], ['gps0'], out=gps[0][:, k3, :], in_=src[:], identity=identf[:])
                for k3, (src, sk, dst, dk) in enumerate(((iF, 'iF', iT, 'iT'), (jF, 'jF', jT, 'jT'), (gFt, 'gFt', gT, 'gT'))):
                    S.op('act', 'copy', ['gps0'], [dk], out=dst[:, g * 128:(g + 1) * 128], in_=gps[0][:, k3, :])
                yield

        def drain(gen, n=None):
            k = 0
            for _ in gen:
                k += 1
                if n is not None and k >= n:
                    return False
            return True

        cur = prep(0, 0)
        n_yield = sum(1 for _ in cur)
        rate = n_yield / float(max(1, n_iblk - 10))
        for ti in range(ntile):
            par = ti % 2
            xk = 'xt%d' % par
            for i0 in range(min(NSLOT - 1, n_iblk)):
                wdma(i0)
            for t in range(256):
                r = t % NR
                bk = (t // 4) % 2
                S.op('dve', 'tensor_scalar', ['iotab', 'iT'], ['OI%d' % r], out=OI[r][:], in0=iotab[:], scalar1=iT[:, t:t + 1], scalar2=None,
                     op0=ALU.is_equal)
                S.op('dve', 'tensor_scalar', ['iotab', 'jT', 'gT'], ['OJ%d' % r], out=OJ[r][:], in0=iotab[:], scalar1=jT[:, t:t + 1],
                     scalar2=gT[:, t:t + 1], op0=ALU.is_equal, op1=ALU.mult)
                S.op('pe', 'matmul', ['OI%d' % r, 'OJ%d' % r], ['gps%d' % bk], gps[bk][:, t % 4, :], lhsT=OJ[r][:], rhs=OI[r][:], start=True, stop=True)
                if t % 4 == 3:
                    S.op('act', 'copy', ['gps%d' % bk], ['Gbuf'], out=Gbuf[:, t - 3:t + 1, :], in_=gps[bk][:])
            nxt = prep(ti + 1, 1 - par) if ti + 1 < ntile else None
            credit = 0.0
            stage1(0, par)
            for i in range(n_iblk):
                if i + NSLOT - 1 < n_iblk:
                    wdma(i + NSLOT - 1)
                if i + 1 < n_iblk:
                    stage1(i + 1, par)
                sl = i % NSLOT
                hv = i % 2
                S.op('act', 'activation', ['aps%d' % hv], ['hf%d' % hv], out=hf[hv][:], in_=aps[hv][:, 0:256], func=AF.Gelu)
                S.op('dve', 'tensor_tensor', ['hf%d' % hv, 'Gbuf'], ['hG%d' % hv], out=hG[hv][:], in0=hf[hv][:], in1=Gbuf[:, :, i], op=ALU.mult)
                for tg in range(2):
                    for dh in range(2):
                        S.op('pe', 'matmul', ['hG%d' % hv, 'Vb%d' % sl], ['ops%d' % (tg * 2 + dh)], ops[tg * 2 + dh][:],
                             lhsT=hG[hv][:, tg * 128:(tg + 1) * 128], rhs=Vb[sl][:, dh * 512:(dh + 1) * 512], start=(i == 0), stop=(i == n_iblk - 1))
                if nxt is not None and i >= 1:
                    credit += rate
                    k_ = int(credit)
                    credit -= k_
                    if k_ > 0 and drain(nxt, k_):
                        nxt = None
            if nxt is not None:
                drain(nxt)
            for tg in range(2):
                for dh in range(2):
                    S.op('dve', 'tensor_tensor', ['ops%d' % (tg * 2 + dh), xk], ['yo'], out=yo[:, tg, dh * 512:(dh + 1) * 512], in0=ops[tg * 2 + dh][:],
                         in1=xt[par][:, tg, dh * 512:(dh + 1) * 512], op=ALU.add)
            if final:
                for tg in range(2):
                    rmsnorm_tile(S, yo[:, tg, :], 'yo', gFin, 'gFin', xt[par][:, tg, :], xk, tmp, ssq, rstd, 'f')
                S.dma('pool', [xk], [], out=x_out[ti], in_=xt[par][:])
            else:
                S.dma('pool', ['yo'], [], out=x_out[ti], in_=yo[:])
        S.end_phase()


def mixer_a(nc, S, D, own):
    t0, nt = own
    with ExitStack() as es:
        C = Ctx(nc, es)
        sb, ps = C.sb, C.ps
        identf = sb("identf", [128, 128], F32)
        identb = sb("identb", [128, 128], BF16)
        Win = sb("Win", [128, 8, 5632], BF16)
        gB = sb("gB", [128, 1024], F32)
        tmp = sb("tmp", [128, 1024], BF16)
        ssq = sb("ssq", [128, 1], F32)
        rstd = sb("rstd", [128, 1], F32)
        xn2 = [sb(f"xn{r}", [128, 1024], BF16) for r in range(2)]
        xnT2 = [sb(f"xnT{r}", [128, 8, 128], BF16) for r in range(2)]
        t1 = sb("t1", [128, 8, 32], F32)
        t2 = sb("t2", [128, 8, 32], F32)
        NB = 2
        xt = [sb(f"xt{r}", [128, 1024], F32) for r in range(NB)]
        cs = [sb(f"cs{r}", [128, 2, 32], F32) for r in range(NB)]
        Pp = [sb(f"Pp{r}", [128, 256], F32) for r in range(NB)]
        rin = [sb(f"rin{r}", [128, 8, 64], F32) for r in range(NB)]
        rot = [sb(f"rot{r}", [128, 8, 64], F32) for r in range(NB)]
        Rq = [sb(f"Rq{r}", [128, 256], BF16) for r in range(NB)]
        Rk = [sb(f"Rk{r}", [128, 256], BF16) for r in range(NB)]
        Rv = [sb(f"Rv{r}", [128, 512], BF16) for r in range(NB)]
        Rg = [sb(f"Rg{r}", [128, 512], F32) for r in range(NB)]
        Nq = [sb(f"Nq{r}", [128, 256], BF16) for r in range(NB)]
        Nk = [sb(f"Nk{r}", [128, 256], BF16) for r in range(NB)]
        Nv = [sb(f"Nv{r}", [128, 256], BF16) for r in range(NB)]
        Gt = [sb(f"Gt{r}", [128, 3072], F32) for r in range(NB)]
        pp = [ps(f"pp{r}", [128, 512], F32) for r in range(4)]
        tps = ps("tps", [128, 8, 128], BF16)

        S.dma('sp', [], ['identf'], out=identf[:], in_=D['ident'])
        S.dma('sp', [], ['gB'], out=gB[:], in_=D['g'].partition_broadcast(128))
        w_v = D['w_in'].rearrange("(c p) n -> p c n", p=128)
        for k in range(11):
            S.dma('pool', [], ['Win%d' % k], out=Win[:, :, k * 512:(k + 1) * 512], in_=w_v[:, :, k * 512:(k + 1) * 512])
        S.op('dve', 'tensor_copy', ['identf'], ['identb'], out=identb[:], in_=identf[:])
        B3 = [128, 8, 32]
        kc = 0
        import os as _os
        for i in range(int(_os.environ.get('MA_NT', '32'))):
            b = i % NB
            sfx = str(b)
            is_own = t0 <= i < t0 + nt
            rows = slice(i * 128, (i + 1) * 128)
            xn, xnT = xn2[b], xnT2[b]
            xnk, xtk = 'xn' + sfx, 'xnT' + sfx
            S.dma('sp', [], ['xt' + sfx], out=xt[b][:], in_=D['x_full'][rows, :])
            S.dma('sp', [], ['cs' + sfx], out=cs[b][:, 0, :], in_=D['cos'][rows, :])
            S.dma('sp', [], ['cs' + sfx], out=cs[b][:, 1, :], in_=D['sin'][rows, :])
            _sub = int(_os.environ.get('MA_SUB', '9'))
            if _sub <= 2:
                S.end_phase()
                return
            rmsnorm_tile(S, xt[b][:], 'xt' + sfx, gB, 'gB', xn[:], xnk, tmp, ssq, rstd, 'a')
            if _sub <= 3:
                S.end_phase()
                return
            for c in range(8):
                S.op('pe', 'transpose', [xnk, 'identb'], ['tps'], out=tps[:, c, :], in_=xn[:, c * 128:(c + 1) * 128], identity=identb[:])
            S.op('act', 'copy', ['tps'], [xtk], out=xnT[:], in_=tps[:])
            if _sub <= 4:
                S.end_phase()
                return
            chunks = list(range(11)) if is_own else [0, 1, 2, 4]
            _lvl = int(_os.environ.get('MA_LVL', '9'))
            if _lvl == 0:
                chunks = [0]
            for ci in chunks:
                bank = pp[kc % 4]
                bk = 'pp%d' % (kc % 4)
                kc += 1
                for dc in range(8):
                    S.op('pe', 'matmul', [xtk, 'Win%d' % ci], [bk], bank[:], lhsT=xnT[:, dc, :], rhs=Win[:, dc, ci * 512:(ci + 1) * 512],
                         start=(dc == 0), stop=(dc == 7))
                lo, hi = bank[:, 0:256], bank[:, 256:512]
                rq = rin[b][:, 0:4, :].rearrange("p a b -> p (a b)")
                rk = rin[b][:, 4:8, :].rearrange("p a b -> p (a b)")
                if ci == 0:
                    S.op('act', 'copy', [bk], ['Pp' + sfx], out=Pp[b][:], in_=lo)
                    S.op('dve', 'tensor_copy', [bk], ['rin' + sfx], out=rq, in_=hi)
                elif ci == 1:
                    S.op('dve', 'tensor_copy', [bk], ['rin' + sfx], out=rk, in_=lo)
                    S.op('act', 'copy', [bk], ['Rv' + sfx], out=Rv[b][:, 0:256], in_=hi)
                elif ci == 2:
                    S.op('act', 'copy', [bk], ['Rv' + sfx], out=Rv[b][:, 256:512], in_=lo)
                    if is_own:
                        S.op('act', 'activation', [bk], ['Rg' + sfx], out=Rg[b][:, 0:256], in_=hi, func=AF.Silu)
                elif ci == 3:
                    S.op('act', 'activation', [bk], ['Rg' + sfx], out=Rg[b][:, 256:512], in_=lo, func=AF.Silu)
                    S.op('dve', 'tensor_copy', [bk], ['Nq' + sfx], out=Nq[b][:], in_=hi)
                elif ci == 4:
                    S.op('dve', 'tensor_copy', [bk], ['Nk' + sfx], out=Nk[b][:], in_=lo)
                    S.op('act', 'copy', [bk], ['Nv' + sfx], out=Nv[b][:], in_=hi)
                else:
                    S.op('act', 'activation', [bk], ['Gt' + sfx], out=Gt[b][:, (ci - 5) * 512:(ci - 4) * 512], in_=bank[:], func=AF.Sigmoid)
            if _lvl == 0:
                S.dma('pool', ['Pp' + sfx], [], out=D['Pp'][rows, :], in_=Pp[b][:])
                continue
            x1, x2 = rin[b][:, :, 0:32], rin[b][:, :, 32:64]
            cosb = cs[b][:, 0, :].unsqueeze(1).to_broadcast(B3)
            sinb = cs[b][:, 1, :].unsqueeze(1).to_broadcast(B3)
            rk_ = ['rin' + sfx, 'cs' + sfx]
            S.op('dve', 'tensor_tensor', rk_, ['t1'], out=t1[:], in0=x1, in1=cosb, op=ALU.mult)
            S.op('dve', 'tensor_tensor', rk_, ['t2'], out=t2[:], in0=x2, in1=sinb, op=ALU.mult)
            S.op('dve', 'tensor_tensor', ['t1', 't2'], ['rot' + sfx], out=rot[b][:, :, 0:32], in0=t1[:], in1=t2[:], op=ALU.subtract)
            S.op('dve', 'tensor_tensor', rk_, ['t1'], out=t1[:], in0=x1, in1=sinb, op=ALU.mult)
            S.op('dve', 'tensor_tensor', rk_, ['t2'], out=t2[:], in0=x2, in1=cosb, op=ALU.mult)
            S.op('dve', 'tensor_tensor', ['t1', 't2'], ['rot' + sfx], out=rot[b][:, :, 32:64], in0=t1[:], in1=t2[:], op=ALU.add)
            S.op('act', 'copy', ['rot' + sfx], ['Rq' + sfx], out=Rq[b][:], in_=rot[b][:, 0:4, :].rearrange("p a b -> p (a b)"))
            S.op('act', 'mul', ['rot' + sfx], ['Rk' + sfx], Rk[b][:], rot[b][:, 4:8, :].rearrange("p a b -> p (a b)"), 0.125)
            S.dma('pool', ['Pp' + sfx], [], out=D['Pp'][rows, :], in_=Pp[b][:])
            S.dma('pool', ['Rk' + sfx], [], out=D['Rk'][rows, :], in_=Rk[b][:])
            S.dma('pool', ['Rv' + sfx], [], out=D['Rv'][rows, :], in_=Rv[b][:])
            S.dma('pool', ['Nk' + sfx], [], out=D['Nk'][rows, :], in_=Nk[b][:])
            S.dma('pool', ['Nv' + sfx], [], out=D['Nv'][rows, :], in_=Nv[b][:])
            if is_own:
                S.dma('pool', ['Rq' + sfx], [], out=D['Rq'][rows, :], in_=Rq[b][:])
                S.dma('pool', ['Rg' + sfx], [], out=D['Rg'][rows, :], in_=Rg[b][:])
                S.dma('pool', ['Nq' + sfx], [], out=D['Nq'][rows, :], in_=Nq[b][:])
                S.dma('pool', ['Gt' + sfx], [], out=D['Gt'][rows, :], in_=Gt[b][:])
        S.end_phase()


def mixer_b(nc, S, D, own):
    t0, nt = own
    with ExitStack() as es:
        C = Ctx(nc, es)
        sb, ps = C.sb, C.ps
        identf = sb("identf", [128, 128], F32)
        identb = sb("identb", [128, 128], BF16)
        Wbp = sb("Wbp", [64, 4, 1024], BF16)
        Wbr = sb("Wbr", [128, 4, 1024], BF16)
        Wbn = sb("Wbn", [128, 2, 1024], BF16)
        Wout = sb("Wout", [128, 8, 1024], BF16)
        poolw = sb("poolw", [64, 4, 64], F32)
        pscale = sb("pscale", [64, 4], F32)
        amat = sb("amat", [128, 4, 5, 128], F32)
        Et = [sb(f"Et{r}", [128, 5, 4, 128], BF16) for r in range(5)]
        rstage = sb("rstage", [128, 5, 4, 128], F32)
        nmask = sb("nmask", [128, 5, 128], F32)
        dl = sb("dl", [128, 8], F32)
        lg = sb("lg", [128, 8], F32)
        pos = sb("pos", [128, 2], F32)
        zf = sb("zf", [128, 4], F32)
        zb = sb("zb", [128, 4], F32)
        dC = sb("dC", [128, 8], F32)
        dmat = sb("dmat", [128, 4, 128], F32)
        xrow = sb("xrow", [128, 2, 128], F32)
        Dt = sb("Dt", [128, 4, 128], F32)
        E1 = sb("E1", [128, 128], F32)
        E2 = sb("E2", [128, 128], F32)
        XF = sb("XF", [64, 4, 128], F32)
        XB = sb("XB", [64, 4, 128], F32)
        Sst = sb("Sst", [64, 4, 128], F32)
        Sob = [sb(f"Sob{r}", [64, 4, 128], BF16) for r in range(2)]
        kd = sb("kd", [128, 4, 64], BF16)
        NB = 2
        xt = [sb(f"xt{r}", [128, 1024], F32) for r in range(NB)]
        Gt = [sb(f"Gt{r}", [128, 3072], F32) for r in range(NB)]
        Rq = [sb(f"Rq{r}", [128, 256], BF16) for r in range(NB)]
        Rk = [sb(f"Rk{r}", [128, 256], BF16) for r in range(NB)]
        Rv = [sb(f"Rv{r}", [128, 512], BF16) for r in range(NB)]
        Rg = [sb(f"Rg{r}", [128, 512], F32) for r in range(NB)]
        SFt = [sb(f"SFt{r}", [64, 4, 128], BF16) for r in range(NB)]
        SBt = [sb(f"SBt{r}", [64, 4, 128], BF16) for r in range(NB)]
        Nq = [sb(f"Nq{r}", [128, 256], BF16) for r in range(NB)]
        Nk = [[sb(f"Nk{r}_{o}", [128, 256], BF16) for o in range(5)] for r in range(NB)]
        Nv1 = [[sb(f"Nv{r}_{o}", [128, 4, 65], BF16) for o in range(5)] for r in range(NB)]
        Pp = [[sb(f"Pp{r}_{o}", [128, 256], F32) for o in range(3)] for r in range(NB)]
        qkT = sb("qkT", [64, 8, 128], BF16)
        qfT = sb("qfT", [64, 4, 128], BF16)
        qbT = sb("qbT", [64, 4, 128], BF16)
        PD = sb("PD", [128, 4, 128], BF16)
        ysq = sb("ysq", [128, 4, 128], F32)
        ssq4 = sb("ssq4", [128, 4], F32)
        rs4 = sb("rs4", [128, 4], F32)
        yr = sb("yr", [128, 4, 128], F32)
        yrb = sb("yrb", [128, 512], BF16)
        yrT = sb("yrT", [128, 4, 128], BF16)
        NTa = sb("NTa", [64, 24, 128], BF16)
        Pm = sb("Pm", [128, 4, 128], BF16)
        PEm = sb("PEm", [128, 4, 128], BF16)
        rden = sb("rden", [128, 4], F32)
        ynb = sb("ynb", [128, 4, 64], BF16)
        ynT = sb("ynT", [128, 2, 128], BF16)
        dT = sb("dT", [64, 4, 128], F32)
        ypT = sb("ypT", [64, 4, 128], BF16)
        mg = sb("mg", [128, 1024], F32)
        tg_ = sb("tg", [128, 1024], F32)
        mgb = sb("mgb", [128, 1024], BF16)
        mT = sb("mT", [128, 8, 128], BF16)
        xo = [sb(f"xo{r}", [128, 1024], F32) for r in range(NB)]
        bank = [ps(f"bk{r}", [128, 512], F32) for r in range(8)]
        sc_ps = bank[0][:].rearrange("p (h c) -> p h c", h=4)
        y_ps = bank[1][:].rearrange("p (h c) -> p h c", h=4)
        st_ps = [bank[2][:].rearrange("p (h c) -> p h c", h=4), bank[6][:].rearrange("p (h c) -> p h c", h=4)]
        st_k = ['bk2', 'bk6']
        o_ps = bank[3][:, 0:260].rearrange("p (h c) -> p h c", h=4)
        pl_ps = bank[4][0:64, :].rearrange("p (h c) -> p h c", h=4)
        tr_ps = bank[5][:].bitcast(BF16).rearrange("p (c t) -> p c t", c=8)
        tr7_ps = bank[7][:].bitcast(BF16).rearrange("p (c t) -> p c t", c=8)

        S.dma('sp', [], ['identf'], out=identf[:], in_=D['ident'])
        S.op('dve', 'tensor_copy', ['identf'], ['identb'], out=identb[:], in_=identf[:])
        for g in range(4):
            S.dma('pool', [], ['Wbp'], out=Wbp[:, g, :], in_=D['wbp'][:, g, :])
        S.dma('pool', [], ['Wbr'], out=Wbr[:], in_=D['wbr'].rearrange("(h e) n -> e h n", e=128))
        S.dma('pool', [], ['Wbn'], out=Wbn[:], in_=D['wbn'].rearrange("(h e) n -> e h n", e=128))
        S.dma('pool', [], ['Wout'], out=Wout[:], in_=D['wout'].rearrange("(h e) n -> e h n", e=128))
        S.dma('sp', [], ['poolw'], out=poolw[:], in_=D['poolw'])
        S.dma('sp', [], ['pscale'], out=pscale[:], in_=D['pscale'])
        S.dma('sp', [], ['amat'], out=amat[:], in_=D['amat'])
        S.dma('sp', [], ['dl'], out=dl[:], in_=D['decay'].partition_broadcast(128))
        S.dma('sp', [], ['pos'], out=pos[:], in_=D['pos'])
        S.dma('sp', [], ['dmat'], out=dmat[:], in_=D['dmat'].rearrange("k p c -> p k c"))
        S.dma('sp', [], ['xrow'], out=xrow[:], in_=D['xrow'])
        for r in range(NB):
            for o in range(5):
                S.op('dve', 'memset', [], ['Nv%d_%d' % (r, o)], Nv1[r][o][:], 1.0)
        for ty in range(5):
            S.dma('sp', [], ['rstage'], out=rstage[:], in_=D['rpbT'][ty])
            S.dma('sp', [], ['nmask'], out=nmask[:], in_=D['nmask'][ty])
            S.op('act', 'activation', ['rstage'], ['rstage'], out=rstage[:], in_=rstage[:], func=AF.Exp)
            S.op('dve', 'tensor_tensor', ['rstage', 'nmask'], ['Et%d' % ty], out=Et[ty][:], in0=rstage[:],
                 in1=nmask[:].unsqueeze(2).to_broadcast([128, 5, 4, 128]), op=ALU.mult)
        S.op('act', 'activation', ['dl'], ['lg'], out=lg[:], in_=dl[:], func=AF.Exp, scale=-1.0)
        S.op('act', 'activation', ['lg'], ['lg'], out=lg[:], in_=lg[:], func=AF.Ln, bias=1.0)
        S.op('act', 'mul', ['lg'], ['lg'], lg[:], lg[:], -1.0)
        S.op('act', 'activation', ['lg', 'pos'], ['zf'], out=zf[:], in_=lg[:, 0:4], func=AF.Exp, scale=pos[:, 0:1])
        S.op('act', 'activation', ['lg', 'pos'], ['zb'], out=zb[:], in_=lg[:, 4:8], func=AF.Exp, scale=pos[:, 1:2])
        S.op('act', 'activation', ['lg'], ['dC'], out=dC[:], in_=lg[:], func=AF.Exp, scale=128.0)
        for h in range(4):
            S.op('act', 'activation', ['lg', 'dmat'], ['E1'], out=E1[:], in_=dmat[:, 0, :], func=AF.Exp, scale=lg[:, h:h + 1])
            S.op('act', 'activation', ['lg', 'dmat'], ['E2'], out=E2[:], in_=dmat[:, 2, :], func=AF.Exp, scale=lg[:, 4 + h:5 + h])
            S.op('dve', 'tensor_tensor', ['E1', 'dmat'], ['E1'], out=E1[:], in0=E1[:], in1=dmat[:, 1, :], op=ALU.mult)
            S.op('dve', 'tensor_tensor', ['E2', 'dmat'], ['E2'], out=E2[:], in0=E2[:], in1=dmat[:, 3, :], op=ALU.mult)
            S.op('dve', 'tensor_tensor', ['E1', 'E2'], ['Dt'], out=Dt[:, h, :], in0=E1[:], in1=E2[:], op=ALU.add)
            S.op('act', 'activation', ['lg', 'xrow'], ['XF'], out=XF[:, h, :], in_=xrow[0:64, 0, :], func=AF.Exp, scale=lg[0:64, h:h + 1])
            S.op('act', 'activation', ['lg', 'xrow'], ['XB'], out=XB[:, h, :], in_=xrow[0:64, 1, :], func=AF.Exp, scale=lg[0:64, 4 + h:5 + h])

        S.end_phase()
        rsb = rstage[:].rearrange("p a b c -> p (a b c)").bitcast(BF16)
        rsf = rstage[:].rearrange("p a b c -> p (a b c)")

        class _V:
            def __init__(self, ap):
                self.ap = ap

            def __getitem__(self, k):
                return self.ap if (k == slice(None)) else self.ap[k]

        Rk2 = [_V(rsb[:, 0:256]), _V(rsb[:, 256:512])]
        Rv2 = [_V(rsb[:, 512:1024]), _V(rsb[:, 1024:1536])]
        kd2 = _V(rsb[:, 1536:1792].rearrange("p (h d) -> p h d", h=4))
        Sob2 = [_V(rsb[0:64, 1792:2304].rearrange("p (h c) -> p h c", h=4)), _V(rsb[0:64, 2304:2816].rearrange("p (h c) -> p h c", h=4))]
        Sst2 = _V(rsf[0:64, 1408:1920].rearrange("p (h c) -> p h c", h=4))

        def sweep(order, zt, zk, dcol, dst, dkey, store_pred, upd_pred, St, Sk, kdt, kdk, Sobt, Sobk, Rkt, Rkk, Rvt, Rvk, bki):
            S.op('dve', 'memset', [], [Sk], St[:], 0.0)
            yield
            for cnt, n in enumerate(order):
                b = cnt % NB
                sfx = str(b)
                rows = slice(n * 128, (n + 1) * 128)
                if store_pred(n):
                    S.op('act', 'copy', [Sk], [Sobk + sfx], out=Sobt[b][:], in_=St[:])
                    S.dma('pool', [Sobk + sfx], [dkey + str(n)], out=dst[n], in_=Sobt[b][:])
                if not upd_pred(n):
                    continue
                S.dma('sp', [], [Rkk + sfx], out=Rkt[b][:], in_=D['Rk'][rows, :])
                S.dma('sp', [], [Rvk + sfx], out=Rvt[b][:], in_=D['Rv'][rows, :])
                yield
                S.op('dve', 'tensor_tensor', [Rkk + sfx, zk], [kdk], out=kdt[:], in0=Rkt[b][:].rearrange("p (h d) -> p h d", h=4),
                     in1=zt[:].unsqueeze(2).to_broadcast([128, 4, 64]), op=ALU.mult)
                yield
                for h in range(4):
                    S.op('pe', 'matmul', [kdk, Rvk + sfx], ['bk%d' % bki], bank[bki][0:64, h * 128:(h + 1) * 128], lhsT=kdt[:, h, :],
                         rhs=Rvt[b][:, h * 128:(h + 1) * 128], start=True, stop=True)
                S.op('dve', 'tensor_tensor', [Sk, 'dC'], [Sk], out=St[:], in0=St[:],
                     in1=dC[0:64, dcol:dcol + 4].unsqueeze(2).to_broadcast([64, 4, 128]), op=ALU.mult)
                yield
                S.op('dve', 'tensor_tensor', [Sk, 'bk%d' % bki], [Sk], out=St[:], in0=St[:],
                     in1=bank[bki][0:64, :].rearrange("p (h c) -> p h c", h=4), op=ALU.add)
                yield

        last = t0 + nt - 1
        sw = [sweep(list(range(0, last + 1)), zf, 'zf', 0, D['SF'], 'dSF', lambda n: n >= t0, lambda n: n < last,
                    Sst, 'Sst', kd, 'kd', Sob, 'Sob', Rk, 'Rk', Rv, 'Rv', 0),
              sweep(list(range(31, t0 - 1, -1)), zb, 'zb', 4, D['SB'], 'dSB', lambda n: n <= last, lambda n: n > t0,
                    Sst2, 'Sst2', kd2, 'kd2', Sob2, 'Sobb', Rk2, 'Rkb', Rv2, 'Rvb', 1)]
        while sw:
            for g_ in list(sw):
                try:
                    next(g_)
                except StopIteration:
                    sw.remove(g_)

        B4 = [128, 4, 128]
        pending_tail = None
        for it in range(nt):
            i = t0 + it
            b = it % NB
            sfx = str(b)
            rows = slice(i * 128, (i + 1) * 128)
            ty = {0: 1, 1: 2, 30: 3, 31: 4}.get(i, 0)
            offs = [o for o in range(NA_OMIN[ty], NA_OMIN[ty] + 5) if 0 <= i + o <= 31]
            poffs = [o for o in range(-1, 2) if 0 <= i + o <= 31]
            S.dma('sp', [], ['xt' + sfx], out=xt[b][:], in_=D['x_full'][rows, :])
            S.dma('sp', [], ['Gt' + sfx], out=Gt[b][:], in_=D['Gt'][rows, :])
            S.dma('sp', [], ['Rq' + sfx], out=Rq[b][:], in_=D['Rq'][rows, :])
            S.dma('sp', [], ['Rk' + sfx], out=Rk[b][:], in_=D['Rk'][rows, :])
            S.dma('sp', [], ['Rv' + sfx], out=Rv[b][:], in_=D['Rv'][rows, :])
            S.dma('sp', [], ['Rg' + sfx], out=Rg[b][:], in_=D['Rg'][rows, :])
            S.dma('sp', ['dSF%d' % i], ['SFt' + sfx], out=SFt[b][:], in_=D['SF'][i])
            S.dma('sp', ['dSB%d' % i], ['SBt' + sfx], out=SBt[b][:], in_=D['SB'][i])
            S.dma('sp', [], ['Nq' + sfx], out=Nq[b][:], in_=D['Nq'][rows, :])
            for oi, o in enumerate(offs):
                r2 = slice((i + o) * 128, (i + o + 1) * 128)
                S.dma('sp', [], ['Nk%s_%d' % (sfx, oi)], out=Nk[b][oi][:], in_=D['Nk'][r2, :])
                S.dma('sp', [], ['Nv%s_%d' % (sfx, oi)], out=Nv1[b][oi][:, :, 0:64], in_=D['Nv'][r2, :].rearrange("p (h d) -> p h d", h=4))
            for oi, o in enumerate(poffs):
                r2 = slice((i + o) * 128, (i + o + 1) * 128)
                S.dma('sp', [], ['Pp%s_%d' % (sfx, oi)], out=Pp[b][oi][:], in_=D['Pp'][r2, :])

            pend_rest = None
            if pending_tail is not None:
                pending_tail[0]()
                pend_rest = pending_tail[1]()
            if 'uTb' in D:
                cpt = -(-128 // nt)
                uT2 = D['uT'].rearrange("i p c e -> i p (c e)")
                for i0 in range(it * cpt, min(128, (it + 1) * cpt)):
                    S.dma('pool', [], [], out=D['uTb'][i0], in_=uT2[i0])
                    S.dma('pool', [], [], out=D['vb'][i0], in_=D['v'][i0 * 128:(i0 + 1) * 128, :])
            def gen_ret():
                for h in range(4):
                    S.op('pe', 'transpose', ['Rq' + sfx, 'identb'], ['bk5'], out=tr_ps[0:64, h, :], in_=Rq[b][:, h * 64:(h + 1) * 64], identity=identb[:])
                    S.op('pe', 'transpose', ['Rk' + sfx, 'identb'], ['bk5'], out=tr_ps[0:64, 4 + h, :], in_=Rk[b][:, h * 64:(h + 1) * 64], identity=identb[:])
                S.op('act', 'copy', ['bk5'], ['qkT'], out=qkT[:], in_=tr_ps[0:64, :, :])
                yield
                S.op('dve', 'tensor_tensor', ['qkT', 'XF'], ['qfT'], out=qfT[:], in0=qkT[:, 0:4, :], in1=XF[:], op=ALU.mult)
                S.op('dve', 'tensor_tensor', ['qkT', 'XB'], ['qbT'], out=qbT[:], in0=qkT[:, 0:4, :], in1=XB[:], op=ALU.mult)
                yield
                for h in range(4):
                    S.op('pe', 'matmul', ['qkT'], ['bk0'], sc_ps[:, h, :], lhsT=qkT[:, 4 + h, :], rhs=qkT[:, h, :], start=True, stop=True)
                yield
                S.op('dve', 'tensor_tensor', ['bk0', 'Dt'], ['PD'], out=PD[:], in0=sc_ps, in1=Dt[:], op=ALU.mult)
                yield
                for h in range(4):
                    S.op('pe', 'matmul', ['PD', 'Rv' + sfx], ['bk1'], y_ps[:, h, :], lhsT=PD[:, h, :], rhs=Rv[b][:, h * 128:(h + 1) * 128],
                         start=(h == 0), stop=False, skip_group_check=True)
                    S.op('pe', 'matmul', ['qfT', 'SFt' + sfx], ['bk1'], y_ps[:, h, :], lhsT=qfT[:, h, :], rhs=SFt[b][:, h, :],
                         start=False, stop=False, skip_group_check=True)
                    S.op('pe', 'matmul', ['qbT', 'SBt' + sfx], ['bk1'], y_ps[:, h, :], lhsT=qbT[:, h, :], rhs=SBt[b][:, h, :],
                         start=False, stop=(h == 3), skip_group_check=True)
                yield
                S.op('act', 'activation', ['bk1'], ['ysq'], out=ysq[:], in_=y_ps, func=AF.Square)
                yield
                S.op('dve', 'tensor_reduce', ['ysq'], ['ssq4'], out=ssq4[:], in_=ysq[:], axis=AX.X, op=ALU.add)
                yield
                S.op('act', 'activation', ['ssq4'], ['rs4'], out=rs4[:], in_=ssq4[:], func=AF.Sqrt, scale=1.0 / 128, bias=EPS)
                yield
                S.op('dve', 'reciprocal', ['rs4'], ['rs4'], out=rs4[:], in_=rs4[:])
                S.op('dve', 'tensor_tensor', ['bk1', 'rs4'], ['yr'], out=yr[:], in0=y_ps, in1=rs4[:].unsqueeze(2).to_broadcast(B4), op=ALU.mult)
                S.op('dve', 'tensor_tensor', ['yr', 'Rg' + sfx], ['yrb'], out=yrb[:], in0=yr[:].rearrange("p h c -> p (h c)"), in1=Rg[b][:], op=ALU.mult)
                yield
                for h in range(4):
                    S.op('pe', 'transpose', ['yrb', 'identb'], ['bk5'], out=tr_ps[:, h, :], in_=yrb[:, h * 128:(h + 1) * 128], identity=identb[:])
                S.op('act', 'copy', ['bk5'], ['yrT'], out=yrT[:], in_=tr_ps[:, 0:4, :])


                yield

            def gen_na():
                srcs = [(Nq[b], 'Nq' + sfx)] + [(Nk[b][oi], 'Nk%s_%d' % (sfx, oi)) for oi in range(len(offs))]
                for r0 in range(0, len(srcs), 2):
                    grp = srcs[r0:r0 + 2]
                    for gi, (src, sk) in enumerate(grp):
                        for h in range(4):
                            S.op('pe', 'transpose', [sk, 'identb'], ['bk7'], out=tr7_ps[0:64, gi * 4 + h, :], in_=src[:, h * 64:(h + 1) * 64], identity=identb[:])
                    n_ = 4 * len(grp)
                    S.op('act', 'copy', ['bk7'], ['NTa'], out=NTa[:, r0 * 4:r0 * 4 + n_, :], in_=tr7_ps[0:64, 0:n_, :])
                yield
                for oi, o in enumerate(offs):
                    sp_, sk_ = st_ps[oi % 2], st_k[oi % 2]
                    for h in range(4):
                        S.op('pe', 'matmul', ['NTa'], [sk_], sp_[:, h, :], lhsT=NTa[:, 4 + 4 * oi + h, :], rhs=NTa[:, h, :], start=True, stop=True)
                    S.op('act', 'activation', [sk_], ['Pm'], out=Pm[:], in_=sp_, func=AF.Exp, scale=0.125)
                    yield
                    S.op('dve', 'tensor_tensor', ['Pm', 'Et%d' % ty], ['PEm'], out=PEm[:], in0=Pm[:], in1=Et[ty][:, o - NA_OMIN[ty], :, :], op=ALU.mult)
                    yield
                    for h in range(4):
                        S.op('pe', 'matmul', ['PEm', 'Nv%s_%d' % (sfx, oi)], ['bk3'], o_ps[:, h, :], lhsT=PEm[:, h, :], rhs=Nv1[b][oi][:, h, :],
                             start=(oi == 0 and h == 0), stop=(oi == len(offs) - 1 and h == 3), skip_group_check=True)
                yield
                S.op('dve', 'reciprocal', ['bk3'], ['rden'], out=rden[:], in_=o_ps[:, :, 64:65].rearrange("p h c -> p (h c)"))
                S.op('dve', 'tensor_tensor', ['bk3', 'rden'], ['ynb'], out=ynb[:], in0=o_ps[:, :, 0:64], in1=rden[:].unsqueeze(2).to_broadcast([128, 4, 64]), op=ALU.mult)
                ynf = ynb[:].rearrange("p h d -> p (h d)")
                yield
                for c in range(2):
                    S.op('pe', 'transpose', ['ynb', 'identb'], ['bk7'], out=tr7_ps[:, c, :], in_=ynf[:, c * 128:(c + 1) * 128], identity=identb[:])
                S.op('act', 'copy', ['bk7'], ['ynT'], out=ynT[:], in_=tr7_ps[:, 0:2, :])


                yield

            def gen_pool():
                for g in range(4):
                    for oi, o in enumerate(poffs):
                        kind = {-1: 0, 1: 2}.get(o, 3 if i == 0 else (4 if i == 31 else 1))
                        S.op('pe', 'matmul', ['Pp%s_%d' % (sfx, oi), 'amat'], ['bk4'], pl_ps[:, g, :], lhsT=Pp[b][oi][:, g * 64:(g + 1) * 64],
                             rhs=amat[:, g, kind, :], start=(oi == 0), stop=(oi == len(poffs) - 1))
                yield
                S.op('act', 'copy', ['bk4'], ['dT'], out=dT[:], in_=pl_ps)
                yield
                for g in range(4):
                    S.op('pe', 'matmul', ['dT', 'poolw'], ['bk4'], pl_ps[:, g, :], lhsT=poolw[:, g, :], rhs=dT[:, g, :], start=True, stop=True)
                yield
                S.op('dve', 'tensor_tensor', ['bk4', 'pscale'], ['ypT'], out=ypT[:], in0=pl_ps, in1=pscale[:].unsqueeze(2).to_broadcast([64, 4, 128]), op=ALU.mult)


                yield

            gens = [gen_ret(), gen_na(), gen_pool()] + ([pend_rest] if pend_rest is not None else [])
            while gens:
                for g_ in list(gens):
                    try:
                        next(g_)
                    except StopIteration:
                        gens.remove(g_)
            def make_tail(b=b, sfx=sfx, it=it):
                def head():
                    def branch(nk, lhs_list, w_tile, goff, first, lastb):
                        for nh in range(2):
                            bkk = bank[6 + nh]
                            for kk in range(nk):
                                S.op('pe', 'matmul', lhs_list[1] + [w_tile[1]], ['bk%d' % (6 + nh)], bkk[:], lhsT=lhs_list[0][:, kk, :],
                                     rhs=w_tile[0][:, kk, nh * 512:(nh + 1) * 512], start=(kk == 0), stop=(kk == nk - 1))
                            cols = slice(nh * 512, (nh + 1) * 512)
                            gsl = Gt[b][:, goff + nh * 512: goff + (nh + 1) * 512]
                            if first:
                                S.op('dve', 'tensor_tensor', ['bk%d' % (6 + nh), 'Gt' + sfx], ['mg'], out=mg[:, cols], in0=bkk[:], in1=gsl, op=ALU.mult)
                            else:
                                S.op('dve', 'tensor_tensor', ['bk%d' % (6 + nh), 'Gt' + sfx], ['tg'], out=tg_[:, cols], in0=bkk[:], in1=gsl, op=ALU.mult)
                                if lastb:
                                    S.op('dve', 'tensor_tensor', ['mg', 'tg'], ['mgb'], out=mgb[:, cols], in0=mg[:, cols], in1=tg_[:, cols], op=ALU.add)
                                else:
                                    S.op('dve', 'tensor_tensor', ['mg', 'tg'], ['mg'], out=mg[:, cols], in0=mg[:, cols], in1=tg_[:, cols], op=ALU.add)
                    branch(4, (ypT, ['ypT']), (Wbp, 'Wbp'), 0, True, False)
                    branch(4, (yrT, ['yrT']), (Wbr, 'Wbr'), 1024, False, False)
                    branch(2, (ynT, ['ynT']), (Wbn, 'Wbn'), 2048, False, True)

                def rest():
                    for c in range(8):
                        S.op('pe', 'transpose', ['mgb', 'identb'], ['bk5'], out=tr_ps[:, c, :], in_=mgb[:, c * 128:(c + 1) * 128], identity=identb[:])
                    S.op('act', 'copy', ['bk5'], ['mT'], out=mT[:], in_=tr_ps)
                    yield
                    for nh in range(2):
                        bkk = bank[6 + nh]
                        for dc in range(8):
                            S.op('pe', 'matmul', ['mT', 'Wout'], ['bk%d' % (6 + nh)], bkk[:], lhsT=mT[:, dc, :], rhs=Wout[:, dc, nh * 512:(nh + 1) * 512],
                                 start=(dc == 0), stop=(dc == 7))
                        cols = slice(nh * 512, (nh + 1) * 512)
                        S.op('dve', 'tensor_tensor', ['bk%d' % (6 + nh), 'xt' + sfx], ['xo' + sfx], out=xo[b][:, cols], in0=bkk[:], in1=xt[b][:, cols], op=ALU.add)
                        yield
                    S.dma('pool', ['xo' + sfx], [], out=D['x_out'][it * 128:(it + 1) * 128, :], in_=xo[b][:])
                    yield
                return head, rest

            pending_tail = make_tail()
        if pending_tail is not None:
            pending_tail[0]()
            for _ in pending_tail[1]():
                pass
        S.end_phase()


POOL_WINDOWS = (2, 4, 8, 16)
SEQ = 4096
NA_OMIN = (-2, 0, -2, -2, -3)
NA_NOFF = (5, 4, 4, 4, 4)


def _const_tables():
    T = {}
    T['ident'] = np.eye(128, dtype=np.float32)
    T['iota'] = np.tile(np.arange(128, dtype=np.float32)[None, :], (128, 1))
    T['cst16'] = np.tile((16 * np.arange(16, dtype=np.float32) + 7.5)[None, :], (128, 1))
    half = 32
    inv = (1.0 / (np.float32(10000.0) ** np.linspace(0.0, 1.0, half, dtype=np.float32))).astype(np.float32)
    ang = (np.arange(SEQ, dtype=np.float32)[:, None] * inv[None, :]).astype(np.float32)
    T['cos'] = np.cos(ang).astype(np.float32)
    T['sin'] = np.sin(ang).astype(np.float32)
    p = np.arange(128, dtype=np.float32)
    T['pos'] = np.stack([127.0 - p, p], axis=1).astype(np.float32)
    m = p[:, None]
    c = p[None, :]
    T['dmat'] = np.stack([np.maximum(c - m, 0), (c >= m).astype(np.float32), np.maximum(m - c, 0), (m > c).astype(np.float32)]).astype(np.float32)
    xr = np.stack([p + 1.0, 128.0 - p], axis=0)
    T['xrow'] = np.tile(xr[None], (128, 1, 1)).astype(np.float32)
    A = np.zeros((128, 4, 5, 128), np.float32)
    for g, w in enumerate(POOL_WINDOWS):
        lo, hi = w // 2, w - w // 2
        for kind, base in ((1, 1024), (3, 0), (4, SEQ - 128)):
            for t in range(128):
                ta = base + t
                a_, b_ = max(ta - lo, 0), min(ta + hi, SEQ)
                cnt = float(b_ - a_)
                for s in range(a_, b_):
                    rel = s - base
                    if 0 <= rel < 128:
                        A[rel, g, kind, t] += 1.0 / cnt
                    elif rel < 0 and kind == 1:
                        A[rel + 128, g, 0, t] += 1.0 / cnt
                    elif rel >= 128 and kind == 1:
                        A[rel - 128, g, 2, t] += 1.0 / cnt
                A[t, g, kind, t] -= 1.0
    T['amat'] = A
    di = np.zeros((5, 128, 5, 128), np.int64)
    dj = np.zeros((5, 128, 5, 128), np.int64)
    mk = np.zeros((5, 128, 5, 128), np.float32)
    q = np.arange(128)
    k = np.arange(128)
    for ty, i in enumerate((5, 0, 1, 30, 31)):
        for oi in range(5):
            o = NA_OMIN[ty] + oi
            j = i + o
            if j < 0 or j > 31:
                continue
            qr = 2 * i + q // 64
            qc = q % 64
            kr = (2 * j + k // 64)[:, None]
            kcol = (k % 64)[:, None]
            rs = np.clip(qr - 4, 0, 56)[None, :]
            vr = (kr >= rs) & (kr < rs + 8)
            cst = np.clip(qc - 8, 0, 48)[None, :]
            vc = (kcol >= cst) & (kcol < cst + 16)
            di[ty, :, oi, :] = np.clip(kr - qr[None, :] + 7, 0, 14)
            dj[ty, :, oi, :] = np.clip(kcol - qc[None, :], -15, 15) + 15
            mk[ty, :, oi, :] = (vr & vc).astype(np.float32)
    T['na_di'], T['na_dj'], T['nmask'] = di, dj, mk
    return T


_TABLES = None


def tables():
    global _TABLES
    if _TABLES is None:
        _TABLES = _const_tables()
    return _TABLES


def layer_layout(l, w_in, pool_w, pool_scale, ret_decay, na_rpb, w_br_pool, w_br_ret, w_br_na, w_out,
                 peer_w_query, peer_sub_keys, peer_u, peer_v):
    T = tables()
    L = {}
    L['w_in'] = np.ascontiguousarray(w_in[l])
    L['wbp'] = np.ascontiguousarray(w_br_pool[l].reshape(4, 64, 1024).transpose(1, 0, 2))
    L['wbr'] = np.ascontiguousarray(w_br_ret[l])
    L['wbn'] = np.ascontiguousarray(w_br_na[l])
    L['wout'] = np.ascontiguousarray(w_out[l])
    L['poolw'] = np.ascontiguousarray(pool_w[l].transpose(1, 0, 2))
    L['pscale'] = np.ascontiguousarray(pool_scale[l].reshape(4, 64).T)
    L['decay'] = np.ascontiguousarray(ret_decay[l].reshape(1, 8))
    rp = na_rpb[l]
    g = rp[:, T['na_di'], T['na_dj']]
    L['rpbT'] = np.ascontiguousarray(g.transpose(1, 2, 3, 0, 4)).astype(np.float32)
    L['wq'] = np.ascontiguousarray(peer_w_query[l].reshape(8, 128, 16, 128).transpose(2, 1, 0, 3))
    L['skT'] = np.ascontiguousarray(peer_sub_keys[l].transpose(2, 0, 1))
    L['uT'] = np.ascontiguousarray(peer_u[l].reshape(128, 128, 8, 128).transpose(0, 3, 2, 1))
    L['v'] = np.ascontiguousarray(peer_v[l])
    return L


N_CORES = 4
NTOK = 4096
DEPTH = 2
_PER_LAYER = ('g_mix', 'w_in', 'wbp', 'wbr', 'wbn', 'wout', 'poolw', 'pscale', 'decay', 'rpbT', 'g_ffn', 'wq', 'skT', 'uT', 'v')
_SHAPES = dict(g_mix=[1, 1024], w_in=[1024, 5632], wbp=[64, 4, 1024], wbr=[512, 1024], wbn=[256, 1024], wout=[1024, 1024],
               poolw=[64, 4, 64], pscale=[64, 4], decay=[1, 8], rpbT=[5, 128, 5, 4, 128], g_ffn=[1, 1024], wq=[16, 128, 8, 128],
               skT=[128, 2, 128], uT=[128, 128, 8, 128], v=[16384, 1024])
_CONST = dict(ident=[128, 128], iota=[128, 128], cst16=[128, 16], cos=[4096, 32], sin=[4096, 32], pos=[128, 2], dmat=[4, 128, 128],
              xrow=[128, 2, 128], amat=[128, 4, 5, 128], nmask=[5, 128, 5, 128])


def build_program(n_iblk=128):
    nc = bass.Bass("TRN2", target_bir_lowering=False)
    dt = lambda nm, shp, ty=F32, kind="ExternalInput": nc.dram_tensor(nm, list(shp), ty, kind=kind).ap()
    x = dt("x", [NTOK, 1024])
    y = dt("y", [NTOK, 1024], F32, "ExternalOutput")
    gfin = dt("gfin", [1, 1024])
    Cn = {k: dt(k, s) for k, s in _CONST.items()}
    W = [{k: dt(f"{k}_{l}", _SHAPES[k]) for k in _PER_LAYER} for l in range(DEPTH)]
    I = "Internal"
    Sc = dict(Pp=dt("s_Pp", [4096, 256], F32, I), Rq=dt("s_Rq", [4096, 256], BF16, I), Rk=dt("s_Rk", [4096, 256], BF16, I),
              Rv=dt("s_Rv", [4096, 512], BF16, I), Rg=dt("s_Rg", [4096, 512], F32, I), Nq=dt("s_Nq", [4096, 256], BF16, I),
              Nk=dt("s_Nk", [4096, 256], BF16, I), Nv=dt("s_Nv", [4096, 256], BF16, I), Gt=dt("s_Gt", [4096, 3072], F32, I),
              SF=dt("s_SF", [32, 64, 4, 128], BF16, I), SB=dt("s_SB", [32, 64, 4, 128], BF16, I))
    x1 = dt("s_x1", [NTOK, 1024], F32, I)
    uTb = dt("s_uTb", [128, 128, 1024], BF16, I)
    vb = dt("s_vb", [128, 128, 1024], BF16, I)
    x2 = dt("s_x2", [NTOK, 1024], F32, I)
    own = (0, NTOK // 128)
    with ExitStack() as es:
        S = Sched(nc, es)
        cur = x
        for l in range(DEPTH):
            D = dict(Cn)
            D.update(Sc)
            D.update(W[l])
            D.update(x_full=cur, g=W[l]['g_mix'], x_out=x1, uTb=uTb, vb=vb)
            mixer_a(nc, S, D, own)
            mixer_b(nc, S, D, own)
            last = (l == DEPTH - 1)
            P = dict(Cn)
            P.update(W[l])
            P.update(x_in=x1, x_out=(y if last else x2), g=W[l]['g_ffn'], gfin=gfin, uTb=uTb, vb=vb, preconverted=True)
            peer_phase(nc, S, P, NTOK, n_iblk=n_iblk, final=last)
            cur = x2
    return nc


_NC_CACHE = {}


def kernel(x, norm_mix, w_in, pool_w, pool_scale, ret_decay, na_rpb, w_br_pool, w_br_ret, w_br_na, w_out, norm_ffn,
           peer_w_query, peer_sub_keys, peer_u, peer_v, norm_final):
    f = lambda a: np.ascontiguousarray(np.asarray(a), dtype=np.float32)
    x = f(x)
    T = tables()
    shared = {k: np.ascontiguousarray(T[k]) for k in _CONST}
    shared['gfin'] = f(norm_final).reshape(1, 1024)
    args = [f(a) for a in (w_in, pool_w, pool_scale, ret_decay, na_rpb, w_br_pool, w_br_ret, w_br_na, w_out,
                           peer_w_query, peer_sub_keys, peer_u, peer_v)]
    nm, nf = f(norm_mix), f(norm_ffn)
    for l in range(DEPTH):
        L = layer_layout(l, *args)
        L['g_mix'] = nm[l].reshape(1, 1024)
        L['g_ffn'] = nf[l].reshape(1, 1024)
        for k in _PER_LAYER:
            shared[f"{k}_{l}"] = L[k]
    if 'nc' not in _NC_CACHE:
        _NC_CACHE['nc'] = build_program()
    nc = _NC_CACHE['nc']
    in_maps = []
    for c in range(N_CORES):
        m = dict(shared)
        m['x'] = np.ascontiguousarray(x[c])
        in_maps.append(m)
    res = run_bass_kernel_spmd(nc, in_maps, core_ids=list(range(N_CORES)))
    out = np.stack([np.asarray(res.results[c]['y'], dtype=np.float32) for c in range(N_CORES)], axis=0)
    return out
```

```python
import numpy as np
from contextlib import ExitStack
import concourse.bass as bass
import concourse.mybir as mybir
from concourse.bass_utils import run_bass_kernel_spmd

F32 = mybir.dt.float32
BF16 = mybir.dt.bfloat16
U32 = mybir.dt.uint32
AF = mybir.ActivationFunctionType
ALU = mybir.AluOpType
AX = mybir.AxisListType

import re as _re
_PSUM_KEY = _re.compile(r"^(bk|pp|gps|aps|ops|tps)\d*$")
EPOCH = 20000
NDMA = 16
EPS = 1e-6


class _Eng:
    def __init__(self, name, strict):
        self.name = name
        self.strict = strict
        self.count = 0
        self.sems = []
        self.known = {}
        self.dsems = []
        self.dcount = []
        self.dnext = 0


class Sched:
    def __init__(self, nc, es):
        self.nc = nc
        self.es = es
        self.E = {
            'pe': _Eng('pe', False),
            'act': _Eng('act', True),
            'dve': _Eng('dve', True),
            'pool': _Eng('pool', True),
            'sp': _Eng('sp', False),
        }
        self.lastw = {}
        self.readers = {}
        self.nsem = 0
        self.prog = {k: [] for k in self.E}

    def _newsem(self, nm):
        self.nsem += 1
        return self.es.enter_context(self.nc.semaphore(f"{nm}_{self.nsem}"))

    def _cur_sem(self, e):
        ep = e.count // EPOCH
        while len(e.sems) <= ep:
            e.sems.append(self._newsem(e.name))
        return e.sems[ep]

    def _wait(self, e, dep):
        sem, val, src = dep
        if src is e and not e.strict:
            return
        k = id(sem)
        if e.known.get(k, 0) >= val:
            return
        self.prog[e.name].append(('w', sem, val))
        e.known[k] = val

    def _deps(self, reads, writes):
        deps = []
        for b in reads:
            if b in self.lastw:
                deps.append(self.lastw[b])
        for b in writes:
            if b in self.lastw:
                deps.append(self.lastw[b])
            deps.extend(self.readers.get(b, []))
        return deps

    def _commit(self, dep, reads, writes):
        for b in reads:
            self.readers.setdefault(b, []).append(dep)
        for b in writes:
            self.lastw[b] = dep
            self.readers[b] = []

    def op(self, eng, meth, reads, writes, *a, **kw):
        fn = lambda h: getattr(h, meth)(*a, **kw)
        e = self.E[eng]
        px = [b for b in reads if _PSUM_KEY.match(b)]
        if px:
            reads = [b for b in reads if b not in px]
            writes = list(writes) + px
        for d in self._deps(reads, writes):
            self._wait(e, d)
        sem = self._cur_sem(e)
        val = e.count % EPOCH + 1
        self.prog[e.name].append(('i', fn, sem, 1))
        e.count += 1
        self._commit((sem, val, e), reads, writes)

    def dma(self, eng, reads, writes, meth='dma_start', **kw):
        fn = lambda h: getattr(h, meth)(**kw)
        e = self.E[eng]
        if not e.dsems:
            e.dsems = [self._newsem(e.name + "d") for _ in range(NDMA)]
            e.dcount = [0] * NDMA
        s = e.dnext
        e.dnext = (e.dnext + 1) % NDMA
        sem = e.dsems[s]
        if e.dcount[s] > 0:
            self._wait(e, (sem, 16 * e.dcount[s], None))
        for d in self._deps(reads, writes):
            self._wait(e, d)
        e.dcount[s] += 1
        self.prog[e.name].append(('i', fn, sem, 16))
        self._commit((sem, 16 * e.dcount[s], None), reads, writes)

    def end_phase(self):
        e = self.E['sp']
        for o in self.E.values():
            if o.count > 0:
                sem = o.sems[(o.count - 1) // EPOCH]
                self._wait(e, (sem, (o.count - 1) % EPOCH + 1, None))
            for s, c in enumerate(o.dcount):
                if c > 0:
                    self._wait(e, (o.dsems[s], 16 * c, None))
        with self.nc.Block() as blk:
            def replay(name):
                def f(h):
                    for it in self.prog[name]:
                        if it[0] == 'w':
                            h.wait_ge(it[1], it[2])
                        else:
                            it[1](h).then_inc(it[2], it[3])
                return f
            blk.sync(replay('sp'))
            blk.scalar(replay('act'))
            blk.vector(replay('dve'))
            blk.gpsimd(replay('pool'))
            blk.tensor(replay('pe'))
        self.prog = {k: [] for k in self.E}
        self.lastw = {}
        self.readers = {}


class Ctx:
    n = 0

    def __init__(self, nc, es):
        self.nc = nc
        self.es = es

    def sb(self, nm, shp, dt):
        Ctx.n += 1
        return self.es.enter_context(self.nc.sbuf_tensor(f"{nm}_{Ctx.n}", list(shp), dt))

    def ps(self, nm, shp, dt):
        Ctx.n += 1
        return self.es.enter_context(self.nc.psum_tensor(f"{nm}_{Ctx.n}", list(shp), dt))


def rmsnorm_tile(S, x_ap, x_key, g_tile, g_key, out_ap, out_key, tmp, ssq, rstd, pfx):
    S.op('act', 'activation', [x_key], [pfx + 'tmp', pfx + 'ssq'], out=tmp[:], in_=x_ap, func=AF.Square, accum_out=ssq[:])
    S.op('act', 'activation', [pfx + 'ssq'], [pfx + 'rstd'], out=rstd[:], in_=ssq[:], func=AF.Sqrt, scale=1.0 / 1024, bias=EPS)
    S.op('dve', 'reciprocal', [pfx + 'rstd'], [pfx + 'rstd'], out=rstd[:], in_=rstd[:])
    S.op('dve', 'scalar_tensor_tensor', [x_key, pfx + 'rstd', g_key], [out_key], out=out_ap, in0=x_ap, scalar=rstd[:, 0:1],
         in1=g_tile[:], op0=ALU.mult, op1=ALU.mult)


def peer_phase(nc, S, D, NT, n_iblk=128, final=False):
    with ExitStack() as es:
        C = Ctx(nc, es)
        sb, ps = C.sb, C.ps
        identf = sb("identf", [128, 128], F32)
        identb = sb("identb", [128, 128], BF16)
        iotaf = sb("iotaf", [128, 128], F32)
        iotab = sb("iotab", [128, 128], BF16)
        c16 = sb("c16", [128, 16], F32)
        skT = sb("skT", [128, 2, 128], F32)
        gB = sb("gB", [128, 1024], F32)
        gFin = sb("gFin", [128, 1024], F32) if final else None
        Gbuf = sb("Gbuf", [128, 256, 128], BF16)
        xt = [sb(f"xt{r}", [128, 2, 1024], F32) for r in range(2)]
        hnT = [sb(f"hnT{r}", [128, 8, 256], BF16) for r in range(2)]
        Wqc = [sb(f"Wqc{r}", [128, 8, 128], BF16) for r in range(2)]
        tmp = sb("tmp", [128, 1024], BF16)
        ssq = sb("ssq", [128, 1], F32)
        rstd = sb("rstd", [128, 1], F32)
        hn = sb("hn", [128, 1024], BF16)
        qc = [sb(f"qc{r}", [128, 256], F32) for r in range(2)]
        sall = sb("sall", [128, 2, 16, 128], F32)
        s2 = sb("s2", [128, 256], F32)
        Vt = sb("Vt", [128, 8, 2, 16], F32)
        It = sb("It", [128, 8, 2, 16], U32)
        cand = sb("cand", [128, 8, 256], F32)
        TV = sb("TV", [128, 8, 16], F32)
        TP = sb("TP", [128, 8, 16], U32)
        TPf = sb("TPf", [128, 8, 16], F32)
        TVs = sb("TVs", [128, 8, 16], F32)
        Ex = sb("Ex", [128, 8, 16], F32)
        Z = sb("Z", [128, 8], F32)
        rZ = sb("rZ", [128, 8], F32)
        I1f = sb("I1f", [128, 8, 16], F32)
        I2f = sb("I2f", [128, 8, 16], F32)
        OH = sb("OH", [128, 8, 16, 16], F32)
        OH2 = sb("OH2", [128, 8, 16, 16], BF16)
        af = sb("af", [128, 8, 16], F32)
        bf = sb("bf", [128, 8, 16], F32)
        iF = sb("iF", [128, 128], F32)
        jF = sb("jF", [128, 128], F32)
        gFt = sb("gFt", [128, 128], F32)
        iT = sb("iT", [128, 256], F32)
        jT = sb("jT", [128, 256], F32)
        gT = sb("gT", [128, 256], F32)
        NR = 8
        OI = [sb(f"OI{r}", [128, 128], BF16) for r in range(NR)]
        OJ = [sb(f"OJ{r}", [128, 128], BF16) for r in range(NR)]
        NSLOT = 5
        UTb = [sb(f"UTb{r}", [128, 8, 128], BF16) for r in range(NSLOT)]
        Vb = [sb(f"Vb{r}", [128, 1024], BF16) for r in range(NSLOT)]
        hf = [sb(f"hf{r}", [128, 256], F32) for r in range(2)]
        hG = [sb(f"hG{r}", [128, 256], BF16) for r in range(2)]
        yo = sb("yo", [128, 2, 1024], F32)
        ops = [ps(f"ops{r}", [128, 512], F32) for r in range(4)]
        aps = [ps(f"aps{r}", [128, 512], F32) for r in range(2)]
        gps = [ps(f"gps{r}", [128, 4, 128], F32) for r in range(2)]
        tps = gps[1][:].rearrange("p a b -> p (a b)").bitcast(BF16).rearrange("p (c t) -> p c t", c=8)

        S.dma('sp', [], ['identf'], out=identf[:], in_=D['ident'])
        S.dma('sp', [], ['iotaf'], out=iotaf[:], in_=D['iota'])
        S.dma('sp', [], ['c16'], out=c16[:], in_=D['cst16'])
        S.dma('sp', [], ['skT'], out=skT[:], in_=D['skT'])
        S.dma('sp', [], ['gB'], out=gB[:], in_=D['g'].partition_broadcast(128))
        if final:
            S.dma('sp', [], ['gFin'], out=gFin[:], in_=D['gfin'].partition_broadcast(128))
        S.op('dve', 'tensor_copy', ['identf'], ['identb'], out=identb[:], in_=identf[:])
        S.op('dve', 'tensor_copy', ['iotaf'], ['iotab'], out=iotab[:], in_=iotaf[:])

        ntile = NT // 256
        x_in = D['x_in'].rearrange("(t g p) d -> t p g d", g=2, p=128)
        x_out = D['x_out'].rearrange("(t g p) d -> t p g d", g=2, p=128)
        iota16_b = iotaf[:, 0:16].unsqueeze(1).unsqueeze(1).to_broadcast([128, 8, 16, 16])
        c16_b = c16[:].unsqueeze(1).unsqueeze(1).to_broadcast([128, 8, 16, 16])
        B4 = [128, 8, 16, 16]

        if not D.get('preconverted', False):
            uT2 = D['uT'].rearrange("i p c e -> i p (c e)")
            for i0 in range(128):
                S.dma('pool', [], ['cvU%d' % i0], out=D['uTb'][i0], in_=uT2[i0])
                S.dma('pool', [], ['cvV%d' % i0], out=D['vb'][i0], in_=D['v'][i0 * 128:(i0 + 1) * 128, :])

        def wdma(i):
            sl = i % NSLOT
            S.dma('sp', ['cvU%d' % i], ['UTb%d' % sl], out=UTb[sl][:].rearrange("p c e -> p (c e)"), in_=D['uTb'][i])
            S.dma('sp', ['cvV%d' % i], ['Vb%d' % sl], out=Vb[sl][:], in_=D['vb'][i])

        def stage1(i, par):
            sl = i % NSLOT
            hv = i % 2
            for dc in range(8):
                S.op('pe', 'matmul', ['UTb%d' % sl, 'hnT%d' % par], ['aps%d' % hv], aps[hv][:, 0:256], lhsT=UTb[sl][:, dc, :],
                     rhs=hnT[par][:, dc, :], start=(dc == 0), stop=(dc == 7))

        def prep(ti, par):
            xk, hk = 'xt%d' % par, 'hnT%d' % par
            S.dma('sp', [], [xk], out=xt[par][:], in_=x_in[ti])
            yield
            for g in range(2):
                rmsnorm_tile(S, xt[par][:, g, :], xk, gB, 'gB', hn[:], 'hn', tmp, ssq, rstd, 'p')
                yield
                for c in range(8):
                    S.op('pe', 'transpose', ['hn', 'identb'], ['gps1'], out=tps[:, c, :], in_=hn[:, c * 128:(c + 1) * 128], identity=identb[:])
                S.op('act', 'copy', ['gps1'], [hk], out=hnT[par][:, :, g * 128:(g + 1) * 128], in_=tps)
                yield
            S.dma('pool', [], ['Wqc0'], out=Wqc[0][:], in_=D['wq'][0])
            for c in range(16):
                gp = gps[c % 2]
                gk = 'gps%d' % (c % 2)
                qk = 'qc%d' % (c % 2)
                wk = 'Wqc%d' % (c % 2)
                if c + 1 < 16:
                    S.dma('pool', [], ['Wqc%d' % ((c + 1) % 2)], out=Wqc[(c + 1) % 2][:], in_=D['wq'][c + 1])
                qv = gp[:, 0:2, :].rearrange("p a b -> p (a b)")
                for dc in range(8):
                    S.op('pe', 'matmul', [hk, wk], [gk], qv, lhsT=Wqc[c % 2][:, dc, :], rhs=hnT[par][:, dc, :],
                         start=(dc == 0), stop=(dc == 7))
                S.op('act', 'copy', [gk], [qk], out=qc[c % 2][:], in_=qv)
                yield
                for g in range(2):
                    S.op('pe', 'matmul', [qk, 'skT'], [gk], gp[:, 2 + g, :], lhsT=qc[c % 2][:, g * 128:(g + 1) * 128], rhs=skT[:, c % 2, :],
                         start=True, stop=True)
                S.op('act', 'copy', [gk], ['sall'], out=sall[:, :, c, :], in_=gp[:, 2:4, :])
                yield
            for g in range(2):
                for c in range(16):
                    h, p = divmod(c, 2)
                    src = sall[:, g, c, :]
                    S.op('dve', 'max', ['sall'], ['Vt'], out=Vt[:, h, p, 0:8], in_=src)
                    yield
                    S.op('dve', 'max_index', ['sall', 'Vt'], ['It'], out=It[:, h, p, 0:8], in_max=Vt[:, h, p, 0:8], in_values=src)
                    yield
                    S.op('dve', 'match_replace', ['sall', 'Vt'], ['s2'], out=s2[:, 0:128], in_to_replace=Vt[:, h, p, 0:8], in_values=src, imm_value=-1e30)
                    yield
                    S.op('dve', 'max', ['s2'], ['Vt'], out=Vt[:, h, p, 8:16], in_=s2[:, 0:128])
                    yield
                    S.op('dve', 'max_index', ['s2', 'Vt'], ['It'], out=It[:, h, p, 8:16], in_max=Vt[:, h, p, 8:16], in_values=s2[:, 0:128])
                    yield
                S.op('dve', 'tensor_tensor', ['Vt'], ['cand'], out=cand[:].rearrange("p h (a b) -> p h a b", a=16),
                     in0=Vt[:, :, 0, :].unsqueeze(3).to_broadcast(B4), in1=Vt[:, :, 1, :].unsqueeze(2).to_broadcast(B4), op=ALU.add)
                yield
                for h in range(8):
                    src = cand[:, h, :]
                    S.op('dve', 'max', ['cand'], ['TV'], out=TV[:, h, 0:8], in_=src)
                    yield
                    S.op('dve', 'max_index', ['cand', 'TV'], ['TP'], out=TP[:, h, 0:8], in_max=TV[:, h, 0:8], in_values=src)
                    yield
                    S.op('dve', 'match_replace', ['cand', 'TV'], ['s2'], out=s2[:], in_to_replace=TV[:, h, 0:8], in_values=src, imm_value=-1e30)
                    yield
                    S.op('dve', 'max', ['s2'], ['TV'], out=TV[:, h, 8:16], in_=s2[:])
                    yield
                    S.op('dve', 'max_index', ['s2', 'TV'], ['TP'], out=TP[:, h, 8:16], in_max=TV[:, h, 8:16], in_values=s2[:])
                    yield
                S.op('dve', 'tensor_tensor', ['TV'], ['TVs'], out=TVs[:], in0=TV[:], in1=TV[:, :, 0:1].to_broadcast([128, 8, 16]), op=ALU.subtract)
                yield
                S.op('act', 'activation', ['TVs'], ['Ex'], out=Ex[:], in_=TVs[:], func=AF.Exp)
                S.op('act', 'copy', ['TP'], ['TPf'], out=TPf[:], in_=TP[:])
                yield
                S.op('act', 'copy', ['It'], ['I1f'], out=I1f[:], in_=It[:, :, 0, :])
                yield
                S.op('act', 'copy', ['It'], ['I2f'], out=I2f[:], in_=It[:, :, 1, :])
                yield
                S.op('dve', 'tensor_reduce', ['Ex'], ['Z'], out=Z[:], in_=Ex[:], axis=AX.X, op=ALU.add)
                yield
                S.op('dve', 'reciprocal', ['Z'], ['rZ'], out=rZ[:], in_=Z[:])
                yield
                S.op('dve', 'tensor_tensor', ['Ex', 'rZ'], ['gFt'], out=gFt[:].rearrange("p (h r) -> p h r", h=8), in0=Ex[:],
                     in1=rZ[:].unsqueeze(2).to_broadcast([128, 8, 16]), op=ALU.mult)
                yield
                S.op('dve', 'tensor_tensor', ['TPf', 'c16'], ['OH'], out=OH[:], in0=TPf[:].unsqueeze(3).to_broadcast(B4), in1=c16_b, op=ALU.subtract)
                yield
                OHf = OH[:].rearrange("p a b c -> p (a b c)")
                OH2f = OH2[:].rearrange("p a b c -> p (a b c)")
                S.op('dve', 'tensor_scalar', ['OH'], ['OH2'], out=OH2f, in0=OHf, scalar1=-8.0, scalar2=None, op0=ALU.is_gt)
                yield
                S.op('dve', 'scalar_tensor_tensor', ['OH', 'OH2'], ['OH'], out=OHf, in0=OHf, scalar=8.0, in1=OH2f, op0=ALU.is_lt, op1=ALU.mult)
                yield
                S.op('dve', 'tensor_tensor', ['OH', 'I1f'], ['OH2'], out=OH2[:], in0=OH[:], in1=I1f[:].unsqueeze(2).to_broadcast(B4), op=ALU.mult)
                yield
                S.op('dve', 'tensor_reduce', ['OH2'], ['iF'], out=iF[:].rearrange("p (h r) -> p h r", h=8), in_=OH2[:], axis=AX.X, op=ALU.add)
                yield
                S.op('dve', 'tensor_tensor', ['OH', 'iotaf'], ['OH2'], out=OH2[:], in0=OH[:], in1=iota16_b, op=ALU.mult)
                yield
                S.op('dve', 'tensor_reduce', ['OH2'], ['af'], out=af[:], in_=OH2[:], axis=AX.X, op=ALU.add)
                yield
                S.op('dve', 'scalar_tensor_tensor', ['af', 'TPf'], ['bf'], out=bf[:].rearrange("p a b -> p (a b)"),
                     in0=af[:].rearrange("p a b -> p (a b)"), scalar=-16.0, in1=TPf[:].rearrange("p a b -> p (a b)"), op0=ALU.mult, op1=ALU.add)
                yield
                S.op('dve', 'tensor_tensor', ['bf', 'iotaf'], ['OH'], out=OH[:], in0=iota16_b, in1=bf[:].unsqueeze(3).to_broadcast(B4), op=ALU.is_equal)
                yield
                S.op('dve', 'tensor_tensor', ['OH', 'I2f'], ['OH2'], out=OH2[:], in0=OH[:], in1=I2f[:].unsqueeze(2).to_broadcast(B4), op=ALU.mult)
                yield
                S.op('dve', 'tensor_reduce', ['OH2'], ['jF'], out=jF[:].rearrange("p (h r) -> p h r", h=8), in_=OH2[:], axis=AX.X, op=ALU.add)
                yield
                for k3, (src, sk, dst, dk) in enumerate(((iF, 'iF', iT, 'iT'), (jF, 'jF', jT, 'jT'), (gFt, 'gFt', gT, 'gT'))):
                    S.op('pe', 'transpose', [sk, 'identf'], ['gps0'], out=gps[0][:, k3, :], in_=src[:], identity=identf[:])
                for k3, (src, sk, dst, dk) in enumerate(((iF, 'iF', iT, 'iT'), (jF, 'jF', jT, 'jT'), (gFt, 'gFt', gT, 'gT'))):
                    S.op('act', 'copy', ['gps0'], [dk], out=dst[:, g * 128:(g + 1) * 128], in_=gps[0][:, k3, :])
                yield

        def drain(gen, n=None):
            k = 0
            for _ in gen:
                k += 1
                if n is not None and k >= n:
                    return False
            return True

        cur = prep(0, 0)
        n_yield = sum(1 for _ in cur)
        rate = n_yield / float(max(1, n_iblk - 10))
        for ti in range(ntile):
            par = ti % 2
            xk = 'xt%d' % par
            for i0 in range(min(NSLOT - 1, n_iblk)):
                wdma(i0)
            for t in range(256):
                r = t % NR
                bk = (t // 4) % 2
                S.op('dve', 'tensor_scalar', ['iotab', 'iT'], ['OI%d' % r], out=OI[r][:], in0=iotab[:], scalar1=iT[:, t:t + 1], scalar2=None,
                     op0=ALU.is_equal)
                S.op('dve', 'tensor_scalar', ['iotab', 'jT', 'gT'], ['OJ%d' % r], out=OJ[r][:], in0=iotab[:], scalar1=jT[:, t:t + 1],
                     scalar2=gT[:, t:t + 1], op0=ALU.is_equal, op1=ALU.mult)
                S.op('pe', 'matmul', ['OI%d' % r, 'OJ%d' % r], ['gps%d' % bk], gps[bk][:, t % 4, :], lhsT=OJ[r][:], rhs=OI[r][:], start=True, stop=True)
                if t % 4 == 3:
                    S.op('act', 'copy', ['gps%d' % bk], ['Gbuf'], out=Gbuf[:, t - 3:t + 1, :], in_=gps[bk][:])
            nxt = prep(ti + 1, 1 - par) if ti + 1 < ntile else None
            credit = 0.0
            stage1(0, par)
            for i in range(n_iblk):
                if i + NSLOT - 1 < n_iblk:
                    wdma(i + NSLOT - 1)
                if i + 1 < n_iblk:
                    stage1(i + 1, par)
                sl = i % NSLOT
                hv = i % 2
                S.op('act', 'activation', ['aps%d' % hv], ['hf%d' % hv], out=hf[hv][:], in_=aps[hv][:, 0:256], func=AF.Gelu)
                S.op('dve', 'tensor_tensor', ['hf%d' % hv, 'Gbuf'], ['hG%d' % hv], out=hG[hv][:], in0=hf[hv][:], in1=Gbuf[:, :, i], op=ALU.mult)
                for tg in range(2):
                    for dh in range(2):
                        S.op('pe', 'matmul', ['hG%d' % hv, 'Vb%d' % sl], ['ops%d' % (tg * 2 + dh)], ops[tg * 2 + dh][:],
                             lhsT=hG[hv][:, tg * 128:(tg + 1) * 128], rhs=Vb[sl][:, dh * 512:(dh + 1) * 512], start=(i == 0), stop=(i == n_iblk - 1))
                if nxt is not None and i >= 1:
                    credit += rate
                    k_ = int(credit)
                    credit -= k_
                    if k_ > 0 and drain(nxt, k_):
                        nxt = None
            if nxt is not None:
                drain(nxt)
            for tg in range(2):
                for dh in range(2):
                    S.op('dve', 'tensor_tensor', ['ops%d' % (tg * 2 + dh), xk], ['yo'], out=yo[:, tg, dh * 512:(dh + 1) * 512], in0=ops[tg * 2 + dh][:],
                         in1=xt[par][:, tg, dh * 512:(dh + 1) * 512], op=ALU.add)
            if final:
                for tg in range(2):
                    rmsnorm_tile(S, yo[:, tg, :], 'yo', gFin, 'gFin', xt[par][:, tg, :], xk, tmp, ssq, rstd, 'f')
                S.dma('pool', [xk], [], out=x_out[ti], in_=xt[par][:])
            else:
                S.dma('pool', ['yo'], [], out=x_out[ti], in_=yo[:])
        S.end_phase()


def mixer_a(nc, S, D, own):
    t0, nt = own
    with ExitStack() as es:
        C = Ctx(nc, es)
        sb, ps = C.sb, C.ps
        identf = sb("identf", [128, 128], F32)
        identb = sb("identb", [128, 128], BF16)
        Win = sb("Win", [128, 8, 5632], BF16)
        gB = sb("gB", [128, 1024], F32)
        tmp = sb("tmp", [128, 1024], BF16)
        ssq = sb("ssq", [128, 1], F32)
        rstd = sb("rstd", [128, 1], F32)
        xn2 = [sb(f"xn{r}", [128, 1024], BF16) for r in range(2)]
        xnT2 = [sb(f"xnT{r}", [128, 8, 128], BF16) for r in range(2)]
        t1 = sb("t1", [128, 8, 32], F32)
        t2 = sb("t2", [128, 8, 32], F32)
        NB = 2
        xt = [sb(f"xt{r}", [128, 1024], F32) for r in range(NB)]
        cs = [sb(f"cs{r}", [128, 2, 32], F32) for r in range(NB)]
        Pp = [sb(f"Pp{r}", [128, 256], F32) for r in range(NB)]
        rin = [sb(f"rin{r}", [128, 8, 64], F32) for r in range(NB)]
        rot = [sb(f"rot{r}", [128, 8, 64], F32) for r in range(NB)]
        Rq = [sb(f"Rq{r}", [128, 256], BF16) for r in range(NB)]
        Rk = [sb(f"Rk{r}", [128, 256], BF16) for r in range(NB)]
        Rv = [sb(f"Rv{r}", [128, 512], BF16) for r in range(NB)]
        Rg = [sb(f"Rg{r}", [128, 512], F32) for r in range(NB)]
        Nq = [sb(f"Nq{r}", [128, 256], BF16) for r in range(NB)]
        Nk = [sb(f"Nk{r}", [128, 256], BF16) for r in range(NB)]
        Nv = [sb(f"Nv{r}", [128, 256], BF16) for r in range(NB)]
        Gt = [sb(f"Gt{r}", [128, 3072], F32) for r in range(NB)]
        pp = [ps(f"pp{r}", [128, 512], F32) for r in range(4)]
        tps = ps("tps", [128, 8, 128], BF16)

        S.dma('sp', [], ['identf'], out=identf[:], in_=D['ident'])
        S.dma('sp', [], ['gB'], out=gB[:], in_=D['g'].partition_broadcast(128))
        w_v = D['w_in'].rearrange("(c p) n -> p c n", p=128)
        for k in range(11):
            S.dma('pool', [], ['Win%d' % k], out=Win[:, :, k * 512:(k + 1) * 512], in_=w_v[:, :, k * 512:(k + 1) * 512])
        S.op('dve', 'tensor_copy', ['identf'], ['identb'], out=identb[:], in_=identf[:])
        B3 = [128, 8, 32]
        kc = 0
        import os as _os
        for i in range(int(_os.environ.get('MA_NT', '32'))):
            b = i % NB
            sfx = str(b)
            is_own = t0 <= i < t0 + nt
            rows = slice(i * 128, (i + 1) * 128)
            xn, xnT = xn2[b], xnT2[b]
            xnk, xtk = 'xn' + sfx, 'xnT' + sfx
            S.dma('sp', [], ['xt' + sfx], out=xt[b][:], in_=D['x_full'][rows, :])
            S.dma('sp', [], ['cs' + sfx], out=cs[b][:, 0, :], in_=D['cos'][rows, :])
            S.dma('sp', [], ['cs' + sfx], out=cs[b][:, 1, :], in_=D['sin'][rows, :])
            _sub = int(_os.environ.get('MA_SUB', '9'))
            if _sub <= 2:
                S.end_phase()
                return
            rmsnorm_tile(S, xt[b][:], 'xt' + sfx, gB, 'gB', xn[:], xnk, tmp, ssq, rstd, 'a')
            if _sub <= 3:
                S.end_phase()
                return
            for c in range(8):
                S.op('pe', 'transpose', [xnk, 'identb'], ['tps'], out=tps[:, c, :], in_=xn[:, c * 128:(c + 1) * 128], identity=identb[:])
            S.op('act', 'copy', ['tps'], [xtk], out=xnT[:], in_=tps[:])
            if _sub <= 4:
                S.end_phase()
                return
            chunks = list(range(11)) if is_own else [0, 1, 2, 4]
            _lvl = int(_os.environ.get('MA_LVL', '9'))
            if _lvl == 0:
                chunks = [0]
            for ci in chunks:
                bank = pp[kc % 4]
                bk = 'pp%d' % (kc % 4)
                kc += 1
                for dc in range(8):
                    S.op('pe', 'matmul', [xtk, 'Win%d' % ci], [bk], bank[:], lhsT=xnT[:, dc, :], rhs=Win[:, dc, ci * 512:(ci + 1) * 512],
                         start=(dc == 0), stop=(dc == 7))
                lo, hi = bank[:, 0:256], bank[:, 256:512]
                rq = rin[b][:, 0:4, :].rearrange("p a b -> p (a b)")
                rk = rin[b][:, 4:8, :].rearrange("p a b -> p (a b)")
                if ci == 0:
                    S.op('act', 'copy', [bk], ['Pp' + sfx], out=Pp[b][:], in_=lo)
                    S.op('dve', 'tensor_copy', [bk], ['rin' + sfx], out=rq, in_=hi)
                elif ci == 1:
                    S.op('dve', 'tensor_copy', [bk], ['rin' + sfx], out=rk, in_=lo)
                    S.op('act', 'copy', [bk], ['Rv' + sfx], out=Rv[b][:, 0:256], in_=hi)
                elif ci == 2:
                    S.op('act', 'copy', [bk], ['Rv' + sfx], out=Rv[b][:, 256:512], in_=lo)
                    if is_own:
                        S.op('act', 'activation', [bk], ['Rg' + sfx], out=Rg[b][:, 0:256], in_=hi, func=AF.Silu)
                elif ci == 3:
                    S.op('act', 'activation', [bk], ['Rg' + sfx], out=Rg[b][:, 256:512], in_=lo, func=AF.Silu)
                    S.op('dve', 'tensor_copy', [bk], ['Nq' + sfx], out=Nq[b][:], in_=hi)
                elif ci == 4:
                    S.op('dve', 'tensor_copy', [bk], ['Nk' + sfx], out=Nk[b][:], in_=lo)
                    S.op('act', 'copy', [bk], ['Nv' + sfx], out=Nv[b][:], in_=hi)
                else:
                    S.op('act', 'activation', [bk], ['Gt' + sfx], out=Gt[b][:, (ci - 5) * 512:(ci - 4) * 512], in_=bank[:], func=AF.Sigmoid)
            if _lvl == 0:
                S.dma('pool', ['Pp' + sfx], [], out=D['Pp'][rows, :], in_=Pp[b][:])
                continue
            x1, x2 = rin[b][:, :, 0:32], rin[b][:, :, 32:64]
            cosb = cs[b][:, 0, :].unsqueeze(1).to_broadcast(B3)
            sinb = cs[b][:, 1, :].unsqueeze(1).to_broadcast(B3)
            rk_ = ['rin' + sfx, 'cs' + sfx]
            S.op('dve', 'tensor_tensor', rk_, ['t1'], out=t1[:], in0=x1, in1=cosb, op=ALU.mult)
            S.op('dve', 'tensor_tensor', rk_, ['t2'], out=t2[:], in0=x2, in1=sinb, op=ALU.mult)
            S.op('dve', 'tensor_tensor', ['t1', 't2'], ['rot' + sfx], out=rot[b][:, :, 0:32], in0=t1[:], in1=t2[:], op=ALU.subtract)
            S.op('dve', 'tensor_tensor', rk_, ['t1'], out=t1[:], in0=x1, in1=sinb, op=ALU.mult)
            S.op('dve', 'tensor_tensor', rk_, ['t2'], out=t2[:], in0=x2, in1=cosb, op=ALU.mult)
            S.op('dve', 'tensor_tensor', ['t1', 't2'], ['rot' + sfx], out=rot[b][:, :, 32:64], in0=t1[:], in1=t2[:], op=ALU.add)
            S.op('act', 'copy', ['rot' + sfx], ['Rq' + sfx], out=Rq[b][:], in_=rot[b][:, 0:4, :].rearrange("p a b -> p (a b)"))
            S.op('act', 'mul', ['rot' + sfx], ['Rk' + sfx], Rk[b][:], rot[b][:, 4:8, :].rearrange("p a b -> p (a b)"), 0.125)
            S.dma('pool', ['Pp' + sfx], [], out=D['Pp'][rows, :], in_=Pp[b][:])
            S.dma('pool', ['Rk' + sfx], [], out=D['Rk'][rows, :], in_=Rk[b][:])
            S.dma('pool', ['Rv' + sfx], [], out=D['Rv'][rows, :], in_=Rv[b][:])
            S.dma('pool', ['Nk' + sfx], [], out=D['Nk'][rows, :], in_=Nk[b][:])
            S.dma('pool', ['Nv' + sfx], [], out=D['Nv'][rows, :], in_=Nv[b][:])
            if is_own:
                S.dma('pool', ['Rq' + sfx], [], out=D['Rq'][rows, :], in_=Rq[b][:])
                S.dma('pool', ['Rg' + sfx], [], out=D['Rg'][rows, :], in_=Rg[b][:])
                S.dma('pool', ['Nq' + sfx], [], out=D['Nq'][rows, :], in_=Nq[b][:])
                S.dma('pool', ['Gt' + sfx], [], out=D['Gt'][rows, :], in_=Gt[b][:])
        S.end_phase()


def mixer_b(nc, S, D, own):
    t0, nt = own
    with ExitStack() as es:
        C = Ctx(nc, es)
        sb, ps = C.sb, C.ps
        identf = sb("identf", [128, 128], F32)
        identb = sb("identb", [128, 128], BF16)
        Wbp = sb("Wbp", [64, 4, 1024], BF16)
        Wbr = sb("Wbr", [128, 4, 1024], BF16)
        Wbn = sb("Wbn", [128, 2, 1024], BF16)
        Wout = sb("Wout", [128, 8, 1024], BF16)
        poolw = sb("poolw", [64, 4, 64], F32)
        pscale = sb("pscale", [64, 4], F32)
        amat = sb("amat", [128, 4, 5, 128], F32)
        Et = [sb(f"Et{r}", [128, 5, 4, 128], BF16) for r in range(5)]
        rstage = sb("rstage", [128, 5, 4, 128], F32)
        nmask = sb("nmask", [128, 5, 128], F32)
        dl = sb("dl", [128, 8], F32)
        lg = sb("lg", [128, 8], F32)
        pos = sb("pos", [128, 2], F32)
        zf = sb("zf", [128, 4], F32)
        zb = sb("zb", [128, 4], F32)
        dC = sb("dC", [128, 8], F32)
        dmat = sb("dmat", [128, 4, 128], F32)
        xrow = sb("xrow", [128, 2, 128], F32)
        Dt = sb("Dt", [128, 4, 128], F32)
        E1 = sb("E1", [128, 128], F32)
        E2 = sb("E2", [128, 128], F32)
        XF = sb("XF", [64, 4, 128], F32)
        XB = sb("XB", [64, 4, 128], F32)
        Sst = sb("Sst", [64, 4, 128], F32)
        Sob = [sb(f"Sob{r}", [64, 4, 128], BF16) for r in range(2)]
        kd = sb("kd", [128, 4, 64], BF16)
        NB = 2
        xt = [sb(f"xt{r}", [128, 1024], F32) for r in range(NB)]
        Gt = [sb(f"Gt{r}", [128, 3072], F32) for r in range(NB)]
        Rq = [sb(f"Rq{r}", [128, 256], BF16) for r in range(NB)]
        Rk = [sb(f"Rk{r}", [128, 256], BF16) for r in range(NB)]
        Rv = [sb(f"Rv{r}", [128, 512], BF16) for r in range(NB)]
        Rg = [sb(f"Rg{r}", [128, 512], F32) for r in range(NB)]
        SFt = [sb(f"SFt{r}", [64, 4, 128], BF16) for r in range(NB)]
        SBt = [sb(f"SBt{r}", [64, 4, 128], BF16) for r in range(NB)]
        Nq = [sb(f"Nq{r}", [128, 256], BF16) for r in range(NB)]
        Nk = [[sb(f"Nk{r}_{o}", [128, 256], BF16) for o in range(5)] for r in range(NB)]
        Nv1 = [[sb(f"Nv{r}_{o}", [128, 4, 65], BF16) for o in range(5)] for r in range(NB)]
        Pp = [[sb(f"Pp{r}_{o}", [128, 256], F32) for o in range(3)] for r in range(NB)]
        qkT = sb("qkT", [64, 8, 128], BF16)
        qfT = sb("qfT", [64, 4, 128], BF16)
        qbT = sb("qbT", [64, 4, 128], BF16)
        PD = sb("PD", [128, 4, 128], BF16)
        ysq = sb("ysq", [128, 4, 128], F32)
        ssq4 = sb("ssq4", [128, 4], F32)
        rs4 = sb("rs4", [128, 4], F32)
        yr = sb("yr", [128, 4, 128], F32)
        yrb = sb("yrb", [128, 512], BF16)
        yrT = sb("yrT", [128, 4, 128], BF16)
        NTa = sb("NTa", [64, 24, 128], BF16)
        Pm = sb("Pm", [128, 4, 128], BF16)
        PEm = sb("PEm", [128, 4, 128], BF16)
        rden = sb("rden", [128, 4], F32)
        ynb = sb("ynb", [128, 4, 64], BF16)
        ynT = sb("ynT", [128, 2, 128], BF16)
        dT = sb("dT", [64, 4, 128], F32)
        ypT = sb("ypT", [64, 4, 128], BF16)
        mg = sb("mg", [128, 1024], F32)
        tg_ = sb("tg", [128, 1024], F32)
        mgb = sb("mgb", [128, 1024], BF16)
        mT = sb("mT", [128, 8, 128], BF16)
        xo = [sb(f"xo{r}", [128, 1024], F32) for r in range(NB)]
        bank = [ps(f"bk{r}", [128, 512], F32) for r in range(8)]
        sc_ps = bank[0][:].rearrange("p (h c) -> p h c", h=4)
        y_ps = bank[1][:].rearrange("p (h c) -> p h c", h=4)
        st_ps = [bank[2][:].rearrange("p (h c) -> p h c", h=4), bank[6][:].rearrange("p (h c) -> p h c", h=4)]
        st_k = ['bk2', 'bk6']
        o_ps = bank[3][:, 0:260].rearrange("p (h c) -> p h c", h=4)
        pl_ps = bank[4][0:64, :].rearrange("p (h c) -> p h c", h=4)
        tr_ps = bank[5][:].bitcast(BF16).rearrange("p (c t) -> p c t", c=8)
        tr7_ps = bank[7][:].bitcast(BF16).rearrange("p (c t) -> p c t", c=8)

        S.dma('sp', [], ['identf'], out=identf[:], in_=D['ident'])
        S.op('dve', 'tensor_copy', ['identf'], ['identb'], out=identb[:], in_=identf[:])
        for g in range(4):
            S.dma('pool', [], ['Wbp'], out=Wbp[:, g, :], in_=D['wbp'][:, g, :])
        S.dma('pool', [], ['Wbr'], out=Wbr[:], in_=D['wbr'].rearrange("(h e) n -> e h n", e=128))
        S.dma('pool', [], ['Wbn'], out=Wbn[:], in_=D['wbn'].rearrange("(h e) n -> e h n", e=128))
        S.dma('pool', [], ['Wout'], out=Wout[:], in_=D['wout'].rearrange("(h e) n -> e h n", e=128))
        S.dma('sp', [], ['poolw'], out=poolw[:], in_=D['poolw'])
        S.dma('sp', [], ['pscale'], out=pscale[:], in_=D['pscale'])
        S.dma('sp', [], ['amat'], out=amat[:], in_=D['amat'])
        S.dma('sp', [], ['dl'], out=dl[:], in_=D['decay'].partition_broadcast(128))
        S.dma('sp', [], ['pos'], out=pos[:], in_=D['pos'])
        S.dma('sp', [], ['dmat'], out=dmat[:], in_=D['dmat'].rearrange("k p c -> p k c"))
        S.dma('sp', [], ['xrow'], out=xrow[:], in_=D['xrow'])
        for r in range(NB):
            for o in range(5):
                S.op('dve', 'memset', [], ['Nv%d_%d' % (r, o)], Nv1[r][o][:], 1.0)
        for ty in range(5):
            S.dma('sp', [], ['rstage'], out=rstage[:], in_=D['rpbT'][ty])
            S.dma('sp', [], ['nmask'], out=nmask[:], in_=D['nmask'][ty])
            S.op('act', 'activation', ['rstage'], ['rstage'], out=rstage[:], in_=rstage[:], func=AF.Exp)
            S.op('dve', 'tensor_tensor', ['rstage', 'nmask'], ['Et%d' % ty], out=Et[ty][:], in0=rstage[:],
                 in1=nmask[:].unsqueeze(2).to_broadcast([128, 5, 4, 128]), op=ALU.mult)
        S.op('act', 'activation', ['dl'], ['lg'], out=lg[:], in_=dl[:], func=AF.Exp, scale=-1.0)
        S.op('act', 'activation', ['lg'], ['lg'], out=lg[:], in_=lg[:], func=AF.Ln, bias=1.0)
        S.op('act', 'mul', ['lg'], ['lg'], lg[:], lg[:], -1.0)
        S.op('act', 'activation', ['lg', 'pos'], ['zf'], out=zf[:], in_=lg[:, 0:4], func=AF.Exp, scale=pos[:, 0:1])
        S.op('act', 'activation', ['lg', 'pos'], ['zb'], out=zb[:], in_=lg[:, 4:8], func=AF.Exp, scale=pos[:, 1:2])
        S.op('act', 'activation', ['lg'], ['dC'], out=dC[:], in_=lg[:], func=AF.Exp, scale=128.0)
        for h in range(4):
            S.op('act', 'activation', ['lg', 'dmat'], ['E1'], out=E1[:], in_=dmat[:, 0, :], func=AF.Exp, scale=lg[:, h:h + 1])
            S.op('act', 'activation', ['lg', 'dmat'], ['E2'], out=E2[:], in_=dmat[:, 2, :], func=AF.Exp, scale=lg[:, 4 + h:5 + h])
            S.op('dve', 'tensor_tensor', ['E1', 'dmat'], ['E1'], out=E1[:], in0=E1[:], in1=dmat[:, 1, :], op=ALU.mult)
            S.op('dve', 'tensor_tensor', ['E2', 'dmat'], ['E2'], out=E2[:], in0=E2[:], in1=dmat[:, 3, :], op=ALU.mult)
            S.op('dve', 'tensor_tensor', ['E1', 'E2'], ['Dt'], out=Dt[:, h, :], in0=E1[:], in1=E2[:], op=ALU.add)
            S.op('act', 'activation', ['lg', 'xrow'], ['XF'], out=XF[:, h, :], in_=xrow[0:64, 0, :], func=AF.Exp, scale=lg[0:64, h:h + 1])
            S.op('act', 'activation', ['lg', 'xrow'], ['XB'], out=XB[:, h, :], in_=xrow[0:64, 1, :], func=AF.Exp, scale=lg[0:64, 4 + h:5 + h])

        S.end_phase()
        rsb = rstage[:].rearrange("p a b c -> p (a b c)").bitcast(BF16)
        rsf = rstage[:].rearrange("p a b c -> p (a b c)")

        class _V:
            def __init__(self, ap):
                self.ap = ap

            def __getitem__(self, k):
                return self.ap if (k == slice(None)) else self.ap[k]

        Rk2 = [_V(rsb[:, 0:256]), _V(rsb[:, 256:512])]
        Rv2 = [_V(rsb[:, 512:1024]), _V(rsb[:, 1024:1536])]
        kd2 = _V(rsb[:, 1536:1792].rearrange("p (h d) -> p h d", h=4))
        Sob2 = [_V(rsb[0:64, 1792:2304].rearrange("p (h c) -> p h c", h=4)), _V(rsb[0:64, 2304:2816].rearrange("p (h c) -> p h c", h=4))]
        Sst2 = _V(rsf[0:64, 1408:1920].rearrange("p (h c) -> p h c", h=4))

        def sweep(order, zt, zk, dcol, dst, dkey, store_pred, upd_pred, St, Sk, kdt, kdk, Sobt, Sobk, Rkt, Rkk, Rvt, Rvk, bki):
            S.op('dve', 'memset', [], [Sk], St[:], 0.0)
            yield
            bk_base = bki
            for cnt, n in enumerate(order):
                b = cnt % NB
                sfx = str(b)
                bki = bk_base + 2 * (cnt % 2)
                rows = slice(n * 128, (n + 1) * 128)
                if store_pred(n):
                    S.op('act', 'copy', [Sk], [Sobk + sfx], out=Sobt[b][:], in_=St[:])
                    S.dma('pool', [Sobk + sfx], [dkey + str(n)], out=dst[n], in_=Sobt[b][:])
                if not upd_pred(n):
                    continue
                S.dma('sp', [], [Rkk + sfx], out=Rkt[b][:], in_=D['Rk'][rows, :])
                S.dma('sp', [], [Rvk + sfx], out=Rvt[b][:], in_=D['Rv'][rows, :])
                yield
                S.op('dve', 'tensor_tensor', [Rkk + sfx, zk], [kdk], out=kdt[:], in0=Rkt[b][:].rearrange("p (h d) -> p h d", h=4),
                     in1=zt[:].unsqueeze(2).to_broadcast([128, 4, 64]), op=ALU.mult)
                yield
                for h in range(4):
                    S.op('pe', 'matmul', [kdk, Rvk + sfx], ['bk%d' % bki], bank[bki][0:64, h * 128:(h + 1) * 128], lhsT=kdt[:, h, :],
                         rhs=Rvt[b][:, h * 128:(h + 1) * 128], start=True, stop=True)
                S.op('dve', 'tensor_tensor', [Sk, 'dC'], [Sk], out=St[:], in0=St[:],
                     in1=dC[0:64, dcol:dcol + 4].unsqueeze(2).to_broadcast([64, 4, 128]), op=ALU.mult)
                yield
                S.op('dve', 'tensor_tensor', [Sk, 'bk%d' % bki], [Sk], out=St[:], in0=St[:],
                     in1=bank[bki][0:64, :].rearrange("p (h c) -> p h c", h=4), op=ALU.add)
                yield

        last = t0 + nt - 1
        sw = [sweep(list(range(0, last + 1)), zf, 'zf', 0, D['SF'], 'dSF', lambda n: n >= t0, lambda n: n < last,
                    Sst, 'Sst', kd, 'kd', Sob, 'Sob', Rk, 'Rk', Rv, 'Rv', 0),
              sweep(list(range(31, t0 - 1, -1)), zb, 'zb', 4, D['SB'], 'dSB', lambda n: n <= last, lambda n: n > t0,
                    Sst2, 'Sst2', kd2, 'kd2', Sob2, 'Sobb', Rk2, 'Rkb', Rv2, 'Rvb', 1)]
        while sw:
            for g_ in list(sw):
                try:
                    next(g_)
                except StopIteration:
                    sw.remove(g_)

        B4 = [128, 4, 128]
        pending_tail = None
        for it in range(nt):
            i = t0 + it
            b = it % NB
            sfx = str(b)
            rows = slice(i * 128, (i + 1) * 128)
            ty = {0: 1, 1: 2, 30: 3, 31: 4}.get(i, 0)
            offs = [o for o in range(NA_OMIN[ty], NA_OMIN[ty] + 5) if 0 <= i + o <= 31]
            poffs = [o for o in range(-1, 2) if 0 <= i + o <= 31]
            S.dma('sp', [], ['xt' + sfx], out=xt[b][:], in_=D['x_full'][rows, :])
            S.dma('sp', [], ['Gt' + sfx], out=Gt[b][:], in_=D['Gt'][rows, :])
            S.dma('sp', [], ['Rq' + sfx], out=Rq[b][:], in_=D['Rq'][rows, :])
            S.dma('sp', [], ['Rk' + sfx], out=Rk[b][:], in_=D['Rk'][rows, :])
            S.dma('sp', [], ['Rv' + sfx], out=Rv[b][:], in_=D['Rv'][rows, :])
            S.dma('sp', [], ['Rg' + sfx], out=Rg[b][:], in_=D['Rg'][rows, :])
            S.dma('sp', ['dSF%d' % i], ['SFt' + sfx], out=SFt[b][:], in_=D['SF'][i])
            S.dma('sp', ['dSB%d' % i], ['SBt' + sfx], out=SBt[b][:], in_=D['SB'][i])
            S.dma('sp', [], ['Nq' + sfx], out=Nq[b][:], in_=D['Nq'][rows, :])
            for oi, o in enumerate(offs):
                r2 = slice((i + o) * 128, (i + o + 1) * 128)
                S.dma('sp', [], ['Nk%s_%d' % (sfx, oi)], out=Nk[b][oi][:], in_=D['Nk'][r2, :])
                S.dma('sp', [], ['Nv%s_%d' % (sfx, oi)], out=Nv1[b][oi][:, :, 0:64], in_=D['Nv'][r2, :].rearrange("p (h d) -> p h d", h=4))
            for oi, o in enumerate(poffs):
                r2 = slice((i + o) * 128, (i + o + 1) * 128)
                S.dma('sp', [], ['Pp%s_%d' % (sfx, oi)], out=Pp[b][oi][:], in_=D['Pp'][r2, :])

            pend_rest = None
            if pending_tail is not None:
                pending_tail[0]()
                pend_rest = pending_tail[1]()
            if 'uTb' in D:
                cpt = -(-128 // nt)
                uT2 = D['uT'].rearrange("i p c e -> i p (c e)")
                for i0 in range(it * cpt, min(128, (it + 1) * cpt)):
                    S.dma('pool', [], [], out=D['uTb'][i0], in_=uT2[i0])
                    S.dma('pool', [], [], out=D['vb'][i0], in_=D['v'][i0 * 128:(i0 + 1) * 128, :])
            def gen_ret():
                for h in range(4):
                    S.op('pe', 'transpose', ['Rq' + sfx, 'identb'], ['bk5'], out=tr_ps[0:64, h, :], in_=Rq[b][:, h * 64:(h + 1) * 64], identity=identb[:])
                    S.op('pe', 'transpose', ['Rk' + sfx, 'identb'], ['bk5'], out=tr_ps[0:64, 4 + h, :], in_=Rk[b][:, h * 64:(h + 1) * 64], identity=identb[:])
                S.op('act', 'copy', ['bk5'], ['qkT'], out=qkT[:], in_=tr_ps[0:64, :, :])
                yield
                S.op('dve', 'tensor_tensor', ['qkT', 'XF'], ['qfT'], out=qfT[:], in0=qkT[:, 0:4, :], in1=XF[:], op=ALU.mult)
                S.op('dve', 'tensor_tensor', ['qkT', 'XB'], ['qbT'], out=qbT[:], in0=qkT[:, 0:4, :], in1=XB[:], op=ALU.mult)
                yield
                for h in range(4):
                    S.op('pe', 'matmul', ['qkT'], ['bk0'], sc_ps[:, h, :], lhsT=qkT[:, 4 + h, :], rhs=qkT[:, h, :], start=True, stop=True)
                yield
                S.op('dve', 'tensor_tensor', ['bk0', 'Dt'], ['PD'], out=PD[:], in0=sc_ps, in1=Dt[:], op=ALU.mult)
                yield
                for h in range(4):
                    S.op('pe', 'matmul', ['PD', 'Rv' + sfx], ['bk1'], y_ps[:, h, :], lhsT=PD[:, h, :], rhs=Rv[b][:, h * 128:(h + 1) * 128],
                         start=(h == 0), stop=False, skip_group_check=True)
                    S.op('pe', 'matmul', ['qfT', 'SFt' + sfx], ['bk1'], y_ps[:, h, :], lhsT=qfT[:, h, :], rhs=SFt[b][:, h, :],
                         start=False, stop=False, skip_group_check=True)
                    S.op('pe', 'matmul', ['qbT', 'SBt' + sfx], ['bk1'], y_ps[:, h, :], lhsT=qbT[:, h, :], rhs=SBt[b][:, h, :],
                         start=False, stop=(h == 3), skip_group_check=True)
                yield
                S.op('act', 'activation', ['bk1'], ['ysq'], out=ysq[:], in_=y_ps, func=AF.Square)
                yield
                S.op('dve', 'tensor_reduce', ['ysq'], ['ssq4'], out=ssq4[:], in_=ysq[:], axis=AX.X, op=ALU.add)
                yield
                S.op('act', 'activation', ['ssq4'], ['rs4'], out=rs4[:], in_=ssq4[:], func=AF.Sqrt, scale=1.0 / 128, bias=EPS)
                yield
                S.op('dve', 'reciprocal', ['rs4'], ['rs4'], out=rs4[:], in_=rs4[:])
                S.op('dve', 'tensor_tensor', ['bk1', 'rs4'], ['yr'], out=yr[:], in0=y_ps, in1=rs4[:].unsqueeze(2).to_broadcast(B4), op=ALU.mult)
                S.op('dve', 'tensor_tensor', ['yr', 'Rg' + sfx], ['yrb'], out=yrb[:], in0=yr[:].rearrange("p h c -> p (h c)"), in1=Rg[b][:], op=ALU.mult)
                yield
                for h in range(4):
                    S.op('pe', 'transpose', ['yrb', 'identb'], ['bk5'], out=tr_ps[:, h, :], in_=yrb[:, h * 128:(h + 1) * 128], identity=identb[:])
                S.op('act', 'copy', ['bk5'], ['yrT'], out=yrT[:], in_=tr_ps[:, 0:4, :])


                yield

            def gen_na():
                srcs = [(Nq[b], 'Nq' + sfx)] + [(Nk[b][oi], 'Nk%s_%d' % (sfx, oi)) for oi in range(len(offs))]
                for r0 in range(0, len(srcs), 2):
                    grp = srcs[r0:r0 + 2]
                    for gi, (src, sk) in enumerate(grp):
                        for h in range(4):
                            S.op('pe', 'transpose', [sk, 'identb'], ['bk7'], out=tr7_ps[0:64, gi * 4 + h, :], in_=src[:, h * 64:(h + 1) * 64], identity=identb[:])
                    n_ = 4 * len(grp)
                    S.op('act', 'copy', ['bk7'], ['NTa'], out=NTa[:, r0 * 4:r0 * 4 + n_, :], in_=tr7_ps[0:64, 0:n_, :])
                yield
                for oi, o in enumerate(offs):
                    sp_, sk_ = st_ps[oi % 2], st_k[oi % 2]
                    for h in range(4):
                        S.op('pe', 'matmul', ['NTa'], [sk_], sp_[:, h, :], lhsT=NTa[:, 4 + 4 * oi + h, :], rhs=NTa[:, h, :], start=True, stop=True)
                    S.op('act', 'activation', [sk_], ['Pm'], out=Pm[:], in_=sp_, func=AF.Exp, scale=0.125)
                    yield
                    S.op('dve', 'tensor_tensor', ['Pm', 'Et%d' % ty], ['PEm'], out=PEm[:], in0=Pm[:], in1=Et[ty][:, o - NA_OMIN[ty], :, :], op=ALU.mult)
                    yield
                    for h in range(4):
                        S.op('pe', 'matmul', ['PEm', 'Nv%s_%d' % (sfx, oi)], ['bk3'], o_ps[:, h, :], lhsT=PEm[:, h, :], rhs=Nv1[b][oi][:, h, :],
                             start=(oi == 0 and h == 0), stop=(oi == len(offs) - 1 and h == 3), skip_group_check=True)
                yield
                S.op('dve', 'reciprocal', ['bk3'], ['rden'], out=rden[:], in_=o_ps[:, :, 64:65].rearrange("p h c -> p (h c)"))
                S.op('dve', 'tensor_tensor', ['bk3', 'rden'], ['ynb'], out=ynb[:], in0=o_ps[:, :, 0:64], in1=rden[:].unsqueeze(2).to_broadcast([128, 4, 64]), op=ALU.mult)
                ynf = ynb[:].rearrange("p h d -> p (h d)")
                yield
                for c in range(2):
                    S.op('pe', 'transpose', ['ynb', 'identb'], ['bk7'], out=tr7_ps[:, c, :], in_=ynf[:, c * 128:(c + 1) * 128], identity=identb[:])
                S.op('act', 'copy', ['bk7'], ['ynT'], out=ynT[:], in_=tr7_ps[:, 0:2, :])


                yield

            def gen_pool():
                for g in range(4):
                    for oi, o in enumerate(poffs):
                        kind = {-1: 0, 1: 2}.get(o, 3 if i == 0 else (4 if i == 31 else 1))
                        S.op('pe', 'matmul', ['Pp%s_%d' % (sfx, oi), 'amat'], ['bk4'], pl_ps[:, g, :], lhsT=Pp[b][oi][:, g * 64:(g + 1) * 64],
                             rhs=amat[:, g, kind, :], start=(oi == 0), stop=(oi == len(poffs) - 1))
                yield
                S.op('act', 'copy', ['bk4'], ['dT'], out=dT[:], in_=pl_ps)
                yield
                for g in range(4):
                    S.op('pe', 'matmul', ['dT', 'poolw'], ['bk4'], pl_ps[:, g, :], lhsT=poolw[:, g, :], rhs=dT[:, g, :], start=True, stop=True)
                yield
                S.op('dve', 'tensor_tensor', ['bk4', 'pscale'], ['ypT'], out=ypT[:], in0=pl_ps, in1=pscale[:].unsqueeze(2).to_broadcast([64, 4, 128]), op=ALU.mult)


                yield

            gens = [gen_ret(), gen_na(), gen_pool()] + ([pend_rest] if pend_rest is not None else [])
            while gens:
                for g_ in list(gens):
                    try:
                        next(g_)
                    except StopIteration:
                        gens.remove(g_)
            def make_tail(b=b, sfx=sfx, it=it):
                def head():
                    def branch(nk, lhs_list, w_tile, goff, first, lastb):
                        for nh in range(2):
                            bkk = bank[6 + nh]
                            for kk in range(nk):
                                S.op('pe', 'matmul', lhs_list[1] + [w_tile[1]], ['bk%d' % (6 + nh)], bkk[:], lhsT=lhs_list[0][:, kk, :],
                                     rhs=w_tile[0][:, kk, nh * 512:(nh + 1) * 512], start=(kk == 0), stop=(kk == nk - 1))
                            cols = slice(nh * 512, (nh + 1) * 512)
                            gsl = Gt[b][:, goff + nh * 512: goff + (nh + 1) * 512]
                            if first:
                                S.op('dve', 'tensor_tensor', ['bk%d' % (6 + nh), 'Gt' + sfx], ['mg'], out=mg[:, cols], in0=bkk[:], in1=gsl, op=ALU.mult)
                            else:
                                S.op('dve', 'tensor_tensor', ['bk%d' % (6 + nh), 'Gt' + sfx], ['tg'], out=tg_[:, cols], in0=bkk[:], in1=gsl, op=ALU.mult)
                                if lastb:
                                    S.op('dve', 'tensor_tensor', ['mg', 'tg'], ['mgb'], out=mgb[:, cols], in0=mg[:, cols], in1=tg_[:, cols], op=ALU.add)
                                else:
                                    S.op('dve', 'tensor_tensor', ['mg', 'tg'], ['mg'], out=mg[:, cols], in0=mg[:, cols], in1=tg_[:, cols], op=ALU.add)
                    branch(4, (ypT, ['ypT']), (Wbp, 'Wbp'), 0, True, False)
                    branch(4, (yrT, ['yrT']), (Wbr, 'Wbr'), 1024, False, False)
                    branch(2, (ynT, ['ynT']), (Wbn, 'Wbn'), 2048, False, True)

                def rest():
                    for c in range(8):
                        S.op('pe', 'transpose', ['mgb', 'identb'], ['bk5'], out=tr_ps[:, c, :], in_=mgb[:, c * 128:(c + 1) * 128], identity=identb[:])
                    S.op('act', 'copy', ['bk5'], ['mT'], out=mT[:], in_=tr_ps)
                    yield
                    for nh in range(2):
                        bkk = bank[6 + nh]
                        for dc in range(8):
                            S.op('pe', 'matmul', ['mT', 'Wout'], ['bk%d' % (6 + nh)], bkk[:], lhsT=mT[:, dc, :], rhs=Wout[:, dc, nh * 512:(nh + 1) * 512],
                                 start=(dc == 0), stop=(dc == 7))
                        cols = slice(nh * 512, (nh + 1) * 512)
                        S.op('dve', 'tensor_tensor', ['bk%d' % (6 + nh), 'xt' + sfx], ['xo' + sfx], out=xo[b][:, cols], in0=bkk[:], in1=xt[b][:, cols], op=ALU.add)
                        yield
                    S.dma('pool', ['xo' + sfx], [], out=D['x_out'][it * 128:(it + 1) * 128, :], in_=xo[b][:])
                    yield
                return head, rest

            pending_tail = make_tail()
        if pending_tail is not None:
            pending_tail[0]()
            for _ in pending_tail[1]():
                pass
        S.end_phase()


POOL_WINDOWS = (2, 4, 8, 16)
SEQ = 4096
NA_OMIN = (-2, 0, -2, -2, -3)
NA_NOFF = (5, 4, 4, 4, 4)


def _const_tables():
    T = {}
    T['ident'] = np.eye(128, dtype=np.float32)
    T['iota'] = np.tile(np.arange(128, dtype=np.float32)[None, :], (128, 1))
    T['cst16'] = np.tile((16 * np.arange(16, dtype=np.float32) + 7.5)[None, :], (128, 1))
    half = 32
    inv = (1.0 / (np.float32(10000.0) ** np.linspace(0.0, 1.0, half, dtype=np.float32))).astype(np.float32)
    ang = (np.arange(SEQ, dtype=np.float32)[:, None] * inv[None, :]).astype(np.float32)
    T['cos'] = np.cos(ang).astype(np.float32)
    T['sin'] = np.sin(ang).astype(np.float32)
    p = np.arange(128, dtype=np.float32)
    T['pos'] = np.stack([127.0 - p, p], axis=1).astype(np.float32)
    m = p[:, None]
    c = p[None, :]
    T['dmat'] = np.stack([np.maximum(c - m, 0), (c >= m).astype(np.float32), np.maximum(m - c, 0), (m > c).astype(np.float32)]).astype(np.float32)
    xr = np.stack([p + 1.0, 128.0 - p], axis=0)
    T['xrow'] = np.tile(xr[None], (128, 1, 1)).astype(np.float32)
    A = np.zeros((128, 4, 5, 128), np.float32)
    for g, w in enumerate(POOL_WINDOWS):
        lo, hi = w // 2, w - w // 2
        for kind, base in ((1, 1024), (3, 0), (4, SEQ - 128)):
            for t in range(128):
                ta = base + t
                a_, b_ = max(ta - lo, 0), min(ta + hi, SEQ)
                cnt = float(b_ - a_)
                for s in range(a_, b_):
                    rel = s - base
                    if 0 <= rel < 128:
                        A[rel, g, kind, t] += 1.0 / cnt
                    elif rel < 0 and kind == 1:
                        A[rel + 128, g, 0, t] += 1.0 / cnt
                    elif rel >= 128 and kind == 1:
                        A[rel - 128, g, 2, t] += 1.0 / cnt
                A[t, g, kind, t] -= 1.0
    T['amat'] = A
    di = np.zeros((5, 128, 5, 128), np.int64)
    dj = np.zeros((5, 128, 5, 128), np.int64)
    mk = np.zeros((5, 128, 5, 128), np.float32)
    q = np.arange(128)
    k = np.arange(128)
    for ty, i in enumerate((5, 0, 1, 30, 31)):
        for oi in range(5):
            o = NA_OMIN[ty] + oi
            j = i + o
            if j < 0 or j > 31:
                continue
            qr = 2 * i + q // 64
            qc = q % 64
            kr = (2 * j + k // 64)[:, None]
            kcol = (k % 64)[:, None]
            rs = np.clip(qr - 4, 0, 56)[None, :]
            vr = (kr >= rs) & (kr < rs + 8)
            cst = np.clip(qc - 8, 0, 48)[None, :]
            vc = (kcol >= cst) & (kcol < cst + 16)
            di[ty, :, oi, :] = np.clip(kr - qr[None, :] + 7, 0, 14)
            dj[ty, :, oi, :] = np.clip(kcol - qc[None, :], -15, 15) + 15
            mk[ty, :, oi, :] = (vr & vc).astype(np.float32)
    T['na_di'], T['na_dj'], T['nmask'] = di, dj, mk
    return T


_TABLES = None


def tables():
    global _TABLES
    if _TABLES is None:
        _TABLES = _const_tables()
    return _TABLES


def layer_layout(l, w_in, pool_w, pool_scale, ret_decay, na_rpb, w_br_pool, w_br_ret, w_br_na, w_out,
                 peer_w_query, peer_sub_keys, peer_u, peer_v):
    T = tables()
    L = {}
    L['w_in'] = np.ascontiguousarray(w_in[l])
    L['wbp'] = np.ascontiguousarray(w_br_pool[l].reshape(4, 64, 1024).transpose(1, 0, 2))
    L['wbr'] = np.ascontiguousarray(w_br_ret[l])
    L['wbn'] = np.ascontiguousarray(w_br_na[l])
    L['wout'] = np.ascontiguousarray(w_out[l])
    L['poolw'] = np.ascontiguousarray(pool_w[l].transpose(1, 0, 2))
    L['pscale'] = np.ascontiguousarray(pool_scale[l].reshape(4, 64).T)
    L['decay'] = np.ascontiguousarray(ret_decay[l].reshape(1, 8))
    rp = na_rpb[l]
    g = rp[:, T['na_di'], T['na_dj']]
    L['rpbT'] = np.ascontiguousarray(g.transpose(1, 2, 3, 0, 4)).astype(np.float32)
    L['wq'] = np.ascontiguousarray(peer_w_query[l].reshape(8, 128, 16, 128).transpose(2, 1, 0, 3))
    L['skT'] = np.ascontiguousarray(peer_sub_keys[l].transpose(2, 0, 1))
    L['uT'] = np.ascontiguousarray(peer_u[l].reshape(128, 128, 8, 128).transpose(0, 3, 2, 1))
    L['v'] = np.ascontiguousarray(peer_v[l])
    return L


N_CORES = 4
NTOK = 4096
DEPTH = 2
_PER_LAYER = ('g_mix', 'w_in', 'wbp', 'wbr', 'wbn', 'wout', 'poolw', 'pscale', 'decay', 'rpbT', 'g_ffn', 'wq', 'skT', 'uT', 'v')
_SHAPES = dict(g_mix=[1, 1024], w_in=[1024, 5632], wbp=[64, 4, 1024], wbr=[512, 1024], wbn=[256, 1024], wout=[1024, 1024],
               poolw=[64, 4, 64], pscale=[64, 4], decay=[1, 8], rpbT=[5, 128, 5, 4, 128], g_ffn=[1, 1024], wq=[16, 128, 8, 128],
               skT=[128, 2, 128], uT=[128, 128, 8, 128], v=[16384, 1024])
_CONST = dict(ident=[128, 128], iota=[128, 128], cst16=[128, 16], cos=[4096, 32], sin=[4096, 32], pos=[128, 2], dmat=[4, 128, 128],
              xrow=[128, 2, 128], amat=[128, 4, 5, 128], nmask=[5, 128, 5, 128])


def build_program(n_iblk=128):
    nc = bass.Bass("TRN2", target_bir_lowering=False)
    dt = lambda nm, shp, ty=F32, kind="ExternalInput": nc.dram_tensor(nm, list(shp), ty, kind=kind).ap()
    x = dt("x", [NTOK, 1024])
    y = dt("y", [NTOK, 1024], F32, "ExternalOutput")
    gfin = dt("gfin", [1, 1024])
    Cn = {k: dt(k, s) for k, s in _CONST.items()}
    W = [{k: dt(f"{k}_{l}", _SHAPES[k]) for k in _PER_LAYER} for l in range(DEPTH)]
    I = "Internal"
    Sc = dict(Pp=dt("s_Pp", [4096, 256], F32, I), Rq=dt("s_Rq", [4096, 256], BF16, I), Rk=dt("s_Rk", [4096, 256], BF16, I),
              Rv=dt("s_Rv", [4096, 512], BF16, I), Rg=dt("s_Rg", [4096, 512], F32, I), Nq=dt("s_Nq", [4096, 256], BF16, I),
              Nk=dt("s_Nk", [4096, 256], BF16, I), Nv=dt("s_Nv", [4096, 256], BF16, I), Gt=dt("s_Gt", [4096, 3072], F32, I),
              SF=dt("s_SF", [32, 64, 4, 128], BF16, I), SB=dt("s_SB", [32, 64, 4, 128], BF16, I))
    x1 = dt("s_x1", [NTOK, 1024], F32, I)
    uTb = dt("s_uTb", [128, 128, 1024], BF16, I)
    vb = dt("s_vb", [128, 128, 1024], BF16, I)
    x2 = dt("s_x2", [NTOK, 1024], F32, I)
    own = (0, NTOK // 128)
    with ExitStack() as es:
        S = Sched(nc, es)
        cur = x
        for l in range(DEPTH):
            D = dict(Cn)
            D.update(Sc)
            D.update(W[l])
            D.update(x_full=cur, g=W[l]['g_mix'], x_out=x1, uTb=uTb, vb=vb)
            mixer_a(nc, S, D, own)
            mixer_b(nc, S, D, own)
            last = (l == DEPTH - 1)
            P = dict(Cn)
            P.update(W[l])
            P.update(x_in=x1, x_out=(y if last else x2), g=W[l]['g_ffn'], gfin=gfin, uTb=uTb, vb=vb, preconverted=True)
            peer_phase(nc, S, P, NTOK, n_iblk=n_iblk, final=last)
            cur = x2
    return nc


_NC_CACHE = {}


def kernel(x, norm_mix, w_in, pool_w, pool_scale, ret_decay, na_rpb, w_br_pool, w_br_ret, w_br_na, w_out, norm_ffn,
           peer_w_query, peer_sub_keys, peer_u, peer_v, norm_final):
    f = lambda a: np.ascontiguousarray(np.asarray(a), dtype=np.float32)
    x = f(x)
    T = tables()
    shared = {k: np.ascontiguousarray(T[k]) for k in _CONST}
    shared['gfin'] = f(norm_final).reshape(1, 1024)
    args = [f(a) for a in (w_in, pool_w, pool_scale, ret_decay, na_rpb, w_br_pool, w_br_ret, w_br_na, w_out,
                           peer_w_query, peer_sub_keys, peer_u, peer_v)]
    nm, nf = f(norm_mix), f(norm_ffn)
    for l in range(DEPTH):
        L = layer_layout(l, *args)
        L['g_mix'] = nm[l].reshape(1, 1024)
        L['g_ffn'] = nf[l].reshape(1, 1024)
        for k in _PER_LAYER:
            shared[f"{k}_{l}"] = L[k]
    if 'nc' not in _NC_CACHE:
        _NC_CACHE['nc'] = build_program()
    nc = _NC_CACHE['nc']
    in_maps = []
    for c in range(N_CORES):
        m = dict(shared)
        m['x'] = np.ascontiguousarray(x[c])
        in_maps.append(m)
    res = run_bass_kernel_spmd(nc, in_maps, core_ids=list(range(N_CORES)))
    out = np.stack([np.asarray(res.results[c]['y'], dtype=np.float32) for c in range(N_CORES)], axis=0)
    return out
```
